# Optimizing a Trainium2 kernel written in Bass

```python
import math
import jax
import jax.numpy as jnp
from jax import lax
import numpy as np

D_MODEL = 4096
BATCH = 4
SEQ = 4096
DEPTH = 1

CTX_LEN = 256
GRID_W = 64
N_MOD = 6
NORM_EPS = 1e-6
HY_WIDTH = D_MODEL // 2
HY_ORDER = 2
HY_EMB_DIM = 33
HY_FILTER_ORDER = 64
HY_SHORT_DECAY_PCT = 0.3
HY_LONG_DECAY_PCT = 1.5
HY_DECAY_TARGET = 1e-2
RET_HEADS = 8
RET_WIDTH = D_MODEL // 2
RET_V_DIM = RET_WIDTH // RET_HEADS
RET_QK_DIM = RET_V_DIM // 2
RET_CHUNK = 128
ROPE_BASE = 10000.0
N_EXPERTS = 16
EXPERT_FF = D_MODEL // 2
EC_CAPACITY_FACTOR = 2
HY_COLS = (HY_ORDER + 1) * HY_WIDTH
Q_COLS = RET_HEADS * RET_QK_DIM
G_COLS = 2 * RET_WIDTH
K_COLS = RET_HEADS * RET_QK_DIM
V_COLS = RET_WIDTH
KV_START = HY_COLS + Q_COLS + G_COLS
IN_WIDTH = KV_START + K_COLS + V_COLS
MIX_WIDTH = HY_WIDTH + RET_WIDTH

kernel_name = 'hybrid_hyena_retention_ec_adaln'

F32 = jnp.float32


def rms_norm(x, g):
    xf = x.astype(F32)
    y = xf * lax.rsqrt(jnp.mean(xf * xf, axis=-1, keepdims=True) + NORM_EPS)
    return (y * g.astype(F32)).astype(x.dtype)


def modulate(h, shift, scale):
    return h * (1.0 + scale) + shift


def ada_params(cond, w, b):
    m = jax.nn.silu(cond) @ w + b
    return jnp.split(m, N_MOD, axis=-1)


def short_conv(u, w, b):
    up = jnp.pad(u, ((0, 0), (1, 1), (0, 0)))
    return up[:, :-2] * w[0] + up[:, 1:-1] * w[1] + up[:, 2:] * w[2] + b


def hyena_filter_spectrum(L, w1, b1, w2, b2, w3, b3, w4, freq):
    bands = (HY_EMB_DIM - 1) // 2
    t = jnp.linspace(0.0, 1.0, L, dtype=F32)[:, None]
    ang = (2.0 * math.pi / L) * jnp.arange(L, dtype=F32)[:, None] * jnp.linspace(1e-4, bands - 1, bands, dtype=F32)[None, :]
    z = jnp.concatenate([t, jnp.cos(ang), -jnp.sin(ang)], axis=-1)
    fr = freq.astype(F32)
    h = jnp.sin(fr * (z @ w1.astype(F32) + b1.astype(F32)))
    h = jnp.sin(fr * (h @ w2.astype(F32) + b2.astype(F32)))
    h = jnp.sin(fr * (h @ w3.astype(F32) + b3.astype(F32)))
    h = (h @ w4.astype(F32)).reshape(L, 2, HY_ORDER, HY_WIDTH)
    max_decay = math.log(HY_DECAY_TARGET) / HY_SHORT_DECAY_PCT
    min_decay = math.log(HY_DECAY_TARGET) / HY_LONG_DECAY_PCT
    deltas = jnp.abs(jnp.linspace(min_decay, max_decay, HY_WIDTH, dtype=F32))
    h = h * jnp.exp(-t * deltas[None, :])[:, None, None, :]
    fwd, bwd = h[:, 0], h[:, 1]
    taps = jnp.concatenate([fwd, jnp.zeros_like(fwd[:1]), bwd[:0:-1]], axis=0)
    taps = taps / jnp.sum(jnp.abs(taps), axis=0, keepdims=True)
    return jnp.fft.rfft(taps, axis=0)


def long_conv(u, spec, bias):
    L = u.shape[1]
    uf = u.astype(F32)
    y = jnp.fft.irfft(jnp.fft.rfft(uf, n=2 * L, axis=1) * spec[None], n=2 * L, axis=1)[:, :L]
    return (y + uf * bias.astype(F32)).astype(u.dtype)


def hyena_mixer(p_hy, short_w, short_b, spec, hy_bias):
    u = short_conv(p_hy, short_w, short_b)
    v, x1, x2 = jnp.split(u, HY_ORDER + 1, axis=-1)
    z = x1 * long_conv(v, spec[:, 0], hy_bias[0])
    return x2 * long_conv(z, spec[:, 1], hy_bias[1])


def rotary_tables(pos_a, pos_b):
    n_freq = RET_QK_DIM // 4
    inv = 1.0 / (ROPE_BASE ** jnp.linspace(0.0, 1.0, n_freq, dtype=F32))
    ang = jnp.concatenate([pos_a[:, None] * inv, pos_b[:, None] * inv], axis=-1)
    return jnp.cos(ang), jnp.sin(ang)


def rotate(x, cos, sin):
    x1, x2 = jnp.split(x, 2, axis=-1)
    cs = cos[None, :, None, :].astype(x.dtype)
    sn = sin[None, :, None, :].astype(x.dtype)
    return jnp.concatenate([x1 * cs - x2 * sn, x1 * sn + x2 * cs], axis=-1)


def log_decay(logit):
    return -jax.nn.softplus(-logit.astype(F32))


def head_rms(o):
    of = o.astype(F32)
    return (of * lax.rsqrt(jnp.mean(of * of, axis=-1, keepdims=True) + NORM_EPS)).astype(o.dtype)


def retention_scan(q, k, v, log_g, s0):
    Bsz, L, H, _ = q.shape
    dv = v.shape[-1]
    n = L // RET_CHUNK
    pos = jnp.arange(RET_CHUNK, dtype=F32)
    diff = pos[:, None] - pos[None, :]
    inner_decay = jnp.where(diff >= 0, jnp.exp(log_g[:, None, None] * jnp.maximum(diff, 0.0)), 0.0).astype(q.dtype)
    q_decay = jnp.exp(log_g[:, None] * (pos + 1.0)).astype(q.dtype)
    k_decay = jnp.exp(log_g[:, None] * (RET_CHUNK - 1.0 - pos)).astype(q.dtype)
    chunk_decay = jnp.exp(log_g * RET_CHUNK).astype(q.dtype)

    def to_chunks(a):
        return a.reshape(Bsz, n, RET_CHUNK, H, a.shape[-1]).transpose(1, 0, 3, 2, 4)

    def step(s, qkv):
        qi, ki, vi = qkv
        att = jnp.einsum('bhid,bhjd->bhij', qi, ki) * inner_decay
        o = jnp.einsum('bhij,bhjv->bhiv', att, vi) + jnp.einsum('bhid,bhdv->bhiv', qi, s) * q_decay[..., None]
        s = s * chunk_decay[:, None, None] + jnp.einsum('bhjd,bhjv->bhdv', ki * k_decay[..., None], vi)
        return s, o

    _, o = lax.scan(step, s0.astype(q.dtype), (to_chunks(q), to_chunks(k), to_chunks(v)))
    return o.transpose(1, 0, 3, 2, 4).reshape(Bsz, L, H, dv)


def context_states(k, v, log_g):
    Lc = k.shape[1]
    j = jnp.arange(Lc, dtype=F32)
    w_f = jnp.exp(log_g[0][None, :] * (Lc - 1.0 - j)[:, None]).astype(k.dtype)
    w_b = jnp.exp(log_g[1][None, :] * j[:, None]).astype(k.dtype)
    s_f = jnp.einsum('blhd,blhv->bhdv', k * w_f[None, :, :, None], v)
    s_b = jnp.einsum('blhd,blhv->bhdv', k * w_b[None, :, :, None], v)
    return s_f, s_b


def retention_mixer(q, k, v, g_f, g_b, log_g, s_f, s_b):
    Bsz, L = q.shape[:2]
    o_f = retention_scan(q, k, v, log_g[0], s_f)
    o_b = retention_scan(q[:, ::-1], k[:, ::-1], v[:, ::-1], log_g[1], s_b)[:, ::-1]
    return (head_rms(o_f).reshape(Bsz, L, RET_WIDTH) * jax.nn.silu(g_f)
            + head_rms(o_b).reshape(Bsz, L, RET_WIDTH) * jax.nn.silu(g_b))


def split_kv(p_kv, cos, sin):
    Bsz, L, _ = p_kv.shape
    k = rotate(p_kv[..., :K_COLS].reshape(Bsz, L, RET_HEADS, RET_QK_DIM), cos, sin) * (RET_QK_DIM ** -0.5)
    v = p_kv[..., K_COLS:].reshape(Bsz, L, RET_HEADS, RET_V_DIM)
    return k, v


def token_mixer(p_main, k, v, cos, sin, short_w, short_b, spec, hy_bias, log_g, s_f, s_b, w_out_l):
    Bsz, L, _ = p_main.shape
    y_hy = hyena_mixer(p_main[..., :HY_COLS], short_w, short_b, spec, hy_bias)
    q = rotate(p_main[..., HY_COLS:HY_COLS + Q_COLS].reshape(Bsz, L, RET_HEADS, RET_QK_DIM), cos, sin)
    g_f, g_b = jnp.split(p_main[..., HY_COLS + Q_COLS:KV_START], 2, axis=-1)
    y_ret = retention_mixer(q, k, v, g_f, g_b, log_g, s_f, s_b)
    return jnp.concatenate([y_hy, y_ret], axis=-1) @ w_out_l


def ec_ffn(h, router_w, w_gate, w_up, w_down):
    Bsz, T, D = h.shape
    cap = EC_CAPACITY_FACTOR * T // N_EXPERTS
    aff = jax.nn.softmax((h @ router_w).astype(F32), axis=-1)
    gates, idx = lax.top_k(jnp.swapaxes(aff, 1, 2), cap)
    xs = jax.vmap(lambda hb, ib: hb[ib])(h, idx)
    hid = jax.nn.silu(jnp.einsum('becd,edf->becf', xs, w_gate)) * jnp.einsum('becd,edf->becf', xs, w_up)
    out = jnp.einsum('becf,efd->becd', hid, w_down) * gates[..., None].astype(h.dtype)
    return jax.vmap(lambda ib, ob: jnp.zeros((T, D), ob.dtype).at[ib.reshape(-1)].add(ob.reshape(-1, D)))(idx, out)


def setup_inputs(seed: int = 0) -> dict:
    key = jax.random.key(seed)
    ks = iter(jax.random.split(key, 32))

    def nrm(shape, scale):
        return jax.random.normal(next(ks), shape, F32) * scale

    D = D_MODEL
    heads = jnp.arange(RET_HEADS, dtype=F32)
    decay_logit = jnp.log(jnp.exp2(5.0 + heads) - 1.0)
    return {
        'x': nrm((BATCH, SEQ, D), 1.0),
        'c': nrm((BATCH, D), 1.0),
        'ctx': nrm((BATCH, CTX_LEN, D), 1.0),
        'c_ctx': nrm((D,), 1.0),
        'ada_w': nrm((DEPTH, D, N_MOD * D), 0.5 * D ** -0.5),
        'ada_b': nrm((DEPTH, N_MOD * D), 0.01),
        'norm1_g': 1.0 + nrm((DEPTH, D), 0.01),
        'norm2_g': 1.0 + nrm((DEPTH, D), 0.01),
        'w_in': nrm((DEPTH, D, IN_WIDTH), D ** -0.5),
        'hy_short_w': nrm((DEPTH, 3, HY_COLS), 3 ** -0.5),
        'hy_short_b': nrm((DEPTH, HY_COLS), 0.01),
        'hy_f_w1': nrm((DEPTH, HY_EMB_DIM, HY_FILTER_ORDER), HY_EMB_DIM ** -0.5),
        'hy_f_b1': nrm((DEPTH, HY_FILTER_ORDER), 0.01),
        'hy_f_w2': nrm((DEPTH, HY_FILTER_ORDER, HY_FILTER_ORDER), HY_FILTER_ORDER ** -0.5),
        'hy_f_b2': nrm((DEPTH, HY_FILTER_ORDER), 0.01),
        'hy_f_w3': nrm((DEPTH, HY_FILTER_ORDER, HY_FILTER_ORDER), HY_FILTER_ORDER ** -0.5),
        'hy_f_b3': nrm((DEPTH, HY_FILTER_ORDER), 0.01),
        'hy_f_w4': nrm((DEPTH, HY_FILTER_ORDER, 2 * HY_ORDER * HY_WIDTH), HY_FILTER_ORDER ** -0.5),
        'hy_sin_freq': 1.0 + nrm((DEPTH, HY_FILTER_ORDER), 0.01),
        'hy_bias': nrm((DEPTH, HY_ORDER, HY_WIDTH), 1.0),
        'ret_decay': decay_logit + nrm((DEPTH, 2, RET_HEADS), 0.01),
        'w_out': nrm((DEPTH, MIX_WIDTH, D), MIX_WIDTH ** -0.5),
        'router_w': nrm((DEPTH, D, N_EXPERTS), D ** -0.5),
        'exp_w_gate': nrm((DEPTH, N_EXPERTS, D, EXPERT_FF), D ** -0.5),
        'exp_w_up': nrm((DEPTH, N_EXPERTS, D, EXPERT_FF), D ** -0.5),
        'exp_w_down': nrm((DEPTH, N_EXPERTS, EXPERT_FF, D), EXPERT_FF ** -0.5),
        'final_g': 1.0 + nrm((D,), 0.01),
    }


def reference(x, c, ctx, c_ctx, ada_w, ada_b, norm1_g, norm2_g, w_in, hy_short_w, hy_short_b,
              hy_f_w1, hy_f_b1, hy_f_w2, hy_f_b2, hy_f_w3, hy_f_b3, hy_f_w4, hy_sin_freq, hy_bias,
              ret_decay, w_out, router_w, exp_w_gate, exp_w_up, exp_w_down, final_g):
    L = x.shape[1]
    Lc = ctx.shape[1]
    rows = L // GRID_W
    lat_row = jnp.repeat(jnp.arange(rows, dtype=F32), GRID_W)
    lat_col = jnp.tile(jnp.arange(GRID_W, dtype=F32), rows)
    ctx_pos = jnp.arange(Lc, dtype=F32)
    cos_x, sin_x = rotary_tables(lat_row, lat_col)
    cos_c, sin_c = rotary_tables(ctx_pos, ctx_pos)
    for layer in range(DEPTH):
        sh1, sc1, gt1, sh2, sc2, gt2 = ada_params(c, ada_w[layer], ada_b[layer])
        csh1, csc1, cgt1, csh2, csc2, cgt2 = ada_params(c_ctx, ada_w[layer], ada_b[layer])
        log_g = log_decay(ret_decay[layer])
        filt = (hy_f_w1[layer], hy_f_b1[layer], hy_f_w2[layer], hy_f_b2[layer],
                hy_f_w3[layer], hy_f_b3[layer], hy_f_w4[layer], hy_sin_freq[layer])
        hc = modulate(rms_norm(ctx, norm1_g[layer]), csh1, csc1)
        kc, vc = split_kv(hc @ w_in[layer, :, KV_START:], cos_c, sin_c)
        s_f, s_b = context_states(kc, vc, log_g)
        hx = modulate(rms_norm(x, norm1_g[layer]), sh1[:, None], sc1[:, None])
        px = hx @ w_in[layer]
        kx, vx = split_kv(px[..., KV_START:], cos_x, sin_x)
        x = x + gt1[:, None] * token_mixer(px[..., :KV_START], kx, vx, cos_x, sin_x,
                                           hy_short_w[layer], hy_short_b[layer],
                                           hyena_filter_spectrum(L, *filt), hy_bias[layer],
                                           log_g, s_f, s_b, w_out[layer])
        hx2 = modulate(rms_norm(x, norm2_g[layer]), sh2[:, None], sc2[:, None])
        x = x + gt2[:, None] * ec_ffn(hx2, router_w[layer], exp_w_gate[layer], exp_w_up[layer], exp_w_down[layer])
        if layer + 1 < DEPTH:
            zero_state = jnp.zeros_like(s_f)
            ctx = ctx + cgt1 * token_mixer(hc @ w_in[layer, :, :KV_START], kc, vc, cos_c, sin_c,
                                           hy_short_w[layer], hy_short_b[layer],
                                           hyena_filter_spectrum(Lc, *filt), hy_bias[layer],
                                           log_g, zero_state, zero_state, w_out[layer])
            hc2 = modulate(rms_norm(ctx, norm2_g[layer]), csh2, csc2)
            ctx = ctx + cgt2 * ec_ffn(hc2, router_w[layer], exp_w_gate[layer], exp_w_up[layer], exp_w_down[layer])
    return rms_norm(x, final_g)
```

```python
import math
from contextlib import ExitStack
import numpy as np
import ml_dtypes
import concourse.bass as bass
import concourse.mybir as mybir
from concourse.bass_utils import run_bass_kernel_spmd

F32 = mybir.dt.float32; BF16 = mybir.dt.bfloat16; I32 = mybir.dt.int32
ALU = mybir.AluOpType; AF = mybir.ActivationFunctionType

D = 4096; T = 4096; LC = 256; NT = 32; P = 128
HYW = 2048; HYC = 6144; QC = 1024; GC = 4096; KC = 1024; VC = 2048
KV0 = HYC + QC + GC; INW = 14336
NE = 16; NL = 16; EFF = 2048; CAP = 512
NFT = 33
EPS = 1e-6


class Res:
    __slots__ = ("name", "w", "r", "t", "dsem")

    def __init__(self, name, t=None):
        self.name = name; self.t = t; self.w = {}; self.r = {}; self.dsem = None


def _is_dram(r):
    return r.t is not None and type(r.t).__name__.lower().startswith("dram")


class Ctx:
    def __init__(self, nc, es):
        self.nc = nc; self.es = es; self.engs = {}; self.sems = {}
        for name, e in (("pe", nc.tensor), ("dve", nc.vector), ("act", nc.scalar), ("pool", nc.gpsimd), ("sp", nc.sync)):
            sem = es.enter_context(nc.semaphore("s_" + name))
            self.engs[name] = dict(e=e, seen={})
            self.sems[name] = [sem, 0]
        self.free_dsems = []
        self.ndsem = 0
        self.stack = [es]
        self.scope_res = [[]]
        self.ninst = 0

    def sb(self, name, shape, dt):
        t = self.stack[-1].enter_context(self.nc.sbuf_tensor(name, list(shape), dt))
        r = Res(name, t); self.scope_res[-1].append(r); return r

    def ps(self, name, shape, dt=F32):
        t = self.stack[-1].enter_context(self.nc.psum_tensor(name, list(shape), dt))
        r = Res(name, t); self.scope_res[-1].append(r); return r

    def dram(self, name, shape, dt, kind="Internal"):
        return Res(name, self.nc.dram_tensor(name, list(shape), dt, kind=kind))

    def push(self):
        es = ExitStack(); self.stack.append(es); self.scope_res.append([]); return es

    def pop(self):
        self.barrier()
        for r in self.scope_res.pop():
            if r.dsem is not None:
                self.free_dsems.append(r.dsem); r.dsem = None
        self.stack.pop().close()

    def _dsem(self, r):
        if r.dsem is None:
            if self.free_dsems:
                r.dsem = self.free_dsems.pop()
            else:
                key = "d%d" % self.ndsem; self.ndsem += 1
                sem = self.es.enter_context(self.nc.semaphore(key))
                self.sems[key] = [sem, 0]; r.dsem = key
        return r.dsem

    def _wait(self, eng, deps):
        E = self.engs[eng]
        for key, val in deps.items():
            if key[0] == "d":
                val = self.sems[key][1]
            elif key == eng and eng == "pe":
                continue
            if E["seen"].get(key, 0) >= val:
                continue
            E["e"].wait_ge(self.sems[key][0], val)
            E["seen"][key] = val

    def _deps(self, reads, writes):
        deps = {}
        for r in reads:
            for k, v in r.w.items():
                if deps.get(k, 0) < v: deps[k] = v
        for r in writes:
            for k, v in r.w.items():
                if deps.get(k, 0) < v: deps[k] = v
            for k, v in r.r.items():
                if deps.get(k, 0) < v: deps[k] = v
        return deps

    def _mark(self, key, val, reads, writes):
        for r in reads:
            if r.r.get(key, 0) < val: r.r[key] = val
        for r in writes:
            r.w = {key: val}; r.r = {}

    def op(self, eng, fn, reads=(), writes=()):
        self._wait(eng, self._deps(reads, writes))
        inst = fn(self.engs[eng]["e"])
        s = self.sems[eng]; s[1] += 1
        inst.then_inc(s[0], 1)
        self._mark(eng, s[1], reads, writes)
        self.ninst += 1
        return inst

    def dma(self, q, fn, reads=(), writes=(), semres=None):
        if semres is None:
            if writes and not _is_dram(writes[0]):
                semres = writes[0]
            else:
                semres = reads[0]
        key = self._dsem(semres)
        self._wait(q, self._deps(reads, writes))
        inst = fn(self.engs[q]["e"])
        s = self.sems[key]; s[1] += 16
        inst.then_inc(s[0], 16)
        self._mark(key, s[1], reads, writes)
        self.ninst += 1
        return inst

    def barrier(self):
        allk = {k: v[1] for k, v in self.sems.items() if v[1] > 0}
        for eng in self.engs:
            self._wait(eng, allk)


def _bf(a):
    return np.ascontiguousarray(a.astype(ml_dtypes.bfloat16))


_CONST_CACHE = {}


def host_constants():
    if _CONST_CACHE:
        return _CONST_CACHE
    c = {}
    n = NFT * P
    idx = np.arange(n, dtype=np.int64)
    prod = (idx[:, None] * idx[None, :]) % 8192
    valid = (idx[:, None] <= 4096) & (idx[None, :] <= 4096)
    ang = prod.astype(np.float64) * (2.0 * math.pi / 8192.0)
    Cm = np.where(valid, np.cos(ang), 0.0)
    Sm = np.where(valid, np.sin(ang), 0.0)
    def tile(M):
        return M.reshape(NFT, P, NFT, P).transpose(2, 1, 0, 3)
    cs = np.stack([tile(Cm), tile(Sm)], axis=2)
    c["cst"] = _bf(cs.astype(np.float32))
    fs = np.zeros(n, np.float32); fs[0] = 1.0 / 8192; fs[1:4096] = 2.0 / 8192; fs[4096] = 1.0 / 8192
    c["fscale"] = np.ascontiguousarray(fs.reshape(NFT, P).T)
    L = T
    bands = 16
    t = np.linspace(0.0, 1.0, L, dtype=np.float32)[:, None]
    ang2 = (np.float32(2.0 * math.pi / L) * np.arange(L, dtype=np.float32)[:, None]) * np.linspace(1e-4, bands - 1, bands, dtype=np.float32)[None, :]
    z = np.concatenate([t, np.cos(ang2), -np.sin(ang2)], axis=-1).astype(np.float32)
    c["zT"] = np.ascontiguousarray(z.T)
    c["negt"] = np.ascontiguousarray((-t[:, 0]).reshape(NT, P).T.astype(np.float32))
    max_decay = math.log(1e-2) / 0.3; min_decay = math.log(1e-2) / 1.5
    deltas = np.abs(np.linspace(min_decay, max_decay, HYW, dtype=np.float32))
    c["deltab"] = np.ascontiguousarray(np.broadcast_to(deltas[None, :], (P, HYW)).astype(np.float32))
    n_freq = 32
    inv = (1.0 / (10000.0 ** np.linspace(0.0, 1.0, n_freq, dtype=np.float32))).astype(np.float32)
    def rot(pa, pb):
        a = np.concatenate([pa[:, None] * inv, pb[:, None] * inv], axis=-1).astype(np.float32)
        return np.cos(a).astype(np.float32), np.sin(a).astype(np.float32)
    rows = L // 64
    lat_row = np.repeat(np.arange(rows, dtype=np.float32), 64)
    lat_col = np.tile(np.arange(64, dtype=np.float32), rows)
    cx, sx = rot(lat_row, lat_col)
    cp = np.arange(LC, dtype=np.float32)
    cc, sc = rot(cp, cp)
    c["rot_x"] = np.ascontiguousarray(np.stack([np.tile(cx, (1, 4)), np.tile(sx, (1, 4))], axis=1))
    c["rot_c"] = np.ascontiguousarray(np.stack([np.tile(cc, (1, 4)), np.tile(sc, (1, 4))], axis=1))
    ks = np.float32(128 ** -0.5)
    c["rot_xk"] = np.ascontiguousarray(c["rot_x"] * ks); c["rot_ck"] = np.ascontiguousarray(c["rot_c"] * ks)
    pidx = np.arange(P, dtype=np.float32)
    m = {}
    m["ident"] = np.eye(P, dtype=np.float32)
    m["ones"] = np.ones((P, P), np.float32)
    jj = pidx[:, None]; ii = pidx[None, :]
    m["diffF"] = np.maximum(ii - jj, 0.0); m["maskF"] = (ii >= jj).astype(np.float32)
    m["diffB"] = np.maximum(jj - ii, 0.0); m["maskB"] = (jj >= ii).astype(np.float32)
    m["ip1"] = np.broadcast_to(ii + 1.0, (P, P)).copy(); m["i128m"] = np.broadcast_to(128.0 - ii, (P, P)).copy()
    m["tri"] = (jj < ii).astype(np.float32)
    m["iota512"] = np.broadcast_to(np.arange(CAP, dtype=np.float32)[None, :], (P, CAP)).copy()
    cols = np.zeros((P, 8), np.float32)
    cols[:, 0] = 127.0 - pidx; cols[:, 1] = pidx; cols[:, 2] = 255.0 - pidx; cols[:, 3] = 127.0 - pidx
    cols[:, 4] = pidx; cols[:, 5] = 128.0 + pidx
    cols[:, 6] = EPS; cols[:, 7] = 1.0
    m["cols"] = cols
    tv = np.zeros((P, NT, 2), np.float32)
    tok = (np.arange(NT)[None, :] * P + np.arange(P)[:, None])
    tv[:, :, 0] = tok // 64; tv[:, :, 1] = tok % 64
    m["tvals"] = tv.reshape(P, NT * 2)
    sel = np.zeros((P, P), np.float32); sel[0, :] = 1.0
    m["sel0"] = sel
    off = {}; parts = []; o = 0
    for k, v in m.items():
        off[k] = (o, v.shape[1]); parts.append(v.astype(np.float32)); o += v.shape[1]
    c["misc"] = np.ascontiguousarray(np.concatenate(parts, axis=1))
    c["misc_off"] = off
    _CONST_CACHE.update(c)
    return c


class Stream:
    def __init__(self, jobs, ahead=1):
        self.jobs = jobs; self.done = {}; self.k = 0; self.ahead = ahead

    def next(self):
        k = self.k
        for j in range(k, min(k + self.ahead + 1, len(self.jobs))):
            if j not in self.done:
                self.done[j] = self.jobs[j]()
        self.k += 1
        return self.done.pop(k)


def col_layout(v):
    v = np.asarray(v, np.float32).reshape(-1, P)
    return np.ascontiguousarray(v.T)


class K:
    def __init__(self, upto="all", dbg=()):
        self.upto = upto; self.dbg = set(dbg)
        self.nc = bass.Bass("TRN2", target_bir_lowering=False)
        self.inputs = {}
        self.outputs = []

    def inp(self, name, shape, dt=F32):
        r = self.C.dram(name, shape, dt, kind="ExternalInput"); self.inputs[name] = r; return r

    def scratch(self, name, shape, dt):
        kind = "ExternalOutput" if name in self.dbg else "Internal"
        if name in self.dbg:
            self.outputs.append(name)
        return self.C.dram(name, shape, dt, kind=kind)

    def load(self, q, dst, dst_ap, src, src_ap):
        return self.C.dma(q, lambda e: e.dma_start(out=dst_ap, in_=src_ap), reads=[src], writes=[dst])

    def store(self, q, dst, dst_ap, src, src_ap):
        return self.C.dma(q, lambda e: e.dma_start(out=dst_ap, in_=src_ap), reads=[src], writes=[dst], semres=src)

    def nextps(self):
        r = self.pbanks[self.pbi % len(self.pbanks)]; self.pbi += 1; return r

    def mm(self, pr, out_ap, pairs, reads):
        n = len(pairs)

        def fn(e):
            inst = None
            for i, (l, r) in enumerate(pairs):
                inst = e.matmul(out_ap, l, r, start=(i == 0), stop=(i == n - 1))
            return inst
        return self.C.op("pe", fn, reads=reads, writes=[pr])

    def cast_eng(self):
        self.ce = (self.ce + 1) % 3
        return ("dve", "act", "pool")[self.ce]

    def copy(self, eng, dst, dst_ap, src, src_ap, extra_reads=()):
        if eng == "act":
            return self.C.op("act", lambda e: e.activation(out=dst_ap, in_=src_ap, func=AF.Copy), reads=[src, *extra_reads], writes=[dst])
        return self.C.op(eng, lambda e: e.tensor_copy(dst_ap, src_ap), reads=[src, *extra_reads], writes=[dst])

    def build(self):
        nc = self.nc
        with ExitStack() as es:
            self.C = C = Ctx(nc, es)
            self.pbi = 0; self.ce = 0
            hc = host_constants()
            self.moff = hc["misc_off"]
            nmisc = hc["misc"].shape[1]
            I = self.inp
            x = I("x", [T, D]); ctx = I("ctx", [LC, D]); cT = I("cT", [P, 64])
            ada_w = I("ada_w", [D, 6 * D]); abT = I("abT", [P, 192])
            g1T = I("g1T", [P, 32]); g2b = I("g2b", [P, D]); fgb = I("fgb", [P, D]); abb = I("abb", [P, 4 * D])
            w_in = I("w_in", [D, INW]); w_out = I("w_out", [D, D])
            swT = I("swT", [P, 48 * 4])
            fw1 = I("fw1", [33, 64]); fw2 = I("fw2", [64, 64]); fw3 = I("fw3", [64, 64]); fw4 = I("fw4", [64, 8192])
            fvec = I("fvec", [64, 4])
            hyb = I("hyb", [P, 4096]); rdec = I("rdec", [P, 16])
            rwT = I("rwT", [P, 32 * 16])
            cst = I("cst", [NFT, P, 2 * NFT * P], BF16); fscale = I("fscale", [P, NFT])
            zT = I("zT", [33, T]); negt = I("negt", [P, NT]); deltab = I("deltab", [P, HYW])
            rot_x = I("rot_x", [T, 512]); rot_c = I("rot_c", [LC, 512]); rot_xk = I("rot_xk", [T, 512]); rot_ck = I("rot_ck", [LC, 512]); misc = I("misc", [P, nmisc])
            if self.upto in ("all", "J"):
                wg = I("wg", [NL, D, EFF]); wu = I("wu", [NL, D, EFF]); wd = I("wd", [NL, EFF, D])
            out = C.dram("out", [T, D], F32, kind="ExternalOutput")
            self.outputs.append("out")
            S = self.scratch
            hxT = S("hxT", [D, T], BF16)
            v_tm = S("v_tm", [T, HYW], BF16); x1_tm = S("x1_tm", [T, HYW], BF16); x2_cm = S("x2_cm", [HYW, T], BF16)
            sg = S("sg", [GC, T], BF16)
            qT_d = S("qT_d", [QC, T], BF16); kT_d = S("kT_d", [KC, T], BF16)
            k_tm = S("k_tm", [T, KC], BF16); vr_tm = S("vr_tm", [T, VC], BF16)
            ed = S("ed", [T, 2 * 4096], BF16)
            spec = S("spec", [NFT * P, 2 * 4096], F32)
            yT = S("yT", [6144, T], BF16)
            x1r = S("x1r", [T, D], F32); hx2 = S("hx2", [T, D], BF16)
            affd = S("affd", [T, NE], F32)
            ffn = [S("ffn%d" % i, [T, 512], F32) for i in range(8)]
            ffr = [S("ffr%d" % i, [T, 512], F32) for i in range(8)]
            dbgs = S("dbgs", [P, 4096], F32)
            self.dbgs_res = dbgs

            mt = C.sb("misc_t", [P, nmisc], F32)
            self.load("sp", mt, mt.t[:], misc, misc.t.ap())

            def M(name, rows=P):
                o, w = self.moff[name]
                return mt.t[0:rows, o:o + w]
            self.M = M
            identb = C.sb("identb", [P, P], BF16); onesb = C.sb("onesb", [P, P], BF16)
            C.op("dve", lambda e: e.tensor_copy(identb.t[:], M("ident")), reads=[mt], writes=[identb])
            C.op("dve", lambda e: e.tensor_copy(onesb.t[:], M("ones")), reads=[mt], writes=[onesb])
            self.identb = identb; self.onesb = onesb; self.mt = mt
            self.pbanks = [C.ps("pb%d" % i, [P, 512], F32) for i in range(7)]
            self.pacc = C.ps("pacc", [P, 512], F32)
            self.slab_i = 0
            modsT = C.sb("modsT", [P, 192, 2], F32)
            AB = C.sb("AB", [P, 4, 32], F32)
            bcd = S("bcd", [P, 4 * D], F32)
            lg = C.sb("lg", [P, 16], F32)
            Sfd = S("Sfd", [P, 16 * 256], F32)

            self.phase_A(cT, ada_w, abT, g1T, g2b, abb, modsT, AB, bcd)
            if "modsT" in self.dbg:
                pass
            if self.upto == "A":
                return self.finish(dbgs, [(modsT, modsT.t[:].rearrange("p a b -> p (a b)"), 384), (AB, AB.t[:].rearrange("p a b -> p (a b)"), 128)])
            C.push()
            hcT = C.sb("hcT", [P, 32, LC], BF16)
            Sf = C.sb("Sf", [P, 16, 256], F32); Sb = C.sb("Sb", [P, 16, 256], BF16)
            self.stg_i = 0
            self.phase_B(x, ctx, AB, hxT, hcT)
            if self.upto == "B":
                tmpd = C.sb("tmpd", [P, 512], F32)
                C.op("dve", lambda e: e.tensor_copy(tmpd.t[:, 0:256], hcT.t[:, 0, :]), reads=[hcT], writes=[tmpd])
                C.op("dve", lambda e: e.tensor_copy(tmpd.t[:, 256:512], hcT.t[:, 31, :]), reads=[hcT], writes=[tmpd])
                return self.finish(dbgs, [(tmpd, tmpd.t[:], 512)])
            self.phase_C(hcT, w_in, rot_ck, rdec, lg, Sf, Sb)
            self.store("sp", Sfd, Sfd.t.ap(), Sf, Sf.t[:].rearrange("p a b -> p (a b)"))
            if self.upto == "C":
                return self.finish(dbgs, [(lg, lg.t[:], 16), (Sf, Sf.t[:, 0, :], 256), (Sf, Sf.t[:, 15, :], 256)])
            C.pop()
            self.phase_D(hxT, w_in, swT, rot_x, rot_xk, v_tm, x1_tm, x2_cm, sg, qT_d, kT_d, k_tm, vr_tm)
            if self.upto == "D":
                return self.finish(dbgs, [])
            self.phase_E(fw1, fw2, fw3, fw4, fvec, zT, negt, deltab, hyb, cst, fscale, spec)
            if self.upto == "E":
                return self.finish(dbgs, [])
            self.phase_F(v_tm, x1_tm, x2_cm, spec, cst, yT)
            if self.upto == "F":
                return self.finish(dbgs, [])
            self.phase_G(qT_d, kT_d, k_tm, vr_tm, sg, Sfd, lg, yT)
            if self.upto == "G":
                return self.finish(dbgs, [])
            aff_tm = C.sb("aff_tm", [P, NT, NE], F32)
            self.phase_H(yT, w_out, x, bcd, x1r, hx2, rwT, aff_tm)
            if self.upto == "H":
                return self.finish(dbgs, [(aff_tm, aff_tm.t[:].rearrange("p a b -> p (a b)"), 512)])
            idx_all = C.sb("idx_all", [P, NL, 4], I32); gate_all = C.sb("gate_all", [P, NL, 4], F32)
            self.phase_I(aff_tm, idx_all, gate_all)
            if self.upto == "I":
                idf2 = C.sb("idf2", [P, 64], F32)
                C.op("dve", lambda e: e.tensor_copy(idf2.t[:], idx_all.t[:].rearrange("p a b -> p (a b)")), reads=[idx_all], writes=[idf2])
                return self.finish(dbgs, [(idf2, idf2.t[:], 64), (gate_all, gate_all.t[:].rearrange("p a b -> p (a b)"), 64)])
            self.phase_J(hx2, wg, wu, wd, idx_all, gate_all, ffn, ffr)
            self.phase_K(x1r, ffn, bcd, fgb, out)
            return self.finish(dbgs, [])

    def finish(self, dbgs, dumps):
        C = self.C
        o = 0
        for (r, ap, w) in dumps:
            self.store("sp", dbgs, dbgs.t.ap()[:, o:o + w], r, ap); o += w
        self.outputs.append("dbgs") if "dbgs" in self.dbg else None
        C.barrier()
        return self.nc

    def phase_A(self, cT, ada_w, abT, g1T, g2b, abb, modsT, AB, bcd):
        C = self.C; M = self.M
        C.push()
        bc = C.sb("bc", [P, 4, D], F32)
        ct = C.sb("ct", [P, 64], F32); sct = C.sb("sct", [P, 32, 2], F32)
        abt = C.sb("abt", [P, 192], F32); g1t = C.sb("g1t", [P, 32], F32)
        self.load("sp", ct, ct.t[:], cT, cT.t.ap())
        self.load("sp", abt, abt.t[:], abT, abT.t.ap())
        self.load("sp", g1t, g1t.t[:], g1T, g1T.t.ap())
        C.op("act", lambda e: e.activation(out=sct.t[:].rearrange("p a b -> p (a b)"), in_=ct.t[:], func=AF.Silu), reads=[ct], writes=[sct])
        wbuf = [C.sb("aw%d" % i, [P, 8, 512], F32) for i in range(5)]
        row = [C.sb("row%d" % i, [2, 512], F32) for i in range(2)]
        awv = ada_w.t.ap().rearrange("(kc p) n -> p kc n", p=P)
        wi = 0
        bmap = {2: 0, 5: 1, 4: 2, 3: 3}
        for nb in range(48):
            pr = self.nextps()
            halves = []
            for h in range(4):
                wb = wbuf[wi % 5]; wi += 1
                self.load("sp" if (wi % 2) else "act", wb, wb.t[:], ada_w, awv[:, h * 8:(h + 1) * 8, nb * 512:(nb + 1) * 512])
                halves.append(wb)
            def fn(e, halves=halves, pr=pr):
                inst = None
                for h in range(4):
                    for k in range(8):
                        kc = h * 8 + k
                        inst = e.matmul(pr.t[0:2, :], sct.t[:, kc, :], halves[h].t[:, k, :], start=(kc == 0), stop=(kc == 31))
                return inst
            C.op("pe", fn, reads=[sct, *halves], writes=[pr])
            rw = row[nb % 2]
            C.op("dve", lambda e, rw=rw, pr=pr: e.tensor_copy(rw.t[:], pr.t[0:2, :]), reads=[pr], writes=[rw])
            pt = self.nextps()
            def fn2(e, rw=rw, pt=pt):
                inst = None
                for j in range(4):
                    inst = e.transpose(pt.t[:, j * 2:j * 2 + 2], rw.t[0:2, j * 128:(j + 1) * 128], M("ident", 2)[:, 0:2])
                return inst
            C.op("pe", fn2, reads=[rw, self.mt], writes=[pt])
            C.op("dve", lambda e, pt=pt, nb=nb: e.tensor_copy(modsT.t[:, nb * 4:(nb + 1) * 4, :].rearrange("p a b -> p (a b)"), pt.t[:, 0:8]), reads=[pt], writes=[modsT])
            mod = nb // 8
            if mod in bmap:
                pb = self.nextps()
                C.op("pe", lambda e, pb=pb, rw=rw: e.matmul(pb.t[:, :], M("sel0", 2), rw.t[0:2, :], start=True, stop=True), reads=[rw, self.mt], writes=[pb])
                j = bmap[mod]; cb = (nb % 8) * 512
                C.op("act", lambda e, pb=pb, j=j, cb=cb: e.activation(out=bc.t[:, j, cb:cb + 512], in_=pb.t[:, :], func=AF.Copy), reads=[pb], writes=[bc])
        for r in range(2):
            C.op("dve", lambda e, r=r: e.tensor_tensor(out=modsT.t[:, :, r], in0=modsT.t[:, :, r], in1=abt.t[:], op=ALU.add), reads=[modsT, abt], writes=[modsT])
        for r in range(2):
            C.op("dve", lambda e, r=r: e.scalar_tensor_tensor(out=AB.t[:, 2 * r, :], in0=modsT.t[:, 32:64, r], scalar=1.0, in1=g1t.t[:], op0=ALU.add, op1=ALU.mult), reads=[modsT, g1t], writes=[AB])
            C.op("dve", lambda e, r=r: e.tensor_copy(AB.t[:, 2 * r + 1, :], modsT.t[:, 0:32, r]), reads=[modsT], writes=[AB])
        tmpb = C.sb("tmpb", [P, D], F32)
        for j in range(4):
            self.load("sp", tmpb, tmpb.t[:], abb, abb.t.ap()[:, j * D:(j + 1) * D])
            C.op("dve", lambda e, j=j: e.tensor_tensor(out=bc.t[:, j, :], in0=bc.t[:, j, :], in1=tmpb.t[:], op=ALU.add), reads=[bc, tmpb], writes=[bc])
        self.load("sp", tmpb, tmpb.t[:], g2b, g2b.t.ap())
        C.op("dve", lambda e: e.scalar_tensor_tensor(out=bc.t[:, 2, :], in0=bc.t[:, 2, :], scalar=1.0, in1=tmpb.t[:], op0=ALU.add, op1=ALU.mult), reads=[bc, tmpb], writes=[bc])
        self.store("sp", bcd, bcd.t.ap(), bc, bc.t[:].rearrange("p a b -> p (a b)"))
        C.pop()

    def psb(self, pr):
        return pr.t[:, :].bitcast(BF16)

    def rms_stats(self, xt, junk, st, width):
        C = self.C; M = self.M
        C.op("act", lambda e: e.activation(out=junk.t[:, 0:width], in_=xt.t[:, 0:width], func=AF.Square, accum_out=st.t[:, 0:1]), reads=[xt], writes=[junk, st])
        C.op("act", lambda e: e.activation(out=st.t[:, 2:3], in_=st.t[:, 0:1], func=AF.Sqrt, scale=1.0 / width, bias=M("cols")[:, 6:7]), reads=[st, self.mt], writes=[st])
        C.op("dve", lambda e: e.reciprocal(st.t[:, 1:2], st.t[:, 2:3]), reads=[st], writes=[st])

    def phase_B(self, x, ctx, AB, hxT, hcT):
        C = self.C; M = self.M
        C.push()
        xt = [C.sb("xt%d" % i, [P, D], F32) for i in range(2)]
        xn = [C.sb("xn%d" % i, [P, D], BF16) for i in range(2)]
        junk = C.sb("junkB", [P, D], BF16)
        sts = [C.sb("stB%d" % i, [P, 4], F32) for i in range(2)]
        hb = [C.sb("hbB%d" % i, [P, 32, 512], BF16) for i in range(2)]
        hview = hxT.t.ap().rearrange("(kc p) t -> p kc t", p=P)
        ev = 0
        for tt in range(NT + 2):
            isc = tt >= NT
            X = xt[tt % 2]; XN = xn[tt % 2]; st = sts[tt % 2]
            if isc:
                self.load("sp", X, X.t[:], ctx, ctx.t.ap()[(tt - NT) * P:(tt - NT + 1) * P, :])
            else:
                self.load("sp" if tt % 2 else "act", X, X.t[:], x, x.t.ap()[tt * P:(tt + 1) * P, :])
            self.rms_stats(X, junk, st, D)
            C.op("pool", lambda e, X=X, XN=XN, st=st: e.tensor_scalar(out=XN.t[:], in0=X.t[:], scalar1=st.t[:, 1:2], scalar2=None, op0=ALU.mult), reads=[X, st], writes=[XN])
            a_i = 2 if isc else 0
            H = hb[(tt // 4) % 2]
            for g in range(4):
                pr = self.nextps()
                pv = self.psb(pr)
                def fn(e, XN=XN, pv=pv, g=g):
                    inst = None
                    for j in range(8):
                        kc = g * 8 + j
                        inst = e.transpose(pv[:, j * P:(j + 1) * P], XN.t[:, kc * P:(kc + 1) * P], self.identb.t[:])
                    return inst
                C.op("pe", fn, reads=[XN, self.identb], writes=[pr])
                for j in range(8):
                    kc = g * 8 + j
                    if isc:
                        dst, dap = hcT, hcT.t[:, kc, (tt - NT) * P:(tt - NT + 1) * P]
                    else:
                        dst, dap = H, H.t[:, kc, (tt % 4) * P:(tt % 4 + 1) * P]
                    ev += 1
                    if ev % 2:
                        C.op("dve", lambda e, dap=dap, pv=pv, j=j, kc=kc, a_i=a_i: e.tensor_scalar(out=dap, in0=pv[:, j * P:(j + 1) * P], scalar1=AB.t[:, a_i, kc:kc + 1], scalar2=AB.t[:, a_i + 1, kc:kc + 1], op0=ALU.mult, op1=ALU.add), reads=[pr, AB], writes=[dst])
                    else:
                        C.op("act", lambda e, dap=dap, pv=pv, j=j, kc=kc, a_i=a_i: e.activation(out=dap, in_=pv[:, j * P:(j + 1) * P], func=AF.Identity, scale=AB.t[:, a_i, kc:kc + 1], bias=AB.t[:, a_i + 1, kc:kc + 1]), reads=[pr, AB], writes=[dst])
            if (not isc) and tt % 4 == 3:
                tb = tt // 4
                self.store("sp", hxT, hview[:, :, tb * 512:(tb + 1) * 512], H, H.t[:])
        C.pop()

    def load_w_bf(self, wsrc, view, stg, wb):
        C = self.C
        for q in range(4):
            s = stg[self.stg_i % len(stg)]; self.stg_i += 1
            self.load("sp" if q % 2 else "act", s, s.t[:], wsrc, view[:, q * 8:(q + 1) * 8, :])
            self.copy(self.cast_eng(), wb, wb.t[:, q * 8:(q + 1) * 8, :], s, s.t[:])

    def rotary(self, pr, rt, rtap, dst, dap, tmps):
        C = self.C
        pv = pr.t[:, :].rearrange("p (h two f) -> p h two f", h=4, two=2)
        x1 = pv[:, :, 0, :]; x2 = pv[:, :, 1, :]
        cs = rtap[:, 0, :].rearrange("p (h f) -> p h f", h=4); sn = rtap[:, 1, :].rearrange("p (h f) -> p h f", h=4)
        t1, t2 = tmps
        t1v = t1.t[:, :].rearrange("p (h f) -> p h f", h=4); t2v = t2.t[:, :].rearrange("p (h f) -> p h f", h=4)
        dv = dap.rearrange("p (h two f) -> p h two f", h=4, two=2)
        C.op("dve", lambda e: e.tensor_tensor(out=t1v, in0=x1, in1=cs, op=ALU.mult), reads=[pr, rt], writes=[t1])
        C.op("dve", lambda e: e.tensor_tensor(out=t2v, in0=x2, in1=sn, op=ALU.mult), reads=[pr, rt], writes=[t2])
        C.op("dve", lambda e: e.tensor_tensor(out=dv[:, :, 0, :], in0=t1v, in1=t2v, op=ALU.subtract), reads=[t1, t2], writes=[dst])
        C.op("dve", lambda e: e.tensor_tensor(out=t1v, in0=x1, in1=sn, op=ALU.mult), reads=[pr, rt], writes=[t1])
        C.op("dve", lambda e: e.tensor_tensor(out=t2v, in0=x2, in1=cs, op=ALU.mult), reads=[pr, rt], writes=[t2])
        C.op("dve", lambda e: e.tensor_tensor(out=dv[:, :, 1, :], in0=t1v, in1=t2v, op=ALU.add), reads=[t1, t2], writes=[dst])

    def phase_C(self, hcT, w_in, rot_ck, rdec, lg, Sf, Sb):
        C = self.C; M = self.M
        C.push()
        rd = C.sb("rd", [P, 16], F32)
        self.load("sp", rd, rd.t[:], rdec, rdec.t.ap())
        C.op("act", lambda e: e.activation(out=rd.t[:], in_=rd.t[:], func=AF.Exp, scale=-1.0), reads=[rd], writes=[rd])
        C.op("act", lambda e: e.activation(out=rd.t[:], in_=rd.t[:], func=AF.Ln, bias=M("cols")[:, 7:8]), reads=[rd, self.mt], writes=[rd])
        C.op("dve", lambda e: e.tensor_scalar(out=lg.t[:], in0=rd.t[:], scalar1=-1.0, scalar2=None, op0=ALU.mult), reads=[rd], writes=[lg])
        wctx = C.sb("wctx", [P, 2, 16], F32)
        for tt in range(2):
            for d in range(2):
                col = 2 + tt if d == 0 else 4 + tt
                C.op("act", lambda e, tt=tt, d=d, col=col: e.activation(out=wctx.t[:, tt, d * 8:(d + 1) * 8], in_=lg.t[:, d * 8:(d + 1) * 8], func=AF.Exp, scale=M("cols")[:, col:col + 1]), reads=[lg, self.mt], writes=[wctx])
        import os
        cstop = int(os.environ.get("CSTOP", "9"))
        if cstop <= 1:
            C.pop(); return
        stg = [C.sb("stgC%d" % i, [P, 8, 512], F32) for i in range(3)]
        wb = [C.sb("wbC%d" % i, [P, 32, 512], BF16) for i in range(2)]
        kct = C.sb("kct", [P, 2, KC], BF16); vct = C.sb("vct", [P, 2, VC], BF16)
        rt = C.sb("rtC", [P, 2, 2, 256], F32)
        tm = [C.sb("tmC%d" % i, [P, 256], F32) for i in range(2)]
        self.load("sp", rt, rt.t[:].rearrange("p a b c -> p a (b c)"), rot_ck, rot_ck.t.ap().rearrange("(a p) c -> p a c", p=P))
        wv = w_in.t.ap().rearrange("(kc p) n -> p kc n", p=P)
        for nb in range(6):
            W = wb[nb % 2]
            self.load_w_bf(w_in, wv[:, :, KV0 + nb * 512:KV0 + (nb + 1) * 512], stg, W)
            for tt in range(2):
                pr = self.nextps()
                self.mm(pr, pr.t[:, :], [(hcT.t[:, kc, tt * P:(tt + 1) * P], W.t[:, kc, :]) for kc in range(32)], [hcT, W])
                if nb < 2 and cstop <= 2:
                    self.copy("act", kct, kct.t[:, tt, nb * 512:(nb + 1) * 512], pr, pr.t[:, :])
                elif nb < 2:
                    self.rotary(pr, rt, rt.t[:, tt], kct, kct.t[:, tt, nb * 512:(nb + 1) * 512], tm)
                else:
                    self.copy("act", vct, vct.t[:, tt, (nb - 2) * 512:(nb - 1) * 512], pr, pr.t[:, :])
        if cstop <= 3:
            C.pop(); return
        kd = [C.sb("kdC%d" % i, [P, P], BF16) for i in range(4)]
        ki = 0
        for d in range(2):
            for h in range(8):
                pr = self.nextps()
                pairs = []
                kds = []
                for tt in range(2):
                    k_ = kd[ki % 4]; ki += 1
                    C.op("dve", lambda e, k_=k_, tt=tt, h=h, d=d: e.tensor_scalar(out=k_.t[:], in0=kct.t[:, tt, h * P:(h + 1) * P], scalar1=wctx.t[:, tt, d * 8 + h:d * 8 + h + 1], scalar2=None, op0=ALU.mult), reads=[kct, wctx], writes=[k_])
                    pairs.append((k_.t[:], vct.t[:, tt, h * 256:(h + 1) * 256])); kds.append(k_)
                if cstop <= 4:
                    continue
                self.mm(pr, pr.t[:, 0:256], pairs, [vct, *kds])
                if cstop <= 5:
                    continue
                C.op("dve", lambda e, pr=pr, d=d, h=h: e.tensor_copy(Sf.t[:, d * 8 + h, :], pr.t[:, 0:256]), reads=[pr], writes=[Sf])
                if cstop <= 6:
                    continue
                C.op("pool", lambda e, d=d, h=h: e.tensor_copy(Sb.t[:, d * 8 + h, :], Sf.t[:, d * 8 + h, :]), reads=[Sf], writes=[Sb])
        C.pop()


    def phase_D(self, hxT, w_in, swT, rot_x, rot_xk, v_tm, x1_tm, x2_cm, sg, qT_d, kT_d, k_tm, vr_tm):
        C = self.C; M = self.M
        C.push()
        stg = [C.sb("stgD%d" % i, [P, 2, 512], F32) for i in range(3)]
        wb = [C.sb("wbD%d" % i, [P, 32, 512], BF16) for i in range(2)]
        hb = [C.sb("hbD%d" % i, [P, 32, 512], BF16) for i in range(2)]
        prow = [C.sb("prow%d" % i, [P, 4098], BF16) for i in range(4)]
        swt = C.sb("swt", [P, 48, 4], F32)
        self.load("sp", swt, swt.t[:].rearrange("p a b -> p (a b)"), swT, swT.t.ap())
        ctmp = C.sb("ctmp", [P, 1024], F32)
        uo = [C.sb("uo%d" % i, [P, 1024], BF16) for i in range(2)]
        utm = [C.sb("utm%d" % i, [P, 8, P], BF16) for i in range(2)]
        rt = [C.sb("rtD%d" % i, [P, 2, 256], F32) for i in range(2)]
        tm = [C.sb("tmD%d" % i, [P, 256], F32) for i in range(2)]
        qk = [C.sb("qk%d" % i, [P, 512], BF16) for i in range(2)]
        qkT = [C.sb("qkT%d" % i, [P, 4, P], BF16) for i in range(2)]
        ot = [C.sb("otD%d" % i, [P, 512], BF16) for i in range(2)]
        for pw in prow:
            C.op("dve", lambda e, pw=pw: e.memset(pw.t[:, 0:1], 0.0), writes=[pw])
            C.op("dve", lambda e, pw=pw: e.memset(pw.t[:, 4097:4098], 0.0), writes=[pw])
        wv = w_in.t.ap().rearrange("(kc p) n -> p kc n", p=P)
        hv = hxT.t.ap().rearrange("(kc p) t -> p kc t", p=P)
        groups = []
        for g in range(28):
            col0 = g * 512
            if g < 12: kind = "hy"
            elif g < 14: kind = "q"
            elif g < 22: kind = "gate"
            elif g < 24: kind = "k"
            else: kind = "v"
            groups.append((col0, kind))
        cnt = dict(stg=0, hb=0, uo=0, utm=0, rt=0, qk=0, qkT=0, ot=0, ev=0)

        def nxt(lst, key):
            r = lst[cnt[key] % len(lst)]; cnt[key] += 1; return r
        def wpieces(g, lo, hi):
            if g >= len(groups):
                return
            c0 = groups[g][0]; Wn = wb[g % 2]
            for q in range(lo, hi):
                s_ = nxt(stg, "stg")
                self.load("sp", s_, s_.t[:], w_in, wv[:, q * 2:(q + 1) * 2, c0:c0 + 512])
                self.copy(self.cast_eng(), Wn, Wn.t[:, q * 2:(q + 1) * 2, :], s_, s_.t[:])

        def hjob(k):
            def job():
                Hn = hb[k % 2]
                self.load("sp", Hn, Hn.t[:], hxT, hv[:, :, (k % 8) * 512:(k % 8 + 1) * 512])
                return Hn
            return job
        hst = Stream([hjob(k) for k in range(28 * 8)])
        wpieces(0, 0, 16)
        for gi, (col0, kind) in enumerate(groups):
            W = wb[gi % 2]
            for tb in range(8):
                H = hst.next()
                wpieces(gi + 1, tb * 2, tb * 2 + 2)
                if kind in ("hy", "gate"):
                    for ct in range(4):
                        pr = self.nextps()
                        self.mm(pr, pr.t[:, :], [(W.t[:, kc, ct * P:(ct + 1) * P], H.t[:, kc, :]) for kc in range(32)], [W, H])
                        if kind == "hy":
                            pw = prow[ct]
                            cnt["ev"] += 1
                            self.copy("act" if cnt["ev"] % 2 else "dve", pw, pw.t[:, 1 + tb * 512:1 + (tb + 1) * 512], pr, pr.t[:, :])
                        else:
                            O = nxt(ot, "ot")
                            C.op("act", lambda e, O=O, pr=pr: e.activation(out=O.t[:], in_=pr.t[:, :], func=AF.Silu), reads=[pr], writes=[O])
                            r0 = (col0 - 7168) + ct * P
                            self.store("sp", sg, sg.t.ap()[r0:r0 + P, tb * 512:(tb + 1) * 512], O, O.t[:])
                else:
                    for tt in range(4):
                        pr = self.nextps()
                        self.mm(pr, pr.t[:, :], [(H.t[:, kc, tt * P:(tt + 1) * P], W.t[:, kc, :]) for kc in range(32)], [W, H])
                        tok0 = tb * 512 + tt * P
                        if kind in ("q", "k"):
                            R = nxt(rt, "rt")
                            tab = rot_x if kind == "q" else rot_xk
                            self.load("sp", R, R.t[:].rearrange("p a b -> p (a b)"), tab, tab.t.ap()[tok0:tok0 + P, :])
                            Q = nxt(qk, "qk")
                            self.rotary(pr, R, R.t[:], Q, Q.t[:, :], tm)
                            if kind == "k":
                                c0 = col0 - 11264
                                self.store("sp", k_tm, k_tm.t.ap()[tok0:tok0 + P, c0:c0 + 512], Q, Q.t[:])
                                dstT = kT_d; hh0 = c0 // P
                            else:
                                dstT = qT_d; hh0 = (col0 - 6144) // P
                            pt = self.nextps(); pv = self.psb(pt)

                            def fnT(e, pv=pv, Q=Q):
                                inst = None
                                for h in range(4):
                                    inst = e.transpose(pv[:, h * P:(h + 1) * P], Q.t[:, h * P:(h + 1) * P], self.identb.t[:])
                                return inst
                            C.op("pe", fnT, reads=[Q, self.identb], writes=[pt])
                            QT = nxt(qkT, "qkT")
                            self.copy("act", QT, QT.t[:].rearrange("p a b -> p (a b)"), pt, pv[:, 0:512])
                            self.store("sp", dstT, dstT.t.ap().rearrange("(h p) t -> p h t", p=P)[:, hh0:hh0 + 4, tok0:tok0 + P], QT, QT.t[:])
                        else:
                            O = nxt(ot, "ot")
                            cnt["ev"] += 1
                            self.copy("act" if cnt["ev"] % 2 else "dve", O, O.t[:], pr, pr.t[:, :])
                            c0 = col0 - 12288
                            self.store("sp", vr_tm, vr_tm.t.ap()[tok0:tok0 + P, c0:c0 + 512], O, O.t[:])
            if kind == "hy":
                for ct in range(4):
                    colt = col0 // P + ct
                    pw = prow[ct]
                    for ch in range(4):
                        s0 = ch * 1024
                        C.op("dve", lambda e, pw=pw, s0=s0, colt=colt: e.tensor_scalar(out=ctmp.t[:], in0=pw.t[:, s0:s0 + 1024], scalar1=swt.t[:, colt, 0:1], scalar2=swt.t[:, colt, 3:4], op0=ALU.mult, op1=ALU.add), reads=[pw, swt], writes=[ctmp])
                        C.op("dve", lambda e, pw=pw, s0=s0, colt=colt: e.scalar_tensor_tensor(out=ctmp.t[:], in0=pw.t[:, s0 + 1:s0 + 1025], scalar=swt.t[:, colt, 1:2], in1=ctmp.t[:], op0=ALU.mult, op1=ALU.add), reads=[pw, swt, ctmp], writes=[ctmp])
                        U = nxt(uo, "uo")
                        C.op("dve", lambda e, pw=pw, s0=s0, colt=colt, U=U: e.scalar_tensor_tensor(out=U.t[:], in0=pw.t[:, s0 + 2:s0 + 1026], scalar=swt.t[:, colt, 2:3], in1=ctmp.t[:], op0=ALU.mult, op1=ALU.add), reads=[pw, swt, ctmp], writes=[U])
                        if col0 >= 4096:
                            r0 = (col0 - 4096) + ct * P
                            self.store("sp", x2_cm, x2_cm.t.ap()[r0:r0 + P, s0:s0 + 1024], U, U.t[:])
                        else:
                            dst = v_tm if col0 < 2048 else x1_tm
                            cc0 = (col0 % 2048) + ct * P
                            pt = self.nextps(); pv = self.psb(pt)

                            def fnU(e, pv=pv, U=U):
                                inst = None
                                for j in range(8):
                                    inst = e.transpose(pv[:, j * P:(j + 1) * P], U.t[:, j * P:(j + 1) * P], self.identb.t[:])
                                return inst
                            C.op("pe", fnU, reads=[U, self.identb], writes=[pt])
                            UT = nxt(utm, "utm")
                            cnt["ev"] += 1
                            self.copy("act" if cnt["ev"] % 2 else "pool" if False else "act", UT, UT.t[:].rearrange("p a b -> p (a b)"), pt, pv[:, 0:1024])
                            self.store("sp", dst, dst.t.ap().rearrange("(tt p) c -> p tt c", p=P)[:, ch * 8:(ch + 1) * 8, cc0:cc0 + P], UT, UT.t[:])
        C.pop()


    def slab_stream(self, cst, seq, slabs):
        def mk(k, mt):
            def job():
                SL = slabs[k % len(slabs)]
                self.load("sp", SL, SL.t[:].rearrange("p a b c -> p (a b c)"), cst, cst.t.ap()[mt])
                return SL
            return job
        return Stream([mk(k, mt) for k, mt in enumerate(seq)])

    def phase_E(self, fw1, fw2, fw3, fw4, fvec, zT, negt, deltab, hyb, cst, fscale, spec):
        C = self.C; M = self.M
        PI = math.pi
        C.push()
        h3 = C.sb("h3", [64, T], F32)
        fv = C.sb("fv", [64, 4], F32)
        self.load("sp", fv, fv.t[:], fvec, fvec.t.ap())
        arg = C.sb("argE", [64, 512], F32); mk = C.sb("mkE", [64, 512], F32)
        C.push()
        zt = C.sb("zt", [33, T], F32); hA = C.sb("hA", [64, T], F32); hB = C.sb("hB", [64, T], F32)
        w1 = C.sb("w1", [33, 64], F32); w2 = C.sb("w2", [64, 64], F32); w3 = C.sb("w3", [64, 64], F32)
        self.load("sp", zt, zt.t[:], zT, zT.t.ap())
        self.load("sp", w1, w1.t[:], fw1, fw1.t.ap()); self.load("sp", w2, w2.t[:], fw2, fw2.t.ap()); self.load("sp", w3, w3.t[:], fw3, fw3.t.ap())
        layers = [(zt, 33, w1, hA), (hA, 64, w2, hB), (hB, 64, w3, h3)]
        for li, (src, kk, w, dst) in enumerate(layers):
            for nb in range(8):
                pr = self.nextps()
                C.op("pe", lambda e, pr=pr, w=w, src=src, kk=kk, nb=nb: e.matmul(pr.t[0:64, :], w.t[0:kk, :], src.t[0:kk, nb * 512:(nb + 1) * 512], start=True, stop=True), reads=[w, src], writes=[pr])
                C.op("dve", lambda e, pr=pr, li=li: e.tensor_scalar(out=arg.t[:], in0=pr.t[0:64, :], scalar1=fv.t[:, li:li + 1], scalar2=fv.t[:, 3:4], op0=ALU.add, op1=ALU.mult), reads=[pr, fv], writes=[arg])
                C.op("dve", lambda e: e.tensor_scalar(out=mk.t[:], in0=arg.t[:], scalar1=PI, scalar2=-2.0 * PI, op0=ALU.is_gt, op1=ALU.mult), reads=[arg], writes=[mk])
                C.op("dve", lambda e: e.tensor_tensor(out=arg.t[:], in0=arg.t[:], in1=mk.t[:], op=ALU.add), reads=[arg, mk], writes=[arg])
                C.op("dve", lambda e: e.tensor_scalar(out=mk.t[:], in0=arg.t[:], scalar1=-PI, scalar2=2.0 * PI, op0=ALU.is_lt, op1=ALU.mult), reads=[arg], writes=[mk])
                C.op("dve", lambda e: e.tensor_tensor(out=arg.t[:], in0=arg.t[:], in1=mk.t[:], op=ALU.add), reads=[arg, mk], writes=[arg])
                C.op("act", lambda e, dst=dst, nb=nb: e.activation(out=dst.t[:, nb * 512:(nb + 1) * 512], in_=arg.t[:], func=AF.Sin), reads=[arg], writes=[dst])
        C.pop()
        w4 = C.sb("w4", [64, 2, 512], F32)
        ngt = C.sb("ngt", [P, NT], F32); delt = C.sb("delt", [P, HYW], F32); hybt = C.sb("hybt", [P, 512], F32); fsc = C.sb("fsc", [P, NFT], F32)
        self.load("sp", ngt, ngt.t[:], negt, negt.t.ap()); self.load("sp", delt, delt.t[:], deltab, deltab.t.ap())
        self.load("sp", fsc, fsc.t[:], fscale, fscale.t.ap())
        edt = C.sb("edt", [P, 32, 2, 512], BF16)
        slabs = [C.sb("slabE%d" % i, [P, 2, NFT, P], BF16) for i in range(2)]
        sst = self.slab_stream(cst, [mt for _ in range(8) for mt in range(NFT)], slabs)
        dect = C.sb("dect", [P, 512], F32)
        tf = [C.sb("tfE%d" % i, [P, 512], F32) for i in range(2)]; tb = [C.sb("tbE%d" % i, [P, 512], F32) for i in range(2)]
        af = C.sb("afE", [P, 512], F32); ab = [C.sb("abE%d" % i, [P, 512], F32) for i in range(2)]
        rn = C.sb("rnE", [P, 512], F32)
        stt = [C.sb("stE%d" % i, [P, 2, 512], F32) for i in range(2)]
        pacc = self.pacc
        specv = spec.t.ap().rearrange("k (two c) -> k two c", two=2)
        si = 0
        for o in range(2):
            for cb in range(4):
                colF = o * 2048 + cb * 512; colB = 4096 + colF
                self.load("sp", hybt, hybt.t[:], hyb, hyb.t.ap()[:, colF:colF + 512])
                self.load("sp", w4, w4.t[:, 0, :], fw4, fw4.t.ap()[:, colF:colF + 512])
                self.load("sp", w4, w4.t[:, 1, :], fw4, fw4.t.ap()[:, colB:colB + 512])
                for lt in range(NT):
                    TF = tf[lt % 2]; TB = tb[lt % 2]; AB_ = ab[lt % 2]
                    prF = self.nextps(); prB = self.nextps()
                    C.op("pe", lambda e, prF=prF, lt=lt, colF=colF: e.matmul(prF.t[:, :], h3.t[:, lt * P:(lt + 1) * P], w4.t[:, 0, :], start=True, stop=True), reads=[h3, w4], writes=[prF])
                    C.op("pe", lambda e, prB=prB, lt=lt, colB=colB: e.matmul(prB.t[:, :], h3.t[:, lt * P:(lt + 1) * P], w4.t[:, 1, :], start=True, stop=True), reads=[h3, w4], writes=[prB])
                    C.op("act", lambda e, lt=lt, cb=cb: e.activation(out=dect.t[:], in_=delt.t[:, cb * 512:(cb + 1) * 512], func=AF.Exp, scale=ngt.t[:, lt:lt + 1]), reads=[delt, ngt], writes=[dect])
                    C.op("dve", lambda e, TF=TF, prF=prF: e.tensor_tensor(out=TF.t[:], in0=prF.t[:, :], in1=dect.t[:], op=ALU.mult), reads=[prF, dect], writes=[TF])
                    C.op("dve", lambda e, TB=TB, prB=prB: e.tensor_tensor(out=TB.t[:], in0=prB.t[:, :], in1=dect.t[:], op=ALU.mult), reads=[prB, dect], writes=[TB])
                    if lt == 0:
                        C.op("dve", lambda e, TB=TB: e.memset(TB.t[0:1, :], 0.0), reads=[TB], writes=[TB])
                    C.op("pool", lambda e, TF=TF, TB=TB, lt=lt: e.tensor_tensor(out=edt.t[:, lt, 0, :], in0=TF.t[:], in1=TB.t[:], op=ALU.add), reads=[TF, TB], writes=[edt])
                    C.op("pool", lambda e, TF=TF, TB=TB, lt=lt: e.tensor_tensor(out=edt.t[:, lt, 1, :], in0=TF.t[:], in1=TB.t[:], op=ALU.subtract), reads=[TF, TB], writes=[edt])
                    C.op("act", lambda e, TF=TF: e.activation(out=af.t[:], in_=TF.t[:], func=AF.Abs), reads=[TF], writes=[af])
                    C.op("act", lambda e, TB=TB, AB_=AB_: e.activation(out=AB_.t[:], in_=TB.t[:], func=AF.Abs), reads=[TB], writes=[AB_])
                    C.op("pool", lambda e, AB_=AB_: e.tensor_tensor(out=AB_.t[:], in0=AB_.t[:], in1=af.t[:], op=ALU.add), reads=[AB_, af], writes=[AB_])
                    C.op("pe", lambda e, AB_=AB_, lt=lt: e.matmul(pacc.t[:, :], M("ones"), AB_.t[:], start=(lt == 0), stop=(lt == NT - 1)), reads=[AB_, self.mt], writes=[pacc])
                C.op("dve", lambda e: e.reciprocal(rn.t[:], pacc.t[:, :]), reads=[pacc], writes=[rn])
                for mt in range(NFT):
                    SL = sst.next()
                    prR = self.nextps(); prW = self.nextps()
                    self.mm(prR, prR.t[:, :], [(SL.t[:, 0, kt, :], edt.t[:, kt, 0, :]) for kt in range(NT)], [SL, edt])
                    self.mm(prW, prW.t[:, :], [(SL.t[:, 1, kt, :], edt.t[:, kt, 1, :]) for kt in range(NT)], [SL, edt])
                    ST = stt[si % 2]; si += 1
                    C.op("dve", lambda e, ST=ST, prR=prR: e.tensor_tensor(out=ST.t[:, 0, :], in0=prR.t[:, :], in1=rn.t[:], op=ALU.mult), reads=[prR, rn], writes=[ST])
                    C.op("dve", lambda e, ST=ST, colF=colF: e.tensor_tensor(out=ST.t[:, 0, :], in0=ST.t[:, 0, :], in1=hybt.t[:], op=ALU.add), reads=[ST, hybt], writes=[ST])
                    C.op("dve", lambda e, ST=ST, prW=prW: e.tensor_tensor(out=ST.t[:, 1, :], in0=prW.t[:, :], in1=rn.t[:], op=ALU.mult), reads=[prW, rn], writes=[ST])
                    C.op("pool", lambda e, ST=ST, mt=mt: e.tensor_scalar(out=ST.t[:].rearrange("p a b -> p (a b)"), in0=ST.t[:].rearrange("p a b -> p (a b)"), scalar1=fsc.t[:, mt:mt + 1], scalar2=None, op0=ALU.mult), reads=[ST, fsc], writes=[ST])
                    self.store("sp", spec, specv[mt * P:(mt + 1) * P, :, colF:colF + 512], ST, ST.t[:])
        C.pop()

    def phase_F(self, v_tm, x1_tm, x2_cm, spec, cst, yT):
        C = self.C; M = self.M
        C.push()
        utm = C.sb("utmF", [P, NT, 512], BF16); x1t = C.sb("x1tF", [P, NT, 512], BF16)
        Y = C.sb("YF", [P, NFT, 2, 512], BF16)
        slabs = [C.sb("slabF%d" % i, [P, 2, NFT, P], BF16) for i in range(2)]
        spt = [C.sb("sptF%d" % i, [P, 2, 512], F32) for i in range(2)]
        sst = self.slab_stream(cst, [m_ for _ in range(8) for m_ in (list(range(NFT)) + list(range(NT)))], slabs)

        def spjob(k):
            cb_, rem = divmod(k, 2 * NFT); o_, mt_ = divmod(rem, NFT)
            c0_ = o_ * 2048 + cb_ * 512

            def job():
                SPn = spt[k % 2]
                self.load("sp", SPn, SPn.t[:], spec, specv[mt_ * P:(mt_ + 1) * P, :, c0_:c0_ + 512])
                return SPn
            return job
        t4 = [C.sb("t4F%d" % i, [P, 512], F32) for i in range(4)]
        yb = [C.sb("ybF%d" % i, [P, 512], BF16) for i in range(2)]
        x2t = [C.sb("x2tF%d" % i, [P, 4, P], BF16) for i in range(2)]
        yo = [C.sb("yoF%d" % i, [P, 4, P], BF16) for i in range(2)]
        specv = spec.t.ap().rearrange("k (two c) -> k two c", two=2)
        cnt = dict(sp=0, yb=0, x2=0, yo=0)
        spst = Stream([spjob(k) for k in range(4 * 2 * NFT)])

        def conv(o, cb, cbfn):
            c0 = o * 2048 + cb * 512
            for mt in range(NFT):
                SL = sst.next()
                prR = self.nextps(); prI = self.nextps()
                self.mm(prR, prR.t[:, :], [(SL.t[:, 0, kt, :], utm.t[:, kt, :]) for kt in range(NT)], [SL, utm])
                self.mm(prI, prI.t[:, :], [(SL.t[:, 1, kt, :], utm.t[:, kt, :]) for kt in range(NT)], [SL, utm])
                SP = spst.next()
                a, b_, c_, d_ = t4
                C.op("dve", lambda e, prR=prR, SP=SP: e.tensor_tensor(out=a.t[:], in0=prR.t[:, :], in1=SP.t[:, 0, :], op=ALU.mult), reads=[prR, SP], writes=[a])
                C.op("dve", lambda e, prI=prI, SP=SP: e.tensor_tensor(out=b_.t[:], in0=prI.t[:, :], in1=SP.t[:, 1, :], op=ALU.mult), reads=[prI, SP], writes=[b_])
                C.op("pool", lambda e, mt=mt: e.tensor_tensor(out=Y.t[:, mt, 0, :], in0=a.t[:], in1=b_.t[:], op=ALU.subtract), reads=[a, b_], writes=[Y])
                C.op("dve", lambda e, prR=prR, SP=SP: e.tensor_tensor(out=c_.t[:], in0=prR.t[:, :], in1=SP.t[:, 1, :], op=ALU.mult), reads=[prR, SP], writes=[c_])
                C.op("dve", lambda e, prI=prI, SP=SP: e.tensor_tensor(out=d_.t[:], in0=prI.t[:, :], in1=SP.t[:, 0, :], op=ALU.mult), reads=[prI, SP], writes=[d_])
                C.op("pool", lambda e, mt=mt: e.tensor_tensor(out=Y.t[:, mt, 1, :], in0=c_.t[:], in1=d_.t[:], op=ALU.add), reads=[c_, d_], writes=[Y])
            for it in range(NT):
                SL = sst.next()
                pr = self.nextps()
                self.mm(pr, pr.t[:, :], [(SL.t[:, 0, kt, :], Y.t[:, kt, 0, :]) for kt in range(NFT)] + [(SL.t[:, 1, kt, :], Y.t[:, kt, 1, :]) for kt in range(NFT)], [SL, Y])
                cbfn(it, pr)

        for cb in range(4):
            self.load("sp", utm, utm.t[:], v_tm, v_tm.t.ap().rearrange("(tt p) c -> p tt c", p=P)[:, :, cb * 512:(cb + 1) * 512])
            self.load("act", x1t, x1t.t[:], x1_tm, x1_tm.t.ap().rearrange("(tt p) c -> p tt c", p=P)[:, :, cb * 512:(cb + 1) * 512])

            def f1(it, pr):
                C.op("dve", lambda e: e.tensor_tensor(out=utm.t[:, it, :], in0=pr.t[:, :], in1=x1t.t[:, it, :], op=ALU.mult), reads=[pr, x1t], writes=[utm])

            def f2(it, pr, cb=cb):
                YB = yb[cnt["yb"] % 2]; cnt["yb"] += 1
                self.copy("act", YB, YB.t[:], pr, pr.t[:, :])
                pt = self.nextps(); pv = self.psb(pt)

                def fnT(e):
                    inst = None
                    for j in range(4):
                        inst = e.transpose(pv[:, j * P:(j + 1) * P], YB.t[:, j * P:(j + 1) * P], self.identb.t[:])
                    return inst
                C.op("pe", fnT, reads=[YB, self.identb], writes=[pt])
                X2 = x2t[cnt["x2"] % 2]; cnt["x2"] += 1
                self.load("sp", X2, X2.t[:], x2_cm, x2_cm.t.ap().rearrange("(j p) t -> p j t", p=P)[:, cb * 4:(cb + 1) * 4, it * P:(it + 1) * P])
                YO = yo[cnt["yo"] % 2]; cnt["yo"] += 1
                C.op("dve", lambda e: e.tensor_tensor(out=YO.t[:].rearrange("p a b -> p (a b)"), in0=pv[:, 0:512], in1=X2.t[:].rearrange("p a b -> p (a b)"), op=ALU.mult), reads=[pt, X2], writes=[YO])
                self.store("sp", yT, yT.t.ap().rearrange("(j p) t -> p j t", p=P)[:, cb * 4:(cb + 1) * 4, it * P:(it + 1) * P], YO, YO.t[:])
            conv(0, cb, f1)
            conv(1, cb, f2)
        C.pop()


    def phase_G(self, qT_d, kT_d, k_tm, vr_tm, sg, Sfd, lg, yT):
        C = self.C; M = self.M
        C.push()
        Sf = [C.sb("SfG%d" % i, [P, 256], F32) for i in range(16)]
        Sb = [C.sb("SbG%d" % i, [P, 256], BF16) for i in range(16)]
        for i in range(16):
            self.load("sp" if i % 2 else "act", Sf[i], Sf[i].t[:], Sfd, Sfd.t.ap()[:, i * 256:(i + 1) * 256])
            C.op("pool", lambda e, i=i: e.tensor_copy(Sb[i].t[:], Sf[i].t[:]), reads=[Sf[i]], writes=[Sb[i]])
        decT = C.sb("decT", [P, 16, P], F32); qdec = C.sb("qdec", [P, 16, P], F32)
        kdec = C.sb("kdec", [P, 16], F32); cdec = C.sb("cdec", [P, 16], F32)
        for d in range(2):
            for h in range(8):
                dh = d * 8 + h
                C.op("act", lambda e, d=d, dh=dh: e.activation(out=decT.t[:, dh, :], in_=M("diffF" if d == 0 else "diffB"), func=AF.Exp, scale=lg.t[:, dh:dh + 1]), reads=[lg, self.mt], writes=[decT])
                C.op("dve", lambda e, d=d, dh=dh: e.tensor_tensor(out=decT.t[:, dh, :], in0=decT.t[:, dh, :], in1=M("maskF" if d == 0 else "maskB"), op=ALU.mult), reads=[decT, self.mt], writes=[decT])
                C.op("act", lambda e, d=d, dh=dh: e.activation(out=qdec.t[:, dh, :], in_=M("ip1" if d == 0 else "i128m"), func=AF.Exp, scale=lg.t[:, dh:dh + 1]), reads=[lg, self.mt], writes=[qdec])
            C.op("act", lambda e, d=d: e.activation(out=kdec.t[:, d * 8:(d + 1) * 8], in_=lg.t[:, d * 8:(d + 1) * 8], func=AF.Exp, scale=M("cols")[:, d:d + 1]), reads=[lg, self.mt], writes=[kdec])
        C.op("act", lambda e: e.activation(out=cdec.t[:], in_=lg.t[:], func=AF.Exp, scale=128.0), reads=[lg], writes=[cdec])
        nb_ = 2
        qTc = [[C.sb("qTc%d%d" % (d, i), [P, 8, P], BF16) for i in range(nb_)] for d in range(2)]
        kTc = [[C.sb("kTc%d%d" % (d, i), [P, 8, P], BF16) for i in range(nb_)] for d in range(2)]
        ktm = [[C.sb("ktm%d%d" % (d, i), [P, KC], BF16) for i in range(nb_)] for d in range(2)]
        vtm = [[C.sb("vtm%d%d" % (d, i), [P, VC], BF16) for i in range(nb_)] for d in range(2)]
        sgc = [[C.sb("sgc%d%d" % (d, i), [P, 16, P], BF16) for i in range(nb_)] for d in range(2)]
        ytl = [[C.sb("ytl%d%d" % (d, i), [P, 16, P], BF16) for i in range(nb_)] for d in range(2)]
        attm = [C.sb("attm%d" % i, [P, P], BF16) for i in range(3)]
        qd = [C.sb("qd%d" % i, [P, P], BF16) for i in range(3)]
        kd = [C.sb("kdG%d" % i, [P, P], BF16) for i in range(3)]
        sq = [C.sb("sq%d" % i, [P, 256], BF16) for i in range(2)]
        sd = [C.sb("sd%d" % i, [P, P], F32) for i in range(2)]
        rs = [C.sb("rs%d" % i, [P, P], F32) for i in range(2)]
        on = [C.sb("on%d" % i, [P, 256], F32) for i in range(2)]
        qv = qT_d.t.ap().rearrange("(h p) t -> p h t", p=P); kv = kT_d.t.ap().rearrange("(h p) t -> p h t", p=P)
        sgv = sg.t.ap().rearrange("(ha p) t -> p ha t", p=P); yv = yT.t.ap().rearrange("(ha p) t -> p ha t", p=P)
        n = 0
        for s_ in range(NT):
            for d in range(2):
                c = s_ if d == 0 else NT - 1 - s_
                bi = s_ % nb_
                Q = qTc[d][bi]; Kt = kTc[d][bi]; KM_ = ktm[d][bi]; V = vtm[d][bi]; G = sgc[d][bi]; YT = ytl[d][bi]
                tk = slice(c * P, (c + 1) * P)
                self.load("sp", Q, Q.t[:], qT_d, qv[:, :, tk]); self.load("act", Kt, Kt.t[:], kT_d, kv[:, :, tk])
                self.load("sp", KM_, KM_.t[:], k_tm, k_tm.t.ap()[tk, :]); self.load("act", V, V.t[:], vr_tm, vr_tm.t.ap()[tk, :])
                self.load("sp", G, G.t[:], sg, sgv[:, d * 16:(d + 1) * 16, tk])
                for h in range(8):
                    dh = d * 8 + h; n += 1
                    AT = attm[n % 3]; QD = qd[n % 3]; KD = kd[n % 3]; SQ = sq[n % 2]; SD = sd[n % 2]; RS = rs[n % 2]; ON = on[n % 2]
                    pa = self.nextps()
                    self.mm(pa, pa.t[:, 0:P], [(Kt.t[:, h, :], Q.t[:, h, :])], [Kt, Q])
                    C.op("dve", lambda e, AT=AT, pa=pa, dh=dh: e.tensor_tensor(out=AT.t[:], in0=pa.t[:, 0:P], in1=decT.t[:, dh, :], op=ALU.mult), reads=[pa, decT], writes=[AT])
                    C.op("pool", lambda e, QD=QD, Q=Q, h=h, dh=dh: e.tensor_tensor(out=QD.t[:], in0=Q.t[:, h, :], in1=qdec.t[:, dh, :], op=ALU.mult), reads=[Q, qdec], writes=[QD])
                    po = self.nextps()

                    def fo(e, po=po, V=V, AT=AT, QD=QD, h=h, dh=dh):
                        inst = None
                        for a in range(2):
                            e.matmul(po.t[:, a * P:(a + 1) * P], V.t[:, h * 256 + a * P:h * 256 + (a + 1) * P], AT.t[:], start=True, stop=False)
                            inst = e.matmul(po.t[:, a * P:(a + 1) * P], Sb[dh].t[:, a * P:(a + 1) * P], QD.t[:], start=False, stop=True)
                        return inst
                    C.op("pe", fo, reads=[V, AT, QD, Sb[dh]], writes=[po])
                    C.op("act", lambda e, SQ=SQ, po=po: e.activation(out=SQ.t[:], in_=po.t[:, 0:256], func=AF.Square), reads=[po], writes=[SQ])
                    pss = self.nextps()
                    self.mm(pss, pss.t[:, 0:P], [(self.onesb.t[:], SQ.t[:, 0:P]), (self.onesb.t[:], SQ.t[:, P:256])], [self.onesb, SQ])
                    C.op("act", lambda e, SD=SD, pss=pss: e.activation(out=SD.t[:], in_=pss.t[:, 0:P], func=AF.Sqrt, scale=1.0 / 256.0, bias=M("cols")[:, 6:7]), reads=[pss, self.mt], writes=[SD])
                    C.op("dve", lambda e, RS=RS, SD=SD: e.reciprocal(RS.t[:], SD.t[:]), reads=[SD], writes=[RS])
                    for a in range(2):
                        C.op("dve", lambda e, ON=ON, po=po, RS=RS, a=a: e.tensor_tensor(out=ON.t[:, a * P:(a + 1) * P], in0=po.t[:, a * P:(a + 1) * P], in1=RS.t[:], op=ALU.mult), reads=[po, RS], writes=[ON])
                    C.op("pool", lambda e, YT=YT, ON=ON, G=G, h=h: e.tensor_tensor(out=YT.t[:, h * 2:h * 2 + 2, :], in0=ON.t[:].rearrange("p (a b) -> p a b", a=2), in1=G.t[:, h * 2:h * 2 + 2, :], op=ALU.mult), reads=[ON, G], writes=[YT])
                    C.op("pool", lambda e, KD=KD, KM_=KM_, h=h, dh=dh: e.tensor_scalar(out=KD.t[:], in0=KM_.t[:, h * P:(h + 1) * P], scalar1=kdec.t[:, dh:dh + 1], scalar2=None, op0=ALU.mult), reads=[KM_, kdec], writes=[KD])
                    psn = self.nextps()
                    self.mm(psn, psn.t[:, 0:256], [(KD.t[:], V.t[:, h * 256:(h + 1) * 256])], [KD, V])
                    C.op("dve", lambda e, psn=psn, dh=dh: e.scalar_tensor_tensor(out=Sf[dh].t[:], in0=Sf[dh].t[:], scalar=cdec.t[:, dh:dh + 1], in1=psn.t[:, 0:256], op0=ALU.mult, op1=ALU.add), reads=[Sf[dh], cdec, psn], writes=[Sf[dh]])
                    C.op("pool", lambda e, dh=dh: e.tensor_copy(Sb[dh].t[:], Sf[dh].t[:]), reads=[Sf[dh]], writes=[Sb[dh]])
                self.store("sp", yT, yv[:, 16 + d * 16:16 + (d + 1) * 16, tk], YT, YT.t[:])
        C.pop()


    def phase_H(self, yT, w_out, x, bcd, x1r, hx2, rwT, aff_tm):
        C = self.C; M = self.M
        C.push()
        stg = [C.sb("stgH%d" % i, [P, 8, 512], F32) for i in range(2)]
        wb = [C.sb("wbH%d" % i, [P, 32, 512], BF16) for i in range(2)]
        g1s = [C.sb("g1s%d" % i, [P, 512], F32) for i in range(2)]
        yh = [C.sb("yh%d" % i, [P, 16, P], BF16) for i in range(2)]
        yf = [C.sb("yf%d" % i, [P, 16, P], BF16) for i in range(2)]
        yk = [C.sb("yk%d" % i, [P, 16, P], BF16) for i in range(2)]
        yr = [C.sb("yr%d" % i, [P, 16, P], BF16) for i in range(2)]
        xs_ = [C.sb("xsH%d" % i, [P, 512], F32) for i in range(3)]
        ob = [C.sb("obH%d" % i, [P, 512], F32) for i in range(3)]
        wv = w_out.t.ap().rearrange("(kc p) n -> p kc n", p=P)
        yv = yT.t.ap().rearrange("(kc p) t -> p kc t", p=P)
        n = 0
        for db in range(8):
            W = wb[db % 2]; cs = slice(db * 512, (db + 1) * 512)
            self.stg_i = 0
            self.load_w_bf(w_out, wv[:, :, cs], stg, W)
            G1 = g1s[db % 2]
            self.load("sp", G1, G1.t[:], bcd, bcd.t.ap()[:, db * 512:(db + 1) * 512])
            for tt in range(NT):
                n += 1
                YH = yh[n % 2]; YF = yf[n % 2]; YK = yk[n % 2]; YR = yr[n % 2]; X = xs_[n % 3]; O = ob[n % 3]
                tk = slice(tt * P, (tt + 1) * P)
                self.load("sp", YH, YH.t[:], yT, yv[:, 0:16, tk]); self.load("act", YF, YF.t[:], yT, yv[:, 16:32, tk]); self.load("sp", YK, YK.t[:], yT, yv[:, 32:48, tk])
                self.load("act", X, X.t[:], x, x.t.ap()[tk, cs])
                C.op("pool", lambda e, YR=YR, YF=YF, YK=YK: e.tensor_tensor(out=YR.t[:], in0=YF.t[:], in1=YK.t[:], op=ALU.add), reads=[YF, YK], writes=[YR])
                pr = self.nextps()
                self.mm(pr, pr.t[:, :], [(YH.t[:, kc, :], W.t[:, kc, :]) for kc in range(16)] + [(YR.t[:, kc, :], W.t[:, 16 + kc, :]) for kc in range(16)], [YH, YR, W])
                C.op("dve", lambda e, O=O, pr=pr, G1=G1: e.tensor_tensor(out=O.t[:], in0=pr.t[:, :], in1=G1.t[:], op=ALU.mult), reads=[pr, G1], writes=[O])
                C.op("pool", lambda e, O=O, X=X: e.tensor_tensor(out=O.t[:], in0=O.t[:], in1=X.t[:], op=ALU.add), reads=[O, X], writes=[O])
                self.store("sp", x1r, x1r.t.ap()[tk, cs], O, O.t[:])
        C.pop()
        C.push()
        A2 = C.sb("A2b", [P, D], F32); B2 = C.sb("B2b", [P, D], F32)
        self.load("sp", A2, A2.t[:], bcd, bcd.t.ap()[:, 2 * D:3 * D]); self.load("act", B2, B2.t[:], bcd, bcd.t.ap()[:, 3 * D:4 * D])
        rw = C.sb("rw", [P, 32, NE], F32)
        self.load("sp", rw, rw.t[:].rearrange("p a b -> p (a b)"), rwT, rwT.t.ap())
        xt = [C.sb("xtH%d" % i, [P, D], F32) for i in range(2)]
        h2 = [C.sb("h2H%d" % i, [P, D], F32) for i in range(2)]
        hb16 = [C.sb("hb16%d" % i, [P, D], BF16) for i in range(2)]
        junk = C.sb("junkH", [P, D], BF16)
        sts = [C.sb("stH%d" % i, [P, 4], F32) for i in range(2)]
        h2T = [C.sb("h2T%d" % i, [P, 32, P], F32) for i in range(2)]
        sm = [C.sb("smH%d" % i, [P, 4], F32) for i in range(2)]
        ex = [C.sb("exH%d" % i, [P, NE], F32) for i in range(2)]
        for tt in range(NT):
            X = xt[tt % 2]; H2 = h2[tt % 2]; HB = hb16[tt % 2]; st = sts[tt % 2]; HT = h2T[tt % 2]; SM = sm[tt % 2]; EX = ex[tt % 2]
            tk = slice(tt * P, (tt + 1) * P)
            self.load("sp" if tt % 2 else "act", X, X.t[:], x1r, x1r.t.ap()[tk, :])
            self.rms_stats(X, junk, st, D)
            C.op("dve", lambda e, H2=H2, X=X, st=st: e.scalar_tensor_tensor(out=H2.t[:], in0=X.t[:], scalar=st.t[:, 1:2], in1=A2.t[:], op0=ALU.mult, op1=ALU.mult), reads=[X, st, A2], writes=[H2])
            C.op("pool", lambda e, H2=H2: e.tensor_tensor(out=H2.t[:], in0=H2.t[:], in1=B2.t[:], op=ALU.add), reads=[H2, B2], writes=[H2])
            C.op("act", lambda e, HB=HB, H2=H2: e.activation(out=HB.t[:], in_=H2.t[:], func=AF.Copy), reads=[H2], writes=[HB])
            self.store("sp", hx2, hx2.t.ap()[tk, :], HB, HB.t[:])
            for g in range(8):
                pt = self.nextps()

                def fnT(e, pt=pt, H2=H2, g=g):
                    inst = None
                    for j in range(4):
                        kc = g * 4 + j
                        inst = e.transpose(pt.t[:, j * P:(j + 1) * P], H2.t[:, kc * P:(kc + 1) * P], M("ident"))
                    return inst
                C.op("pe", fnT, reads=[H2, self.mt], writes=[pt])
                self.copy("act" if g % 2 else "dve", HT, HT.t[:, g * 4:(g + 1) * 4, :].rearrange("p a b -> p (a b)"), pt, pt.t[:, :])
            pl = self.nextps()
            self.mm(pl, pl.t[:, 0:NE], [(HT.t[:, kc, :], rw.t[:, kc, :]) for kc in range(32)], [HT, rw])
            C.op("dve", lambda e, SM=SM, pl=pl: e.tensor_reduce(out=SM.t[:, 0:1], in_=pl.t[:, 0:NE], axis=mybir.AxisListType.X, op=ALU.max), reads=[pl], writes=[SM])
            C.op("dve", lambda e, SM=SM: e.tensor_scalar(out=SM.t[:, 1:2], in0=SM.t[:, 0:1], scalar1=-1.0, scalar2=None, op0=ALU.mult), reads=[SM], writes=[SM])
            C.op("act", lambda e, EX=EX, pl=pl, SM=SM: e.activation(out=EX.t[:], in_=pl.t[:, 0:NE], func=AF.Exp, bias=SM.t[:, 1:2], accum_out=SM.t[:, 2:3]), reads=[pl, SM], writes=[EX, SM])
            C.op("dve", lambda e, SM=SM: e.reciprocal(SM.t[:, 3:4], SM.t[:, 2:3]), reads=[SM], writes=[SM])
            C.op("dve", lambda e, EX=EX, SM=SM, tt=tt: e.tensor_scalar(out=aff_tm.t[:, tt, :], in0=EX.t[:], scalar1=SM.t[:, 3:4], scalar2=None, op0=ALU.mult), reads=[EX, SM], writes=[aff_tm])
        C.pop()


    def phase_I(self, aff_tm, idx_all, gate_all):
        C = self.C; M = self.M
        C.push()
        affT = C.sb("affT", [NE, T], F32); junk = C.sb("junkI", [NE, T], F32)
        for g in range(8):
            pt = self.nextps()

            def fnT(e, pt=pt, g=g):
                inst = None
                for j in range(4):
                    tt = g * 4 + j
                    inst = e.transpose(pt.t[0:NE, j * P:(j + 1) * P], aff_tm.t[:, tt, :], M("ident"))
                return inst
            C.op("pe", fnT, reads=[aff_tm, self.mt], writes=[pt])
            self.copy("dve", affT, affT.t[:, g * 512:(g + 1) * 512], pt, pt.t[0:NE, :])
        bs = C.sb("bsI", [NE, 8], F32)
        C.op("dve", lambda e: e.memset(bs.t[:, 0:1], 0.0), writes=[bs])
        C.op("dve", lambda e: e.memset(bs.t[:, 1:2], 1.0), reads=[bs], writes=[bs])
        for it in range(34):
            C.op("dve", lambda e: e.tensor_scalar(out=bs.t[:, 2:3], in0=bs.t[:, 0:1], scalar1=bs.t[:, 1:2], scalar2=0.5, op0=ALU.add, op1=ALU.mult), reads=[bs], writes=[bs])
            C.op("dve", lambda e: e.tensor_scalar(out=junk.t[:], in0=affT.t[:], scalar1=bs.t[:, 2:3], scalar2=0.0, op0=ALU.is_ge, op1=ALU.add, accum_out=bs.t[:, 3:4]), reads=[affT, bs], writes=[junk, bs])
            C.op("dve", lambda e: e.tensor_scalar(out=bs.t[:, 4:5], in0=bs.t[:, 3:4], scalar1=float(CAP) - 0.5, scalar2=None, op0=ALU.is_gt), reads=[bs], writes=[bs])
            C.op("dve", lambda e: e.tensor_tensor(out=bs.t[:, 5:6], in0=bs.t[:, 2:3], in1=bs.t[:, 0:1], op=ALU.subtract), reads=[bs], writes=[bs])
            C.op("dve", lambda e: e.tensor_tensor(out=bs.t[:, 6:7], in0=bs.t[:, 1:2], in1=bs.t[:, 2:3], op=ALU.subtract), reads=[bs], writes=[bs])
            C.op("dve", lambda e: e.scalar_tensor_tensor(out=bs.t[:, 0:1], in0=bs.t[:, 5:6], scalar=bs.t[:, 4:5], in1=bs.t[:, 0:1], op0=ALU.mult, op1=ALU.add), reads=[bs], writes=[bs])
            C.op("dve", lambda e: e.scalar_tensor_tensor(out=bs.t[:, 1:2], in0=bs.t[:, 6:7], scalar=bs.t[:, 4:5], in1=bs.t[:, 2:3], op0=ALU.mult, op1=ALU.add), reads=[bs], writes=[bs])
        thrB = C.sb("thrB", [NE, P], F32)
        C.op("dve", lambda e: e.tensor_scalar(out=thrB.t[:], in0=M("ones", NE), scalar1=bs.t[:, 0:1], scalar2=None, op0=ALU.mult), reads=[bs, self.mt], writes=[thrB])
        pb = self.nextps()
        C.op("pe", lambda e: e.matmul(pb.t[:, 0:NE], thrB.t[:], M("ident", NE)[:, 0:NE], start=True, stop=True), reads=[thrB, self.mt], writes=[pb])
        thr = C.sb("thrI", [P, NE], F32)
        self.copy("dve", thr, thr.t[:], pb, pb.t[:, 0:NE])
        mask = C.sb("maskI", [P, NT, NE], F32); slot = C.sb("slotI", [P, NT, NE], F32); msum = C.sb("msumI", [P, NE], F32)
        for tt in range(NT):
            C.op("dve", lambda e, tt=tt: e.tensor_tensor(out=mask.t[:, tt, :], in0=aff_tm.t[:, tt, :], in1=thr.t[:], op=ALU.is_ge), reads=[aff_tm, thr], writes=[mask])
        C.op("dve", lambda e: e.memset(msum.t[:], 0.0), writes=[msum])
        for tt in range(NT):
            pr = self.nextps()

            def fn(e, pr=pr, tt=tt):
                e.matmul(pr.t[:, 0:NE], M("tri"), mask.t[:, tt, :], start=True, stop=False)
                return e.matmul(pr.t[:, 0:NE], M("ones"), msum.t[:], start=False, stop=True)
            C.op("pe", fn, reads=[mask, msum, self.mt], writes=[pr])
            self.copy("act", slot, slot.t[:, tt, :], pr, pr.t[:, 0:NE])
            C.op("dve", lambda e, tt=tt: e.tensor_tensor(out=msum.t[:], in0=msum.t[:], in1=mask.t[:, tt, :], op=ALU.add), reads=[msum, mask], writes=[msum])
        sv = slot.t[:].rearrange("p a b -> p (a b)"); mv = mask.t[:].rearrange("p a b -> p (a b)")
        C.op("dve", lambda e: e.scalar_tensor_tensor(out=sv, in0=sv, scalar=1.0, in1=mv, op0=ALU.add, op1=ALU.mult), reads=[slot, mask], writes=[slot])
        C.op("dve", lambda e: e.tensor_scalar(out=sv, in0=sv, scalar1=-1.0, scalar2=None, op0=ALU.add), reads=[slot], writes=[slot])
        if "dbgs" in self.dbg:
            self.store("sp", self.dbgs_res, self.dbgs_res.t.ap()[:, 128:640], slot, sv)
            self.store("sp", self.dbgs_res, self.dbgs_res.t.ap()[:, 640:1152], mask, mv)
            self.store("sp", self.dbgs_res, self.dbgs_res.t.ap()[:, 1152:1168], thr, thr.t[:])
        rhsE = C.sb("rhsE", [P, NT, 4], F32)
        C.op("dve", lambda e: e.memset(rhsE.t[:], 0.0), writes=[rhsE])
        C.op("dve", lambda e: e.tensor_copy(rhsE.t[:, :, 0:2], M("tvals").rearrange("p (a b) -> p a b", b=2)), reads=[self.mt, rhsE], writes=[rhsE])
        oh = [[C.sb("ohI%d_%d" % (b, i), [P, CAP], F32) for i in range(NT)] for b in range(2)]
        idf = C.sb("idfI", [P, 4], F32); pis = C.sb("pisI", [P, 16], F32)
        for ex in range(NL):
            C.op("dve", lambda e, ex=ex: e.tensor_copy(rhsE.t[:, :, 2], aff_tm.t[:, :, ex]), reads=[aff_tm, rhsE], writes=[rhsE])
            pi = self.nextps()
            OHs = oh[ex % 2]
            for tt in range(NT):
                OH = OHs[tt]
                C.op("dve" if tt % 2 else "pool", lambda e, OH=OH, tt=tt, ex=ex: e.tensor_scalar(out=OH.t[:], in0=M("iota512"), scalar1=slot.t[:, tt, ex:ex + 1], scalar2=None, op0=ALU.is_equal), reads=[slot, self.mt], writes=[OH])

            def fm(e, OHs=OHs, pi=pi):
                inst = None
                for st in range(4):
                    for tt in range(NT):
                        inst = e.matmul(pi.t[:, st * 4:(st + 1) * 4], OHs[tt].t[:, st * P:(st + 1) * P], rhsE.t[:, tt, :], start=(tt == 0), stop=(tt == NT - 1))
                return inst
            C.op("pe", fm, reads=[*OHs, rhsE], writes=[pi])
            C.op("dve", lambda e, pi=pi: e.tensor_copy(pis.t[:], pi.t[:, 0:16]), reads=[pi], writes=[pis])
            pv = pis.t[:].rearrange("p (a b) -> p a b", b=4)
            C.op("dve", lambda e, pv=pv: e.scalar_tensor_tensor(out=idf.t[:], in0=pv[:, :, 0], scalar=64.0, in1=pv[:, :, 1], op0=ALU.mult, op1=ALU.add), reads=[pis], writes=[idf])
            C.op("dve", lambda e, ex=ex: e.tensor_copy(idx_all.t[:, ex, :], idf.t[:]), reads=[idf], writes=[idx_all])
            C.op("dve", lambda e, ex=ex, pv=pv: e.tensor_copy(gate_all.t[:, ex, :], pv[:, :, 2]), reads=[pis], writes=[gate_all])
        C.pop()

    def phase_J(self, hx2, wg, wu, wd, idx_all, gate_all, ffn, ffr):
        C = self.C; M = self.M
        C.push()
        zt = C.sb("ztJ", [P, 512], F32)
        C.op("dve", lambda e: e.memset(zt.t[:], 0.0), writes=[zt])
        fres = [Res("ffnres%d" % i) for i in range(8)]
        for db in range(8):
            for tt in range(NT):
                C.dma("sp" if tt % 2 else "act", lambda e, db=db, tt=tt: e.dma_start(out=ffn[db].t.ap()[tt * P:(tt + 1) * P, :], in_=zt.t[:]), reads=[zt], writes=[fres[db]], semres=zt)
        stg = [C.sb("stgJ%d" % i, [P, 4096], F32) for i in range(3)]
        wgb = [C.sb("wgb%d" % i, [P, 32, P], BF16) for i in range(2)]
        wub = [C.sb("wub%d" % i, [P, 32, P], BF16) for i in range(2)]
        wdb = [C.sb("wdb%d" % i, [P, 16, 512], BF16) for i in range(2)]
        xs = [C.sb("xsJ%d" % i, [P, D], BF16) for i in range(2)]
        xsT = C.sb("xsT", [P, 32, CAP], BF16); hidT = C.sb("hidT", [P, 16, CAP], BF16)
        sgt = [C.sb("sgt%d" % i, [P, CAP], F32) for i in range(2)]
        ot = [C.sb("otJ%d" % i, [P, 512], F32) for i in range(4)]
        cnt = dict(stg=0, ot=0, ev=0)

        def nstg():
            r = stg[cnt["stg"] % 3]; cnt["stg"] += 1; return r
        def gujob(ex, fi):
            def job():
                WG = wgb[fi % 2]; WU = wub[fi % 2]
                gv = wg.t.ap()[ex].rearrange("(kc p) f -> p kc f", p=P); uv = wu.t.ap()[ex].rearrange("(kc p) f -> p kc f", p=P)
                for (src, view, Wd_) in ((wg, gv, WG), (wu, uv, WU)):
                    S_ = nstg()
                    self.load("sp", S_, S_.t[:].rearrange("p (a b) -> p a b", a=32), src, view[:, :, fi * P:(fi + 1) * P])
                    self.copy(self.cast_eng(), Wd_, Wd_.t[:].rearrange("p a b -> p (a b)"), S_, S_.t[:])
                return (WG, WU)
            return job

        def djob(ex, db):
            def job():
                WD = wdb[db % 2]
                dv = wd.t.ap()[ex].rearrange("(fc p) d -> p fc d", p=P)
                for hf in range(2):
                    S_ = nstg()
                    self.load("sp", S_, S_.t[:].rearrange("p (a b) -> p a b", a=8), wd, dv[:, hf * 8:(hf + 1) * 8, db * 512:(db + 1) * 512])
                    self.copy(self.cast_eng(), WD, WD.t[:, hf * 8:(hf + 1) * 8, :].rearrange("p a b -> p (a b)"), S_, S_.t[:])
                return WD
            return job
        wst = Stream([j for ex in range(NL) for j in ([gujob(ex, fi) for fi in range(16)] + [djob(ex, db) for db in range(8)])])
        for ex in range(NL):
            for st in range(4):
                X = xs[st % 2]
                C.dma("pool", lambda e, X=X, ex=ex, st=st: e.indirect_dma_start(out=X.t[:], out_offset=None, in_=hx2.t.ap(), in_offset=bass.IndirectOffsetOnAxis(ap=idx_all.t[:, ex, st:st + 1], axis=0)), reads=[hx2, idx_all], writes=[X])
                for g in range(8):
                    pt = self.nextps(); pv = self.psb(pt)

                    def fnT(e, pv=pv, X=X, g=g):
                        inst = None
                        for j in range(4):
                            kc = g * 4 + j
                            inst = e.transpose(pv[:, j * P:(j + 1) * P], X.t[:, kc * P:(kc + 1) * P], self.identb.t[:])
                        return inst
                    C.op("pe", fnT, reads=[X, self.identb], writes=[pt])
                    cnt["ev"] += 1
                    self.copy("act" if cnt["ev"] % 2 else "dve", xsT, xsT.t[:, g * 4:(g + 1) * 4, st * P:(st + 1) * P], pt, pv[:, 0:512].rearrange("p (a b) -> p a b", a=4))
            for fi in range(16):
                WG, WU = wst.next()
                pg = self.nextps(); pu = self.nextps()
                self.mm(pg, pg.t[:, :], [(WG.t[:, kc, :], xsT.t[:, kc, :]) for kc in range(32)], [WG, xsT])
                self.mm(pu, pu.t[:, :], [(WU.t[:, kc, :], xsT.t[:, kc, :]) for kc in range(32)], [WU, xsT])
                SG = sgt[fi % 2]
                C.op("act", lambda e, SG=SG, pg=pg: e.activation(out=SG.t[:], in_=pg.t[:, :], func=AF.Silu), reads=[pg], writes=[SG])
                C.op("dve", lambda e, SG=SG, pu=pu, fi=fi: e.tensor_tensor(out=hidT.t[:, fi, :], in0=pu.t[:, :], in1=SG.t[:], op=ALU.mult), reads=[pu, SG], writes=[hidT])
            for db in range(8):
                WD = wst.next()
                for st in range(4):
                    po = self.nextps()
                    self.mm(po, po.t[:, :], [(hidT.t[:, fc, st * P:(st + 1) * P], WD.t[:, fc, :]) for fc in range(16)], [hidT, WD])
                    O = ot[cnt["ot"] % 4]; cnt["ot"] += 1
                    C.op("act", lambda e, O=O, po=po, ex=ex, st=st: e.activation(out=O.t[:], in_=po.t[:, :], func=AF.Copy, scale=gate_all.t[:, ex, st:st + 1]), reads=[po, gate_all], writes=[O])
                    rd_ = [O, idx_all] + ([] if st == 0 else [fres[db]])
                    wr_ = [fres[db]] if st == 0 else []
                    C.dma("pool", lambda e, O=O, ex=ex, st=st, db=db: e.indirect_dma_start(out=ffn[db].t.ap(), out_offset=bass.IndirectOffsetOnAxis(ap=idx_all.t[:, ex, st:st + 1], axis=0), in_=O.t[:], in_offset=None, compute_op=ALU.add), reads=rd_, writes=wr_, semres=O)
        self.fres = fres
        C.pop()

    def phase_K(self, x1r, ffn, bcd, fgb, out):
        C = self.C; M = self.M
        C.push()
        g2 = C.sb("gt2b", [P, D], F32); fg = C.sb("fgbt", [P, D], F32)
        self.load("sp", g2, g2.t[:], bcd, bcd.t.ap()[:, D:2 * D]); self.load("act", fg, fg.t[:], fgb, fgb.t.ap())
        xt = [C.sb("xtK%d" % i, [P, D], F32) for i in range(2)]
        ft = [C.sb("ftK%d" % i, [P, D], F32) for i in range(2)]
        junk = C.sb("junkK", [P, D], BF16)
        sts = [C.sb("stK%d" % i, [P, 4], F32) for i in range(2)]
        for tt in range(NT):
            X = xt[tt % 2]; Fd = ft[tt % 2]; st = sts[tt % 2]
            tk = slice(tt * P, (tt + 1) * P)
            self.load("sp", X, X.t[:], x1r, x1r.t.ap()[tk, :])
            for db in range(8):
                C.dma("act" if db % 2 else "sp", lambda e, Fd=Fd, db=db, tk=tk: e.dma_start(out=Fd.t[:, db * 512:(db + 1) * 512], in_=ffn[db].t.ap()[tk, :]), reads=[self.fres[db]], writes=[Fd])
            C.op("dve", lambda e, Fd=Fd: e.tensor_tensor(out=Fd.t[:], in0=Fd.t[:], in1=g2.t[:], op=ALU.mult), reads=[Fd, g2], writes=[Fd])
            C.op("pool", lambda e, Fd=Fd, X=X: e.tensor_tensor(out=Fd.t[:], in0=Fd.t[:], in1=X.t[:], op=ALU.add), reads=[Fd, X], writes=[Fd])
            self.rms_stats(Fd, junk, st, D)
            C.op("dve", lambda e, Fd=Fd, st=st, X=X: e.scalar_tensor_tensor(out=X.t[:], in0=Fd.t[:], scalar=st.t[:, 1:2], in1=fg.t[:], op0=ALU.mult, op1=ALU.mult), reads=[Fd, st, fg, X], writes=[X])
            self.store("sp", out, out.t.ap()[tk, :], X, X.t[:])
        C.pop()


def prep_inputs(inp, b, names, r=0):
    hc = host_constants()
    f = lambda a: np.ascontiguousarray(np.asarray(a, np.float32))
    bro = lambda v: np.ascontiguousarray(np.broadcast_to(np.asarray(v, np.float32).reshape(1, -1), (P, np.asarray(v).size)))
    m = {}
    m["x"] = f(inp["x"][b]); m["ctx"] = f(inp["ctx"][b])
    cc = np.stack([col_layout(inp["c"][b]), col_layout(inp["c_ctx"])], axis=2)
    m["cT"] = f(cc.reshape(P, 64))
    m["ada_w"] = f(inp["ada_w"][0]); m["abT"] = col_layout(inp["ada_b"][0])
    ab = np.asarray(inp["ada_b"][0], np.float32).reshape(6, D)
    m["abb"] = np.ascontiguousarray(np.concatenate([bro(ab[2]), bro(ab[5]), bro(ab[4]), bro(ab[3])], axis=1))
    m["g1T"] = col_layout(inp["norm1_g"][0]); m["g2b"] = bro(inp["norm2_g"][0]); m["fgb"] = bro(inp["final_g"])
    m["w_in"] = f(inp["w_in"][0]); m["w_out"] = f(inp["w_out"][0])
    sw = np.asarray(inp["hy_short_w"][0], np.float32); sb_ = np.asarray(inp["hy_short_b"][0], np.float32)
    swt = np.stack([col_layout(sw[0]), col_layout(sw[1]), col_layout(sw[2]), col_layout(sb_)], axis=2)
    m["swT"] = f(swt.reshape(P, 48 * 4))
    m["fw1"] = f(inp["hy_f_w1"][0]); m["fw2"] = f(inp["hy_f_w2"][0]); m["fw3"] = f(inp["hy_f_w3"][0]); m["fw4"] = f(inp["hy_f_w4"][0])
    m["fvec"] = f(np.stack([inp["hy_f_b1"][0], inp["hy_f_b2"][0], inp["hy_f_b3"][0], inp["hy_sin_freq"][0]], axis=1))
    m["hyb"] = bro(np.asarray(inp["hy_bias"][0]).reshape(-1)); m["rdec"] = bro(np.asarray(inp["ret_decay"][0]).reshape(-1))
    perm = [(e + NL * r) % NE for e in range(NE)]
    rw = np.asarray(inp["router_w"][0], np.float32)[:, perm].reshape(32, P, NE).transpose(1, 0, 2)
    m["rwT"] = f(rw.reshape(P, 32 * NE))
    m["cst"] = hc["cst"].reshape(NFT, P, 2 * NFT * P); m["fscale"] = hc["fscale"]
    m["zT"] = hc["zT"]; m["negt"] = hc["negt"]; m["deltab"] = hc["deltab"]
    m["rot_x"] = hc["rot_x"].reshape(T, 512); m["rot_c"] = hc["rot_c"].reshape(LC, 512)
    m["rot_xk"] = hc["rot_xk"].reshape(T, 512); m["rot_ck"] = hc["rot_ck"].reshape(LC, 512)
    m["misc"] = hc["misc"]
    if "wg" in names:
        sl = slice(NL * r, NL * (r + 1))
        m["wg"] = f(inp["exp_w_gate"][0][sl]); m["wu"] = f(inp["exp_w_up"][0][sl]); m["wd"] = f(inp["exp_w_down"][0][sl])
    return {k: v for k, v in m.items() if k in names}


def run(inputs, upto="all", dbg=(), cores=8):
    kb = K(upto=upto, dbg=dbg)
    nc = kb.build()
    names = set(kb.inputs.keys())
    maps = {}
    in_maps = []
    for cid in range(cores):
        b = cid // 2
        if b not in maps:
            maps[b] = prep_inputs(inputs, b, names, 0)
        in_maps.append(maps[b])
    res = run_bass_kernel_spmd(nc, in_maps, core_ids=list(range(cores)))
    return res, kb


def kernel(**inputs):
    res, kb = run(inputs)
    out = np.stack([np.asarray(res.results[2 * b]["out"], np.float32) for b in range(4)], axis=0)
    return out
```

```python
import math
from contextlib import ExitStack
import numpy as np
import ml_dtypes
import concourse.bass as bass
import concourse.mybir as mybir
from concourse.bass_utils import run_bass_kernel_spmd

F32 = mybir.dt.float32; BF16 = mybir.dt.bfloat16; I32 = mybir.dt.int32
ALU = mybir.AluOpType; AF = mybir.ActivationFunctionType

D = 4096; T = 4096; LC = 256; NT = 32; P = 128
HYW = 2048; HYC = 6144; QC = 1024; GC = 4096; KC = 1024; VC = 2048
KV0 = HYC + QC + GC; INW = 14336
NE = 16; NL = 16; EFF = 2048; CAP = 512
NFT = 33
EPS = 1e-6


class Res:
    __slots__ = ("name", "w", "r", "t", "dsem")

    def __init__(self, name, t=None):
        self.name = name; self.t = t; self.w = {}; self.r = {}; self.dsem = None


def _is_dram(r):
    return r.t is not None and type(r.t).__name__.lower().startswith("dram")


class Ctx:
    def __init__(self, nc, es):
        self.nc = nc; self.es = es; self.engs = {}; self.sems = {}
        for name, e in (("pe", nc.tensor), ("dve", nc.vector), ("act", nc.scalar), ("pool", nc.gpsimd), ("sp", nc.sync)):
            sem = es.enter_context(nc.semaphore("s_" + name))
            self.engs[name] = dict(e=e, seen={})
            self.sems[name] = [sem, 0]
        self.free_dsems = []
        self.ndsem = 0
        self.stack = [es]
        self.scope_res = [[]]
        self.ninst = 0

    def sb(self, name, shape, dt):
        t = self.stack[-1].enter_context(self.nc.sbuf_tensor(name, list(shape), dt))
        r = Res(name, t); self.scope_res[-1].append(r); return r

    def ps(self, name, shape, dt=F32):
        t = self.stack[-1].enter_context(self.nc.psum_tensor(name, list(shape), dt))
        r = Res(name, t); self.scope_res[-1].append(r); return r

    def dram(self, name, shape, dt, kind="Internal"):
        return Res(name, self.nc.dram_tensor(name, list(shape), dt, kind=kind))

    def push(self):
        es = ExitStack(); self.stack.append(es); self.scope_res.append([]); return es

    def pop(self):
        self.barrier()
        for r in self.scope_res.pop():
            if r.dsem is not None:
                self.free_dsems.append(r.dsem); r.dsem = None
        self.stack.pop().close()

    def _dsem(self, r):
        if r.dsem is None:
            if self.free_dsems:
                r.dsem = self.free_dsems.pop()
            else:
                key = "d%d" % self.ndsem; self.ndsem += 1
                sem = self.es.enter_context(self.nc.semaphore(key))
                self.sems[key] = [sem, 0]; r.dsem = key
        return r.dsem

    def _wait(self, eng, deps):
        E = self.engs[eng]
        for key, val in deps.items():
            if key[0] == "d":
                val = self.sems[key][1]
            elif key == eng and eng == "pe":
                continue
            if E["seen"].get(key, 0) >= val:
                continue
            E["e"].wait_ge(self.sems[key][0], val)
            E["seen"][key] = val

    def _deps(self, reads, writes):
        deps = {}
        for r in reads:
            for k, v in r.w.items():
                if deps.get(k, 0) < v: deps[k] = v
        for r in writes:
            for k, v in r.w.items():
                if deps.get(k, 0) < v: deps[k] = v
            for k, v in r.r.items():
                if deps.get(k, 0) < v: deps[k] = v
        return deps

    def _mark(self, key, val, reads, writes):
        for r in reads:
            if r.r.get(key, 0) < val: r.r[key] = val
        for r in writes:
            r.w = {key: val}; r.r = {}

    def op(self, eng, fn, reads=(), writes=()):
        self._wait(eng, self._deps(reads, writes))
        inst = fn(self.engs[eng]["e"])
        s = self.sems[eng]; s[1] += 1
        inst.then_inc(s[0], 1)
        self._mark(eng, s[1], reads, writes)
        self.ninst += 1
        return inst

    def dma(self, q, fn, reads=(), writes=(), semres=None):
        if semres is None:
            if writes and not _is_dram(writes[0]):
                semres = writes[0]
            else:
                semres = reads[0]
        key = self._dsem(semres)
        self._wait(q, self._deps(reads, writes))
        inst = fn(self.engs[q]["e"])
        s = self.sems[key]; s[1] += 16
        inst.then_inc(s[0], 16)
        self._mark(key, s[1], reads, writes)
        self.ninst += 1
        return inst

    def barrier(self):
        allk = {k: v[1] for k, v in self.sems.items() if v[1] > 0}
        for eng in self.engs:
            self._wait(eng, allk)


def _bf(a):
    return np.ascontiguousarray(a.astype(ml_dtypes.bfloat16))


_CONST_CACHE = {}


def host_constants():
    if _CONST_CACHE:
        return _CONST_CACHE
    c = {}
    n = NFT * P
    idx = np.arange(n, dtype=np.int64)
    prod = (idx[:, None] * idx[None, :]) % 8192
    valid = (idx[:, None] <= 4096) & (idx[None, :] <= 4096)
    ang = prod.astype(np.float64) * (2.0 * math.pi / 8192.0)
    Cm = np.where(valid, np.cos(ang), 0.0)
    Sm = np.where(valid, np.sin(ang), 0.0)
    def tile(M):
        return M.reshape(NFT, P, NFT, P).transpose(2, 1, 0, 3)
    cs = np.stack([tile(Cm), tile(Sm)], axis=2)
    c["cst"] = _bf(cs.astype(np.float32))
    fs = np.zeros(n, np.float32); fs[0] = 1.0 / 8192; fs[1:4096] = 2.0 / 8192; fs[4096] = 1.0 / 8192
    c["fscale"] = np.ascontiguousarray(fs.reshape(NFT, P).T)
    L = T
    bands = 16
    t = np.linspace(0.0, 1.0, L, dtype=np.float32)[:, None]
    ang2 = (np.float32(2.0 * math.pi / L) * np.arange(L, dtype=np.float32)[:, None]) * np.linspace(1e-4, bands - 1, bands, dtype=np.float32)[None, :]
    z = np.concatenate([t, np.cos(ang2), -np.sin(ang2)], axis=-1).astype(np.float32)
    c["zT"] = np.ascontiguousarray(z.T)
    c["negt"] = np.ascontiguousarray((-t[:, 0]).reshape(NT, P).T.astype(np.float32))
    max_decay = math.log(1e-2) / 0.3; min_decay = math.log(1e-2) / 1.5
    deltas = np.abs(np.linspace(min_decay, max_decay, HYW, dtype=np.float32))
    c["deltab"] = np.ascontiguousarray(np.broadcast_to(deltas[None, :], (P, HYW)).astype(np.float32))
    n_freq = 32
    inv = (1.0 / (10000.0 ** np.linspace(0.0, 1.0, n_freq, dtype=np.float32))).astype(np.float32)
    def rot(pa, pb):
        a = np.concatenate([pa[:, None] * inv, pb[:, None] * inv], axis=-1).astype(np.float32)
        return np.cos(a).astype(np.float32), np.sin(a).astype(np.float32)
    rows = L // 64
    lat_row = np.repeat(np.arange(rows, dtype=np.float32), 64)
    lat_col = np.tile(np.arange(64, dtype=np.float32), rows)
    cx, sx = rot(lat_row, lat_col)
    cp = np.arange(LC, dtype=np.float32)
    cc, sc = rot(cp, cp)
    c["rot_x"] = np.ascontiguousarray(np.stack([np.tile(cx, (1, 4)), np.tile(sx, (1, 4))], axis=1))
    c["rot_c"] = np.ascontiguousarray(np.stack([np.tile(cc, (1, 4)), np.tile(sc, (1, 4))], axis=1))
    ks = np.float32(128 ** -0.5)
    c["rot_xk"] = np.ascontiguousarray(c["rot_x"] * ks); c["rot_ck"] = np.ascontiguousarray(c["rot_c"] * ks)
    pidx = np.arange(P, dtype=np.float32)
    m = {}
    m["ident"] = np.eye(P, dtype=np.float32)
    m["ones"] = np.ones((P, P), np.float32)
    jj = pidx[:, None]; ii = pidx[None, :]
    m["diffF"] = np.maximum(ii - jj, 0.0); m["maskF"] = (ii >= jj).astype(np.float32)
    m["diffB"] = np.maximum(jj - ii, 0.0); m["maskB"] = (jj >= ii).astype(np.float32)
    m["ip1"] = np.broadcast_to(ii + 1.0, (P, P)).copy(); m["i128m"] = np.broadcast_to(128.0 - ii, (P, P)).copy()
    m["tri"] = (jj < ii).astype(np.float32)
    m["iota512"] = np.broadcast_to(np.arange(CAP, dtype=np.float32)[None, :], (P, CAP)).copy()
    cols = np.zeros((P, 8), np.float32)
    cols[:, 0] = 127.0 - pidx; cols[:, 1] = pidx; cols[:, 2] = 255.0 - pidx; cols[:, 3] = 127.0 - pidx
    cols[:, 4] = pidx; cols[:, 5] = 128.0 + pidx
    cols[:, 6] = EPS; cols[:, 7] = 1.0
    m["cols"] = cols
    tv = np.zeros((P, NT, 2), np.float32)
    tok = (np.arange(NT)[None, :] * P + np.arange(P)[:, None])
    tv[:, :, 0] = tok // 64; tv[:, :, 1] = tok % 64
    m["tvals"] = tv.reshape(P, NT * 2)
    sel = np.zeros((P, P), np.float32); sel[0, :] = 1.0
    m["sel0"] = sel
    off = {}; parts = []; o = 0
    for k, v in m.items():
        off[k] = (o, v.shape[1]); parts.append(v.astype(np.float32)); o += v.shape[1]
    c["misc"] = np.ascontiguousarray(np.concatenate(parts, axis=1))
    c["misc_off"] = off
    _CONST_CACHE.update(c)
    return c


class Stream:
    def __init__(self, jobs, ahead=1):
        self.jobs = jobs; self.done = {}; self.k = 0; self.ahead = ahead

    def next(self):
        k = self.k
        for j in range(k, min(k + self.ahead + 1, len(self.jobs))):
            if j not in self.done:
                self.done[j] = self.jobs[j]()
        self.k += 1
        return self.done.pop(k)


def col_layout(v):
    v = np.asarray(v, np.float32).reshape(-1, P)
    return np.ascontiguousarray(v.T)


class K:
    def __init__(self, upto="all", dbg=()):
        self.upto = upto; self.dbg = set(dbg)
        self.nc = bass.Bass("TRN2", target_bir_lowering=False)
        self.inputs = {}
        self.outputs = []

    def inp(self, name, shape, dt=F32):
        r = self.C.dram(name, shape, dt, kind="ExternalInput"); self.inputs[name] = r; return r

    def scratch(self, name, shape, dt):
        kind = "ExternalOutput" if name in self.dbg else "Internal"
        if name in self.dbg:
            self.outputs.append(name)
        return self.C.dram(name, shape, dt, kind=kind)

    def load(self, q, dst, dst_ap, src, src_ap):
        return self.C.dma(q, lambda e: e.dma_start(out=dst_ap, in_=src_ap), reads=[src], writes=[dst])

    def store(self, q, dst, dst_ap, src, src_ap):
        return self.C.dma(q, lambda e: e.dma_start(out=dst_ap, in_=src_ap), reads=[src], writes=[dst], semres=src)

    def nextps(self):
        r = self.pbanks[self.pbi % len(self.pbanks)]; self.pbi += 1; return r

    def mm(self, pr, out_ap, pairs, reads):
        n = len(pairs)

        def fn(e):
            inst = None
            for i, (l, r) in enumerate(pairs):
                inst = e.matmul(out_ap, l, r, start=(i == 0), stop=(i == n - 1))
            return inst
        return self.C.op("pe", fn, reads=reads, writes=[pr])

    def cast_eng(self):
        self.ce = (self.ce + 1) % 3
        return ("dve", "act", "pool")[self.ce]

    def copy(self, eng, dst, dst_ap, src, src_ap, extra_reads=()):
        if eng == "act":
            return self.C.op("act", lambda e: e.activation(out=dst_ap, in_=src_ap, func=AF.Copy), reads=[src, *extra_reads], writes=[dst])
        return self.C.op(eng, lambda e: e.tensor_copy(dst_ap, src_ap), reads=[src, *extra_reads], writes=[dst])

    def build(self):
        nc = self.nc
        with ExitStack() as es:
            self.C = C = Ctx(nc, es)
            self.pbi = 0; self.ce = 0
            hc = host_constants()
            self.moff = hc["misc_off"]
            nmisc = hc["misc"].shape[1]
            I = self.inp
            x = I("x", [T, D]); ctx = I("ctx", [LC, D]); cT = I("cT", [P, 64])
            ada_w = I("ada_w", [D, 6 * D]); abT = I("abT", [P, 192])
            g1T = I("g1T", [P, 32]); g2b = I("g2b", [P, D]); fgb = I("fgb", [P, D]); abb = I("abb", [P, 4 * D])
            w_in = I("w_in", [D, INW]); w_out = I("w_out", [D, D])
            swT = I("swT", [P, 48 * 4])
            fw1 = I("fw1", [33, 64]); fw2 = I("fw2", [64, 64]); fw3 = I("fw3", [64, 64]); fw4 = I("fw4", [64, 8192])
            fvec = I("fvec", [64, 4])
            hyb = I("hyb", [P, 4096]); rdec = I("rdec", [P, 16])
            rwT = I("rwT", [P, 32 * 16])
            cst = I("cst", [NFT, P, 2 * NFT * P], BF16); fscale = I("fscale", [P, NFT])
            zT = I("zT", [33, T]); negt = I("negt", [P, NT]); deltab = I("deltab", [P, HYW])
            rot_x = I("rot_x", [T, 512]); rot_c = I("rot_c", [LC, 512]); rot_xk = I("rot_xk", [T, 512]); rot_ck = I("rot_ck", [LC, 512]); misc = I("misc", [P, nmisc])
            if self.upto in ("all", "J"):
                wg = I("wg", [NL, D, EFF]); wu = I("wu", [NL, D, EFF]); wd = I("wd", [NL, EFF, D])
            out = C.dram("out", [T, D], F32, kind="ExternalOutput")
            self.outputs.append("out")
            S = self.scratch
            hxT = S("hxT", [D, T], BF16)
            v_tm = S("v_tm", [T, HYW], BF16); x1_tm = S("x1_tm", [T, HYW], BF16); x2_cm = S("x2_cm", [HYW, T], BF16)
            sg = S("sg", [GC, T], BF16)
            qT_d = S("qT_d", [QC, T], BF16); kT_d = S("kT_d", [KC, T], BF16)
            k_tm = S("k_tm", [T, KC], BF16); vr_tm = S("vr_tm", [T, VC], BF16)
            ed = S("ed", [T, 2 * 4096], BF16)
            spec = S("spec", [NFT * P, 2 * 4096], F32)
            yT = S("yT", [6144, T], BF16)
            x1r = S("x1r", [T, D], F32); hx2 = S("hx2", [T, D], BF16)
            affd = S("affd", [T, NE], F32)
            ffn = [S("ffn%d" % i, [T, 512], F32) for i in range(8)]
            ffr = [S("ffr%d" % i, [T, 512], F32) for i in range(8)]
            dbgs = S("dbgs", [P, 4096], F32)
            self.dbgs_res = dbgs

            mt = C.sb("misc_t", [P, nmisc], F32)
            self.load("sp", mt, mt.t[:], misc, misc.t.ap())

            def M(name, rows=P):
                o, w = self.moff[name]
                return mt.t[0:rows, o:o + w]
            self.M = M
            identb = C.sb("identb", [P, P], BF16); onesb = C.sb("onesb", [P, P], BF16)
            C.op("dve", lambda e: e.tensor_copy(identb.t[:], M("ident")), reads=[mt], writes=[identb])
            C.op("dve", lambda e: e.tensor_copy(onesb.t[:], M("ones")), reads=[mt], writes=[onesb])
            self.identb = identb; self.onesb = onesb; self.mt = mt
            self.pbanks = [C.ps("pb%d" % i, [P, 512], F32) for i in range(7)]
            self.pacc = C.ps("pacc", [P, 512], F32)
            self.slab_i = 0
            modsT = C.sb("modsT", [P, 192, 2], F32)
            AB = C.sb("AB", [P, 4, 32], F32)
            bcd = S("bcd", [P, 4 * D], F32)
            lg = C.sb("lg", [P, 16], F32)
            Sfd = S("Sfd", [P, 16 * 256], F32)

            self.phase_A(cT, ada_w, abT, g1T, g2b, abb, modsT, AB, bcd)
            if "modsT" in self.dbg:
                pass
            if self.upto == "A":
                return self.finish(dbgs, [(modsT, modsT.t[:].rearrange("p a b -> p (a b)"), 384), (AB, AB.t[:].rearrange("p a b -> p (a b)"), 128)])
            C.push()
            hcT = C.sb("hcT", [P, 32, LC], BF16)
            Sf = C.sb("Sf", [P, 16, 256], F32); Sb = C.sb("Sb", [P, 16, 256], BF16)
            self.stg_i = 0
            self.phase_B(x, ctx, AB, hxT, hcT)
            if self.upto == "B":
                tmpd = C.sb("tmpd", [P, 512], F32)
                C.op("dve", lambda e: e.tensor_copy(tmpd.t[:, 0:256], hcT.t[:, 0, :]), reads=[hcT], writes=[tmpd])
                C.op("dve", lambda e: e.tensor_copy(tmpd.t[:, 256:512], hcT.t[:, 31, :]), reads=[hcT], writes=[tmpd])
                return self.finish(dbgs, [(tmpd, tmpd.t[:], 512)])
            self.phase_C(hcT, w_in, rot_ck, rdec, lg, Sf, Sb)
            self.store("sp", Sfd, Sfd.t.ap(), Sf, Sf.t[:].rearrange("p a b -> p (a b)"))
            if self.upto == "C":
                return self.finish(dbgs, [(lg, lg.t[:], 16), (Sf, Sf.t[:, 0, :], 256), (Sf, Sf.t[:, 15, :], 256)])
            C.pop()
            self.phase_D(hxT, w_in, swT, rot_x, rot_xk, v_tm, x1_tm, x2_cm, sg, qT_d, kT_d, k_tm, vr_tm)
            if self.upto == "D":
                return self.finish(dbgs, [])
            self.phase_E(fw1, fw2, fw3, fw4, fvec, zT, negt, deltab, hyb, cst, fscale, spec)
            if self.upto == "E":
                return self.finish(dbgs, [])
            self.phase_F(v_tm, x1_tm, x2_cm, spec, cst, yT)
            if self.upto == "F":
                return self.finish(dbgs, [])
            self.phase_G(qT_d, kT_d, k_tm, vr_tm, sg, Sfd, lg, yT)
            if self.upto == "G":
                return self.finish(dbgs, [])
            aff_tm = C.sb("aff_tm", [P, NT, NE], F32)
            self.phase_H(yT, w_out, x, bcd, x1r, hx2, rwT, aff_tm)
            if self.upto == "H":
                return self.finish(dbgs, [(aff_tm, aff_tm.t[:].rearrange("p a b -> p (a b)"), 512)])
            idx_all = C.sb("idx_all", [P, NL, 4], I32); gate_all = C.sb("gate_all", [P, NL, 4], F32)
            self.phase_I(aff_tm, idx_all, gate_all)
            if self.upto == "I":
                idf2 = C.sb("idf2", [P, 64], F32)
                C.op("dve", lambda e: e.tensor_copy(idf2.t[:], idx_all.t[:].rearrange("p a b -> p (a b)")), reads=[idx_all], writes=[idf2])
                return self.finish(dbgs, [(idf2, idf2.t[:], 64), (gate_all, gate_all.t[:].rearrange("p a b -> p (a b)"), 64)])
            self.phase_J(hx2, wg, wu, wd, idx_all, gate_all, ffn, ffr)
            self.phase_K(x1r, ffn, bcd, fgb, out)
            return self.finish(dbgs, [])

    def finish(self, dbgs, dumps):
        C = self.C
        o = 0
        for (r, ap, w) in dumps:
            self.store("sp", dbgs, dbgs.t.ap()[:, o:o + w], r, ap); o += w
        self.outputs.append("dbgs") if "dbgs" in self.dbg else None
        C.barrier()
        return self.nc

    def phase_A(self, cT, ada_w, abT, g1T, g2b, abb, modsT, AB, bcd):
        C = self.C; M = self.M
        C.push()
        bc = C.sb("bc", [P, 4, D], F32)
        ct = C.sb("ct", [P, 64], F32); sct = C.sb("sct", [P, 32, 2], F32)
        abt = C.sb("abt", [P, 192], F32); g1t = C.sb("g1t", [P, 32], F32)
        self.load("sp", ct, ct.t[:], cT, cT.t.ap())
        self.load("sp", abt, abt.t[:], abT, abT.t.ap())
        self.load("sp", g1t, g1t.t[:], g1T, g1T.t.ap())
        C.op("act", lambda e: e.activation(out=sct.t[:].rearrange("p a b -> p (a b)"), in_=ct.t[:], func=AF.Silu), reads=[ct], writes=[sct])
        wbuf = [C.sb("aw%d" % i, [P, 8, 512], F32) for i in range(5)]
        row = [C.sb("row%d" % i, [2, 512], F32) for i in range(2)]
        awv = ada_w.t.ap().rearrange("(kc p) n -> p kc n", p=P)
        wi = 0
        bmap = {2: 0, 5: 1, 4: 2, 3: 3}
        for nb in range(48):
            pr = self.nextps()
            halves = []
            for h in range(4):
                wb = wbuf[wi % 5]; wi += 1
                self.load("sp" if (wi % 2) else "act", wb, wb.t[:], ada_w, awv[:, h * 8:(h + 1) * 8, nb * 512:(nb + 1) * 512])
                halves.append(wb)
            def fn(e, halves=halves, pr=pr):
                inst = None
                for h in range(4):
                    for k in range(8):
                        kc = h * 8 + k
                        inst = e.matmul(pr.t[0:2, :], sct.t[:, kc, :], halves[h].t[:, k, :], start=(kc == 0), stop=(kc == 31))
                return inst
            C.op("pe", fn, reads=[sct, *halves], writes=[pr])
            rw = row[nb % 2]
            C.op("dve", lambda e, rw=rw, pr=pr: e.tensor_copy(rw.t[:], pr.t[0:2, :]), reads=[pr], writes=[rw])
            pt = self.nextps()
            def fn2(e, rw=rw, pt=pt):
                inst = None
                for j in range(4):
                    inst = e.transpose(pt.t[:, j * 2:j * 2 + 2], rw.t[0:2, j * 128:(j + 1) * 128], M("ident", 2)[:, 0:2])
                return inst
            C.op("pe", fn2, reads=[rw, self.mt], writes=[pt])
            C.op("dve", lambda e, pt=pt, nb=nb: e.tensor_copy(modsT.t[:, nb * 4:(nb + 1) * 4, :].rearrange("p a b -> p (a b)"), pt.t[:, 0:8]), reads=[pt], writes=[modsT])
            mod = nb // 8
            if mod in bmap:
                pb = self.nextps()
                C.op("pe", lambda e, pb=pb, rw=rw: e.matmul(pb.t[:, :], M("sel0", 2), rw.t[0:2, :], start=True, stop=True), reads=[rw, self.mt], writes=[pb])
                j = bmap[mod]; cb = (nb % 8) * 512
                C.op("act", lambda e, pb=pb, j=j, cb=cb: e.activation(out=bc.t[:, j, cb:cb + 512], in_=pb.t[:, :], func=AF.Copy), reads=[pb], writes=[bc])
        for r in range(2):
            C.op("dve", lambda e, r=r: e.tensor_tensor(out=modsT.t[:, :, r], in0=modsT.t[:, :, r], in1=abt.t[:], op=ALU.add), reads=[modsT, abt], writes=[modsT])
        for r in range(2):
            C.op("dve", lambda e, r=r: e.scalar_tensor_tensor(out=AB.t[:, 2 * r, :], in0=modsT.t[:, 32:64, r], scalar=1.0, in1=g1t.t[:], op0=ALU.add, op1=ALU.mult), reads=[modsT, g1t], writes=[AB])
            C.op("dve", lambda e, r=r: e.tensor_copy(AB.t[:, 2 * r + 1, :], modsT.t[:, 0:32, r]), reads=[modsT], writes=[AB])
        tmpb = C.sb("tmpb", [P, D], F32)
        for j in range(4):
            self.load("sp", tmpb, tmpb.t[:], abb, abb.t.ap()[:, j * D:(j + 1) * D])
            C.op("dve", lambda e, j=j: e.tensor_tensor(out=bc.t[:, j, :], in0=bc.t[:, j, :], in1=tmpb.t[:], op=ALU.add), reads=[bc, tmpb], writes=[bc])
        self.load("sp", tmpb, tmpb.t[:], g2b, g2b.t.ap())
        C.op("dve", lambda e: e.scalar_tensor_tensor(out=bc.t[:, 2, :], in0=bc.t[:, 2, :], scalar=1.0, in1=tmpb.t[:], op0=ALU.add, op1=ALU.mult), reads=[bc, tmpb], writes=[bc])
        self.store("sp", bcd, bcd.t.ap(), bc, bc.t[:].rearrange("p a b -> p (a b)"))
        C.pop()

    def psb(self, pr):
        return pr.t[:, :].bitcast(BF16)

    def rms_stats(self, xt, junk, st, width):
        C = self.C; M = self.M
        C.op("act", lambda e: e.activation(out=junk.t[:, 0:width], in_=xt.t[:, 0:width], func=AF.Square, accum_out=st.t[:, 0:1]), reads=[xt], writes=[junk, st])
        C.op("act", lambda e: e.activation(out=st.t[:, 2:3], in_=st.t[:, 0:1], func=AF.Sqrt, scale=1.0 / width, bias=M("cols")[:, 6:7]), reads=[st, self.mt], writes=[st])
        C.op("dve", lambda e: e.reciprocal(st.t[:, 1:2], st.t[:, 2:3]), reads=[st], writes=[st])

    def phase_B(self, x, ctx, AB, hxT, hcT):
        C = self.C; M = self.M
        C.push()
        xt = [C.sb("xt%d" % i, [P, D], F32) for i in range(2)]
        xn = [C.sb("xn%d" % i, [P, D], BF16) for i in range(2)]
        junk = C.sb("junkB", [P, D], BF16)
        sts = [C.sb("stB%d" % i, [P, 4], F32) for i in range(2)]
        hb = [C.sb("hbB%d" % i, [P, 32, 512], BF16) for i in range(2)]
        hview = hxT.t.ap().rearrange("(kc p) t -> p kc t", p=P)
        ev = 0
        for tt in range(NT + 2):
            isc = tt >= NT
            X = xt[tt % 2]; XN = xn[tt % 2]; st = sts[tt % 2]
            if isc:
                self.load("sp", X, X.t[:], ctx, ctx.t.ap()[(tt - NT) * P:(tt - NT + 1) * P, :])
            else:
                self.load("sp" if tt % 2 else "act", X, X.t[:], x, x.t.ap()[tt * P:(tt + 1) * P, :])
            self.rms_stats(X, junk, st, D)
            C.op("act", lambda e, X=X, XN=XN, st=st: e.activation(out=XN.t[:], in_=X.t[:], func=AF.Copy, scale=st.t[:, 1:2]), reads=[X, st], writes=[XN])
            a_i = 2 if isc else 0
            H = hb[(tt // 4) % 2]
            for g in range(4):
                pr = self.nextps()
                pv = self.psb(pr)
                def fn(e, XN=XN, pv=pv, g=g):
                    inst = None
                    for j in range(8):
                        kc = g * 8 + j
                        inst = e.transpose(pv[:, j * P:(j + 1) * P], XN.t[:, kc * P:(kc + 1) * P], self.identb.t[:])
                    return inst
                C.op("pe", fn, reads=[XN, self.identb], writes=[pr])
                for j in range(8):
                    kc = g * 8 + j
                    if isc:
                        dst, dap = hcT, hcT.t[:, kc, (tt - NT) * P:(tt - NT + 1) * P]
                    else:
                        dst, dap = H, H.t[:, kc, (tt % 4) * P:(tt % 4 + 1) * P]
                    ev += 1
                    if ev % 2:
                        C.op("dve", lambda e, dap=dap, pv=pv, j=j, kc=kc, a_i=a_i: e.tensor_scalar(out=dap, in0=pv[:, j * P:(j + 1) * P], scalar1=AB.t[:, a_i, kc:kc + 1], scalar2=AB.t[:, a_i + 1, kc:kc + 1], op0=ALU.mult, op1=ALU.add), reads=[pr, AB], writes=[dst])
                    else:
                        C.op("act", lambda e, dap=dap, pv=pv, j=j, kc=kc, a_i=a_i: e.activation(out=dap, in_=pv[:, j * P:(j + 1) * P], func=AF.Identity, scale=AB.t[:, a_i, kc:kc + 1], bias=AB.t[:, a_i + 1, kc:kc + 1]), reads=[pr, AB], writes=[dst])
            if (not isc) and tt % 4 == 3:
                tb = tt // 4
                self.store("sp", hxT, hview[:, :, tb * 512:(tb + 1) * 512], H, H.t[:])
        C.pop()

    def load_w_bf(self, wsrc, view, stg, wb):
        C = self.C
        for q in range(4):
            s = stg[self.stg_i % len(stg)]; self.stg_i += 1
            self.load("sp" if q % 2 else "act", s, s.t[:], wsrc, view[:, q * 8:(q + 1) * 8, :])
            self.copy(self.cast_eng(), wb, wb.t[:, q * 8:(q + 1) * 8, :], s, s.t[:])

    def rotary(self, pr, rt, rtap, dst, dap, tmps):
        C = self.C
        pv = pr.t[:, :].rearrange("p (h two f) -> p h two f", h=4, two=2)
        x1 = pv[:, :, 0, :]; x2 = pv[:, :, 1, :]
        cs = rtap[:, 0, :].rearrange("p (h f) -> p h f", h=4); sn = rtap[:, 1, :].rearrange("p (h f) -> p h f", h=4)
        t1, t2 = tmps
        t1v = t1.t[:, :].rearrange("p (h f) -> p h f", h=4); t2v = t2.t[:, :].rearrange("p (h f) -> p h f", h=4)
        dv = dap.rearrange("p (h two f) -> p h two f", h=4, two=2)
        C.op("dve", lambda e: e.tensor_tensor(out=t1v, in0=x1, in1=cs, op=ALU.mult), reads=[pr, rt], writes=[t1])
        C.op("dve", lambda e: e.tensor_tensor(out=t2v, in0=x2, in1=sn, op=ALU.mult), reads=[pr, rt], writes=[t2])
        C.op("dve", lambda e: e.tensor_tensor(out=dv[:, :, 0, :], in0=t1v, in1=t2v, op=ALU.subtract), reads=[t1, t2], writes=[dst])
        C.op("dve", lambda e: e.tensor_tensor(out=t1v, in0=x1, in1=sn, op=ALU.mult), reads=[pr, rt], writes=[t1])
        C.op("dve", lambda e: e.tensor_tensor(out=t2v, in0=x2, in1=cs, op=ALU.mult), reads=[pr, rt], writes=[t2])
        C.op("dve", lambda e: e.tensor_tensor(out=dv[:, :, 1, :], in0=t1v, in1=t2v, op=ALU.add), reads=[t1, t2], writes=[dst])

    def phase_C(self, hcT, w_in, rot_ck, rdec, lg, Sf, Sb):
        C = self.C; M = self.M
        C.push()
        rd = C.sb("rd", [P, 16], F32)
        self.load("sp", rd, rd.t[:], rdec, rdec.t.ap())
        C.op("act", lambda e: e.activation(out=rd.t[:], in_=rd.t[:], func=AF.Exp, scale=-1.0), reads=[rd], writes=[rd])
        C.op("act", lambda e: e.activation(out=rd.t[:], in_=rd.t[:], func=AF.Ln, bias=M("cols")[:, 7:8]), reads=[rd, self.mt], writes=[rd])
        C.op("dve", lambda e: e.tensor_scalar(out=lg.t[:], in0=rd.t[:], scalar1=-1.0, scalar2=None, op0=ALU.mult), reads=[rd], writes=[lg])
        wctx = C.sb("wctx", [P, 2, 16], F32)
        for tt in range(2):
            for d in range(2):
                col = 2 + tt if d == 0 else 4 + tt
                C.op("act", lambda e, tt=tt, d=d, col=col: e.activation(out=wctx.t[:, tt, d * 8:(d + 1) * 8], in_=lg.t[:, d * 8:(d + 1) * 8], func=AF.Exp, scale=M("cols")[:, col:col + 1]), reads=[lg, self.mt], writes=[wctx])
        import os
        cstop = int(os.environ.get("CSTOP", "9"))
        if cstop <= 1:
            C.pop(); return
        stg = [C.sb("stgC%d" % i, [P, 8, 512], F32) for i in range(3)]
        wb = [C.sb("wbC%d" % i, [P, 32, 512], BF16) for i in range(2)]
        kct = C.sb("kct", [P, 2, KC], BF16); vct = C.sb("vct", [P, 2, VC], BF16)
        rt = C.sb("rtC", [P, 2, 2, 256], F32)
        tm = [C.sb("tmC%d" % i, [P, 256], F32) for i in range(2)]
        self.load("sp", rt, rt.t[:].rearrange("p a b c -> p a (b c)"), rot_ck, rot_ck.t.ap().rearrange("(a p) c -> p a c", p=P))
        wv = w_in.t.ap().rearrange("(kc p) n -> p kc n", p=P)
        for nb in range(6):
            W = wb[nb % 2]
            self.load_w_bf(w_in, wv[:, :, KV0 + nb * 512:KV0 + (nb + 1) * 512], stg, W)
            for tt in range(2):
                pr = self.nextps()
                self.mm(pr, pr.t[:, :], [(hcT.t[:, kc, tt * P:(tt + 1) * P], W.t[:, kc, :]) for kc in range(32)], [hcT, W])
                if nb < 2 and cstop <= 2:
                    self.copy("act", kct, kct.t[:, tt, nb * 512:(nb + 1) * 512], pr, pr.t[:, :])
                elif nb < 2:
                    self.rotary(pr, rt, rt.t[:, tt], kct, kct.t[:, tt, nb * 512:(nb + 1) * 512], tm)
                else:
                    self.copy("act", vct, vct.t[:, tt, (nb - 2) * 512:(nb - 1) * 512], pr, pr.t[:, :])
        if cstop <= 3:
            C.pop(); return
        kd = [C.sb("kdC%d" % i, [P, P], BF16) for i in range(4)]
        ki = 0
        for d in range(2):
            for h in range(8):
                pr = self.nextps()
                pairs = []
                kds = []
                for tt in range(2):
                    k_ = kd[ki % 4]; ki += 1
                    C.op("dve", lambda e, k_=k_, tt=tt, h=h, d=d: e.tensor_scalar(out=k_.t[:], in0=kct.t[:, tt, h * P:(h + 1) * P], scalar1=wctx.t[:, tt, d * 8 + h:d * 8 + h + 1], scalar2=None, op0=ALU.mult), reads=[kct, wctx], writes=[k_])
                    pairs.append((k_.t[:], vct.t[:, tt, h * 256:(h + 1) * 256])); kds.append(k_)
                if cstop <= 4:
                    continue
                self.mm(pr, pr.t[:, 0:256], pairs, [vct, *kds])
                if cstop <= 5:
                    continue
                C.op("dve", lambda e, pr=pr, d=d, h=h: e.tensor_copy(Sf.t[:, d * 8 + h, :], pr.t[:, 0:256]), reads=[pr], writes=[Sf])
                if cstop <= 6:
                    continue
                C.op("pool", lambda e, d=d, h=h: e.tensor_copy(Sb.t[:, d * 8 + h, :], Sf.t[:, d * 8 + h, :]), reads=[Sf], writes=[Sb])
        C.pop()


    def phase_D(self, hxT, w_in, swT, rot_x, rot_xk, v_tm, x1_tm, x2_cm, sg, qT_d, kT_d, k_tm, vr_tm):
        C = self.C; M = self.M
        C.push()
        stg = [C.sb("stgD%d" % i, [P, 2, 512], F32) for i in range(3)]
        wb = [C.sb("wbD%d" % i, [P, 32, 512], BF16) for i in range(2)]
        hb = [C.sb("hbD%d" % i, [P, 32, 512], BF16) for i in range(2)]
        prow = [C.sb("prow%d" % i, [P, 4098], BF16) for i in range(4)]
        swt = C.sb("swt", [P, 48, 4], F32)
        self.load("sp", swt, swt.t[:].rearrange("p a b -> p (a b)"), swT, swT.t.ap())
        ctmp = C.sb("ctmp", [P, 1024], F32)
        uo = [C.sb("uo%d" % i, [P, 1024], BF16) for i in range(2)]
        utm = [C.sb("utm%d" % i, [P, 8, P], BF16) for i in range(2)]
        rt = [C.sb("rtD%d" % i, [P, 2, 256], F32) for i in range(2)]
        tm = [C.sb("tmD%d" % i, [P, 256], F32) for i in range(2)]
        qk = [C.sb("qk%d" % i, [P, 512], BF16) for i in range(2)]
        qkT = [C.sb("qkT%d" % i, [P, 4, P], BF16) for i in range(2)]
        ot = [C.sb("otD%d" % i, [P, 512], BF16) for i in range(2)]
        for pw in prow:
            C.op("dve", lambda e, pw=pw: e.memset(pw.t[:, 0:1], 0.0), writes=[pw])
            C.op("dve", lambda e, pw=pw: e.memset(pw.t[:, 4097:4098], 0.0), writes=[pw])
        wv = w_in.t.ap().rearrange("(kc p) n -> p kc n", p=P)
        hv = hxT.t.ap().rearrange("(kc p) t -> p kc t", p=P)
        groups = []
        for g in range(28):
            col0 = g * 512
            if g < 12: kind = "hy"
            elif g < 14: kind = "q"
            elif g < 22: kind = "gate"
            elif g < 24: kind = "k"
            else: kind = "v"
            groups.append((col0, kind))
        cnt = dict(stg=0, hb=0, uo=0, utm=0, rt=0, qk=0, qkT=0, ot=0, ev=0)

        def nxt(lst, key):
            r = lst[cnt[key] % len(lst)]; cnt[key] += 1; return r
        def wpieces(g, lo, hi):
            if g >= len(groups):
                return
            c0 = groups[g][0]; Wn = wb[g % 2]
            for q in range(lo, hi):
                s_ = nxt(stg, "stg")
                self.load("sp", s_, s_.t[:], w_in, wv[:, q * 2:(q + 1) * 2, c0:c0 + 512])
                self.copy(self.cast_eng(), Wn, Wn.t[:, q * 2:(q + 1) * 2, :], s_, s_.t[:])

        def hjob(k):
            def job():
                Hn = hb[k % 2]
                self.load("sp", Hn, Hn.t[:], hxT, hv[:, :, (k % 8) * 512:(k % 8 + 1) * 512])
                return Hn
            return job
        hst = Stream([hjob(k) for k in range(28 * 8)])
        wpieces(0, 0, 16)
        for gi, (col0, kind) in enumerate(groups):
            W = wb[gi % 2]
            for tb in range(8):
                H = hst.next()
                wpieces(gi + 1, tb * 2, tb * 2 + 2)
                if kind in ("hy", "gate"):
                    for ct in range(4):
                        pr = self.nextps()
                        self.mm(pr, pr.t[:, :], [(W.t[:, kc, ct * P:(ct + 1) * P], H.t[:, kc, :]) for kc in range(32)], [W, H])
                        if kind == "hy":
                            pw = prow[ct]
                            cnt["ev"] += 1
                            self.copy("act" if cnt["ev"] % 2 else "dve", pw, pw.t[:, 1 + tb * 512:1 + (tb + 1) * 512], pr, pr.t[:, :])
                        else:
                            O = nxt(ot, "ot")
                            C.op("act", lambda e, O=O, pr=pr: e.activation(out=O.t[:], in_=pr.t[:, :], func=AF.Silu), reads=[pr], writes=[O])
                            r0 = (col0 - 7168) + ct * P
                            self.store("sp", sg, sg.t.ap()[r0:r0 + P, tb * 512:(tb + 1) * 512], O, O.t[:])
                else:
                    for tt in range(4):
                        pr = self.nextps()
                        self.mm(pr, pr.t[:, :], [(H.t[:, kc, tt * P:(tt + 1) * P], W.t[:, kc, :]) for kc in range(32)], [W, H])
                        tok0 = tb * 512 + tt * P
                        if kind in ("q", "k"):
                            R = nxt(rt, "rt")
                            tab = rot_x if kind == "q" else rot_xk
                            self.load("sp", R, R.t[:].rearrange("p a b -> p (a b)"), tab, tab.t.ap()[tok0:tok0 + P, :])
                            Q = nxt(qk, "qk")
                            self.rotary(pr, R, R.t[:], Q, Q.t[:, :], tm)
                            if kind == "k":
                                c0 = col0 - 11264
                                self.store("sp", k_tm, k_tm.t.ap()[tok0:tok0 + P, c0:c0 + 512], Q, Q.t[:])
                                dstT = kT_d; hh0 = c0 // P
                            else:
                                dstT = qT_d; hh0 = (col0 - 6144) // P
                            pt = self.nextps(); pv = self.psb(pt)

                            def fnT(e, pv=pv, Q=Q):
                                inst = None
                                for h in range(4):
                                    inst = e.transpose(pv[:, h * P:(h + 1) * P], Q.t[:, h * P:(h + 1) * P], self.identb.t[:])
                                return inst
                            C.op("pe", fnT, reads=[Q, self.identb], writes=[pt])
                            QT = nxt(qkT, "qkT")
                            self.copy("act", QT, QT.t[:].rearrange("p a b -> p (a b)"), pt, pv[:, 0:512])
                            self.store("sp", dstT, dstT.t.ap().rearrange("(h p) t -> p h t", p=P)[:, hh0:hh0 + 4, tok0:tok0 + P], QT, QT.t[:])
                        else:
                            O = nxt(ot, "ot")
                            cnt["ev"] += 1
                            self.copy("act" if cnt["ev"] % 2 else "dve", O, O.t[:], pr, pr.t[:, :])
                            c0 = col0 - 12288
                            self.store("sp", vr_tm, vr_tm.t.ap()[tok0:tok0 + P, c0:c0 + 512], O, O.t[:])
            if kind == "hy":
                for ct in range(4):
                    colt = col0 // P + ct
                    pw = prow[ct]
                    for ch in range(4):
                        s0 = ch * 1024
                        C.op("dve", lambda e, pw=pw, s0=s0, colt=colt: e.tensor_scalar(out=ctmp.t[:], in0=pw.t[:, s0:s0 + 1024], scalar1=swt.t[:, colt, 0:1], scalar2=swt.t[:, colt, 3:4], op0=ALU.mult, op1=ALU.add), reads=[pw, swt], writes=[ctmp])
                        C.op("dve", lambda e, pw=pw, s0=s0, colt=colt: e.scalar_tensor_tensor(out=ctmp.t[:], in0=pw.t[:, s0 + 1:s0 + 1025], scalar=swt.t[:, colt, 1:2], in1=ctmp.t[:], op0=ALU.mult, op1=ALU.add), reads=[pw, swt, ctmp], writes=[ctmp])
                        U = nxt(uo, "uo")
                        C.op("dve", lambda e, pw=pw, s0=s0, colt=colt, U=U: e.scalar_tensor_tensor(out=U.t[:], in0=pw.t[:, s0 + 2:s0 + 1026], scalar=swt.t[:, colt, 2:3], in1=ctmp.t[:], op0=ALU.mult, op1=ALU.add), reads=[pw, swt, ctmp], writes=[U])
                        if col0 >= 4096:
                            r0 = (col0 - 4096) + ct * P
                            self.store("sp", x2_cm, x2_cm.t.ap()[r0:r0 + P, s0:s0 + 1024], U, U.t[:])
                        else:
                            dst = v_tm if col0 < 2048 else x1_tm
                            cc0 = (col0 % 2048) + ct * P
                            pt = self.nextps(); pv = self.psb(pt)

                            def fnU(e, pv=pv, U=U):
                                inst = None
                                for j in range(8):
                                    inst = e.transpose(pv[:, j * P:(j + 1) * P], U.t[:, j * P:(j + 1) * P], self.identb.t[:])
                                return inst
                            C.op("pe", fnU, reads=[U, self.identb], writes=[pt])
                            UT = nxt(utm, "utm")
                            cnt["ev"] += 1
                            self.copy("act" if cnt["ev"] % 2 else "pool" if False else "act", UT, UT.t[:].rearrange("p a b -> p (a b)"), pt, pv[:, 0:1024])
                            self.store("sp", dst, dst.t.ap().rearrange("(tt p) c -> p tt c", p=P)[:, ch * 8:(ch + 1) * 8, cc0:cc0 + P], UT, UT.t[:])
        C.pop()


    def slab_stream(self, cst, seq, slabs):
        def mk(k, mt):
            def job():
                SL = slabs[k % len(slabs)]
                self.load("sp", SL, SL.t[:].rearrange("p a b c -> p (a b c)"), cst, cst.t.ap()[mt])
                return SL
            return job
        return Stream([mk(k, mt) for k, mt in enumerate(seq)])

    def phase_E(self, fw1, fw2, fw3, fw4, fvec, zT, negt, deltab, hyb, cst, fscale, spec):
        C = self.C; M = self.M
        PI = math.pi
        C.push()
        h3 = C.sb("h3", [64, T], F32)
        fv = C.sb("fv", [64, 4], F32)
        self.load("sp", fv, fv.t[:], fvec, fvec.t.ap())
        arg = C.sb("argE", [64, 512], F32); mk = C.sb("mkE", [64, 512], F32)
        C.push()
        zt = C.sb("zt", [33, T], F32); hA = C.sb("hA", [64, T], F32); hB = C.sb("hB", [64, T], F32)
        w1 = C.sb("w1", [33, 64], F32); w2 = C.sb("w2", [64, 64], F32); w3 = C.sb("w3", [64, 64], F32)
        self.load("sp", zt, zt.t[:], zT, zT.t.ap())
        self.load("sp", w1, w1.t[:], fw1, fw1.t.ap()); self.load("sp", w2, w2.t[:], fw2, fw2.t.ap()); self.load("sp", w3, w3.t[:], fw3, fw3.t.ap())
        layers = [(zt, 33, w1, hA), (hA, 64, w2, hB), (hB, 64, w3, h3)]
        for li, (src, kk, w, dst) in enumerate(layers):
            for nb in range(8):
                pr = self.nextps()
                C.op("pe", lambda e, pr=pr, w=w, src=src, kk=kk, nb=nb: e.matmul(pr.t[0:64, :], w.t[0:kk, :], src.t[0:kk, nb * 512:(nb + 1) * 512], start=True, stop=True), reads=[w, src], writes=[pr])
                C.op("dve", lambda e, pr=pr, li=li: e.tensor_scalar(out=arg.t[:], in0=pr.t[0:64, :], scalar1=fv.t[:, li:li + 1], scalar2=fv.t[:, 3:4], op0=ALU.add, op1=ALU.mult), reads=[pr, fv], writes=[arg])
                C.op("dve", lambda e: e.tensor_scalar(out=mk.t[:], in0=arg.t[:], scalar1=PI, scalar2=-2.0 * PI, op0=ALU.is_gt, op1=ALU.mult), reads=[arg], writes=[mk])
                C.op("dve", lambda e: e.tensor_tensor(out=arg.t[:], in0=arg.t[:], in1=mk.t[:], op=ALU.add), reads=[arg, mk], writes=[arg])
                C.op("dve", lambda e: e.tensor_scalar(out=mk.t[:], in0=arg.t[:], scalar1=-PI, scalar2=2.0 * PI, op0=ALU.is_lt, op1=ALU.mult), reads=[arg], writes=[mk])
                C.op("dve", lambda e: e.tensor_tensor(out=arg.t[:], in0=arg.t[:], in1=mk.t[:], op=ALU.add), reads=[arg, mk], writes=[arg])
                C.op("act", lambda e, dst=dst, nb=nb: e.activation(out=dst.t[:, nb * 512:(nb + 1) * 512], in_=arg.t[:], func=AF.Sin), reads=[arg], writes=[dst])
        C.pop()
        w4 = C.sb("w4", [64, 2, 512], F32)
        ngt = C.sb("ngt", [P, NT], F32); delt = C.sb("delt", [P, HYW], F32); hybt = C.sb("hybt", [P, 512], F32); fsc = C.sb("fsc", [P, NFT], F32)
        self.load("sp", ngt, ngt.t[:], negt, negt.t.ap()); self.load("sp", delt, delt.t[:], deltab, deltab.t.ap())
        self.load("sp", fsc, fsc.t[:], fscale, fscale.t.ap())
        edt = C.sb("edt", [P, 32, 2, 512], BF16)
        slabs = [C.sb("slabE%d" % i, [P, 2, NFT, P], BF16) for i in range(2)]
        sst = self.slab_stream(cst, [mt for _ in range(8) for mt in range(NFT)], slabs)
        dect = C.sb("dect", [P, 512], F32)
        tf = [C.sb("tfE%d" % i, [P, 512], F32) for i in range(2)]; tb = [C.sb("tbE%d" % i, [P, 512], F32) for i in range(2)]
        af = C.sb("afE", [P, 512], F32); ab = [C.sb("abE%d" % i, [P, 512], F32) for i in range(2)]
        rn = C.sb("rnE", [P, 512], F32)
        stt = [C.sb("stE%d" % i, [P, 2, 512], F32) for i in range(2)]
        pacc = self.pacc
        specv = spec.t.ap().rearrange("k (two c) -> k two c", two=2)
        si = 0
        for o in range(2):
            for cb in range(4):
                colF = o * 2048 + cb * 512; colB = 4096 + colF
                self.load("sp", hybt, hybt.t[:], hyb, hyb.t.ap()[:, colF:colF + 512])
                self.load("sp", w4, w4.t[:, 0, :], fw4, fw4.t.ap()[:, colF:colF + 512])
                self.load("sp", w4, w4.t[:, 1, :], fw4, fw4.t.ap()[:, colB:colB + 512])
                for lt in range(NT):
                    TF = tf[lt % 2]; TB = tb[lt % 2]; AB_ = ab[lt % 2]
                    prF = self.nextps(); prB = self.nextps()
                    C.op("pe", lambda e, prF=prF, lt=lt, colF=colF: e.matmul(prF.t[:, :], h3.t[:, lt * P:(lt + 1) * P], w4.t[:, 0, :], start=True, stop=True), reads=[h3, w4], writes=[prF])
                    C.op("pe", lambda e, prB=prB, lt=lt, colB=colB: e.matmul(prB.t[:, :], h3.t[:, lt * P:(lt + 1) * P], w4.t[:, 1, :], start=True, stop=True), reads=[h3, w4], writes=[prB])
                    C.op("act", lambda e, lt=lt, cb=cb: e.activation(out=dect.t[:], in_=delt.t[:, cb * 512:(cb + 1) * 512], func=AF.Exp, scale=ngt.t[:, lt:lt + 1]), reads=[delt, ngt], writes=[dect])
                    C.op("dve", lambda e, TF=TF, prF=prF: e.tensor_tensor(out=TF.t[:], in0=prF.t[:, :], in1=dect.t[:], op=ALU.mult), reads=[prF, dect], writes=[TF])
                    C.op("dve", lambda e, TB=TB, prB=prB: e.tensor_tensor(out=TB.t[:], in0=prB.t[:, :], in1=dect.t[:], op=ALU.mult), reads=[prB, dect], writes=[TB])
                    if lt == 0:
                        C.op("dve", lambda e, TB=TB: e.memset(TB.t[0:1, :], 0.0), reads=[TB], writes=[TB])
                    C.op("pool", lambda e, TF=TF, TB=TB, lt=lt: e.tensor_tensor(out=edt.t[:, lt, 0, :], in0=TF.t[:], in1=TB.t[:], op=ALU.add), reads=[TF, TB], writes=[edt])
                    C.op("pool", lambda e, TF=TF, TB=TB, lt=lt: e.tensor_tensor(out=edt.t[:, lt, 1, :], in0=TF.t[:], in1=TB.t[:], op=ALU.subtract), reads=[TF, TB], writes=[edt])
                    C.op("act", lambda e, TF=TF: e.activation(out=af.t[:], in_=TF.t[:], func=AF.Abs), reads=[TF], writes=[af])
                    C.op("act", lambda e, TB=TB, AB_=AB_: e.activation(out=AB_.t[:], in_=TB.t[:], func=AF.Abs), reads=[TB], writes=[AB_])
                    C.op("pool", lambda e, AB_=AB_: e.tensor_tensor(out=AB_.t[:], in0=AB_.t[:], in1=af.t[:], op=ALU.add), reads=[AB_, af], writes=[AB_])
                    C.op("pe", lambda e, AB_=AB_, lt=lt: e.matmul(pacc.t[:, :], M("ones"), AB_.t[:], start=(lt == 0), stop=(lt == NT - 1)), reads=[AB_, self.mt], writes=[pacc])
                C.op("dve", lambda e: e.reciprocal(rn.t[:], pacc.t[:, :]), reads=[pacc], writes=[rn])
                for mt in range(NFT):
                    SL = sst.next()
                    prR = self.nextps(); prW = self.nextps()
                    self.mm(prR, prR.t[:, :], [(SL.t[:, 0, kt, :], edt.t[:, kt, 0, :]) for kt in range(NT)], [SL, edt])
                    self.mm(prW, prW.t[:, :], [(SL.t[:, 1, kt, :], edt.t[:, kt, 1, :]) for kt in range(NT)], [SL, edt])
                    ST = stt[si % 2]; si += 1
                    C.op("dve", lambda e, ST=ST, prR=prR: e.tensor_tensor(out=ST.t[:, 0, :], in0=prR.t[:, :], in1=rn.t[:], op=ALU.mult), reads=[prR, rn], writes=[ST])
                    C.op("dve", lambda e, ST=ST, colF=colF: e.tensor_tensor(out=ST.t[:, 0, :], in0=ST.t[:, 0, :], in1=hybt.t[:], op=ALU.add), reads=[ST, hybt], writes=[ST])
                    C.op("dve", lambda e, ST=ST, prW=prW: e.tensor_tensor(out=ST.t[:, 1, :], in0=prW.t[:, :], in1=rn.t[:], op=ALU.mult), reads=[prW, rn], writes=[ST])
                    C.op("pool", lambda e, ST=ST, mt=mt: e.tensor_scalar(out=ST.t[:].rearrange("p a b -> p (a b)"), in0=ST.t[:].rearrange("p a b -> p (a b)"), scalar1=fsc.t[:, mt:mt + 1], scalar2=None, op0=ALU.mult), reads=[ST, fsc], writes=[ST])
                    self.store("sp", spec, specv[mt * P:(mt + 1) * P, :, colF:colF + 512], ST, ST.t[:])
        C.pop()

    def phase_F(self, v_tm, x1_tm, x2_cm, spec, cst, yT):
        C = self.C; M = self.M
        C.push()
        utm = C.sb("utmF", [P, NT, 512], BF16); x1t = C.sb("x1tF", [P, NT, 512], BF16)
        Y = C.sb("YF", [P, NFT, 2, 512], BF16)
        slabs = [C.sb("slabF%d" % i, [P, 2, NFT, P], BF16) for i in range(2)]
        spt = [C.sb("sptF%d" % i, [P, 2, 512], F32) for i in range(2)]
        sst = self.slab_stream(cst, [m_ for _ in range(8) for m_ in (list(range(NFT)) + list(range(NT)))], slabs)

        def spjob(k):
            cb_, rem = divmod(k, 2 * NFT); o_, mt_ = divmod(rem, NFT)
            c0_ = o_ * 2048 + cb_ * 512

            def job():
                SPn = spt[k % 2]
                self.load("sp", SPn, SPn.t[:], spec, specv[mt_ * P:(mt_ + 1) * P, :, c0_:c0_ + 512])
                return SPn
            return job
        t4 = [C.sb("t4F%d" % i, [P, 512], F32) for i in range(4)]
        yb = [C.sb("ybF%d" % i, [P, 512], BF16) for i in range(2)]
        x2t = [C.sb("x2tF%d" % i, [P, 4, P], BF16) for i in range(2)]
        yo = [C.sb("yoF%d" % i, [P, 4, P], BF16) for i in range(2)]
        specv = spec.t.ap().rearrange("k (two c) -> k two c", two=2)
        cnt = dict(sp=0, yb=0, x2=0, yo=0)
        spst = Stream([spjob(k) for k in range(4 * 2 * NFT)])

        def conv(o, cb, cbfn):
            c0 = o * 2048 + cb * 512
            for mt in range(NFT):
                SL = sst.next()
                prR = self.nextps(); prI = self.nextps()
                self.mm(prR, prR.t[:, :], [(SL.t[:, 0, kt, :], utm.t[:, kt, :]) for kt in range(NT)], [SL, utm])
                self.mm(prI, prI.t[:, :], [(SL.t[:, 1, kt, :], utm.t[:, kt, :]) for kt in range(NT)], [SL, utm])
                SP = spst.next()
                a, b_, c_, d_ = t4
                C.op("dve", lambda e, prR=prR, SP=SP: e.tensor_tensor(out=a.t[:], in0=prR.t[:, :], in1=SP.t[:, 0, :], op=ALU.mult), reads=[prR, SP], writes=[a])
                C.op("dve", lambda e, prI=prI, SP=SP: e.tensor_tensor(out=b_.t[:], in0=prI.t[:, :], in1=SP.t[:, 1, :], op=ALU.mult), reads=[prI, SP], writes=[b_])
                C.op("pool", lambda e, mt=mt: e.tensor_tensor(out=Y.t[:, mt, 0, :], in0=a.t[:], in1=b_.t[:], op=ALU.subtract), reads=[a, b_], writes=[Y])
                C.op("dve", lambda e, prR=prR, SP=SP: e.tensor_tensor(out=c_.t[:], in0=prR.t[:, :], in1=SP.t[:, 1, :], op=ALU.mult), reads=[prR, SP], writes=[c_])
                C.op("dve", lambda e, prI=prI, SP=SP: e.tensor_tensor(out=d_.t[:], in0=prI.t[:, :], in1=SP.t[:, 0, :], op=ALU.mult), reads=[prI, SP], writes=[d_])
                C.op("pool", lambda e, mt=mt: e.tensor_tensor(out=Y.t[:, mt, 1, :], in0=c_.t[:], in1=d_.t[:], op=ALU.add), reads=[c_, d_], writes=[Y])
            for it in range(NT):
                SL = sst.next()
                pr = self.nextps()
                self.mm(pr, pr.t[:, :], [(SL.t[:, 0, kt, :], Y.t[:, kt, 0, :]) for kt in range(NFT)] + [(SL.t[:, 1, kt, :], Y.t[:, kt, 1, :]) for kt in range(NFT)], [SL, Y])
                cbfn(it, pr)

        for cb in range(4):
            self.load("sp", utm, utm.t[:], v_tm, v_tm.t.ap().rearrange("(tt p) c -> p tt c", p=P)[:, :, cb * 512:(cb + 1) * 512])
            self.load("act", x1t, x1t.t[:], x1_tm, x1_tm.t.ap().rearrange("(tt p) c -> p tt c", p=P)[:, :, cb * 512:(cb + 1) * 512])

            def f1(it, pr):
                C.op("dve", lambda e: e.tensor_tensor(out=utm.t[:, it, :], in0=pr.t[:, :], in1=x1t.t[:, it, :], op=ALU.mult), reads=[pr, x1t], writes=[utm])

            def f2(it, pr, cb=cb):
                YB = yb[cnt["yb"] % 2]; cnt["yb"] += 1
                self.copy("act", YB, YB.t[:], pr, pr.t[:, :])
                pt = self.nextps(); pv = self.psb(pt)

                def fnT(e):
                    inst = None
                    for j in range(4):
                        inst = e.transpose(pv[:, j * P:(j + 1) * P], YB.t[:, j * P:(j + 1) * P], self.identb.t[:])
                    return inst
                C.op("pe", fnT, reads=[YB, self.identb], writes=[pt])
                X2 = x2t[cnt["x2"] % 2]; cnt["x2"] += 1
                self.load("sp", X2, X2.t[:], x2_cm, x2_cm.t.ap().rearrange("(j p) t -> p j t", p=P)[:, cb * 4:(cb + 1) * 4, it * P:(it + 1) * P])
                YO = yo[cnt["yo"] % 2]; cnt["yo"] += 1
                C.op("dve", lambda e: e.tensor_tensor(out=YO.t[:].rearrange("p a b -> p (a b)"), in0=pv[:, 0:512], in1=X2.t[:].rearrange("p a b -> p (a b)"), op=ALU.mult), reads=[pt, X2], writes=[YO])
                self.store("sp", yT, yT.t.ap().rearrange("(j p) t -> p j t", p=P)[:, cb * 4:(cb + 1) * 4, it * P:(it + 1) * P], YO, YO.t[:])
            conv(0, cb, f1)
            conv(1, cb, f2)
        C.pop()


    def phase_G(self, qT_d, kT_d, k_tm, vr_tm, sg, Sfd, lg, yT):
        C = self.C; M = self.M
        C.push()
        Sf = [C.sb("SfG%d" % i, [P, 256], F32) for i in range(16)]
        Sb = [C.sb("SbG%d" % i, [P, 256], BF16) for i in range(16)]
        for i in range(16):
            self.load("sp" if i % 2 else "act", Sf[i], Sf[i].t[:], Sfd, Sfd.t.ap()[:, i * 256:(i + 1) * 256])
            C.op("pool", lambda e, i=i: e.tensor_copy(Sb[i].t[:], Sf[i].t[:]), reads=[Sf[i]], writes=[Sb[i]])
        decT = C.sb("decT", [P, 16, P], F32); qdec = C.sb("qdec", [P, 16, P], F32)
        kdec = C.sb("kdec", [P, 16], F32); cdec = C.sb("cdec", [P, 16], F32)
        for d in range(2):
            for h in range(8):
                dh = d * 8 + h
                C.op("act", lambda e, d=d, dh=dh: e.activation(out=decT.t[:, dh, :], in_=M("diffF" if d == 0 else "diffB"), func=AF.Exp, scale=lg.t[:, dh:dh + 1]), reads=[lg, self.mt], writes=[decT])
                C.op("dve", lambda e, d=d, dh=dh: e.tensor_tensor(out=decT.t[:, dh, :], in0=decT.t[:, dh, :], in1=M("maskF" if d == 0 else "maskB"), op=ALU.mult), reads=[decT, self.mt], writes=[decT])
                C.op("act", lambda e, d=d, dh=dh: e.activation(out=qdec.t[:, dh, :], in_=M("ip1" if d == 0 else "i128m"), func=AF.Exp, scale=lg.t[:, dh:dh + 1]), reads=[lg, self.mt], writes=[qdec])
            C.op("act", lambda e, d=d: e.activation(out=kdec.t[:, d * 8:(d + 1) * 8], in_=lg.t[:, d * 8:(d + 1) * 8], func=AF.Exp, scale=M("cols")[:, d:d + 1]), reads=[lg, self.mt], writes=[kdec])
        C.op("act", lambda e: e.activation(out=cdec.t[:], in_=lg.t[:], func=AF.Exp, scale=128.0), reads=[lg], writes=[cdec])
        nb_ = 2
        qTc = [[C.sb("qTc%d%d" % (d, i), [P, 8, P], BF16) for i in range(nb_)] for d in range(2)]
        kTc = [[C.sb("kTc%d%d" % (d, i), [P, 8, P], BF16) for i in range(nb_)] for d in range(2)]
        ktm = [[C.sb("ktm%d%d" % (d, i), [P, KC], BF16) for i in range(nb_)] for d in range(2)]
        vtm = [[C.sb("vtm%d%d" % (d, i), [P, VC], BF16) for i in range(nb_)] for d in range(2)]
        sgc = [[C.sb("sgc%d%d" % (d, i), [P, 16, P], BF16) for i in range(nb_)] for d in range(2)]
        ytl = [[C.sb("ytl%d%d" % (d, i), [P, 16, P], BF16) for i in range(nb_)] for d in range(2)]
        attm = [C.sb("attm%d" % i, [P, P], BF16) for i in range(3)]
        qd = [C.sb("qd%d" % i, [P, P], BF16) for i in range(3)]
        kd = [C.sb("kdG%d" % i, [P, P], BF16) for i in range(3)]
        sq = [C.sb("sq%d" % i, [P, 256], BF16) for i in range(2)]
        sd = [C.sb("sd%d" % i, [P, P], F32) for i in range(2)]
        rs = [C.sb("rs%d" % i, [P, P], F32) for i in range(2)]
        on = [C.sb("on%d" % i, [P, 256], F32) for i in range(2)]
        qv = qT_d.t.ap().rearrange("(h p) t -> p h t", p=P); kv = kT_d.t.ap().rearrange("(h p) t -> p h t", p=P)
        sgv = sg.t.ap().rearrange("(ha p) t -> p ha t", p=P); yv = yT.t.ap().rearrange("(ha p) t -> p ha t", p=P)
        n = 0
        for s_ in range(NT):
            for d in range(2):
                c = s_ if d == 0 else NT - 1 - s_
                bi = s_ % nb_
                Q = qTc[d][bi]; Kt = kTc[d][bi]; KM_ = ktm[d][bi]; V = vtm[d][bi]; G = sgc[d][bi]; YT = ytl[d][bi]
                tk = slice(c * P, (c + 1) * P)
                self.load("sp", Q, Q.t[:], qT_d, qv[:, :, tk]); self.load("act", Kt, Kt.t[:], kT_d, kv[:, :, tk])
                self.load("sp", KM_, KM_.t[:], k_tm, k_tm.t.ap()[tk, :]); self.load("act", V, V.t[:], vr_tm, vr_tm.t.ap()[tk, :])
                self.load("sp", G, G.t[:], sg, sgv[:, d * 16:(d + 1) * 16, tk])
                for h in range(8):
                    dh = d * 8 + h; n += 1
                    AT = attm[n % 3]; QD = qd[n % 3]; KD = kd[n % 3]; SQ = sq[n % 2]; SD = sd[n % 2]; RS = rs[n % 2]; ON = on[n % 2]
                    pa = self.nextps()
                    self.mm(pa, pa.t[:, 0:P], [(Kt.t[:, h, :], Q.t[:, h, :])], [Kt, Q])
                    C.op("dve", lambda e, AT=AT, pa=pa, dh=dh: e.tensor_tensor(out=AT.t[:], in0=pa.t[:, 0:P], in1=decT.t[:, dh, :], op=ALU.mult), reads=[pa, decT], writes=[AT])
                    C.op("pool", lambda e, QD=QD, Q=Q, h=h, dh=dh: e.tensor_tensor(out=QD.t[:], in0=Q.t[:, h, :], in1=qdec.t[:, dh, :], op=ALU.mult), reads=[Q, qdec], writes=[QD])
                    po = self.nextps()

                    def fo(e, po=po, V=V, AT=AT, QD=QD, h=h, dh=dh):
                        inst = None
                        for a in range(2):
                            e.matmul(po.t[:, a * P:(a + 1) * P], V.t[:, h * 256 + a * P:h * 256 + (a + 1) * P], AT.t[:], start=True, stop=False)
                            inst = e.matmul(po.t[:, a * P:(a + 1) * P], Sb[dh].t[:, a * P:(a + 1) * P], QD.t[:], start=False, stop=True)
                        return inst
                    C.op("pe", fo, reads=[V, AT, QD, Sb[dh]], writes=[po])
                    C.op("act", lambda e, SQ=SQ, po=po: e.activation(out=SQ.t[:], in_=po.t[:, 0:256], func=AF.Square), reads=[po], writes=[SQ])
                    pss = self.nextps()
                    self.mm(pss, pss.t[:, 0:P], [(self.onesb.t[:], SQ.t[:, 0:P]), (self.onesb.t[:], SQ.t[:, P:256])], [self.onesb, SQ])
                    C.op("act", lambda e, SD=SD, pss=pss: e.activation(out=SD.t[:], in_=pss.t[:, 0:P], func=AF.Sqrt, scale=1.0 / 256.0, bias=M("cols")[:, 6:7]), reads=[pss, self.mt], writes=[SD])
                    C.op("dve", lambda e, RS=RS, SD=SD: e.reciprocal(RS.t[:], SD.t[:]), reads=[SD], writes=[RS])
                    for a in range(2):
                        C.op("dve", lambda e, ON=ON, po=po, RS=RS, a=a: e.tensor_tensor(out=ON.t[:, a * P:(a + 1) * P], in0=po.t[:, a * P:(a + 1) * P], in1=RS.t[:], op=ALU.mult), reads=[po, RS], writes=[ON])
                    C.op("pool", lambda e, YT=YT, ON=ON, G=G, h=h: e.tensor_tensor(out=YT.t[:, h * 2:h * 2 + 2, :], in0=ON.t[:].rearrange("p (a b) -> p a b", a=2), in1=G.t[:, h * 2:h * 2 + 2, :], op=ALU.mult), reads=[ON, G], writes=[YT])
                    C.op("pool", lambda e, KD=KD, KM_=KM_, h=h, dh=dh: e.tensor_scalar(out=KD.t[:], in0=KM_.t[:, h * P:(h + 1) * P], scalar1=kdec.t[:, dh:dh + 1], scalar2=None, op0=ALU.mult), reads=[KM_, kdec], writes=[KD])
                    psn = self.nextps()
                    self.mm(psn, psn.t[:, 0:256], [(KD.t[:], V.t[:, h * 256:(h + 1) * 256])], [KD, V])
                    C.op("dve", lambda e, psn=psn, dh=dh: e.scalar_tensor_tensor(out=Sf[dh].t[:], in0=Sf[dh].t[:], scalar=cdec.t[:, dh:dh + 1], in1=psn.t[:, 0:256], op0=ALU.mult, op1=ALU.add), reads=[Sf[dh], cdec, psn], writes=[Sf[dh]])
                    C.op("pool", lambda e, dh=dh: e.tensor_copy(Sb[dh].t[:], Sf[dh].t[:]), reads=[Sf[dh]], writes=[Sb[dh]])
                self.store("sp", yT, yv[:, 16 + d * 16:16 + (d + 1) * 16, tk], YT, YT.t[:])
        C.pop()


    def phase_H(self, yT, w_out, x, bcd, x1r, hx2, rwT, aff_tm):
        C = self.C; M = self.M
        yv = yT.t.ap().rearrange("(kc p) t -> p kc t", p=P)
        C.push()
        ya = [C.sb("yaH%d" % i, [P, 16, 512], BF16) for i in range(2)]
        yb_ = [C.sb("ybH%d" % i, [P, 16, 512], BF16) for i in range(2)]
        for tb in range(8):
            A_ = ya[tb % 2]; B_ = yb_[tb % 2]; ts_ = slice(tb * 512, (tb + 1) * 512)
            self.load("sp", A_, A_.t[:], yT, yv[:, 16:32, ts_]); self.load("sp", B_, B_.t[:], yT, yv[:, 32:48, ts_])
            C.op("dve", lambda e, A_=A_, B_=B_: e.tensor_tensor(out=A_.t[:].rearrange("p a b -> p (a b)"), in0=A_.t[:].rearrange("p a b -> p (a b)"), in1=B_.t[:].rearrange("p a b -> p (a b)"), op=ALU.add), reads=[A_, B_], writes=[A_])
            self.store("sp", yT, yv[:, 16:32, ts_], A_, A_.t[:])
        C.pop()
        C.push()
        stg = [C.sb("stgH%d" % i, [P, 8, 512], F32) for i in range(2)]
        wb = [C.sb("wbH%d" % i, [P, 32, 512], BF16) for i in range(2)]
        g1s = [C.sb("g1s%d" % i, [P, 512], F32) for i in range(2)]
        yh = [C.sb("yh%d" % i, [P, 32, 512], BF16) for i in range(2)]
        xs_ = [C.sb("xsH%d" % i, [P, 4, 512], F32) for i in range(2)]
        ob = [C.sb("obH%d" % i, [P, 4, 512], F32) for i in range(2)]
        wv = w_out.t.ap().rearrange("(kc p) n -> p kc n", p=P)
        xv = x.t.ap().rearrange("(tt p) c -> p tt c", p=P); ov = x1r.t.ap().rearrange("(tt p) c -> p tt c", p=P)
        sgi = [0]

        def wpieces(db, lo, hi):
            if db >= 8:
                return
            Wn = wb[db % 2]
            for q in range(lo, hi):
                s_ = stg[sgi[0] % 2]; sgi[0] += 1
                self.load("sp", s_, s_.t[:], w_out, wv[:, q * 8:(q + 1) * 8, db * 512:(db + 1) * 512])
                self.copy(self.cast_eng(), Wn, Wn.t[:, q * 8:(q + 1) * 8, :], s_, s_.t[:])
            if hi == 4:
                Gn = g1s[db % 2]
                self.load("sp", Gn, Gn.t[:], bcd, bcd.t.ap()[:, db * 512:(db + 1) * 512])

        def yjob(k):
            def job():
                db_, tb_ = divmod(k, 8)
                Yn = yh[k % 2]; Xn = xs_[k % 2]
                self.load("sp", Yn, Yn.t[:], yT, yv[:, 0:32, tb_ * 512:(tb_ + 1) * 512])
                self.load("sp", Xn, Xn.t[:], x, xv[:, tb_ * 4:(tb_ + 1) * 4, db_ * 512:(db_ + 1) * 512])
                return (Yn, Xn)
            return job
        yst = Stream([yjob(k) for k in range(64)])
        wpieces(0, 0, 4)
        n = 0
        for db in range(8):
            W = wb[db % 2]; G1 = g1s[db % 2]
            for tb in range(8):
                Y, X = yst.next()
                if tb % 2 == 0:
                    wpieces(db + 1, tb // 2, tb // 2 + 1)
                O = ob[n % 2]; n += 1
                for tt in range(4):
                    pr = self.nextps()
                    self.mm(pr, pr.t[:, :], [(Y.t[:, kc, tt * P:(tt + 1) * P], W.t[:, kc, :]) for kc in range(32)], [Y, W])
                    C.op("dve", lambda e, O=O, pr=pr, G1=G1, tt=tt: e.tensor_tensor(out=O.t[:, tt, :], in0=pr.t[:, :], in1=G1.t[:], op=ALU.mult), reads=[pr, G1], writes=[O])
                C.op("dve", lambda e, O=O, X=X: e.tensor_tensor(out=O.t[:].rearrange("p a b -> p (a b)"), in0=O.t[:].rearrange("p a b -> p (a b)"), in1=X.t[:].rearrange("p a b -> p (a b)"), op=ALU.add), reads=[O, X], writes=[O])
                self.store("sp", x1r, ov[:, tb * 4:(tb + 1) * 4, db * 512:(db + 1) * 512], O, O.t[:])
        C.pop()
        C.push()
        A2 = C.sb("A2b", [P, D], F32); B2 = C.sb("B2b", [P, D], F32)
        self.load("sp", A2, A2.t[:], bcd, bcd.t.ap()[:, 2 * D:3 * D]); self.load("act", B2, B2.t[:], bcd, bcd.t.ap()[:, 3 * D:4 * D])
        rw = C.sb("rw", [P, 32, NE], F32)
        self.load("sp", rw, rw.t[:].rearrange("p a b -> p (a b)"), rwT, rwT.t.ap())
        xt = [C.sb("xtH%d" % i, [P, D], F32) for i in range(2)]
        h2 = [C.sb("h2H%d" % i, [P, D], F32) for i in range(2)]
        hb16 = [C.sb("hb16%d" % i, [P, D], BF16) for i in range(2)]
        junk = C.sb("junkH", [P, D], BF16)
        sts = [C.sb("stH%d" % i, [P, 4], F32) for i in range(2)]
        h2T = [C.sb("h2T%d" % i, [P, 32, P], F32) for i in range(2)]
        sm = [C.sb("smH%d" % i, [P, 4], F32) for i in range(2)]
        ex = [C.sb("exH%d" % i, [P, NE], F32) for i in range(2)]
        for tt in range(NT):
            X = xt[tt % 2]; H2 = h2[tt % 2]; HB = hb16[tt % 2]; st = sts[tt % 2]; HT = h2T[tt % 2]; SM = sm[tt % 2]; EX = ex[tt % 2]
            tk = slice(tt * P, (tt + 1) * P)
            self.load("sp" if tt % 2 else "act", X, X.t[:], x1r, x1r.t.ap()[tk, :])
            self.rms_stats(X, junk, st, D)
            C.op("dve", lambda e, H2=H2, X=X, st=st: e.scalar_tensor_tensor(out=H2.t[:], in0=X.t[:], scalar=st.t[:, 1:2], in1=A2.t[:], op0=ALU.mult, op1=ALU.mult), reads=[X, st, A2], writes=[H2])
            C.op("pool", lambda e, H2=H2: e.tensor_tensor(out=H2.t[:], in0=H2.t[:], in1=B2.t[:], op=ALU.add), reads=[H2, B2], writes=[H2])
            C.op("act", lambda e, HB=HB, H2=H2: e.activation(out=HB.t[:], in_=H2.t[:], func=AF.Copy), reads=[H2], writes=[HB])
            self.store("sp", hx2, hx2.t.ap()[tk, :], HB, HB.t[:])
            for g in range(8):
                pt = self.nextps()

                def fnT(e, pt=pt, H2=H2, g=g):
                    inst = None
                    for j in range(4):
                        kc = g * 4 + j
                        inst = e.transpose(pt.t[:, j * P:(j + 1) * P], H2.t[:, kc * P:(kc + 1) * P], M("ident"))
                    return inst
                C.op("pe", fnT, reads=[H2, self.mt], writes=[pt])
                self.copy("act" if g % 2 else "dve", HT, HT.t[:, g * 4:(g + 1) * 4, :].rearrange("p a b -> p (a b)"), pt, pt.t[:, :])
            pl = self.nextps()
            self.mm(pl, pl.t[:, 0:NE], [(HT.t[:, kc, :], rw.t[:, kc, :]) for kc in range(32)], [HT, rw])
            C.op("dve", lambda e, SM=SM, pl=pl: e.tensor_reduce(out=SM.t[:, 0:1], in_=pl.t[:, 0:NE], axis=mybir.AxisListType.X, op=ALU.max), reads=[pl], writes=[SM])
            C.op("dve", lambda e, SM=SM: e.tensor_scalar(out=SM.t[:, 1:2], in0=SM.t[:, 0:1], scalar1=-1.0, scalar2=None, op0=ALU.mult), reads=[SM], writes=[SM])
            C.op("act", lambda e, EX=EX, pl=pl, SM=SM: e.activation(out=EX.t[:], in_=pl.t[:, 0:NE], func=AF.Exp, bias=SM.t[:, 1:2], accum_out=SM.t[:, 2:3]), reads=[pl, SM], writes=[EX, SM])
            C.op("dve", lambda e, SM=SM: e.reciprocal(SM.t[:, 3:4], SM.t[:, 2:3]), reads=[SM], writes=[SM])
            C.op("dve", lambda e, EX=EX, SM=SM, tt=tt: e.tensor_scalar(out=aff_tm.t[:, tt, :], in0=EX.t[:], scalar1=SM.t[:, 3:4], scalar2=None, op0=ALU.mult), reads=[EX, SM], writes=[aff_tm])
        C.pop()


    def phase_I(self, aff_tm, idx_all, gate_all):
        C = self.C; M = self.M
        C.push()
        affT = C.sb("affT", [NE, T], F32); junk = C.sb("junkI", [NE, T], F32)
        for g in range(8):
            pt = self.nextps()

            def fnT(e, pt=pt, g=g):
                inst = None
                for j in range(4):
                    tt = g * 4 + j
                    inst = e.transpose(pt.t[0:NE, j * P:(j + 1) * P], aff_tm.t[:, tt, :], M("ident"))
                return inst
            C.op("pe", fnT, reads=[aff_tm, self.mt], writes=[pt])
            self.copy("dve", affT, affT.t[:, g * 512:(g + 1) * 512], pt, pt.t[0:NE, :])
        bs = C.sb("bsI", [NE, 8], F32)
        C.op("dve", lambda e: e.memset(bs.t[:, 0:1], 0.0), writes=[bs])
        C.op("dve", lambda e: e.memset(bs.t[:, 1:2], 1.0), reads=[bs], writes=[bs])
        for it in range(34):
            C.op("dve", lambda e: e.tensor_scalar(out=bs.t[:, 2:3], in0=bs.t[:, 0:1], scalar1=bs.t[:, 1:2], scalar2=0.5, op0=ALU.add, op1=ALU.mult), reads=[bs], writes=[bs])
            C.op("dve", lambda e: e.tensor_scalar(out=junk.t[:], in0=affT.t[:], scalar1=bs.t[:, 2:3], scalar2=0.0, op0=ALU.is_ge, op1=ALU.add, accum_out=bs.t[:, 3:4]), reads=[affT, bs], writes=[junk, bs])
            C.op("dve", lambda e: e.tensor_scalar(out=bs.t[:, 4:5], in0=bs.t[:, 3:4], scalar1=float(CAP) - 0.5, scalar2=None, op0=ALU.is_gt), reads=[bs], writes=[bs])
            C.op("dve", lambda e: e.tensor_tensor(out=bs.t[:, 5:6], in0=bs.t[:, 2:3], in1=bs.t[:, 0:1], op=ALU.subtract), reads=[bs], writes=[bs])
            C.op("dve", lambda e: e.tensor_tensor(out=bs.t[:, 6:7], in0=bs.t[:, 1:2], in1=bs.t[:, 2:3], op=ALU.subtract), reads=[bs], writes=[bs])
            C.op("dve", lambda e: e.scalar_tensor_tensor(out=bs.t[:, 0:1], in0=bs.t[:, 5:6], scalar=bs.t[:, 4:5], in1=bs.t[:, 0:1], op0=ALU.mult, op1=ALU.add), reads=[bs], writes=[bs])
            C.op("dve", lambda e: e.scalar_tensor_tensor(out=bs.t[:, 1:2], in0=bs.t[:, 6:7], scalar=bs.t[:, 4:5], in1=bs.t[:, 2:3], op0=ALU.mult, op1=ALU.add), reads=[bs], writes=[bs])
        thrB = C.sb("thrB", [NE, P], F32)
        C.op("dve", lambda e: e.tensor_scalar(out=thrB.t[:], in0=M("ones", NE), scalar1=bs.t[:, 0:1], scalar2=None, op0=ALU.mult), reads=[bs, self.mt], writes=[thrB])
        pb = self.nextps()
        C.op("pe", lambda e: e.matmul(pb.t[:, 0:NE], thrB.t[:], M("ident", NE)[:, 0:NE], start=True, stop=True), reads=[thrB, self.mt], writes=[pb])
        thr = C.sb("thrI", [P, NE], F32)
        self.copy("dve", thr, thr.t[:], pb, pb.t[:, 0:NE])
        mask = C.sb("maskI", [P, NT, NE], F32); slot = C.sb("slotI", [P, NT, NE], F32); msum = C.sb("msumI", [P, NE], F32)
        for tt in range(NT):
            C.op("dve", lambda e, tt=tt: e.tensor_tensor(out=mask.t[:, tt, :], in0=aff_tm.t[:, tt, :], in1=thr.t[:], op=ALU.is_ge), reads=[aff_tm, thr], writes=[mask])
        C.op("dve", lambda e: e.memset(msum.t[:], 0.0), writes=[msum])
        for tt in range(NT):
            pr = self.nextps()

            def fn(e, pr=pr, tt=tt):
                e.matmul(pr.t[:, 0:NE], M("tri"), mask.t[:, tt, :], start=True, stop=False)
                return e.matmul(pr.t[:, 0:NE], M("ones"), msum.t[:], start=False, stop=True)
            C.op("pe", fn, reads=[mask, msum, self.mt], writes=[pr])
            self.copy("act", slot, slot.t[:, tt, :], pr, pr.t[:, 0:NE])
            C.op("dve", lambda e, tt=tt: e.tensor_tensor(out=msum.t[:], in0=msum.t[:], in1=mask.t[:, tt, :], op=ALU.add), reads=[msum, mask], writes=[msum])
        sv = slot.t[:].rearrange("p a b -> p (a b)"); mv = mask.t[:].rearrange("p a b -> p (a b)")
        C.op("dve", lambda e: e.scalar_tensor_tensor(out=sv, in0=sv, scalar=1.0, in1=mv, op0=ALU.add, op1=ALU.mult), reads=[slot, mask], writes=[slot])
        C.op("dve", lambda e: e.tensor_scalar(out=sv, in0=sv, scalar1=-1.0, scalar2=None, op0=ALU.add), reads=[slot], writes=[slot])
        if "dbgs" in self.dbg:
            self.store("sp", self.dbgs_res, self.dbgs_res.t.ap()[:, 128:640], slot, sv)
            self.store("sp", self.dbgs_res, self.dbgs_res.t.ap()[:, 640:1152], mask, mv)
            self.store("sp", self.dbgs_res, self.dbgs_res.t.ap()[:, 1152:1168], thr, thr.t[:])
        rhsE = C.sb("rhsE", [P, NT, 4], F32)
        C.op("dve", lambda e: e.memset(rhsE.t[:], 0.0), writes=[rhsE])
        C.op("dve", lambda e: e.tensor_copy(rhsE.t[:, :, 0:2], M("tvals").rearrange("p (a b) -> p a b", b=2)), reads=[self.mt, rhsE], writes=[rhsE])
        oh = [[C.sb("ohI%d_%d" % (b, i), [P, CAP], F32) for i in range(NT)] for b in range(2)]
        idf = C.sb("idfI", [P, 4], F32); pis = C.sb("pisI", [P, 16], F32)
        for ex in range(NL):
            C.op("dve", lambda e, ex=ex: e.tensor_copy(rhsE.t[:, :, 2], aff_tm.t[:, :, ex]), reads=[aff_tm, rhsE], writes=[rhsE])
            pi = self.nextps()
            OHs = oh[ex % 2]
            for tt in range(NT):
                OH = OHs[tt]
                C.op("dve" if tt % 2 else "pool", lambda e, OH=OH, tt=tt, ex=ex: e.tensor_scalar(out=OH.t[:], in0=M("iota512"), scalar1=slot.t[:, tt, ex:ex + 1], scalar2=None, op0=ALU.is_equal), reads=[slot, self.mt], writes=[OH])

            def fm(e, OHs=OHs, pi=pi):
                inst = None
                for st in range(4):
                    for tt in range(NT):
                        inst = e.matmul(pi.t[:, st * 4:(st + 1) * 4], OHs[tt].t[:, st * P:(st + 1) * P], rhsE.t[:, tt, :], start=(tt == 0), stop=(tt == NT - 1))
                return inst
            C.op("pe", fm, reads=[*OHs, rhsE], writes=[pi])
            C.op("dve", lambda e, pi=pi: e.tensor_copy(pis.t[:], pi.t[:, 0:16]), reads=[pi], writes=[pis])
            pv = pis.t[:].rearrange("p (a b) -> p a b", b=4)
            C.op("dve", lambda e, pv=pv: e.scalar_tensor_tensor(out=idf.t[:], in0=pv[:, :, 0], scalar=64.0, in1=pv[:, :, 1], op0=ALU.mult, op1=ALU.add), reads=[pis], writes=[idf])
            C.op("dve", lambda e, ex=ex: e.tensor_copy(idx_all.t[:, ex, :], idf.t[:]), reads=[idf], writes=[idx_all])
            C.op("dve", lambda e, ex=ex, pv=pv: e.tensor_copy(gate_all.t[:, ex, :], pv[:, :, 2]), reads=[pis], writes=[gate_all])
        C.pop()

    def phase_J(self, hx2, wg, wu, wd, idx_all, gate_all, ffn, ffr):
        C = self.C; M = self.M
        C.push()
        zt = C.sb("ztJ", [P, 512], F32)
        C.op("dve", lambda e: e.memset(zt.t[:], 0.0), writes=[zt])
        fres = [Res("ffnres%d" % i) for i in range(8)]
        for db in range(8):
            for tt in range(NT):
                C.dma("sp" if tt % 2 else "act", lambda e, db=db, tt=tt: e.dma_start(out=ffn[db].t.ap()[tt * P:(tt + 1) * P, :], in_=zt.t[:]), reads=[zt], writes=[fres[db]], semres=zt)
        stg = [C.sb("stgJ%d" % i, [P, 4096], F32) for i in range(3)]
        wgb = [C.sb("wgb%d" % i, [P, 32, P], BF16) for i in range(2)]
        wub = [C.sb("wub%d" % i, [P, 32, P], BF16) for i in range(2)]
        wdb = [C.sb("wdb%d" % i, [P, 16, 512], BF16) for i in range(2)]
        xs = [C.sb("xsJ%d" % i, [P, D], BF16) for i in range(2)]
        xsT = C.sb("xsT", [P, 32, CAP], BF16); hidT = C.sb("hidT", [P, 16, CAP], BF16)
        sgt = [C.sb("sgt%d" % i, [P, CAP], F32) for i in range(2)]
        ot = [C.sb("otJ%d" % i, [P, 512], F32) for i in range(4)]
        cnt = dict(stg=0, ot=0, ev=0)

        def nstg():
            r = stg[cnt["stg"] % 3]; cnt["stg"] += 1; return r
        def gujob(ex, fi):
            def job():
                WG = wgb[fi % 2]; WU = wub[fi % 2]
                gv = wg.t.ap()[ex].rearrange("(kc p) f -> p kc f", p=P); uv = wu.t.ap()[ex].rearrange("(kc p) f -> p kc f", p=P)
                for (src, view, Wd_) in ((wg, gv, WG), (wu, uv, WU)):
                    S_ = nstg()
                    self.load("sp", S_, S_.t[:].rearrange("p (a b) -> p a b", a=32), src, view[:, :, fi * P:(fi + 1) * P])
                    self.copy(self.cast_eng(), Wd_, Wd_.t[:].rearrange("p a b -> p (a b)"), S_, S_.t[:])
                return (WG, WU)
            return job

        def djob(ex, db):
            def job():
                WD = wdb[db % 2]
                dv = wd.t.ap()[ex].rearrange("(fc p) d -> p fc d", p=P)
                for hf in range(2):
                    S_ = nstg()
                    self.load("sp", S_, S_.t[:].rearrange("p (a b) -> p a b", a=8), wd, dv[:, hf * 8:(hf + 1) * 8, db * 512:(db + 1) * 512])
                    self.copy(self.cast_eng(), WD, WD.t[:, hf * 8:(hf + 1) * 8, :].rearrange("p a b -> p (a b)"), S_, S_.t[:])
                return WD
            return job
        wst = Stream([j for ex in range(NL) for j in ([gujob(ex, fi) for fi in range(16)] + [djob(ex, db) for db in range(8)])])
        for ex in range(NL):
            for st in range(4):
                X = xs[st % 2]
                C.dma("pool", lambda e, X=X, ex=ex, st=st: e.indirect_dma_start(out=X.t[:], out_offset=None, in_=hx2.t.ap(), in_offset=bass.IndirectOffsetOnAxis(ap=idx_all.t[:, ex, st:st + 1], axis=0)), reads=[hx2, idx_all], writes=[X])
                for g in range(8):
                    pt = self.nextps(); pv = self.psb(pt)

                    def fnT(e, pv=pv, X=X, g=g):
                        inst = None
                        for j in range(4):
                            kc = g * 4 + j
                            inst = e.transpose(pv[:, j * P:(j + 1) * P], X.t[:, kc * P:(kc + 1) * P], self.identb.t[:])
                        return inst
                    C.op("pe", fnT, reads=[X, self.identb], writes=[pt])
                    cnt["ev"] += 1
                    self.copy("act" if cnt["ev"] % 2 else "dve", xsT, xsT.t[:, g * 4:(g + 1) * 4, st * P:(st + 1) * P], pt, pv[:, 0:512].rearrange("p (a b) -> p a b", a=4))
            for fi in range(16):
                WG, WU = wst.next()
                pg = self.nextps(); pu = self.nextps()
                self.mm(pg, pg.t[:, :], [(WG.t[:, kc, :], xsT.t[:, kc, :]) for kc in range(32)], [WG, xsT])
                self.mm(pu, pu.t[:, :], [(WU.t[:, kc, :], xsT.t[:, kc, :]) for kc in range(32)], [WU, xsT])
                SG = sgt[fi % 2]
                C.op("act", lambda e, SG=SG, pg=pg: e.activation(out=SG.t[:], in_=pg.t[:, :], func=AF.Silu), reads=[pg], writes=[SG])
                C.op("dve", lambda e, SG=SG, pu=pu, fi=fi: e.tensor_tensor(out=hidT.t[:, fi, :], in0=pu.t[:, :], in1=SG.t[:], op=ALU.mult), reads=[pu, SG], writes=[hidT])
            for db in range(8):
                WD = wst.next()
                for st in range(4):
                    po = self.nextps()
                    self.mm(po, po.t[:, :], [(hidT.t[:, fc, st * P:(st + 1) * P], WD.t[:, fc, :]) for fc in range(16)], [hidT, WD])
                    O = ot[cnt["ot"] % 4]; cnt["ot"] += 1
                    C.op("act", lambda e, O=O, po=po, ex=ex, st=st: e.activation(out=O.t[:], in_=po.t[:, :], func=AF.Copy, scale=gate_all.t[:, ex, st:st + 1]), reads=[po, gate_all], writes=[O])
                    rd_ = [O, idx_all] + ([] if st == 0 else [fres[db]])
                    wr_ = [fres[db]] if st == 0 else []
                    C.dma("pool", lambda e, O=O, ex=ex, st=st, db=db: e.indirect_dma_start(out=ffn[db].t.ap(), out_offset=bass.IndirectOffsetOnAxis(ap=idx_all.t[:, ex, st:st + 1], axis=0), in_=O.t[:], in_offset=None, compute_op=ALU.add), reads=rd_, writes=wr_, semres=O)
        self.fres = fres
        C.pop()

    def phase_K(self, x1r, ffn, bcd, fgb, out):
        C = self.C; M = self.M
        C.push()
        g2 = C.sb("gt2b", [P, D], F32); fg = C.sb("fgbt", [P, D], F32)
        self.load("sp", g2, g2.t[:], bcd, bcd.t.ap()[:, D:2 * D]); self.load("act", fg, fg.t[:], fgb, fgb.t.ap())
        xt = [C.sb("xtK%d" % i, [P, D], F32) for i in range(2)]
        ft = [C.sb("ftK%d" % i, [P, D], F32) for i in range(2)]
        junk = C.sb("junkK", [P, D], BF16)
        sts = [C.sb("stK%d" % i, [P, 4], F32) for i in range(2)]
        for tt in range(NT):
            X = xt[tt % 2]; Fd = ft[tt % 2]; st = sts[tt % 2]
            tk = slice(tt * P, (tt + 1) * P)
            self.load("sp", X, X.t[:], x1r, x1r.t.ap()[tk, :])
            for db in range(8):
                C.dma("act" if db % 2 else "sp", lambda e, Fd=Fd, db=db, tk=tk: e.dma_start(out=Fd.t[:, db * 512:(db + 1) * 512], in_=ffn[db].t.ap()[tk, :]), reads=[self.fres[db]], writes=[Fd])
            C.op("dve", lambda e, Fd=Fd: e.tensor_tensor(out=Fd.t[:], in0=Fd.t[:], in1=g2.t[:], op=ALU.mult), reads=[Fd, g2], writes=[Fd])
            C.op("pool", lambda e, Fd=Fd, X=X: e.tensor_tensor(out=Fd.t[:], in0=Fd.t[:], in1=X.t[:], op=ALU.add), reads=[Fd, X], writes=[Fd])
            self.rms_stats(Fd, junk, st, D)
            C.op("dve", lambda e, Fd=Fd, st=st, X=X: e.scalar_tensor_tensor(out=X.t[:], in0=Fd.t[:], scalar=st.t[:, 1:2], in1=fg.t[:], op0=ALU.mult, op1=ALU.mult), reads=[Fd, st, fg, X], writes=[X])
            self.store("sp", out, out.t.ap()[tk, :], X, X.t[:])
        C.pop()


def prep_inputs(inp, b, names, r=0):
    hc = host_constants()
    f = lambda a: np.ascontiguousarray(np.asarray(a, np.float32))
    bro = lambda v: np.ascontiguousarray(np.broadcast_to(np.asarray(v, np.float32).reshape(1, -1), (P, np.asarray(v).size)))
    m = {}
    m["x"] = f(inp["x"][b]); m["ctx"] = f(inp["ctx"][b])
    cc = np.stack([col_layout(inp["c"][b]), col_layout(inp["c_ctx"])], axis=2)
    m["cT"] = f(cc.reshape(P, 64))
    m["ada_w"] = f(inp["ada_w"][0]); m["abT"] = col_layout(inp["ada_b"][0])
    ab = np.asarray(inp["ada_b"][0], np.float32).reshape(6, D)
    m["abb"] = np.ascontiguousarray(np.concatenate([bro(ab[2]), bro(ab[5]), bro(ab[4]), bro(ab[3])], axis=1))
    m["g1T"] = col_layout(inp["norm1_g"][0]); m["g2b"] = bro(inp["norm2_g"][0]); m["fgb"] = bro(inp["final_g"])
    m["w_in"] = f(inp["w_in"][0]); m["w_out"] = f(inp["w_out"][0])
    sw = np.asarray(inp["hy_short_w"][0], np.float32); sb_ = np.asarray(inp["hy_short_b"][0], np.float32)
    swt = np.stack([col_layout(sw[0]), col_layout(sw[1]), col_layout(sw[2]), col_layout(sb_)], axis=2)
    m["swT"] = f(swt.reshape(P, 48 * 4))
    m["fw1"] = f(inp["hy_f_w1"][0]); m["fw2"] = f(inp["hy_f_w2"][0]); m["fw3"] = f(inp["hy_f_w3"][0]); m["fw4"] = f(inp["hy_f_w4"][0])
    m["fvec"] = f(np.stack([inp["hy_f_b1"][0], inp["hy_f_b2"][0], inp["hy_f_b3"][0], inp["hy_sin_freq"][0]], axis=1))
    m["hyb"] = bro(np.asarray(inp["hy_bias"][0]).reshape(-1)); m["rdec"] = bro(np.asarray(inp["ret_decay"][0]).reshape(-1))
    perm = [(e + NL * r) % NE for e in range(NE)]
    rw = np.asarray(inp["router_w"][0], np.float32)[:, perm].reshape(32, P, NE).transpose(1, 0, 2)
    m["rwT"] = f(rw.reshape(P, 32 * NE))
    m["cst"] = hc["cst"].reshape(NFT, P, 2 * NFT * P); m["fscale"] = hc["fscale"]
    m["zT"] = hc["zT"]; m["negt"] = hc["negt"]; m["deltab"] = hc["deltab"]
    m["rot_x"] = hc["rot_x"].reshape(T, 512); m["rot_c"] = hc["rot_c"].reshape(LC, 512)
    m["rot_xk"] = hc["rot_xk"].reshape(T, 512); m["rot_ck"] = hc["rot_ck"].reshape(LC, 512)
    m["misc"] = hc["misc"]
    if "wg" in names:
        sl = slice(NL * r, NL * (r + 1))
        m["wg"] = f(inp["exp_w_gate"][0][sl]); m["wu"] = f(inp["exp_w_up"][0][sl]); m["wd"] = f(inp["exp_w_down"][0][sl])
    return {k: v for k, v in m.items() if k in names}


def run(inputs, upto="all", dbg=(), cores=8):
    kb = K(upto=upto, dbg=dbg)
    nc = kb.build()
    names = set(kb.inputs.keys())
    maps = {}
    in_maps = []
    for cid in range(cores):
        b = cid // 2
        if b not in maps:
            maps[b] = prep_inputs(inputs, b, names, 0)
        in_maps.append(maps[b])
    res = run_bass_kernel_spmd(nc, in_maps, core_ids=list(range(cores)))
    return res, kb


def kernel(**inputs):
    res, kb = run(inputs)
    out = np.stack([np.asarray(res.results[2 * b]["out"], np.float32) for b in range(4)], axis=0)
    return out
```

```python
import math
from contextlib import ExitStack
import numpy as np
import ml_dtypes
import concourse.bass as bass
import concourse.mybir as mybir
from concourse.bass_utils import run_bass_kernel_spmd

F32 = mybir.dt.float32; BF16 = mybir.dt.bfloat16; I32 = mybir.dt.int32
ALU = mybir.AluOpType; AF = mybir.ActivationFunctionType

D = 4096; T = 4096; LC = 256; NT = 32; P = 128
HYW = 2048; HYC = 6144; QC = 1024; GC = 4096; KC = 1024; VC = 2048
KV0 = HYC + QC + GC; INW = 14336
NE = 16; NL = 16; EFF = 2048; CAP = 512
NFT = 33
EPS = 1e-6


class Res:
    __slots__ = ("name", "w", "r", "t", "dsem")

    def __init__(self, name, t=None):
        self.name = name; self.t = t; self.w = {}; self.r = {}; self.dsem = None


def _is_dram(r):
    return r.t is not None and type(r.t).__name__.lower().startswith("dram")


class Ctx:
    def __init__(self, nc, es):
        self.nc = nc; self.es = es; self.engs = {}; self.sems = {}
        for name, e in (("pe", nc.tensor), ("dve", nc.vector), ("act", nc.scalar), ("pool", nc.gpsimd), ("sp", nc.sync)):
            sem = es.enter_context(nc.semaphore("s_" + name))
            self.engs[name] = dict(e=e, seen={})
            self.sems[name] = [sem, 0]
        self.free_dsems = []
        self.ndsem = 0
        self.stack = [es]
        self.scope_res = [[]]
        self.ninst = 0

    def sb(self, name, shape, dt):
        t = self.stack[-1].enter_context(self.nc.sbuf_tensor(name, list(shape), dt))
        r = Res(name, t); self.scope_res[-1].append(r); return r

    def ps(self, name, shape, dt=F32):
        t = self.stack[-1].enter_context(self.nc.psum_tensor(name, list(shape), dt))
        r = Res(name, t); self.scope_res[-1].append(r); return r

    def dram(self, name, shape, dt, kind="Internal"):
        return Res(name, self.nc.dram_tensor(name, list(shape), dt, kind=kind))

    def push(self):
        es = ExitStack(); self.stack.append(es); self.scope_res.append([]); return es

    def pop(self):
        self.barrier()
        for r in self.scope_res.pop():
            if r.dsem is not None:
                self.free_dsems.append(r.dsem); r.dsem = None
        self.stack.pop().close()

    def _dsem(self, r):
        if r.dsem is None:
            if self.free_dsems:
                r.dsem = self.free_dsems.pop()
            else:
                key = "d%d" % self.ndsem; self.ndsem += 1
                sem = self.es.enter_context(self.nc.semaphore(key))
                self.sems[key] = [sem, 0]; r.dsem = key
        return r.dsem

    def _wait(self, eng, deps):
        E = self.engs[eng]
        for key, val in deps.items():
            if key[0] == "d":
                val = self.sems[key][1]
            elif key == eng and eng == "pe":
                continue
            if E["seen"].get(key, 0) >= val:
                continue
            E["e"].wait_ge(self.sems[key][0], val)
            E["seen"][key] = val

    def _deps(self, reads, writes):
        deps = {}
        for r in reads:
            for k, v in r.w.items():
                if deps.get(k, 0) < v: deps[k] = v
        for r in writes:
            for k, v in r.w.items():
                if deps.get(k, 0) < v: deps[k] = v
            for k, v in r.r.items():
                if deps.get(k, 0) < v: deps[k] = v
        return deps

    def _mark(self, key, val, reads, writes):
        for r in reads:
            if r.r.get(key, 0) < val: r.r[key] = val
        for r in writes:
            r.w = {key: val}; r.r = {}

    def op(self, eng, fn, reads=(), writes=()):
        self._wait(eng, self._deps(reads, writes))
        inst = fn(self.engs[eng]["e"])
        s = self.sems[eng]; s[1] += 1
        inst.then_inc(s[0], 1)
        self._mark(eng, s[1], reads, writes)
        self.ninst += 1
        return inst

    def dma(self, q, fn, reads=(), writes=(), semres=None):
        if semres is None:
            if writes and not _is_dram(writes[0]):
                semres = writes[0]
            else:
                semres = reads[0]
        key = self._dsem(semres)
        self._wait(q, self._deps(reads, writes))
        inst = fn(self.engs[q]["e"])
        s = self.sems[key]; s[1] += 16
        inst.then_inc(s[0], 16)
        self._mark(key, s[1], reads, writes)
        self.ninst += 1
        return inst

    def barrier(self):
        allk = {k: v[1] for k, v in self.sems.items() if v[1] > 0}
        for eng in self.engs:
            self._wait(eng, allk)


def _bf(a):
    return np.ascontiguousarray(a.astype(ml_dtypes.bfloat16))


_CONST_CACHE = {}


def host_constants():
    if _CONST_CACHE:
        return _CONST_CACHE
    c = {}
    n = NFT * P
    idx = np.arange(n, dtype=np.int64)
    prod = (idx[:, None] * idx[None, :]) % 8192
    valid = (idx[:, None] <= 4096) & (idx[None, :] <= 4096)
    ang = prod.astype(np.float64) * (2.0 * math.pi / 8192.0)
    Cm = np.where(valid, np.cos(ang), 0.0)
    Sm = np.where(valid, np.sin(ang), 0.0)
    def tile(M):
        return M.reshape(NFT, P, NFT, P).transpose(2, 1, 0, 3)
    cs = np.stack([tile(Cm), tile(Sm)], axis=2)
    c["cst"] = _bf(cs.astype(np.float32))
    fs = np.zeros(n, np.float32); fs[0] = 1.0 / 8192; fs[1:4096] = 2.0 / 8192; fs[4096] = 1.0 / 8192
    c["fscale"] = np.ascontiguousarray(fs.reshape(NFT, P).T)
    L = T
    bands = 16
    t = np.linspace(0.0, 1.0, L, dtype=np.float32)[:, None]
    ang2 = (np.float32(2.0 * math.pi / L) * np.arange(L, dtype=np.float32)[:, None]) * np.linspace(1e-4, bands - 1, bands, dtype=np.float32)[None, :]
    z = np.concatenate([t, np.cos(ang2), -np.sin(ang2)], axis=-1).astype(np.float32)
    c["zT"] = np.ascontiguousarray(z.T)
    c["negt"] = np.ascontiguousarray((-t[:, 0]).reshape(NT, P).T.astype(np.float32))
    max_decay = math.log(1e-2) / 0.3; min_decay = math.log(1e-2) / 1.5
    deltas = np.abs(np.linspace(min_decay, max_decay, HYW, dtype=np.float32))
    c["deltab"] = np.ascontiguousarray(np.broadcast_to(deltas[None, :], (P, HYW)).astype(np.float32))
    n_freq = 32
    inv = (1.0 / (10000.0 ** np.linspace(0.0, 1.0, n_freq, dtype=np.float32))).astype(np.float32)
    def rot(pa, pb):
        a = np.concatenate([pa[:, None] * inv, pb[:, None] * inv], axis=-1).astype(np.float32)
        return np.cos(a).astype(np.float32), np.sin(a).astype(np.float32)
    rows = L // 64
    lat_row = np.repeat(np.arange(rows, dtype=np.float32), 64)
    lat_col = np.tile(np.arange(64, dtype=np.float32), rows)
    cx, sx = rot(lat_row, lat_col)
    cp = np.arange(LC, dtype=np.float32)
    cc, sc = rot(cp, cp)
    c["rot_x"] = np.ascontiguousarray(np.stack([np.tile(cx, (1, 4)), np.tile(sx, (1, 4))], axis=1))
    c["rot_c"] = np.ascontiguousarray(np.stack([np.tile(cc, (1, 4)), np.tile(sc, (1, 4))], axis=1))
    ks = np.float32(128 ** -0.5)
    c["rot_xk"] = np.ascontiguousarray(c["rot_x"] * ks); c["rot_ck"] = np.ascontiguousarray(c["rot_c"] * ks)
    pidx = np.arange(P, dtype=np.float32)
    m = {}
    m["ident"] = np.eye(P, dtype=np.float32)
    m["ones"] = np.ones((P, P), np.float32)
    jj = pidx[:, None]; ii = pidx[None, :]
    m["diffF"] = np.maximum(ii - jj, 0.0); m["maskF"] = (ii >= jj).astype(np.float32)
    m["diffB"] = np.maximum(jj - ii, 0.0); m["maskB"] = (jj >= ii).astype(np.float32)
    m["ip1"] = np.broadcast_to(ii + 1.0, (P, P)).copy(); m["i128m"] = np.broadcast_to(128.0 - ii, (P, P)).copy()
    m["tri"] = (jj < ii).astype(np.float32)
    m["iota512"] = np.broadcast_to(np.arange(CAP, dtype=np.float32)[None, :], (P, CAP)).copy()
    cols = np.zeros((P, 8), np.float32)
    cols[:, 0] = 127.0 - pidx; cols[:, 1] = pidx; cols[:, 2] = 255.0 - pidx; cols[:, 3] = 127.0 - pidx
    cols[:, 4] = pidx; cols[:, 5] = 128.0 + pidx
    cols[:, 6] = EPS; cols[:, 7] = 1.0
    m["cols"] = cols
    tv = np.zeros((P, NT, 2), np.float32)
    tok = (np.arange(NT)[None, :] * P + np.arange(P)[:, None])
    tv[:, :, 0] = tok // 64; tv[:, :, 1] = tok % 64
    m["tvals"] = tv.reshape(P, NT * 2)
    sel = np.zeros((P, P), np.float32); sel[0, :] = 1.0
    m["sel0"] = sel
    off = {}; parts = []; o = 0
    for k, v in m.items():
        off[k] = (o, v.shape[1]); parts.append(v.astype(np.float32)); o += v.shape[1]
    c["misc"] = np.ascontiguousarray(np.concatenate(parts, axis=1))
    c["misc_off"] = off
    _CONST_CACHE.update(c)
    return c


class Stream:
    def __init__(self, jobs, ahead=1):
        self.jobs = jobs; self.done = {}; self.k = 0; self.ahead = ahead

    def next(self):
        k = self.k
        for j in range(k, min(k + self.ahead + 1, len(self.jobs))):
            if j not in self.done:
                self.done[j] = self.jobs[j]()
        self.k += 1
        return self.done.pop(k)


def col_layout(v):
    v = np.asarray(v, np.float32).reshape(-1, P)
    return np.ascontiguousarray(v.T)


class K:
    def __init__(self, upto="all", dbg=()):
        self.upto = upto; self.dbg = set(dbg)
        self.nc = bass.Bass("TRN2", target_bir_lowering=False)
        self.inputs = {}
        self.outputs = []

    def inp(self, name, shape, dt=F32):
        r = self.C.dram(name, shape, dt, kind="ExternalInput"); self.inputs[name] = r; return r

    def scratch(self, name, shape, dt):
        kind = "ExternalOutput" if name in self.dbg else "Internal"
        if name in self.dbg:
            self.outputs.append(name)
        return self.C.dram(name, shape, dt, kind=kind)

    def load(self, q, dst, dst_ap, src, src_ap):
        return self.C.dma(q, lambda e: e.dma_start(out=dst_ap, in_=src_ap), reads=[src], writes=[dst])

    def store(self, q, dst, dst_ap, src, src_ap):
        return self.C.dma(q, lambda e: e.dma_start(out=dst_ap, in_=src_ap), reads=[src], writes=[dst], semres=src)

    def nextps(self):
        r = self.pbanks[self.pbi % len(self.pbanks)]; self.pbi += 1; return r

    def mm(self, pr, out_ap, pairs, reads):
        n = len(pairs)

        def fn(e):
            inst = None
            for i, (l, r) in enumerate(pairs):
                inst = e.matmul(out_ap, l, r, start=(i == 0), stop=(i == n - 1))
            return inst
        return self.C.op("pe", fn, reads=reads, writes=[pr])

    def cast_eng(self):
        self.ce = (self.ce + 1) % 3
        return ("dve", "act", "pool")[self.ce]

    def copy(self, eng, dst, dst_ap, src, src_ap, extra_reads=()):
        if eng == "act":
            return self.C.op("act", lambda e: e.activation(out=dst_ap, in_=src_ap, func=AF.Copy), reads=[src, *extra_reads], writes=[dst])
        return self.C.op(eng, lambda e: e.tensor_copy(dst_ap, src_ap), reads=[src, *extra_reads], writes=[dst])

    def build(self):
        nc = self.nc
        with ExitStack() as es:
            self.C = C = Ctx(nc, es)
            self.pbi = 0; self.ce = 0
            hc = host_constants()
            self.moff = hc["misc_off"]
            nmisc = hc["misc"].shape[1]
            I = self.inp
            x = I("x", [T, D]); ctx = I("ctx", [LC, D]); cT = I("cT", [P, 64])
            ada_w = I("ada_w", [D, 6 * D]); abT = I("abT", [P, 192])
            g1T = I("g1T", [P, 32]); g2b = I("g2b", [P, D]); fgb = I("fgb", [P, D]); abb = I("abb", [P, 4 * D])
            w_in = I("w_in", [D, INW]); w_out = I("w_out", [D, D])
            swT = I("swT", [P, 48 * 4])
            fw1 = I("fw1", [33, 64]); fw2 = I("fw2", [64, 64]); fw3 = I("fw3", [64, 64]); fw4 = I("fw4", [64, 8192])
            fvec = I("fvec", [64, 4])
            hyb = I("hyb", [P, 4096]); rdec = I("rdec", [P, 16])
            rwT = I("rwT", [P, 32 * 16])
            cst = I("cst", [NFT, P, 2 * NFT * P], BF16); fscale = I("fscale", [P, NFT])
            zT = I("zT", [33, T]); negt = I("negt", [P, NT]); deltab = I("deltab", [P, HYW])
            rot_x = I("rot_x", [T, 512]); rot_c = I("rot_c", [LC, 512]); rot_xk = I("rot_xk", [T, 512]); rot_ck = I("rot_ck", [LC, 512]); misc = I("misc", [P, nmisc])
            if self.upto in ("all", "J"):
                wg = I("wg", [NL, D, EFF]); wu = I("wu", [NL, D, EFF]); wd = I("wd", [NL, EFF, D])
            out = C.dram("out", [T, D], F32, kind="ExternalOutput")
            self.outputs.append("out")
            S = self.scratch
            hxT = S("hxT", [D, T], BF16)
            v_tm = S("v_tm", [T, HYW], BF16); x1_tm = S("x1_tm", [T, HYW], BF16); x2_cm = S("x2_cm", [HYW, T], BF16)
            sg = S("sg", [GC, T], BF16)
            qT_d = S("qT_d", [QC, T], BF16); kT_d = S("kT_d", [KC, T], BF16)
            k_tm = S("k_tm", [T, KC], BF16); vr_tm = S("vr_tm", [T, VC], BF16)
            ed = S("ed", [T, 2 * 4096], BF16)
            spec = S("spec", [NFT * P, 2 * 4096], F32)
            yT = S("yT", [6144, T], BF16)
            x1r = S("x1r", [T, D], F32); hx2 = S("hx2", [T, D], BF16)
            affd = S("affd", [T, NE], F32)
            ffn = [S("ffn%d" % i, [T, 512], F32) for i in range(8)]
            ffr = [S("ffr%d" % i, [T, 512], F32) for i in range(8)]
            dbgs = S("dbgs", [P, 4096], F32)
            self.dbgs_res = dbgs

            mt = C.sb("misc_t", [P, nmisc], F32)
            self.load("sp", mt, mt.t[:], misc, misc.t.ap())

            def M(name, rows=P):
                o, w = self.moff[name]
                return mt.t[0:rows, o:o + w]
            self.M = M
            identb = C.sb("identb", [P, P], BF16); onesb = C.sb("onesb", [P, P], BF16)
            C.op("dve", lambda e: e.tensor_copy(identb.t[:], M("ident")), reads=[mt], writes=[identb])
            C.op("dve", lambda e: e.tensor_copy(onesb.t[:], M("ones")), reads=[mt], writes=[onesb])
            self.identb = identb; self.onesb = onesb; self.mt = mt
            self.pbanks = [C.ps("pb%d" % i, [P, 512], F32) for i in range(7)]
            self.pacc = C.ps("pacc", [P, 512], F32)
            self.slab_i = 0
            modsT = C.sb("modsT", [P, 192, 2], F32)
            AB = C.sb("AB", [P, 4, 32], F32)
            bcd = S("bcd", [P, 4 * D], F32)
            lg = C.sb("lg", [P, 16], F32)
            Sfd = S("Sfd", [P, 16 * 256], F32)

            self.phase_A(cT, ada_w, abT, g1T, g2b, abb, modsT, AB, bcd)
            if "modsT" in self.dbg:
                pass
            if self.upto == "A":
                return self.finish(dbgs, [(modsT, modsT.t[:].rearrange("p a b -> p (a b)"), 384), (AB, AB.t[:].rearrange("p a b -> p (a b)"), 128)])
            C.push()
            hcT = C.sb("hcT", [P, 32, LC], BF16)
            Sf = C.sb("Sf", [P, 16, 256], F32); Sb = C.sb("Sb", [P, 16, 256], BF16)
            self.stg_i = 0
            self.phase_B(x, ctx, AB, hxT, hcT)
            if self.upto == "B":
                tmpd = C.sb("tmpd", [P, 512], F32)
                C.op("dve", lambda e: e.tensor_copy(tmpd.t[:, 0:256], hcT.t[:, 0, :]), reads=[hcT], writes=[tmpd])
                C.op("dve", lambda e: e.tensor_copy(tmpd.t[:, 256:512], hcT.t[:, 31, :]), reads=[hcT], writes=[tmpd])
                return self.finish(dbgs, [(tmpd, tmpd.t[:], 512)])
            self.phase_C(hcT, w_in, rot_ck, rdec, lg, Sf, Sb)
            self.store("sp", Sfd, Sfd.t.ap(), Sf, Sf.t[:].rearrange("p a b -> p (a b)"))
            if self.upto == "C":
                return self.finish(dbgs, [(lg, lg.t[:], 16), (Sf, Sf.t[:, 0, :], 256), (Sf, Sf.t[:, 15, :], 256)])
            C.pop()
            self.phase_D(hxT, w_in, swT, rot_x, rot_xk, v_tm, x1_tm, x2_cm, sg, qT_d, kT_d, k_tm, vr_tm)
            if self.upto == "D":
                return self.finish(dbgs, [])
            self.phase_E(fw1, fw2, fw3, fw4, fvec, zT, negt, deltab, hyb, cst, fscale, spec)
            if self.upto == "E":
                return self.finish(dbgs, [])
            self.phase_F(v_tm, x1_tm, x2_cm, spec, cst, yT)
            if self.upto == "F":
                return self.finish(dbgs, [])
            self.phase_G(qT_d, kT_d, k_tm, vr_tm, sg, Sfd, lg, yT)
            if self.upto == "G":
                return self.finish(dbgs, [])
            aff_tm = C.sb("aff_tm", [P, NT, NE], F32)
            self.phase_H(yT, w_out, x, bcd, x1r, hx2, rwT, aff_tm)
            if self.upto == "H":
                return self.finish(dbgs, [(aff_tm, aff_tm.t[:].rearrange("p a b -> p (a b)"), 512)])
            idx_all = C.sb("idx_all", [P, NL, 4], I32); gate_all = C.sb("gate_all", [P, NL, 4], F32)
            self.phase_I(aff_tm, idx_all, gate_all)
            if self.upto == "I":
                idf2 = C.sb("idf2", [P, 64], F32)
                C.op("dve", lambda e: e.tensor_copy(idf2.t[:], idx_all.t[:].rearrange("p a b -> p (a b)")), reads=[idx_all], writes=[idf2])
                return self.finish(dbgs, [(idf2, idf2.t[:], 64), (gate_all, gate_all.t[:].rearrange("p a b -> p (a b)"), 64)])
            self.phase_J(hx2, wg, wu, wd, idx_all, gate_all, ffn, ffr)
            self.phase_K(x1r, ffn, bcd, fgb, out)
            return self.finish(dbgs, [])

    def finish(self, dbgs, dumps):
        C = self.C
        o = 0
        for (r, ap, w) in dumps:
            self.store("sp", dbgs, dbgs.t.ap()[:, o:o + w], r, ap); o += w
        self.outputs.append("dbgs") if "dbgs" in self.dbg else None
        C.barrier()
        return self.nc

    def phase_A(self, cT, ada_w, abT, g1T, g2b, abb, modsT, AB, bcd):
        C = self.C; M = self.M
        C.push()
        bc = C.sb("bc", [P, 4, D], F32)
        ct = C.sb("ct", [P, 64], F32); sct = C.sb("sct", [P, 32, 2], F32)
        abt = C.sb("abt", [P, 192], F32); g1t = C.sb("g1t", [P, 32], F32)
        self.load("sp", ct, ct.t[:], cT, cT.t.ap())
        self.load("sp", abt, abt.t[:], abT, abT.t.ap())
        self.load("sp", g1t, g1t.t[:], g1T, g1T.t.ap())
        C.op("act", lambda e: e.activation(out=sct.t[:].rearrange("p a b -> p (a b)"), in_=ct.t[:], func=AF.Silu), reads=[ct], writes=[sct])
        wbuf = [C.sb("aw%d" % i, [P, 8, 512], F32) for i in range(5)]
        row = [C.sb("row%d" % i, [2, 512], F32) for i in range(2)]
        awv = ada_w.t.ap().rearrange("(kc p) n -> p kc n", p=P)
        wi = 0
        bmap = {2: 0, 5: 1, 4: 2, 3: 3}
        for nb in range(48):
            pr = self.nextps()
            halves = []
            for h in range(4):
                wb = wbuf[wi % 5]; wi += 1
                self.load("sp" if (wi % 2) else "act", wb, wb.t[:], ada_w, awv[:, h * 8:(h + 1) * 8, nb * 512:(nb + 1) * 512])
                halves.append(wb)
            def fn(e, halves=halves, pr=pr):
                inst = None
                for h in range(4):
                    for k in range(8):
                        kc = h * 8 + k
                        inst = e.matmul(pr.t[0:2, :], sct.t[:, kc, :], halves[h].t[:, k, :], start=(kc == 0), stop=(kc == 31))
                return inst
            C.op("pe", fn, reads=[sct, *halves], writes=[pr])
            rw = row[nb % 2]
            C.op("dve", lambda e, rw=rw, pr=pr: e.tensor_copy(rw.t[:], pr.t[0:2, :]), reads=[pr], writes=[rw])
            pt = self.nextps()
            def fn2(e, rw=rw, pt=pt):
                inst = None
                for j in range(4):
                    inst = e.transpose(pt.t[:, j * 2:j * 2 + 2], rw.t[0:2, j * 128:(j + 1) * 128], M("ident", 2)[:, 0:2])
                return inst
            C.op("pe", fn2, reads=[rw, self.mt], writes=[pt])
            C.op("dve", lambda e, pt=pt, nb=nb: e.tensor_copy(modsT.t[:, nb * 4:(nb + 1) * 4, :].rearrange("p a b -> p (a b)"), pt.t[:, 0:8]), reads=[pt], writes=[modsT])
            mod = nb // 8
            if mod in bmap:
                pb = self.nextps()
                C.op("pe", lambda e, pb=pb, rw=rw: e.matmul(pb.t[:, :], M("sel0", 2), rw.t[0:2, :], start=True, stop=True), reads=[rw, self.mt], writes=[pb])
                j = bmap[mod]; cb = (nb % 8) * 512
                C.op("act", lambda e, pb=pb, j=j, cb=cb: e.activation(out=bc.t[:, j, cb:cb + 512], in_=pb.t[:, :], func=AF.Copy), reads=[pb], writes=[bc])
        for r in range(2):
            C.op("dve", lambda e, r=r: e.tensor_tensor(out=modsT.t[:, :, r], in0=modsT.t[:, :, r], in1=abt.t[:], op=ALU.add), reads=[modsT, abt], writes=[modsT])
        for r in range(2):
            C.op("dve", lambda e, r=r: e.scalar_tensor_tensor(out=AB.t[:, 2 * r, :], in0=modsT.t[:, 32:64, r], scalar=1.0, in1=g1t.t[:], op0=ALU.add, op1=ALU.mult), reads=[modsT, g1t], writes=[AB])
            C.op("dve", lambda e, r=r: e.tensor_copy(AB.t[:, 2 * r + 1, :], modsT.t[:, 0:32, r]), reads=[modsT], writes=[AB])
        tmpb = C.sb("tmpb", [P, D], F32)
        for j in range(4):
            self.load("sp", tmpb, tmpb.t[:], abb, abb.t.ap()[:, j * D:(j + 1) * D])
            C.op("dve", lambda e, j=j: e.tensor_tensor(out=bc.t[:, j, :], in0=bc.t[:, j, :], in1=tmpb.t[:], op=ALU.add), reads=[bc, tmpb], writes=[bc])
        self.load("sp", tmpb, tmpb.t[:], g2b, g2b.t.ap())
        C.op("dve", lambda e: e.scalar_tensor_tensor(out=bc.t[:, 2, :], in0=bc.t[:, 2, :], scalar=1.0, in1=tmpb.t[:], op0=ALU.add, op1=ALU.mult), reads=[bc, tmpb], writes=[bc])
        self.store("sp", bcd, bcd.t.ap(), bc, bc.t[:].rearrange("p a b -> p (a b)"))
        C.pop()

    def psb(self, pr):
        return pr.t[:, :].bitcast(BF16)

    def rms_stats(self, xt, junk, st, width):
        C = self.C; M = self.M
        C.op("act", lambda e: e.activation(out=junk.t[:, 0:width], in_=xt.t[:, 0:width], func=AF.Square, accum_out=st.t[:, 0:1]), reads=[xt], writes=[junk, st])
        C.op("act", lambda e: e.activation(out=st.t[:, 2:3], in_=st.t[:, 0:1], func=AF.Sqrt, scale=1.0 / width, bias=M("cols")[:, 6:7]), reads=[st, self.mt], writes=[st])
        C.op("dve", lambda e: e.reciprocal(st.t[:, 1:2], st.t[:, 2:3]), reads=[st], writes=[st])

    def phase_B(self, x, ctx, AB, hxT, hcT):
        C = self.C; M = self.M
        C.push()
        xt = [C.sb("xt%d" % i, [P, D], F32) for i in range(2)]
        xn = [C.sb("xn%d" % i, [P, D], BF16) for i in range(2)]
        junk = C.sb("junkB", [P, D], BF16)
        sts = [C.sb("stB%d" % i, [P, 4], F32) for i in range(2)]
        hb = [C.sb("hbB%d" % i, [P, 32, 512], BF16) for i in range(2)]
        hview = hxT.t.ap().rearrange("(kc p) t -> p kc t", p=P)
        ev = 0
        for tt in range(NT + 2):
            isc = tt >= NT
            X = xt[tt % 2]; XN = xn[tt % 2]; st = sts[tt % 2]
            if isc:
                self.load("sp", X, X.t[:], ctx, ctx.t.ap()[(tt - NT) * P:(tt - NT + 1) * P, :])
            else:
                self.load("sp" if tt % 2 else "act", X, X.t[:], x, x.t.ap()[tt * P:(tt + 1) * P, :])
            self.rms_stats(X, junk, st, D)
            C.op("act", lambda e, X=X, XN=XN, st=st: e.activation(out=XN.t[:], in_=X.t[:], func=AF.Copy, scale=st.t[:, 1:2]), reads=[X, st], writes=[XN])
            a_i = 2 if isc else 0
            H = hb[(tt // 4) % 2]
            for g in range(4):
                pr = self.nextps()
                pv = self.psb(pr)
                def fn(e, XN=XN, pv=pv, g=g):
                    inst = None
                    for j in range(8):
                        kc = g * 8 + j
                        inst = e.transpose(pv[:, j * P:(j + 1) * P], XN.t[:, kc * P:(kc + 1) * P], self.identb.t[:])
                    return inst
                C.op("pe", fn, reads=[XN, self.identb], writes=[pr])
                for j in range(8):
                    kc = g * 8 + j
                    if isc:
                        dst, dap = hcT, hcT.t[:, kc, (tt - NT) * P:(tt - NT + 1) * P]
                    else:
                        dst, dap = H, H.t[:, kc, (tt % 4) * P:(tt % 4 + 1) * P]
                    ev += 1
                    if ev % 2:
                        C.op("dve", lambda e, dap=dap, pv=pv, j=j, kc=kc, a_i=a_i: e.tensor_scalar(out=dap, in0=pv[:, j * P:(j + 1) * P], scalar1=AB.t[:, a_i, kc:kc + 1], scalar2=AB.t[:, a_i + 1, kc:kc + 1], op0=ALU.mult, op1=ALU.add), reads=[pr, AB], writes=[dst])
                    else:
                        C.op("act", lambda e, dap=dap, pv=pv, j=j, kc=kc, a_i=a_i: e.activation(out=dap, in_=pv[:, j * P:(j + 1) * P], func=AF.Identity, scale=AB.t[:, a_i, kc:kc + 1], bias=AB.t[:, a_i + 1, kc:kc + 1]), reads=[pr, AB], writes=[dst])
            if (not isc) and tt % 4 == 3:
                tb = tt // 4
                self.store("sp", hxT, hview[:, :, tb * 512:(tb + 1) * 512], H, H.t[:])
        C.pop()

    def load_w_bf(self, wsrc, view, stg, wb):
        C = self.C
        for q in range(4):
            s = stg[self.stg_i % len(stg)]; self.stg_i += 1
            self.load("sp" if q % 2 else "act", s, s.t[:], wsrc, view[:, q * 8:(q + 1) * 8, :])
            self.copy(self.cast_eng(), wb, wb.t[:, q * 8:(q + 1) * 8, :], s, s.t[:])

    def rotary(self, pr, rt, rtap, dst, dap, tmps):
        C = self.C
        pv = pr.t[:, :].rearrange("p (h two f) -> p h two f", h=4, two=2)
        x1 = pv[:, :, 0, :]; x2 = pv[:, :, 1, :]
        cs = rtap[:, 0, :].rearrange("p (h f) -> p h f", h=4); sn = rtap[:, 1, :].rearrange("p (h f) -> p h f", h=4)
        t1, t2 = tmps
        t1v = t1.t[:, :].rearrange("p (h f) -> p h f", h=4); t2v = t2.t[:, :].rearrange("p (h f) -> p h f", h=4)
        dv = dap.rearrange("p (h two f) -> p h two f", h=4, two=2)
        C.op("dve", lambda e: e.tensor_tensor(out=t1v, in0=x1, in1=cs, op=ALU.mult), reads=[pr, rt], writes=[t1])
        C.op("dve", lambda e: e.tensor_tensor(out=t2v, in0=x2, in1=sn, op=ALU.mult), reads=[pr, rt], writes=[t2])
        C.op("dve", lambda e: e.tensor_tensor(out=dv[:, :, 0, :], in0=t1v, in1=t2v, op=ALU.subtract), reads=[t1, t2], writes=[dst])
        C.op("dve", lambda e: e.tensor_tensor(out=t1v, in0=x1, in1=sn, op=ALU.mult), reads=[pr, rt], writes=[t1])
        C.op("dve", lambda e: e.tensor_tensor(out=t2v, in0=x2, in1=cs, op=ALU.mult), reads=[pr, rt], writes=[t2])
        C.op("dve", lambda e: e.tensor_tensor(out=dv[:, :, 1, :], in0=t1v, in1=t2v, op=ALU.add), reads=[t1, t2], writes=[dst])

    def phase_C(self, hcT, w_in, rot_ck, rdec, lg, Sf, Sb):
        C = self.C; M = self.M
        C.push()
        rd = C.sb("rd", [P, 16], F32)
        self.load("sp", rd, rd.t[:], rdec, rdec.t.ap())
        C.op("act", lambda e: e.activation(out=rd.t[:], in_=rd.t[:], func=AF.Exp, scale=-1.0), reads=[rd], writes=[rd])
        C.op("act", lambda e: e.activation(out=rd.t[:], in_=rd.t[:], func=AF.Ln, bias=M("cols")[:, 7:8]), reads=[rd, self.mt], writes=[rd])
        C.op("dve", lambda e: e.tensor_scalar(out=lg.t[:], in0=rd.t[:], scalar1=-1.0, scalar2=None, op0=ALU.mult), reads=[rd], writes=[lg])
        wctx = C.sb("wctx", [P, 2, 16], F32)
        for tt in range(2):
            for d in range(2):
                col = 2 + tt if d == 0 else 4 + tt
                C.op("act", lambda e, tt=tt, d=d, col=col: e.activation(out=wctx.t[:, tt, d * 8:(d + 1) * 8], in_=lg.t[:, d * 8:(d + 1) * 8], func=AF.Exp, scale=M("cols")[:, col:col + 1]), reads=[lg, self.mt], writes=[wctx])
        import os
        cstop = int(os.environ.get("CSTOP", "9"))
        if cstop <= 1:
            C.pop(); return
        stg = [C.sb("stgC%d" % i, [P, 8, 512], F32) for i in range(3)]
        wb = [C.sb("wbC%d" % i, [P, 32, 512], BF16) for i in range(2)]
        kct = C.sb("kct", [P, 2, KC], BF16); vct = C.sb("vct", [P, 2, VC], BF16)
        rt = C.sb("rtC", [P, 2, 2, 256], F32)
        tm = [C.sb("tmC%d" % i, [P, 256], F32) for i in range(2)]
        self.load("sp", rt, rt.t[:].rearrange("p a b c -> p a (b c)"), rot_ck, rot_ck.t.ap().rearrange("(a p) c -> p a c", p=P))
        wv = w_in.t.ap().rearrange("(kc p) n -> p kc n", p=P)
        for nb in range(6):
            W = wb[nb % 2]
            self.load_w_bf(w_in, wv[:, :, KV0 + nb * 512:KV0 + (nb + 1) * 512], stg, W)
            for tt in range(2):
                pr = self.nextps()
                self.mm(pr, pr.t[:, :], [(hcT.t[:, kc, tt * P:(tt + 1) * P], W.t[:, kc, :]) for kc in range(32)], [hcT, W])
                if nb < 2 and cstop <= 2:
                    self.copy("act", kct, kct.t[:, tt, nb * 512:(nb + 1) * 512], pr, pr.t[:, :])
                elif nb < 2:
                    self.rotary(pr, rt, rt.t[:, tt], kct, kct.t[:, tt, nb * 512:(nb + 1) * 512], tm)
                else:
                    self.copy("act", vct, vct.t[:, tt, (nb - 2) * 512:(nb - 1) * 512], pr, pr.t[:, :])
        if cstop <= 3:
            C.pop(); return
        kd = [C.sb("kdC%d" % i, [P, P], BF16) for i in range(4)]
        ki = 0
        for d in range(2):
            for h in range(8):
                pr = self.nextps()
                pairs = []
                kds = []
                for tt in range(2):
                    k_ = kd[ki % 4]; ki += 1
                    C.op("dve", lambda e, k_=k_, tt=tt, h=h, d=d: e.tensor_scalar(out=k_.t[:], in0=kct.t[:, tt, h * P:(h + 1) * P], scalar1=wctx.t[:, tt, d * 8 + h:d * 8 + h + 1], scalar2=None, op0=ALU.mult), reads=[kct, wctx], writes=[k_])
                    pairs.append((k_.t[:], vct.t[:, tt, h * 256:(h + 1) * 256])); kds.append(k_)
                if cstop <= 4:
                    continue
                self.mm(pr, pr.t[:, 0:256], pairs, [vct, *kds])
                if cstop <= 5:
                    continue
                C.op("dve", lambda e, pr=pr, d=d, h=h: e.tensor_copy(Sf.t[:, d * 8 + h, :], pr.t[:, 0:256]), reads=[pr], writes=[Sf])
                if cstop <= 6:
                    continue
                C.op("pool", lambda e, d=d, h=h: e.tensor_copy(Sb.t[:, d * 8 + h, :], Sf.t[:, d * 8 + h, :]), reads=[Sf], writes=[Sb])
        C.pop()


    def phase_D(self, hxT, w_in, swT, rot_x, rot_xk, v_tm, x1_tm, x2_cm, sg, qT_d, kT_d, k_tm, vr_tm):
        C = self.C; M = self.M
        C.push()
        stg = [C.sb("stgD%d" % i, [P, 2, 512], F32) for i in range(3)]
        wb = [C.sb("wbD%d" % i, [P, 32, 512], BF16) for i in range(2)]
        hb = [C.sb("hbD%d" % i, [P, 32, 512], BF16) for i in range(2)]
        prow = [C.sb("prow%d" % i, [P, 4098], BF16) for i in range(4)]
        swt = C.sb("swt", [P, 48, 4], F32)
        self.load("sp", swt, swt.t[:].rearrange("p a b -> p (a b)"), swT, swT.t.ap())
        ctmp = C.sb("ctmp", [P, 1024], F32)
        uo = [C.sb("uo%d" % i, [P, 1024], BF16) for i in range(2)]
        utm = [C.sb("utm%d" % i, [P, 8, P], BF16) for i in range(2)]
        rt = [C.sb("rtD%d" % i, [P, 2, 256], F32) for i in range(2)]
        tm = [C.sb("tmD%d" % i, [P, 256], F32) for i in range(2)]
        qk = [C.sb("qk%d" % i, [P, 512], BF16) for i in range(2)]
        qkT = [C.sb("qkT%d" % i, [P, 4, P], BF16) for i in range(2)]
        ot = [C.sb("otD%d" % i, [P, 512], BF16) for i in range(2)]
        for pw in prow:
            C.op("dve", lambda e, pw=pw: e.memset(pw.t[:, 0:1], 0.0), writes=[pw])
            C.op("dve", lambda e, pw=pw: e.memset(pw.t[:, 4097:4098], 0.0), writes=[pw])
        wv = w_in.t.ap().rearrange("(kc p) n -> p kc n", p=P)
        hv = hxT.t.ap().rearrange("(kc p) t -> p kc t", p=P)
        groups = []
        for g in range(28):
            col0 = g * 512
            if g < 12: kind = "hy"
            elif g < 14: kind = "q"
            elif g < 22: kind = "gate"
            elif g < 24: kind = "k"
            else: kind = "v"
            groups.append((col0, kind))
        cnt = dict(stg=0, hb=0, uo=0, utm=0, rt=0, qk=0, qkT=0, ot=0, ev=0)

        def nxt(lst, key):
            r = lst[cnt[key] % len(lst)]; cnt[key] += 1; return r
        def wpieces(g, lo, hi):
            if g >= len(groups):
                return
            c0 = groups[g][0]; Wn = wb[g % 2]
            for q in range(lo, hi):
                s_ = nxt(stg, "stg")
                self.load("sp", s_, s_.t[:], w_in, wv[:, q * 2:(q + 1) * 2, c0:c0 + 512])
                self.copy(self.cast_eng(), Wn, Wn.t[:, q * 2:(q + 1) * 2, :], s_, s_.t[:])

        def hjob(k):
            def job():
                Hn = hb[k % 2]
                self.load("sp", Hn, Hn.t[:], hxT, hv[:, :, (k % 8) * 512:(k % 8 + 1) * 512])
                return Hn
            return job
        hst = Stream([hjob(k) for k in range(28 * 8)])
        wpieces(0, 0, 16)
        for gi, (col0, kind) in enumerate(groups):
            W = wb[gi % 2]
            for tb in range(8):
                H = hst.next()
                wpieces(gi + 1, tb * 2, tb * 2 + 2)
                if kind in ("hy", "gate"):
                    for ct in range(4):
                        pr = self.nextps()
                        self.mm(pr, pr.t[:, :], [(W.t[:, kc, ct * P:(ct + 1) * P], H.t[:, kc, :]) for kc in range(32)], [W, H])
                        if kind == "hy":
                            pw = prow[ct]
                            cnt["ev"] += 1
                            self.copy("act" if cnt["ev"] % 2 else "dve", pw, pw.t[:, 1 + tb * 512:1 + (tb + 1) * 512], pr, pr.t[:, :])
                        else:
                            O = nxt(ot, "ot")
                            C.op("act", lambda e, O=O, pr=pr: e.activation(out=O.t[:], in_=pr.t[:, :], func=AF.Silu), reads=[pr], writes=[O])
                            r0 = (col0 - 7168) + ct * P
                            self.store("sp", sg, sg.t.ap()[r0:r0 + P, tb * 512:(tb + 1) * 512], O, O.t[:])
                else:
                    for tt in range(4):
                        pr = self.nextps()
                        self.mm(pr, pr.t[:, :], [(H.t[:, kc, tt * P:(tt + 1) * P], W.t[:, kc, :]) for kc in range(32)], [W, H])
                        tok0 = tb * 512 + tt * P
                        if kind in ("q", "k"):
                            R = nxt(rt, "rt")
                            tab = rot_x if kind == "q" else rot_xk
                            self.load("sp", R, R.t[:].rearrange("p a b -> p (a b)"), tab, tab.t.ap()[tok0:tok0 + P, :])
                            Q = nxt(qk, "qk")
                            self.rotary(pr, R, R.t[:], Q, Q.t[:, :], tm)
                            if kind == "k":
                                c0 = col0 - 11264
                                self.store("sp", k_tm, k_tm.t.ap()[tok0:tok0 + P, c0:c0 + 512], Q, Q.t[:])
                                dstT = kT_d; hh0 = c0 // P
                            else:
                                dstT = qT_d; hh0 = (col0 - 6144) // P
                            pt = self.nextps(); pv = self.psb(pt)

                            def fnT(e, pv=pv, Q=Q):
                                inst = None
                                for h in range(4):
                                    inst = e.transpose(pv[:, h * P:(h + 1) * P], Q.t[:, h * P:(h + 1) * P], self.identb.t[:])
                                return inst
                            C.op("pe", fnT, reads=[Q, self.identb], writes=[pt])
                            QT = nxt(qkT, "qkT")
                            self.copy("act", QT, QT.t[:].rearrange("p a b -> p (a b)"), pt, pv[:, 0:512])
                            self.store("sp", dstT, dstT.t.ap().rearrange("(h p) t -> p h t", p=P)[:, hh0:hh0 + 4, tok0:tok0 + P], QT, QT.t[:])
                        else:
                            O = nxt(ot, "ot")
                            cnt["ev"] += 1
                            self.copy("act" if cnt["ev"] % 2 else "dve", O, O.t[:], pr, pr.t[:, :])
                            c0 = col0 - 12288
                            self.store("sp", vr_tm, vr_tm.t.ap()[tok0:tok0 + P, c0:c0 + 512], O, O.t[:])
            if kind == "hy":
                for ct in range(4):
                    colt = col0 // P + ct
                    pw = prow[ct]
                    for ch in range(4):
                        s0 = ch * 1024
                        C.op("dve", lambda e, pw=pw, s0=s0, colt=colt: e.tensor_scalar(out=ctmp.t[:], in0=pw.t[:, s0:s0 + 1024], scalar1=swt.t[:, colt, 0:1], scalar2=swt.t[:, colt, 3:4], op0=ALU.mult, op1=ALU.add), reads=[pw, swt], writes=[ctmp])
                        C.op("dve", lambda e, pw=pw, s0=s0, colt=colt: e.scalar_tensor_tensor(out=ctmp.t[:], in0=pw.t[:, s0 + 1:s0 + 1025], scalar=swt.t[:, colt, 1:2], in1=ctmp.t[:], op0=ALU.mult, op1=ALU.add), reads=[pw, swt, ctmp], writes=[ctmp])
                        U = nxt(uo, "uo")
                        C.op("dve", lambda e, pw=pw, s0=s0, colt=colt, U=U: e.scalar_tensor_tensor(out=U.t[:], in0=pw.t[:, s0 + 2:s0 + 1026], scalar=swt.t[:, colt, 2:3], in1=ctmp.t[:], op0=ALU.mult, op1=ALU.add), reads=[pw, swt, ctmp], writes=[U])
                        if col0 >= 4096:
                            r0 = (col0 - 4096) + ct * P
                            self.store("sp", x2_cm, x2_cm.t.ap()[r0:r0 + P, s0:s0 + 1024], U, U.t[:])
                        else:
                            dst = v_tm if col0 < 2048 else x1_tm
                            cc0 = (col0 % 2048) + ct * P
                            pt = self.nextps(); pv = self.psb(pt)

                            def fnU(e, pv=pv, U=U):
                                inst = None
                                for j in range(8):
                                    inst = e.transpose(pv[:, j * P:(j + 1) * P], U.t[:, j * P:(j + 1) * P], self.identb.t[:])
                                return inst
                            C.op("pe", fnU, reads=[U, self.identb], writes=[pt])
                            UT = nxt(utm, "utm")
                            cnt["ev"] += 1
                            self.copy("act" if cnt["ev"] % 2 else "pool" if False else "act", UT, UT.t[:].rearrange("p a b -> p (a b)"), pt, pv[:, 0:1024])
                            self.store("sp", dst, dst.t.ap().rearrange("(tt p) c -> p tt c", p=P)[:, ch * 8:(ch + 1) * 8, cc0:cc0 + P], UT, UT.t[:])
        C.pop()


    def slab_stream(self, cst, seq, slabs):
        def mk(k, mt):
            def job():
                SL = slabs[k % len(slabs)]
                self.load("sp", SL, SL.t[:].rearrange("p a b c -> p (a b c)"), cst, cst.t.ap()[mt])
                return SL
            return job
        return Stream([mk(k, mt) for k, mt in enumerate(seq)])

    def phase_E(self, fw1, fw2, fw3, fw4, fvec, zT, negt, deltab, hyb, cst, fscale, spec):
        C = self.C; M = self.M
        PI = math.pi
        C.push()
        h3 = C.sb("h3", [64, T], F32)
        fv = C.sb("fv", [64, 4], F32)
        self.load("sp", fv, fv.t[:], fvec, fvec.t.ap())
        arg = C.sb("argE", [64, 512], F32); mk = C.sb("mkE", [64, 512], F32)
        C.push()
        zt = C.sb("zt", [33, T], F32); hA = C.sb("hA", [64, T], F32); hB = C.sb("hB", [64, T], F32)
        w1 = C.sb("w1", [33, 64], F32); w2 = C.sb("w2", [64, 64], F32); w3 = C.sb("w3", [64, 64], F32)
        self.load("sp", zt, zt.t[:], zT, zT.t.ap())
        self.load("sp", w1, w1.t[:], fw1, fw1.t.ap()); self.load("sp", w2, w2.t[:], fw2, fw2.t.ap()); self.load("sp", w3, w3.t[:], fw3, fw3.t.ap())
        layers = [(zt, 33, w1, hA), (hA, 64, w2, hB), (hB, 64, w3, h3)]
        for li, (src, kk, w, dst) in enumerate(layers):
            for nb in range(8):
                pr = self.nextps()
                C.op("pe", lambda e, pr=pr, w=w, src=src, kk=kk, nb=nb: e.matmul(pr.t[0:64, :], w.t[0:kk, :], src.t[0:kk, nb * 512:(nb + 1) * 512], start=True, stop=True), reads=[w, src], writes=[pr])
                C.op("dve", lambda e, pr=pr, li=li: e.tensor_scalar(out=arg.t[:], in0=pr.t[0:64, :], scalar1=fv.t[:, li:li + 1], scalar2=fv.t[:, 3:4], op0=ALU.add, op1=ALU.mult), reads=[pr, fv], writes=[arg])
                C.op("dve", lambda e: e.tensor_scalar(out=mk.t[:], in0=arg.t[:], scalar1=PI, scalar2=-2.0 * PI, op0=ALU.is_gt, op1=ALU.mult), reads=[arg], writes=[mk])
                C.op("dve", lambda e: e.tensor_tensor(out=arg.t[:], in0=arg.t[:], in1=mk.t[:], op=ALU.add), reads=[arg, mk], writes=[arg])
                C.op("dve", lambda e: e.tensor_scalar(out=mk.t[:], in0=arg.t[:], scalar1=-PI, scalar2=2.0 * PI, op0=ALU.is_lt, op1=ALU.mult), reads=[arg], writes=[mk])
                C.op("dve", lambda e: e.tensor_tensor(out=arg.t[:], in0=arg.t[:], in1=mk.t[:], op=ALU.add), reads=[arg, mk], writes=[arg])
                C.op("act", lambda e, dst=dst, nb=nb: e.activation(out=dst.t[:, nb * 512:(nb + 1) * 512], in_=arg.t[:], func=AF.Sin), reads=[arg], writes=[dst])
        C.pop()
        w4 = C.sb("w4", [64, 2, 512], F32)
        ngt = C.sb("ngt", [P, NT], F32); delt = C.sb("delt", [P, HYW], F32); hybt = C.sb("hybt", [P, 512], F32); fsc = C.sb("fsc", [P, NFT], F32)
        self.load("sp", ngt, ngt.t[:], negt, negt.t.ap()); self.load("sp", delt, delt.t[:], deltab, deltab.t.ap())
        self.load("sp", fsc, fsc.t[:], fscale, fscale.t.ap())
        edt = C.sb("edt", [P, 32, 2, 512], BF16)
        slabs = [C.sb("slabE%d" % i, [P, 2, NFT, P], BF16) for i in range(2)]
        sst = self.slab_stream(cst, [mt for _ in range(8) for mt in range(NFT)], slabs)
        dect = C.sb("dect", [P, 512], F32)
        tf = [C.sb("tfE%d" % i, [P, 512], F32) for i in range(2)]; tb = [C.sb("tbE%d" % i, [P, 512], F32) for i in range(2)]
        af = C.sb("afE", [P, 512], F32); ab = [C.sb("abE%d" % i, [P, 512], F32) for i in range(2)]
        rn = C.sb("rnE", [P, 512], F32)
        stt = [C.sb("stE%d" % i, [P, 2, 512], F32) for i in range(2)]
        pacc = self.pacc
        specv = spec.t.ap().rearrange("k (two c) -> k two c", two=2)
        si = 0
        for o in range(2):
            for cb in range(4):
                colF = o * 2048 + cb * 512; colB = 4096 + colF
                self.load("sp", hybt, hybt.t[:], hyb, hyb.t.ap()[:, colF:colF + 512])
                self.load("sp", w4, w4.t[:, 0, :], fw4, fw4.t.ap()[:, colF:colF + 512])
                self.load("sp", w4, w4.t[:, 1, :], fw4, fw4.t.ap()[:, colB:colB + 512])
                for lt in range(NT):
                    TF = tf[lt % 2]; TB = tb[lt % 2]; AB_ = ab[lt % 2]
                    prF = self.nextps(); prB = self.nextps()
                    C.op("pe", lambda e, prF=prF, lt=lt, colF=colF: e.matmul(prF.t[:, :], h3.t[:, lt * P:(lt + 1) * P], w4.t[:, 0, :], start=True, stop=True), reads=[h3, w4], writes=[prF])
                    C.op("pe", lambda e, prB=prB, lt=lt, colB=colB: e.matmul(prB.t[:, :], h3.t[:, lt * P:(lt + 1) * P], w4.t[:, 1, :], start=True, stop=True), reads=[h3, w4], writes=[prB])
                    C.op("act", lambda e, lt=lt, cb=cb: e.activation(out=dect.t[:], in_=delt.t[:, cb * 512:(cb + 1) * 512], func=AF.Exp, scale=ngt.t[:, lt:lt + 1]), reads=[delt, ngt], writes=[dect])
                    C.op("dve", lambda e, TF=TF, prF=prF: e.tensor_tensor(out=TF.t[:], in0=prF.t[:, :], in1=dect.t[:], op=ALU.mult), reads=[prF, dect], writes=[TF])
                    C.op("dve", lambda e, TB=TB, prB=prB: e.tensor_tensor(out=TB.t[:], in0=prB.t[:, :], in1=dect.t[:], op=ALU.mult), reads=[prB, dect], writes=[TB])
                    if lt == 0:
                        C.op("dve", lambda e, TB=TB: e.memset(TB.t[0:1, :], 0.0), reads=[TB], writes=[TB])
                    C.op("pool", lambda e, TF=TF, TB=TB, lt=lt: e.tensor_tensor(out=edt.t[:, lt, 0, :], in0=TF.t[:], in1=TB.t[:], op=ALU.add), reads=[TF, TB], writes=[edt])
                    C.op("pool", lambda e, TF=TF, TB=TB, lt=lt: e.tensor_tensor(out=edt.t[:, lt, 1, :], in0=TF.t[:], in1=TB.t[:], op=ALU.subtract), reads=[TF, TB], writes=[edt])
                    C.op("act", lambda e, TF=TF: e.activation(out=af.t[:], in_=TF.t[:], func=AF.Abs), reads=[TF], writes=[af])
                    C.op("act", lambda e, TB=TB, AB_=AB_: e.activation(out=AB_.t[:], in_=TB.t[:], func=AF.Abs), reads=[TB], writes=[AB_])
                    C.op("pool", lambda e, AB_=AB_: e.tensor_tensor(out=AB_.t[:], in0=AB_.t[:], in1=af.t[:], op=ALU.add), reads=[AB_, af], writes=[AB_])
                    C.op("pe", lambda e, AB_=AB_, lt=lt: e.matmul(pacc.t[:, :], M("ones"), AB_.t[:], start=(lt == 0), stop=(lt == NT - 1)), reads=[AB_, self.mt], writes=[pacc])
                C.op("dve", lambda e: e.reciprocal(rn.t[:], pacc.t[:, :]), reads=[pacc], writes=[rn])
                for mt in range(NFT):
                    SL = sst.next()
                    prR = self.nextps(); prW = self.nextps()
                    self.mm(prR, prR.t[:, :], [(SL.t[:, 0, kt, :], edt.t[:, kt, 0, :]) for kt in range(NT)], [SL, edt])
                    self.mm(prW, prW.t[:, :], [(SL.t[:, 1, kt, :], edt.t[:, kt, 1, :]) for kt in range(NT)], [SL, edt])
                    ST = stt[si % 2]; si += 1
                    C.op("dve", lambda e, ST=ST, prR=prR: e.tensor_tensor(out=ST.t[:, 0, :], in0=prR.t[:, :], in1=rn.t[:], op=ALU.mult), reads=[prR, rn], writes=[ST])
                    C.op("dve", lambda e, ST=ST, colF=colF: e.tensor_tensor(out=ST.t[:, 0, :], in0=ST.t[:, 0, :], in1=hybt.t[:], op=ALU.add), reads=[ST, hybt], writes=[ST])
                    C.op("dve", lambda e, ST=ST, prW=prW: e.tensor_tensor(out=ST.t[:, 1, :], in0=prW.t[:, :], in1=rn.t[:], op=ALU.mult), reads=[prW, rn], writes=[ST])
                    C.op("pool", lambda e, ST=ST, mt=mt: e.tensor_scalar(out=ST.t[:].rearrange("p a b -> p (a b)"), in0=ST.t[:].rearrange("p a b -> p (a b)"), scalar1=fsc.t[:, mt:mt + 1], scalar2=None, op0=ALU.mult), reads=[ST, fsc], writes=[ST])
                    self.store("sp", spec, specv[mt * P:(mt + 1) * P, :, colF:colF + 512], ST, ST.t[:])
        C.pop()

    def phase_F(self, v_tm, x1_tm, x2_cm, spec, cst, yT):
        C = self.C; M = self.M
        C.push()
        utm = C.sb("utmF", [P, NT, 512], BF16); x1t = C.sb("x1tF", [P, NT, 512], BF16)
        Y = C.sb("YF", [P, NFT, 2, 512], BF16)
        slabs = [C.sb("slabF%d" % i, [P, 2, NFT, P], BF16) for i in range(2)]
        spt = [C.sb("sptF%d" % i, [P, 2, 512], F32) for i in range(2)]
        sst = self.slab_stream(cst, [m_ for _ in range(8) for m_ in (list(range(NFT)) + list(range(NT)))], slabs)

        def spjob(k):
            cb_, rem = divmod(k, 2 * NFT); o_, mt_ = divmod(rem, NFT)
            c0_ = o_ * 2048 + cb_ * 512

            def job():
                SPn = spt[k % 2]
                self.load("sp", SPn, SPn.t[:], spec, specv[mt_ * P:(mt_ + 1) * P, :, c0_:c0_ + 512])
                return SPn
            return job
        t4 = [C.sb("t4F%d" % i, [P, 512], F32) for i in range(4)]
        yb = [C.sb("ybF%d" % i, [P, 512], BF16) for i in range(2)]
        x2t = [C.sb("x2tF%d" % i, [P, 4, P], BF16) for i in range(2)]
        yo = [C.sb("yoF%d" % i, [P, 4, P], BF16) for i in range(2)]
        specv = spec.t.ap().rearrange("k (two c) -> k two c", two=2)
        cnt = dict(sp=0, yb=0, x2=0, yo=0)
        spst = Stream([spjob(k) for k in range(4 * 2 * NFT)])

        def conv(o, cb, cbfn):
            c0 = o * 2048 + cb * 512
            for mt in range(NFT):
                SL = sst.next()
                prR = self.nextps(); prI = self.nextps()
                self.mm(prR, prR.t[:, :], [(SL.t[:, 0, kt, :], utm.t[:, kt, :]) for kt in range(NT)], [SL, utm])
                self.mm(prI, prI.t[:, :], [(SL.t[:, 1, kt, :], utm.t[:, kt, :]) for kt in range(NT)], [SL, utm])
                SP = spst.next()
                a, b_, c_, d_ = t4
                C.op("dve", lambda e, prR=prR, SP=SP: e.tensor_tensor(out=a.t[:], in0=prR.t[:, :], in1=SP.t[:, 0, :], op=ALU.mult), reads=[prR, SP], writes=[a])
                C.op("dve", lambda e, prI=prI, SP=SP: e.tensor_tensor(out=b_.t[:], in0=prI.t[:, :], in1=SP.t[:, 1, :], op=ALU.mult), reads=[prI, SP], writes=[b_])
                C.op("pool", lambda e, mt=mt: e.tensor_tensor(out=Y.t[:, mt, 0, :], in0=a.t[:], in1=b_.t[:], op=ALU.subtract), reads=[a, b_], writes=[Y])
                C.op("dve", lambda e, prR=prR, SP=SP: e.tensor_tensor(out=c_.t[:], in0=prR.t[:, :], in1=SP.t[:, 1, :], op=ALU.mult), reads=[prR, SP], writes=[c_])
                C.op("dve", lambda e, prI=prI, SP=SP: e.tensor_tensor(out=d_.t[:], in0=prI.t[:, :], in1=SP.t[:, 0, :], op=ALU.mult), reads=[prI, SP], writes=[d_])
                C.op("pool", lambda e, mt=mt: e.tensor_tensor(out=Y.t[:, mt, 1, :], in0=c_.t[:], in1=d_.t[:], op=ALU.add), reads=[c_, d_], writes=[Y])
            for it in range(NT):
                SL = sst.next()
                pr = self.nextps()
                self.mm(pr, pr.t[:, :], [(SL.t[:, 0, kt, :], Y.t[:, kt, 0, :]) for kt in range(NFT)] + [(SL.t[:, 1, kt, :], Y.t[:, kt, 1, :]) for kt in range(NFT)], [SL, Y])
                cbfn(it, pr)

        for cb in range(4):
            self.load("sp", utm, utm.t[:], v_tm, v_tm.t.ap().rearrange("(tt p) c -> p tt c", p=P)[:, :, cb * 512:(cb + 1) * 512])
            self.load("act", x1t, x1t.t[:], x1_tm, x1_tm.t.ap().rearrange("(tt p) c -> p tt c", p=P)[:, :, cb * 512:(cb + 1) * 512])

            def f1(it, pr):
                C.op("dve", lambda e: e.tensor_tensor(out=utm.t[:, it, :], in0=pr.t[:, :], in1=x1t.t[:, it, :], op=ALU.mult), reads=[pr, x1t], writes=[utm])

            def f2(it, pr, cb=cb):
                YB = yb[cnt["yb"] % 2]; cnt["yb"] += 1
                self.copy("act", YB, YB.t[:], pr, pr.t[:, :])
                pt = self.nextps(); pv = self.psb(pt)

                def fnT(e):
                    inst = None
                    for j in range(4):
                        inst = e.transpose(pv[:, j * P:(j + 1) * P], YB.t[:, j * P:(j + 1) * P], self.identb.t[:])
                    return inst
                C.op("pe", fnT, reads=[YB, self.identb], writes=[pt])
                X2 = x2t[cnt["x2"] % 2]; cnt["x2"] += 1
                self.load("sp", X2, X2.t[:], x2_cm, x2_cm.t.ap().rearrange("(j p) t -> p j t", p=P)[:, cb * 4:(cb + 1) * 4, it * P:(it + 1) * P])
                YO = yo[cnt["yo"] % 2]; cnt["yo"] += 1
                C.op("dve", lambda e: e.tensor_tensor(out=YO.t[:].rearrange("p a b -> p (a b)"), in0=pv[:, 0:512], in1=X2.t[:].rearrange("p a b -> p (a b)"), op=ALU.mult), reads=[pt, X2], writes=[YO])
                self.store("sp", yT, yT.t.ap().rearrange("(j p) t -> p j t", p=P)[:, cb * 4:(cb + 1) * 4, it * P:(it + 1) * P], YO, YO.t[:])
            conv(0, cb, f1)
            conv(1, cb, f2)
        C.pop()


    def phase_G(self, qT_d, kT_d, k_tm, vr_tm, sg, Sfd, lg, yT):
        C = self.C; M = self.M
        C.push()
        Sf = [C.sb("SfG%d" % i, [P, 256], F32) for i in range(16)]
        Sb = [C.sb("SbG%d" % i, [P, 256], BF16) for i in range(16)]
        for i in range(16):
            self.load("sp" if i % 2 else "act", Sf[i], Sf[i].t[:], Sfd, Sfd.t.ap()[:, i * 256:(i + 1) * 256])
            C.op("pool", lambda e, i=i: e.tensor_copy(Sb[i].t[:], Sf[i].t[:]), reads=[Sf[i]], writes=[Sb[i]])
        decT = C.sb("decT", [P, 16, P], F32); qdec = C.sb("qdec", [P, 16, P], F32)
        kdec = C.sb("kdec", [P, 16], F32); cdec = C.sb("cdec", [P, 16], F32)
        for d in range(2):
            for h in range(8):
                dh = d * 8 + h
                C.op("act", lambda e, d=d, dh=dh: e.activation(out=decT.t[:, dh, :], in_=M("diffF" if d == 0 else "diffB"), func=AF.Exp, scale=lg.t[:, dh:dh + 1]), reads=[lg, self.mt], writes=[decT])
                C.op("dve", lambda e, d=d, dh=dh: e.tensor_tensor(out=decT.t[:, dh, :], in0=decT.t[:, dh, :], in1=M("maskF" if d == 0 else "maskB"), op=ALU.mult), reads=[decT, self.mt], writes=[decT])
                C.op("act", lambda e, d=d, dh=dh: e.activation(out=qdec.t[:, dh, :], in_=M("ip1" if d == 0 else "i128m"), func=AF.Exp, scale=lg.t[:, dh:dh + 1]), reads=[lg, self.mt], writes=[qdec])
            C.op("act", lambda e, d=d: e.activation(out=kdec.t[:, d * 8:(d + 1) * 8], in_=lg.t[:, d * 8:(d + 1) * 8], func=AF.Exp, scale=M("cols")[:, d:d + 1]), reads=[lg, self.mt], writes=[kdec])
        C.op("act", lambda e: e.activation(out=cdec.t[:], in_=lg.t[:], func=AF.Exp, scale=128.0), reads=[lg], writes=[cdec])
        nb_ = 3
        qTc = [[C.sb("qTc%d%d" % (d, i), [P, 8, P], BF16) for i in range(nb_)] for d in range(2)]
        kTc = [[C.sb("kTc%d%d" % (d, i), [P, 8, P], BF16) for i in range(nb_)] for d in range(2)]
        ktm = [[C.sb("ktm%d%d" % (d, i), [P, KC], BF16) for i in range(nb_)] for d in range(2)]
        vtm = [[C.sb("vtm%d%d" % (d, i), [P, VC], BF16) for i in range(nb_)] for d in range(2)]
        sgc = [[C.sb("sgc%d%d" % (d, i), [P, 16, P], BF16) for i in range(nb_)] for d in range(2)]
        ytl = [[C.sb("ytl%d%d" % (d, i), [P, 16, P], BF16) for i in range(nb_)] for d in range(2)]
        qv = qT_d.t.ap().rearrange("(h p) t -> p h t", p=P); kv = kT_d.t.ap().rearrange("(h p) t -> p h t", p=P)
        sgv = sg.t.ap().rearrange("(ha p) t -> p ha t", p=P); yv = yT.t.ap().rearrange("(ha p) t -> p ha t", p=P)
        attm = [C.sb("attmP%d" % i, [P, P], BF16) for i in range(4)]
        qd = [C.sb("qdP%d" % i, [P, P], BF16) for i in range(4)]
        kd = [C.sb("kdP%d" % i, [P, P], BF16) for i in range(4)]
        sq = [C.sb("sqP%d" % i, [P, 256], BF16) for i in range(3)]
        sd = [C.sb("sdP%d" % i, [P, P], F32) for i in range(3)]
        rs = [C.sb("rsP%d" % i, [P, P], F32) for i in range(3)]
        on = [C.sb("onP%d" % i, [P, 256], F32) for i in range(3)]
        items = []
        for s_ in range(NT):
            for d in range(2):
                c = s_ if d == 0 else NT - 1 - s_
                for h in range(8):
                    items.append(dict(s=s_, d=d, c=c, h=h, dh=d * 8 + h, n=len(items)))

        def stage_load(it):
            d = it["d"]; bi = it["s"] % nb_; c = it["c"]
            bufs = dict(Q=qTc[d][bi], Kt=kTc[d][bi], KM=ktm[d][bi], V=vtm[d][bi], G=sgc[d][bi], YT=ytl[d][bi])
            if it["h"] == 0:
                tk = slice(c * P, (c + 1) * P)
                self.load("sp", bufs["Q"], bufs["Q"].t[:], qT_d, qv[:, :, tk]); self.load("sp", bufs["Kt"], bufs["Kt"].t[:], kT_d, kv[:, :, tk])
                self.load("sp", bufs["KM"], bufs["KM"].t[:], k_tm, k_tm.t.ap()[tk, :]); self.load("sp", bufs["V"], bufs["V"].t[:], vr_tm, vr_tm.t.ap()[tk, :])
                self.load("sp", bufs["G"], bufs["G"].t[:], sg, sgv[:, d * 16:(d + 1) * 16, tk])
            it.update(bufs)

        def stage_a(it):
            h = it["h"]; dh = it["dh"]; n = it["n"]
            Q = it["Q"]; Kt = it["Kt"]; KM_ = it["KM"]
            AT = attm[n % 4]; QD = qd[n % 4]; KD = kd[n % 4]
            it.update(AT=AT, QD=QD, KD=KD)
            pa = self.nextps()
            self.mm(pa, pa.t[:, 0:P], [(Kt.t[:, h, :], Q.t[:, h, :])], [Kt, Q])
            C.op("dve", lambda e: e.tensor_tensor(out=AT.t[:], in0=pa.t[:, 0:P], in1=decT.t[:, dh, :], op=ALU.mult), reads=[pa, decT], writes=[AT])
            C.op("pool", lambda e: e.tensor_tensor(out=QD.t[:], in0=Q.t[:, h, :], in1=qdec.t[:, dh, :], op=ALU.mult), reads=[Q, qdec], writes=[QD])
            C.op("pool", lambda e: e.tensor_scalar(out=KD.t[:], in0=KM_.t[:, h * P:(h + 1) * P], scalar1=kdec.t[:, dh:dh + 1], scalar2=None, op0=ALU.mult), reads=[KM_, kdec], writes=[KD])

        def stage_b(it):
            h = it["h"]; dh = it["dh"]; n = it["n"]; V = it["V"]; AT = it["AT"]; QD = it["QD"]; KD = it["KD"]
            SQ = sq[n % 3]
            po = self.nextps()
            it.update(po=po, SQ=SQ)

            def fo(e):
                inst = None
                for a_ in range(2):
                    e.matmul(po.t[:, a_ * P:(a_ + 1) * P], V.t[:, h * 256 + a_ * P:h * 256 + (a_ + 1) * P], AT.t[:], start=True, stop=False)
                    inst = e.matmul(po.t[:, a_ * P:(a_ + 1) * P], Sb[dh].t[:, a_ * P:(a_ + 1) * P], QD.t[:], start=False, stop=True)
                return inst
            C.op("pe", fo, reads=[V, AT, QD, Sb[dh]], writes=[po])
            C.op("act", lambda e: e.activation(out=SQ.t[:], in_=po.t[:, 0:256], func=AF.Square), reads=[po], writes=[SQ])
            psn = self.nextps()
            self.mm(psn, psn.t[:, 0:256], [(KD.t[:], V.t[:, h * 256:(h + 1) * 256])], [KD, V])
            C.op("dve", lambda e: e.scalar_tensor_tensor(out=Sf[dh].t[:], in0=Sf[dh].t[:], scalar=cdec.t[:, dh:dh + 1], in1=psn.t[:, 0:256], op0=ALU.mult, op1=ALU.add), reads=[Sf[dh], cdec, psn], writes=[Sf[dh]])
            C.op("pool", lambda e: e.tensor_copy(Sb[dh].t[:], Sf[dh].t[:]), reads=[Sf[dh]], writes=[Sb[dh]])

        def stage_c(it):
            h = it["h"]; n = it["n"]; po = it["po"]; SQ = it["SQ"]; G = it["G"]; YT = it["YT"]
            SD = sd[n % 3]; RS = rs[n % 3]; ON = on[n % 3]
            pss = self.nextps()
            self.mm(pss, pss.t[:, 0:P], [(self.onesb.t[:], SQ.t[:, 0:P]), (self.onesb.t[:], SQ.t[:, P:256])], [self.onesb, SQ])
            C.op("act", lambda e: e.activation(out=SD.t[:], in_=pss.t[:, 0:P], func=AF.Sqrt, scale=1.0 / 256.0, bias=M("cols")[:, 6:7]), reads=[pss, self.mt], writes=[SD])
            C.op("dve", lambda e: e.reciprocal(RS.t[:], SD.t[:]), reads=[SD], writes=[RS])
            for a_ in range(2):
                C.op("dve", lambda e, a_=a_: e.tensor_tensor(out=ON.t[:, a_ * P:(a_ + 1) * P], in0=po.t[:, a_ * P:(a_ + 1) * P], in1=RS.t[:], op=ALU.mult), reads=[po, RS], writes=[ON])
            C.op("pool", lambda e: e.tensor_tensor(out=YT.t[:, h * 2:h * 2 + 2, :], in0=ON.t[:].rearrange("p (a b) -> p a b", a=2), in1=G.t[:, h * 2:h * 2 + 2, :], op=ALU.mult), reads=[ON, G], writes=[YT])
            if h == 7:
                tk = slice(it["c"] * P, (it["c"] + 1) * P)
                self.store("sp", yT, yv[:, 16 + it["d"] * 16:16 + (it["d"] + 1) * 16, tk], YT, YT.t[:])
        NI = len(items)
        for i in range(NI + 2):
            if i < NI:
                stage_load(items[i]); stage_a(items[i])
            if 0 <= i - 1 < NI:
                stage_b(items[i - 1])
            if 0 <= i - 2 < NI:
                stage_c(items[i - 2])
        C.pop()


    def phase_H(self, yT, w_out, x, bcd, x1r, hx2, rwT, aff_tm):
        C = self.C; M = self.M
        yv = yT.t.ap().rearrange("(kc p) t -> p kc t", p=P)
        C.push()
        ya = [C.sb("yaH%d" % i, [P, 16, 512], BF16) for i in range(2)]
        yb_ = [C.sb("ybH%d" % i, [P, 16, 512], BF16) for i in range(2)]
        for tb in range(8):
            A_ = ya[tb % 2]; B_ = yb_[tb % 2]; ts_ = slice(tb * 512, (tb + 1) * 512)
            self.load("sp", A_, A_.t[:], yT, yv[:, 16:32, ts_]); self.load("sp", B_, B_.t[:], yT, yv[:, 32:48, ts_])
            C.op("dve", lambda e, A_=A_, B_=B_: e.tensor_tensor(out=A_.t[:].rearrange("p a b -> p (a b)"), in0=A_.t[:].rearrange("p a b -> p (a b)"), in1=B_.t[:].rearrange("p a b -> p (a b)"), op=ALU.add), reads=[A_, B_], writes=[A_])
            self.store("sp", yT, yv[:, 16:32, ts_], A_, A_.t[:])
        C.pop()
        C.push()
        stg = [C.sb("stgH%d" % i, [P, 8, 512], F32) for i in range(2)]
        wb = [C.sb("wbH%d" % i, [P, 32, 512], BF16) for i in range(2)]
        g1s = [C.sb("g1s%d" % i, [P, 512], F32) for i in range(2)]
        yh = [C.sb("yh%d" % i, [P, 32, 512], BF16) for i in range(2)]
        xs_ = [C.sb("xsH%d" % i, [P, 4, 512], F32) for i in range(2)]
        ob = [C.sb("obH%d" % i, [P, 4, 512], F32) for i in range(2)]
        wv = w_out.t.ap().rearrange("(kc p) n -> p kc n", p=P)
        xv = x.t.ap().rearrange("(tt p) c -> p tt c", p=P); ov = x1r.t.ap().rearrange("(tt p) c -> p tt c", p=P)
        sgi = [0]

        def wpieces(db, lo, hi):
            if db >= 8:
                return
            Wn = wb[db % 2]
            for q in range(lo, hi):
                s_ = stg[sgi[0] % 2]; sgi[0] += 1
                self.load("sp", s_, s_.t[:], w_out, wv[:, q * 8:(q + 1) * 8, db * 512:(db + 1) * 512])
                self.copy(self.cast_eng(), Wn, Wn.t[:, q * 8:(q + 1) * 8, :], s_, s_.t[:])
            if hi == 4:
                Gn = g1s[db % 2]
                self.load("sp", Gn, Gn.t[:], bcd, bcd.t.ap()[:, db * 512:(db + 1) * 512])

        def yjob(k):
            def job():
                db_, tb_ = divmod(k, 8)
                Yn = yh[k % 2]; Xn = xs_[k % 2]
                self.load("sp", Yn, Yn.t[:], yT, yv[:, 0:32, tb_ * 512:(tb_ + 1) * 512])
                self.load("sp", Xn, Xn.t[:], x, xv[:, tb_ * 4:(tb_ + 1) * 4, db_ * 512:(db_ + 1) * 512])
                return (Yn, Xn)
            return job
        yst = Stream([yjob(k) for k in range(64)])
        wpieces(0, 0, 4)
        n = 0
        for db in range(8):
            W = wb[db % 2]; G1 = g1s[db % 2]
            for tb in range(8):
                Y, X = yst.next()
                if tb % 2 == 0:
                    wpieces(db + 1, tb // 2, tb // 2 + 1)
                O = ob[n % 2]; n += 1
                for tt in range(4):
                    pr = self.nextps()
                    self.mm(pr, pr.t[:, :], [(Y.t[:, kc, tt * P:(tt + 1) * P], W.t[:, kc, :]) for kc in range(32)], [Y, W])
                    C.op("dve", lambda e, O=O, pr=pr, G1=G1, tt=tt: e.tensor_tensor(out=O.t[:, tt, :], in0=pr.t[:, :], in1=G1.t[:], op=ALU.mult), reads=[pr, G1], writes=[O])
                C.op("dve", lambda e, O=O, X=X: e.tensor_tensor(out=O.t[:].rearrange("p a b -> p (a b)"), in0=O.t[:].rearrange("p a b -> p (a b)"), in1=X.t[:].rearrange("p a b -> p (a b)"), op=ALU.add), reads=[O, X], writes=[O])
                self.store("sp", x1r, ov[:, tb * 4:(tb + 1) * 4, db * 512:(db + 1) * 512], O, O.t[:])
        C.pop()
        C.push()
        A2 = C.sb("A2b", [P, D], F32); B2 = C.sb("B2b", [P, D], F32)
        self.load("sp", A2, A2.t[:], bcd, bcd.t.ap()[:, 2 * D:3 * D]); self.load("act", B2, B2.t[:], bcd, bcd.t.ap()[:, 3 * D:4 * D])
        rw = C.sb("rw", [P, 32, NE], F32)
        self.load("sp", rw, rw.t[:].rearrange("p a b -> p (a b)"), rwT, rwT.t.ap())
        xt = [C.sb("xtH%d" % i, [P, D], F32) for i in range(2)]
        h2 = [C.sb("h2H%d" % i, [P, D], F32) for i in range(2)]
        hb16 = [C.sb("hb16%d" % i, [P, D], BF16) for i in range(2)]
        junk = C.sb("junkH", [P, D], BF16)
        sts = [C.sb("stH%d" % i, [P, 4], F32) for i in range(2)]
        h2T = [C.sb("h2T%d" % i, [P, 32, P], F32) for i in range(2)]
        sm = [C.sb("smH%d" % i, [P, 4], F32) for i in range(2)]
        ex = [C.sb("exH%d" % i, [P, NE], F32) for i in range(2)]
        for tt in range(NT):
            X = xt[tt % 2]; H2 = h2[tt % 2]; HB = hb16[tt % 2]; st = sts[tt % 2]; HT = h2T[tt % 2]; SM = sm[tt % 2]; EX = ex[tt % 2]
            tk = slice(tt * P, (tt + 1) * P)
            self.load("sp" if tt % 2 else "act", X, X.t[:], x1r, x1r.t.ap()[tk, :])
            self.rms_stats(X, junk, st, D)
            C.op("dve", lambda e, H2=H2, X=X, st=st: e.scalar_tensor_tensor(out=H2.t[:], in0=X.t[:], scalar=st.t[:, 1:2], in1=A2.t[:], op0=ALU.mult, op1=ALU.mult), reads=[X, st, A2], writes=[H2])
            C.op("pool", lambda e, H2=H2: e.tensor_tensor(out=H2.t[:], in0=H2.t[:], in1=B2.t[:], op=ALU.add), reads=[H2, B2], writes=[H2])
            C.op("act", lambda e, HB=HB, H2=H2: e.activation(out=HB.t[:], in_=H2.t[:], func=AF.Copy), reads=[H2], writes=[HB])
            self.store("sp", hx2, hx2.t.ap()[tk, :], HB, HB.t[:])
            for g in range(8):
                pt = self.nextps()

                def fnT(e, pt=pt, H2=H2, g=g):
                    inst = None
                    for j in range(4):
                        kc = g * 4 + j
                        inst = e.transpose(pt.t[:, j * P:(j + 1) * P], H2.t[:, kc * P:(kc + 1) * P], M("ident"))
                    return inst
                C.op("pe", fnT, reads=[H2, self.mt], writes=[pt])
                self.copy("act" if g % 2 else "dve", HT, HT.t[:, g * 4:(g + 1) * 4, :].rearrange("p a b -> p (a b)"), pt, pt.t[:, :])
            pl = self.nextps()
            self.mm(pl, pl.t[:, 0:NE], [(HT.t[:, kc, :], rw.t[:, kc, :]) for kc in range(32)], [HT, rw])
            C.op("dve", lambda e, SM=SM, pl=pl: e.tensor_reduce(out=SM.t[:, 0:1], in_=pl.t[:, 0:NE], axis=mybir.AxisListType.X, op=ALU.max), reads=[pl], writes=[SM])
            C.op("dve", lambda e, SM=SM: e.tensor_scalar(out=SM.t[:, 1:2], in0=SM.t[:, 0:1], scalar1=-1.0, scalar2=None, op0=ALU.mult), reads=[SM], writes=[SM])
            C.op("act", lambda e, EX=EX, pl=pl, SM=SM: e.activation(out=EX.t[:], in_=pl.t[:, 0:NE], func=AF.Exp, bias=SM.t[:, 1:2], accum_out=SM.t[:, 2:3]), reads=[pl, SM], writes=[EX, SM])
            C.op("dve", lambda e, SM=SM: e.reciprocal(SM.t[:, 3:4], SM.t[:, 2:3]), reads=[SM], writes=[SM])
            C.op("dve", lambda e, EX=EX, SM=SM, tt=tt: e.tensor_scalar(out=aff_tm.t[:, tt, :], in0=EX.t[:], scalar1=SM.t[:, 3:4], scalar2=None, op0=ALU.mult), reads=[EX, SM], writes=[aff_tm])
        C.pop()


    def phase_I(self, aff_tm, idx_all, gate_all):
        C = self.C; M = self.M
        C.push()
        affT = C.sb("affT", [NE, T], F32); junk = C.sb("junkI", [NE, T], F32)
        for g in range(8):
            pt = self.nextps()

            def fnT(e, pt=pt, g=g):
                inst = None
                for j in range(4):
                    tt = g * 4 + j
                    inst = e.transpose(pt.t[0:NE, j * P:(j + 1) * P], aff_tm.t[:, tt, :], M("ident"))
                return inst
            C.op("pe", fnT, reads=[aff_tm, self.mt], writes=[pt])
            self.copy("dve", affT, affT.t[:, g * 512:(g + 1) * 512], pt, pt.t[0:NE, :])
        bs = C.sb("bsI", [NE, 8], F32)
        C.op("dve", lambda e: e.memset(bs.t[:, 0:1], 0.0), writes=[bs])
        C.op("dve", lambda e: e.memset(bs.t[:, 1:2], 1.0), reads=[bs], writes=[bs])
        for it in range(34):
            C.op("dve", lambda e: e.tensor_scalar(out=bs.t[:, 2:3], in0=bs.t[:, 0:1], scalar1=bs.t[:, 1:2], scalar2=0.5, op0=ALU.add, op1=ALU.mult), reads=[bs], writes=[bs])
            C.op("dve", lambda e: e.tensor_scalar(out=junk.t[:], in0=affT.t[:], scalar1=bs.t[:, 2:3], scalar2=0.0, op0=ALU.is_ge, op1=ALU.add, accum_out=bs.t[:, 3:4]), reads=[affT, bs], writes=[junk, bs])
            C.op("dve", lambda e: e.tensor_scalar(out=bs.t[:, 4:5], in0=bs.t[:, 3:4], scalar1=float(CAP) - 0.5, scalar2=None, op0=ALU.is_gt), reads=[bs], writes=[bs])
            C.op("dve", lambda e: e.tensor_tensor(out=bs.t[:, 5:6], in0=bs.t[:, 2:3], in1=bs.t[:, 0:1], op=ALU.subtract), reads=[bs], writes=[bs])
            C.op("dve", lambda e: e.tensor_tensor(out=bs.t[:, 6:7], in0=bs.t[:, 1:2], in1=bs.t[:, 2:3], op=ALU.subtract), reads=[bs], writes=[bs])
            C.op("dve", lambda e: e.scalar_tensor_tensor(out=bs.t[:, 0:1], in0=bs.t[:, 5:6], scalar=bs.t[:, 4:5], in1=bs.t[:, 0:1], op0=ALU.mult, op1=ALU.add), reads=[bs], writes=[bs])
            C.op("dve", lambda e: e.scalar_tensor_tensor(out=bs.t[:, 1:2], in0=bs.t[:, 6:7], scalar=bs.t[:, 4:5], in1=bs.t[:, 2:3], op0=ALU.mult, op1=ALU.add), reads=[bs], writes=[bs])
        thrB = C.sb("thrB", [NE, P], F32)
        C.op("dve", lambda e: e.tensor_scalar(out=thrB.t[:], in0=M("ones", NE), scalar1=bs.t[:, 0:1], scalar2=None, op0=ALU.mult), reads=[bs, self.mt], writes=[thrB])
        pb = self.nextps()
        C.op("pe", lambda e: e.matmul(pb.t[:, 0:NE], thrB.t[:], M("ident", NE)[:, 0:NE], start=True, stop=True), reads=[thrB, self.mt], writes=[pb])
        thr = C.sb("thrI", [P, NE], F32)
        self.copy("dve", thr, thr.t[:], pb, pb.t[:, 0:NE])
        mask = C.sb("maskI", [P, NT, NE], F32); slot = C.sb("slotI", [P, NT, NE], F32); msum = C.sb("msumI", [P, NE], F32)
        for tt in range(NT):
            C.op("dve", lambda e, tt=tt: e.tensor_tensor(out=mask.t[:, tt, :], in0=aff_tm.t[:, tt, :], in1=thr.t[:], op=ALU.is_ge), reads=[aff_tm, thr], writes=[mask])
        C.op("dve", lambda e: e.memset(msum.t[:], 0.0), writes=[msum])
        for tt in range(NT):
            pr = self.nextps()

            def fn(e, pr=pr, tt=tt):
                e.matmul(pr.t[:, 0:NE], M("tri"), mask.t[:, tt, :], start=True, stop=False)
                return e.matmul(pr.t[:, 0:NE], M("ones"), msum.t[:], start=False, stop=True)
            C.op("pe", fn, reads=[mask, msum, self.mt], writes=[pr])
            self.copy("act", slot, slot.t[:, tt, :], pr, pr.t[:, 0:NE])
            C.op("dve", lambda e, tt=tt: e.tensor_tensor(out=msum.t[:], in0=msum.t[:], in1=mask.t[:, tt, :], op=ALU.add), reads=[msum, mask], writes=[msum])
        sv = slot.t[:].rearrange("p a b -> p (a b)"); mv = mask.t[:].rearrange("p a b -> p (a b)")
        C.op("dve", lambda e: e.scalar_tensor_tensor(out=sv, in0=sv, scalar=1.0, in1=mv, op0=ALU.add, op1=ALU.mult), reads=[slot, mask], writes=[slot])
        C.op("dve", lambda e: e.tensor_scalar(out=sv, in0=sv, scalar1=-1.0, scalar2=None, op0=ALU.add), reads=[slot], writes=[slot])
        if "dbgs" in self.dbg:
            self.store("sp", self.dbgs_res, self.dbgs_res.t.ap()[:, 128:640], slot, sv)
            self.store("sp", self.dbgs_res, self.dbgs_res.t.ap()[:, 640:1152], mask, mv)
            self.store("sp", self.dbgs_res, self.dbgs_res.t.ap()[:, 1152:1168], thr, thr.t[:])
        rhsE = C.sb("rhsE", [P, NT, 4], BF16)
        C.op("dve", lambda e: e.tensor_copy(rhsE.t[:, :, 0:2], M("tvals").rearrange("p (a b) -> p a b", b=2)), reads=[self.mt], writes=[rhsE])
        ahi = C.sb("ahiI", [P, NT], BF16); ahf = C.sb("ahfI", [P, NT], F32); alo = C.sb("aloI", [P, NT], F32)
        oh = [[C.sb("ohI%d_%d" % (b, i), [P, CAP], BF16) for i in range(NT)] for b in range(2)]
        idf = C.sb("idfI", [P, 4], F32); pis = C.sb("pisI", [P, 16], F32)
        for ex in range(NL):
            C.op("dve", lambda e, ex=ex: e.tensor_copy(ahi.t[:], aff_tm.t[:, :, ex]), reads=[aff_tm], writes=[ahi])
            C.op("dve", lambda e: e.tensor_copy(ahf.t[:], ahi.t[:]), reads=[ahi], writes=[ahf])
            C.op("dve", lambda e, ex=ex: e.tensor_tensor(out=alo.t[:], in0=aff_tm.t[:, :, ex], in1=ahf.t[:], op=ALU.subtract), reads=[aff_tm, ahf], writes=[alo])
            C.op("dve", lambda e: e.tensor_copy(rhsE.t[:, :, 2], ahi.t[:]), reads=[ahi, rhsE], writes=[rhsE])
            C.op("dve", lambda e: e.tensor_copy(rhsE.t[:, :, 3], alo.t[:]), reads=[alo, rhsE], writes=[rhsE])
            pi = self.nextps()
            OHs = oh[ex % 2]
            for tt in range(NT):
                OH = OHs[tt]
                C.op("dve", lambda e, OH=OH, tt=tt, ex=ex: e.tensor_scalar(out=OH.t[:], in0=M("iota512"), scalar1=slot.t[:, tt, ex:ex + 1], scalar2=None, op0=ALU.is_equal), reads=[slot, self.mt], writes=[OH])

            def fm(e, OHs=OHs, pi=pi):
                inst = None
                for st in range(4):
                    for tt in range(NT):
                        inst = e.matmul(pi.t[:, st * 4:(st + 1) * 4], OHs[tt].t[:, st * P:(st + 1) * P], rhsE.t[:, tt, :], start=(tt == 0), stop=(tt == NT - 1))
                return inst
            C.op("pe", fm, reads=[*OHs, rhsE], writes=[pi])
            C.op("dve", lambda e, pi=pi: e.tensor_copy(pis.t[:], pi.t[:, 0:16]), reads=[pi], writes=[pis])
            pv = pis.t[:].rearrange("p (a b) -> p a b", b=4)
            C.op("dve", lambda e, pv=pv: e.scalar_tensor_tensor(out=idf.t[:], in0=pv[:, :, 0], scalar=64.0, in1=pv[:, :, 1], op0=ALU.mult, op1=ALU.add), reads=[pis], writes=[idf])
            C.op("dve", lambda e, ex=ex: e.tensor_copy(idx_all.t[:, ex, :], idf.t[:]), reads=[idf], writes=[idx_all])
            C.op("dve", lambda e, ex=ex, pv=pv: e.tensor_tensor(out=gate_all.t[:, ex, :], in0=pv[:, :, 2], in1=pv[:, :, 3], op=ALU.add), reads=[pis], writes=[gate_all])
        C.pop()

    def phase_J(self, hx2, wg, wu, wd, idx_all, gate_all, ffn, ffr):
        C = self.C; M = self.M
        C.push()
        zt = C.sb("ztJ", [P, 512], F32)
        C.op("dve", lambda e: e.memset(zt.t[:], 0.0), writes=[zt])
        fres = [Res("ffnres%d" % i) for i in range(8)]
        for db in range(8):
            for tt in range(NT):
                C.dma("sp" if tt % 2 else "act", lambda e, db=db, tt=tt: e.dma_start(out=ffn[db].t.ap()[tt * P:(tt + 1) * P, :], in_=zt.t[:]), reads=[zt], writes=[fres[db]], semres=zt)
        stg = [C.sb("stgJ%d" % i, [P, 4096], F32) for i in range(3)]
        wgb = [C.sb("wgb%d" % i, [P, 32, P], BF16) for i in range(2)]
        wub = [C.sb("wub%d" % i, [P, 32, P], BF16) for i in range(2)]
        wdb = [C.sb("wdb%d" % i, [P, 16, 512], BF16) for i in range(2)]
        xs = [C.sb("xsJ%d" % i, [P, D], BF16) for i in range(2)]
        xsT = C.sb("xsT", [P, 32, CAP], BF16); hidT = C.sb("hidT", [P, 16, CAP], BF16)
        sgt = [C.sb("sgt%d" % i, [P, CAP], F32) for i in range(2)]
        ot = [C.sb("otJ%d" % i, [P, 512], F32) for i in range(4)]
        cnt = dict(stg=0, ot=0, ev=0)

        def nstg():
            r = stg[cnt["stg"] % 3]; cnt["stg"] += 1; return r
        def gujob(ex, fi):
            def job():
                WG = wgb[fi % 2]; WU = wub[fi % 2]
                gv = wg.t.ap()[ex].rearrange("(kc p) f -> p kc f", p=P); uv = wu.t.ap()[ex].rearrange("(kc p) f -> p kc f", p=P)
                for (src, view, Wd_) in ((wg, gv, WG), (wu, uv, WU)):
                    S_ = nstg()
                    self.load("sp", S_, S_.t[:].rearrange("p (a b) -> p a b", a=32), src, view[:, :, fi * P:(fi + 1) * P])
                    self.copy(self.cast_eng(), Wd_, Wd_.t[:].rearrange("p a b -> p (a b)"), S_, S_.t[:])
                return (WG, WU)
            return job

        def djob(ex, db):
            def job():
                WD = wdb[db % 2]
                dv = wd.t.ap()[ex].rearrange("(fc p) d -> p fc d", p=P)
                for hf in range(2):
                    S_ = nstg()
                    self.load("sp", S_, S_.t[:].rearrange("p (a b) -> p a b", a=8), wd, dv[:, hf * 8:(hf + 1) * 8, db * 512:(db + 1) * 512])
                    self.copy(self.cast_eng(), WD, WD.t[:, hf * 8:(hf + 1) * 8, :].rearrange("p a b -> p (a b)"), S_, S_.t[:])
                return WD
            return job
        wst = Stream([j for ex in range(NL) for j in ([gujob(ex, fi) for fi in range(16)] + [djob(ex, db) for db in range(8)])])
        for ex in range(NL):
            for st in range(4):
                X = xs[st % 2]
                C.dma("pool", lambda e, X=X, ex=ex, st=st: e.indirect_dma_start(out=X.t[:], out_offset=None, in_=hx2.t.ap(), in_offset=bass.IndirectOffsetOnAxis(ap=idx_all.t[:, ex, st:st + 1], axis=0)), reads=[hx2, idx_all], writes=[X])
                for g in range(8):
                    pt = self.nextps(); pv = self.psb(pt)

                    def fnT(e, pv=pv, X=X, g=g):
                        inst = None
                        for j in range(4):
                            kc = g * 4 + j
                            inst = e.transpose(pv[:, j * P:(j + 1) * P], X.t[:, kc * P:(kc + 1) * P], self.identb.t[:])
                        return inst
                    C.op("pe", fnT, reads=[X, self.identb], writes=[pt])
                    cnt["ev"] += 1
                    self.copy("act" if cnt["ev"] % 2 else "dve", xsT, xsT.t[:, g * 4:(g + 1) * 4, st * P:(st + 1) * P], pt, pv[:, 0:512].rearrange("p (a b) -> p a b", a=4))
            for fi in range(16):
                WG, WU = wst.next()
                pg = self.nextps(); pu = self.nextps()
                self.mm(pg, pg.t[:, :], [(WG.t[:, kc, :], xsT.t[:, kc, :]) for kc in range(32)], [WG, xsT])
                self.mm(pu, pu.t[:, :], [(WU.t[:, kc, :], xsT.t[:, kc, :]) for kc in range(32)], [WU, xsT])
                SG = sgt[fi % 2]
                C.op("act", lambda e, SG=SG, pg=pg: e.activation(out=SG.t[:], in_=pg.t[:, :], func=AF.Silu), reads=[pg], writes=[SG])
                C.op("dve", lambda e, SG=SG, pu=pu, fi=fi: e.tensor_tensor(out=hidT.t[:, fi, :], in0=pu.t[:, :], in1=SG.t[:], op=ALU.mult), reads=[pu, SG], writes=[hidT])
            for db in range(8):
                WD = wst.next()
                for st in range(4):
                    po = self.nextps()
                    self.mm(po, po.t[:, :], [(hidT.t[:, fc, st * P:(st + 1) * P], WD.t[:, fc, :]) for fc in range(16)], [hidT, WD])
                    O = ot[cnt["ot"] % 4]; cnt["ot"] += 1
                    C.op("act", lambda e, O=O, po=po, ex=ex, st=st: e.activation(out=O.t[:], in_=po.t[:, :], func=AF.Copy, scale=gate_all.t[:, ex, st:st + 1]), reads=[po, gate_all], writes=[O])
                    rd_ = [O, idx_all] + ([] if st == 0 else [fres[db]])
                    wr_ = [fres[db]] if st == 0 else []
                    C.dma("pool", lambda e, O=O, ex=ex, st=st, db=db: e.indirect_dma_start(out=ffn[db].t.ap(), out_offset=bass.IndirectOffsetOnAxis(ap=idx_all.t[:, ex, st:st + 1], axis=0), in_=O.t[:], in_offset=None, compute_op=ALU.add), reads=rd_, writes=wr_, semres=O)
        self.fres = fres
        C.pop()

    def phase_K(self, x1r, ffn, bcd, fgb, out):
        C = self.C; M = self.M
        C.push()
        g2 = C.sb("gt2b", [P, D], F32); fg = C.sb("fgbt", [P, D], F32)
        self.load("sp", g2, g2.t[:], bcd, bcd.t.ap()[:, D:2 * D]); self.load("act", fg, fg.t[:], fgb, fgb.t.ap())
        xt = [C.sb("xtK%d" % i, [P, D], F32) for i in range(2)]
        ft = [C.sb("ftK%d" % i, [P, D], F32) for i in range(2)]
        junk = C.sb("junkK", [P, D], BF16)
        sts = [C.sb("stK%d" % i, [P, 4], F32) for i in range(2)]
        for tt in range(NT):
            X = xt[tt % 2]; Fd = ft[tt % 2]; st = sts[tt % 2]
            tk = slice(tt * P, (tt + 1) * P)
            self.load("sp", X, X.t[:], x1r, x1r.t.ap()[tk, :])
            for db in range(8):
                C.dma("act" if db % 2 else "sp", lambda e, Fd=Fd, db=db, tk=tk: e.dma_start(out=Fd.t[:, db * 512:(db + 1) * 512], in_=ffn[db].t.ap()[tk, :]), reads=[self.fres[db]], writes=[Fd])
            C.op("dve", lambda e, Fd=Fd: e.tensor_tensor(out=Fd.t[:], in0=Fd.t[:], in1=g2.t[:], op=ALU.mult), reads=[Fd, g2], writes=[Fd])
            C.op("pool", lambda e, Fd=Fd, X=X: e.tensor_tensor(out=Fd.t[:], in0=Fd.t[:], in1=X.t[:], op=ALU.add), reads=[Fd, X], writes=[Fd])
            self.rms_stats(Fd, junk, st, D)
            C.op("dve", lambda e, Fd=Fd, st=st, X=X: e.scalar_tensor_tensor(out=X.t[:], in0=Fd.t[:], scalar=st.t[:, 1:2], in1=fg.t[:], op0=ALU.mult, op1=ALU.mult), reads=[Fd, st, fg, X], writes=[X])
            self.store("sp", out, out.t.ap()[tk, :], X, X.t[:])
        C.pop()


def prep_inputs(inp, b, names, r=0):
    hc = host_constants()
    f = lambda a: np.ascontiguousarray(np.asarray(a, np.float32))
    bro = lambda v: np.ascontiguousarray(np.broadcast_to(np.asarray(v, np.float32).reshape(1, -1), (P, np.asarray(v).size)))
    m = {}
    m["x"] = f(inp["x"][b]); m["ctx"] = f(inp["ctx"][b])
    cc = np.stack([col_layout(inp["c"][b]), col_layout(inp["c_ctx"])], axis=2)
    m["cT"] = f(cc.reshape(P, 64))
    m["ada_w"] = f(inp["ada_w"][0]); m["abT"] = col_layout(inp["ada_b"][0])
    ab = np.asarray(inp["ada_b"][0], np.float32).reshape(6, D)
    m["abb"] = np.ascontiguousarray(np.concatenate([bro(ab[2]), bro(ab[5]), bro(ab[4]), bro(ab[3])], axis=1))
    m["g1T"] = col_layout(inp["norm1_g"][0]); m["g2b"] = bro(inp["norm2_g"][0]); m["fgb"] = bro(inp["final_g"])
    m["w_in"] = f(inp["w_in"][0]); m["w_out"] = f(inp["w_out"][0])
    sw = np.asarray(inp["hy_short_w"][0], np.float32); sb_ = np.asarray(inp["hy_short_b"][0], np.float32)
    swt = np.stack([col_layout(sw[0]), col_layout(sw[1]), col_layout(sw[2]), col_layout(sb_)], axis=2)
    m["swT"] = f(swt.reshape(P, 48 * 4))
    m["fw1"] = f(inp["hy_f_w1"][0]); m["fw2"] = f(inp["hy_f_w2"][0]); m["fw3"] = f(inp["hy_f_w3"][0]); m["fw4"] = f(inp["hy_f_w4"][0])
    m["fvec"] = f(np.stack([inp["hy_f_b1"][0], inp["hy_f_b2"][0], inp["hy_f_b3"][0], inp["hy_sin_freq"][0]], axis=1))
    m["hyb"] = bro(np.asarray(inp["hy_bias"][0]).reshape(-1)); m["rdec"] = bro(np.asarray(inp["ret_decay"][0]).reshape(-1))
    perm = [(e + NL * r) % NE for e in range(NE)]
    rw = np.asarray(inp["router_w"][0], np.float32)[:, perm].reshape(32, P, NE).transpose(1, 0, 2)
    m["rwT"] = f(rw.reshape(P, 32 * NE))
    m["cst"] = hc["cst"].reshape(NFT, P, 2 * NFT * P); m["fscale"] = hc["fscale"]
    m["zT"] = hc["zT"]; m["negt"] = hc["negt"]; m["deltab"] = hc["deltab"]
    m["rot_x"] = hc["rot_x"].reshape(T, 512); m["rot_c"] = hc["rot_c"].reshape(LC, 512)
    m["rot_xk"] = hc["rot_xk"].reshape(T, 512); m["rot_ck"] = hc["rot_ck"].reshape(LC, 512)
    m["misc"] = hc["misc"]
    if "wg" in names:
        sl = slice(NL * r, NL * (r + 1))
        m["wg"] = f(inp["exp_w_gate"][0][sl]); m["wu"] = f(inp["exp_w_up"][0][sl]); m["wd"] = f(inp["exp_w_down"][0][sl])
    return {k: v for k, v in m.items() if k in names}


def run(inputs, upto="all", dbg=(), cores=8):
    kb = K(upto=upto, dbg=dbg)
    nc = kb.build()
    names = set(kb.inputs.keys())
    maps = {}
    in_maps = []
    for cid in range(cores):
        b = cid // 2
        if b not in maps:
            maps[b] = prep_inputs(inputs, b, names, 0)
        in_maps.append(maps[b])
    res = run_bass_kernel_spmd(nc, in_maps, core_ids=list(range(cores)))
    return res, kb


def kernel(**inputs):
    res, kb = run(inputs)
    out = np.stack([np.asarray(res.results[2 * b]["out"], np.float32) for b in range(4)], axis=0)
    return out
```

```python
import math
from contextlib import ExitStack
import numpy as np
import ml_dtypes
import concourse.bass as bass
import concourse.mybir as mybir
from concourse.bass_utils import run_bass_kernel_spmd

F32 = mybir.dt.float32; BF16 = mybir.dt.bfloat16; I32 = mybir.dt.int32
ALU = mybir.AluOpType; AF = mybir.ActivationFunctionType

D = 4096; T = 4096; LC = 256; NT = 32; P = 128
HYW = 2048; HYC = 6144; QC = 1024; GC = 4096; KC = 1024; VC = 2048
KV0 = HYC + QC + GC; INW = 14336
NE = 16; NL = 16; EFF = 2048; CAP = 512
NFT = 33
EPS = 1e-6


class Res:
    __slots__ = ("name", "w", "r", "t", "dsem")

    def __init__(self, name, t=None):
        self.name = name; self.t = t; self.w = {}; self.r = {}; self.dsem = None


def _is_dram(r):
    return r.t is not None and type(r.t).__name__.lower().startswith("dram")


class Ctx:
    def __init__(self, nc, es):
        self.nc = nc; self.es = es; self.engs = {}; self.sems = {}
        for name, e in (("pe", nc.tensor), ("dve", nc.vector), ("act", nc.scalar), ("pool", nc.gpsimd), ("sp", nc.sync)):
            sem = es.enter_context(nc.semaphore("s_" + name))
            self.engs[name] = dict(e=e, seen={})
            self.sems[name] = [sem, 0]
        self.free_dsems = []
        self.ndsem = 0
        self.stack = [es]
        self.scope_res = [[]]
        self.ninst = 0

    def sb(self, name, shape, dt):
        t = self.stack[-1].enter_context(self.nc.sbuf_tensor(name, list(shape), dt))
        r = Res(name, t); self.scope_res[-1].append(r); return r

    def ps(self, name, shape, dt=F32):
        t = self.stack[-1].enter_context(self.nc.psum_tensor(name, list(shape), dt))
        r = Res(name, t); self.scope_res[-1].append(r); return r

    def dram(self, name, shape, dt, kind="Internal"):
        return Res(name, self.nc.dram_tensor(name, list(shape), dt, kind=kind))

    def push(self):
        es = ExitStack(); self.stack.append(es); self.scope_res.append([]); return es

    def pop(self):
        self.barrier()
        for r in self.scope_res.pop():
            if r.dsem is not None:
                self.free_dsems.append(r.dsem); r.dsem = None
        self.stack.pop().close()

    def _dsem(self, r):
        if r.dsem is None:
            if self.free_dsems:
                r.dsem = self.free_dsems.pop()
            else:
                key = "d%d" % self.ndsem; self.ndsem += 1
                sem = self.es.enter_context(self.nc.semaphore(key))
                self.sems[key] = [sem, 0]; r.dsem = key
        return r.dsem

    def _wait(self, eng, deps):
        E = self.engs[eng]
        for key, val in deps.items():
            if key[0] == "d":
                val = self.sems[key][1]
            elif key == eng and eng == "pe":
                continue
            if E["seen"].get(key, 0) >= val:
                continue
            E["e"].wait_ge(self.sems[key][0], val)
            E["seen"][key] = val

    def _deps(self, reads, writes):
        deps = {}
        for r in reads:
            for k, v in r.w.items():
                if deps.get(k, 0) < v: deps[k] = v
        for r in writes:
            for k, v in r.w.items():
                if deps.get(k, 0) < v: deps[k] = v
            for k, v in r.r.items():
                if deps.get(k, 0) < v: deps[k] = v
        return deps

    def _mark(self, key, val, reads, writes):
        for r in reads:
            if r.r.get(key, 0) < val: r.r[key] = val
        for r in writes:
            r.w = {key: val}; r.r = {}

    def op(self, eng, fn, reads=(), writes=()):
        self._wait(eng, self._deps(reads, writes))
        inst = fn(self.engs[eng]["e"])
        s = self.sems[eng]; s[1] += 1
        inst.then_inc(s[0], 1)
        self._mark(eng, s[1], reads, writes)
        self.ninst += 1
        return inst

    def dma(self, q, fn, reads=(), writes=(), semres=None):
        if semres is None:
            if writes and not _is_dram(writes[0]):
                semres = writes[0]
            else:
                semres = reads[0]
        key = self._dsem(semres)
        self._wait(q, self._deps(reads, writes))
        inst = fn(self.engs[q]["e"])
        s = self.sems[key]; s[1] += 16
        inst.then_inc(s[0], 16)
        self._mark(key, s[1], reads, writes)
        self.ninst += 1
        return inst

    def barrier(self):
        allk = {k: v[1] for k, v in self.sems.items() if v[1] > 0}
        for eng in self.engs:
            self._wait(eng, allk)


def _bf(a):
    return np.ascontiguousarray(a.astype(ml_dtypes.bfloat16))


_CONST_CACHE = {}


def host_constants():
    if _CONST_CACHE:
        return _CONST_CACHE
    c = {}
    n = NFT * P
    idx = np.arange(n, dtype=np.int64)
    prod = (idx[:, None] * idx[None, :]) % 8192
    valid = (idx[:, None] <= 4096) & (idx[None, :] <= 4096)
    ang = prod.astype(np.float64) * (2.0 * math.pi / 8192.0)
    Cm = np.where(valid, np.cos(ang), 0.0)
    Sm = np.where(valid, np.sin(ang), 0.0)
    def tile(M):
        return M.reshape(NFT, P, NFT, P).transpose(2, 1, 0, 3)
    cs = np.stack([tile(Cm), tile(Sm)], axis=2)
    c["cst"] = _bf(cs.astype(np.float32))
    fs = np.zeros(n, np.float32); fs[0] = 1.0 / 8192; fs[1:4096] = 2.0 / 8192; fs[4096] = 1.0 / 8192
    c["fscale"] = np.ascontiguousarray(fs.reshape(NFT, P).T)
    L = T
    bands = 16
    t = np.linspace(0.0, 1.0, L, dtype=np.float32)[:, None]
    ang2 = (np.float32(2.0 * math.pi / L) * np.arange(L, dtype=np.float32)[:, None]) * np.linspace(1e-4, bands - 1, bands, dtype=np.float32)[None, :]
    z = np.concatenate([t, np.cos(ang2), -np.sin(ang2)], axis=-1).astype(np.float32)
    c["zT"] = np.ascontiguousarray(z.T)
    c["negt"] = np.ascontiguousarray((-t[:, 0]).reshape(NT, P).T.astype(np.float32))
    max_decay = math.log(1e-2) / 0.3; min_decay = math.log(1e-2) / 1.5
    deltas = np.abs(np.linspace(min_decay, max_decay, HYW, dtype=np.float32))
    c["deltab"] = np.ascontiguousarray(np.broadcast_to(deltas[None, :], (P, HYW)).astype(np.float32))
    n_freq = 32
    inv = (1.0 / (10000.0 ** np.linspace(0.0, 1.0, n_freq, dtype=np.float32))).astype(np.float32)
    def rot(pa, pb):
        a = np.concatenate([pa[:, None] * inv, pb[:, None] * inv], axis=-1).astype(np.float32)
        return np.cos(a).astype(np.float32), np.sin(a).astype(np.float32)
    rows = L // 64
    lat_row = np.repeat(np.arange(rows, dtype=np.float32), 64)
    lat_col = np.tile(np.arange(64, dtype=np.float32), rows)
    cx, sx = rot(lat_row, lat_col)
    cp = np.arange(LC, dtype=np.float32)
    cc, sc = rot(cp, cp)
    c["rot_x"] = np.ascontiguousarray(np.stack([np.tile(cx, (1, 4)), np.tile(sx, (1, 4))], axis=1))
    c["rot_c"] = np.ascontiguousarray(np.stack([np.tile(cc, (1, 4)), np.tile(sc, (1, 4))], axis=1))
    ks = np.float32(128 ** -0.5)
    c["rot_xk"] = np.ascontiguousarray(c["rot_x"] * ks); c["rot_ck"] = np.ascontiguousarray(c["rot_c"] * ks)
    pidx = np.arange(P, dtype=np.float32)
    m = {}
    m["ident"] = np.eye(P, dtype=np.float32)
    m["ones"] = np.ones((P, P), np.float32)
    jj = pidx[:, None]; ii = pidx[None, :]
    m["diffF"] = np.maximum(ii - jj, 0.0); m["maskF"] = (ii >= jj).astype(np.float32)
    m["diffB"] = np.maximum(jj - ii, 0.0); m["maskB"] = (jj >= ii).astype(np.float32)
    m["ip1"] = np.broadcast_to(ii + 1.0, (P, P)).copy(); m["i128m"] = np.broadcast_to(128.0 - ii, (P, P)).copy()
    m["tri"] = (jj < ii).astype(np.float32)
    m["iota512"] = np.broadcast_to(np.arange(CAP, dtype=np.float32)[None, :], (P, CAP)).copy()
    cols = np.zeros((P, 8), np.float32)
    cols[:, 0] = 127.0 - pidx; cols[:, 1] = pidx; cols[:, 2] = 255.0 - pidx; cols[:, 3] = 127.0 - pidx
    cols[:, 4] = pidx; cols[:, 5] = 128.0 + pidx
    cols[:, 6] = EPS; cols[:, 7] = 1.0
    m["cols"] = cols
    tv = np.zeros((P, NT, 2), np.float32)
    tok = (np.arange(NT)[None, :] * P + np.arange(P)[:, None])
    tv[:, :, 0] = tok // 64; tv[:, :, 1] = tok % 64
    m["tvals"] = tv.reshape(P, NT * 2)
    sel = np.zeros((P, P), np.float32); sel[0, :] = 1.0
    m["sel0"] = sel
    off = {}; parts = []; o = 0
    for k, v in m.items():
        off[k] = (o, v.shape[1]); parts.append(v.astype(np.float32)); o += v.shape[1]
    c["misc"] = np.ascontiguousarray(np.concatenate(parts, axis=1))
    c["misc_off"] = off
    _CONST_CACHE.update(c)
    return c


class Stream:
    def __init__(self, jobs, ahead=1):
        self.jobs = jobs; self.done = {}; self.k = 0; self.ahead = ahead

    def next(self):
        k = self.k
        for j in range(k, min(k + self.ahead + 1, len(self.jobs))):
            if j not in self.done:
                self.done[j] = self.jobs[j]()
        self.k += 1
        return self.done.pop(k)


def col_layout(v):
    v = np.asarray(v, np.float32).reshape(-1, P)
    return np.ascontiguousarray(v.T)


class K:
    def __init__(self, upto="all", dbg=()):
        self.upto = upto; self.dbg = set(dbg)
        self.nc = bass.Bass("TRN2", target_bir_lowering=False)
        self.inputs = {}
        self.outputs = []

    def inp(self, name, shape, dt=F32):
        r = self.C.dram(name, shape, dt, kind="ExternalInput"); self.inputs[name] = r; return r

    def scratch(self, name, shape, dt):
        kind = "ExternalOutput" if name in self.dbg else "Internal"
        if name in self.dbg:
            self.outputs.append(name)
        return self.C.dram(name, shape, dt, kind=kind)

    def load(self, q, dst, dst_ap, src, src_ap):
        return self.C.dma(q, lambda e: e.dma_start(out=dst_ap, in_=src_ap), reads=[src], writes=[dst])

    def store(self, q, dst, dst_ap, src, src_ap):
        return self.C.dma(q, lambda e: e.dma_start(out=dst_ap, in_=src_ap), reads=[src], writes=[dst], semres=src)

    def nextps(self):
        r = self.pbanks[self.pbi % len(self.pbanks)]; self.pbi += 1; return r

    def mm(self, pr, out_ap, pairs, reads):
        n = len(pairs)

        def fn(e):
            inst = None
            for i, (l, r) in enumerate(pairs):
                inst = e.matmul(out_ap, l, r, start=(i == 0), stop=(i == n - 1))
            return inst
        return self.C.op("pe", fn, reads=reads, writes=[pr])

    def cast_eng(self):
        self.ce = (self.ce + 1) % 3
        return ("dve", "act", "pool")[self.ce]

    def copy(self, eng, dst, dst_ap, src, src_ap, extra_reads=()):
        if eng == "act":
            return self.C.op("act", lambda e: e.activation(out=dst_ap, in_=src_ap, func=AF.Copy), reads=[src, *extra_reads], writes=[dst])
        return self.C.op(eng, lambda e: e.tensor_copy(dst_ap, src_ap), reads=[src, *extra_reads], writes=[dst])

    def build(self):
        nc = self.nc
        with ExitStack() as es:
            self.C = C = Ctx(nc, es)
            self.pbi = 0; self.ce = 0
            hc = host_constants()
            self.moff = hc["misc_off"]
            nmisc = hc["misc"].shape[1]
            I = self.inp
            x = I("x", [T, D]); ctx = I("ctx", [LC, D]); cT = I("cT", [P, 64])
            ada_w = I("ada_w", [D, 6 * D]); abT = I("abT", [P, 192])
            g1T = I("g1T", [P, 32]); g2b = I("g2b", [P, D]); fgb = I("fgb", [P, D]); abb = I("abb", [P, 4 * D])
            w_in = I("w_in", [D, INW]); w_out = I("w_out", [D, D])
            swT = I("swT", [P, 48 * 4])
            fw1 = I("fw1", [33, 64]); fw2 = I("fw2", [64, 64]); fw3 = I("fw3", [64, 64]); fw4 = I("fw4", [64, 8192])
            fvec = I("fvec", [64, 4])
            hyb = I("hyb", [P, 4096]); rdec = I("rdec", [P, 16])
            rwT = I("rwT", [P, 32 * 16])
            cst = I("cst", [NFT, P, 2 * NFT * P], BF16); fscale = I("fscale", [P, NFT])
            zT = I("zT", [33, T]); negt = I("negt", [P, NT]); deltab = I("deltab", [P, HYW])
            rot_x = I("rot_x", [T, 512]); rot_c = I("rot_c", [LC, 512]); rot_xk = I("rot_xk", [T, 512]); rot_ck = I("rot_ck", [LC, 512]); misc = I("misc", [P, nmisc])
            if self.upto in ("all", "J"):
                wg = I("wg", [NL, D, EFF]); wu = I("wu", [NL, D, EFF]); wd = I("wd", [NL, EFF, D])
            out = C.dram("out", [T, D], F32, kind="ExternalOutput")
            self.outputs.append("out")
            S = self.scratch
            hxT = S("hxT", [D, T], BF16)
            v_tm = S("v_tm", [T, HYW], BF16); x1_tm = S("x1_tm", [T, HYW], BF16); x2_cm = S("x2_cm", [HYW, T], BF16)
            sg = S("sg", [GC, T], BF16)
            qT_d = S("qT_d", [QC, T], BF16); kT_d = S("kT_d", [KC, T], BF16)
            k_tm = S("k_tm", [T, KC], BF16); vr_tm = S("vr_tm", [T, VC], BF16)
            ed = S("ed", [T, 2 * 4096], BF16)
            spec = S("spec", [NFT * P, 2 * 4096], F32)
            yT = S("yT", [6144, T], BF16)
            x1r = S("x1r", [T, D], F32); hx2 = S("hx2", [T, D], BF16)
            affd = S("affd", [T, NE], F32)
            ffn = [S("ffn%d" % i, [T, 512], F32) for i in range(8)]
            ffr = [S("ffr%d" % i, [T, 512], F32) for i in range(8)]
            dbgs = S("dbgs", [P, 4096], F32)
            self.dbgs_res = dbgs

            mt = C.sb("misc_t", [P, nmisc], F32)
            self.load("sp", mt, mt.t[:], misc, misc.t.ap())

            def M(name, rows=P):
                o, w = self.moff[name]
                return mt.t[0:rows, o:o + w]
            self.M = M
            identb = C.sb("identb", [P, P], BF16); onesb = C.sb("onesb", [P, P], BF16)
            C.op("dve", lambda e: e.tensor_copy(identb.t[:], M("ident")), reads=[mt], writes=[identb])
            C.op("dve", lambda e: e.tensor_copy(onesb.t[:], M("ones")), reads=[mt], writes=[onesb])
            self.identb = identb; self.onesb = onesb; self.mt = mt
            self.pbanks = [C.ps("pb%d" % i, [P, 512], F32) for i in range(7)]
            self.pacc = C.ps("pacc", [P, 512], F32)
            self.slab_i = 0
            modsT = C.sb("modsT", [P, 192, 2], F32)
            AB = C.sb("AB", [P, 4, 32], F32)
            bcd = S("bcd", [P, 4 * D], F32)
            lg = C.sb("lg", [P, 16], F32)
            Sfd = S("Sfd", [P, 16 * 256], F32)

            self.phase_A(cT, ada_w, abT, g1T, g2b, abb, modsT, AB, bcd)
            if "modsT" in self.dbg:
                pass
            if self.upto == "A":
                return self.finish(dbgs, [(modsT, modsT.t[:].rearrange("p a b -> p (a b)"), 384), (AB, AB.t[:].rearrange("p a b -> p (a b)"), 128)])
            C.push()
            hcT = C.sb("hcT", [P, 32, LC], BF16)
            Sf = C.sb("Sf", [P, 16, 256], F32); Sb = C.sb("Sb", [P, 16, 256], BF16)
            self.stg_i = 0
            self.phase_B(x, ctx, AB, hxT, hcT)
            if self.upto == "B":
                tmpd = C.sb("tmpd", [P, 512], F32)
                C.op("dve", lambda e: e.tensor_copy(tmpd.t[:, 0:256], hcT.t[:, 0, :]), reads=[hcT], writes=[tmpd])
                C.op("dve", lambda e: e.tensor_copy(tmpd.t[:, 256:512], hcT.t[:, 31, :]), reads=[hcT], writes=[tmpd])
                return self.finish(dbgs, [(tmpd, tmpd.t[:], 512)])
            self.phase_C(hcT, w_in, rot_ck, rdec, lg, Sf, Sb)
            self.store("sp", Sfd, Sfd.t.ap(), Sf, Sf.t[:].rearrange("p a b -> p (a b)"))
            if self.upto == "C":
                return self.finish(dbgs, [(lg, lg.t[:], 16), (Sf, Sf.t[:, 0, :], 256), (Sf, Sf.t[:, 15, :], 256)])
            C.pop()
            self.phase_D(hxT, w_in, swT, rot_x, rot_xk, v_tm, x1_tm, x2_cm, sg, qT_d, kT_d, k_tm, vr_tm)
            if self.upto == "D":
                return self.finish(dbgs, [])
            self.phase_E(fw1, fw2, fw3, fw4, fvec, zT, negt, deltab, hyb, cst, fscale, spec)
            if self.upto == "E":
                return self.finish(dbgs, [])
            self.phase_F(v_tm, x1_tm, x2_cm, spec, cst, yT)
            if self.upto == "F":
                return self.finish(dbgs, [])
            self.phase_G(qT_d, kT_d, k_tm, vr_tm, sg, Sfd, lg, yT)
            if self.upto == "G":
                return self.finish(dbgs, [])
            aff_tm = C.sb("aff_tm", [P, NT, NE], F32)
            self.phase_H(yT, w_out, x, bcd, x1r, hx2, rwT, aff_tm)
            if self.upto == "H":
                return self.finish(dbgs, [(aff_tm, aff_tm.t[:].rearrange("p a b -> p (a b)"), 512)])
            idx_all = C.sb("idx_all", [P, NL, 4], I32); gate_all = C.sb("gate_all", [P, NL, 4], F32)
            self.phase_I(aff_tm, idx_all, gate_all)
            if self.upto == "I":
                idf2 = C.sb("idf2", [P, 64], F32)
                C.op("dve", lambda e: e.tensor_copy(idf2.t[:], idx_all.t[:].rearrange("p a b -> p (a b)")), reads=[idx_all], writes=[idf2])
                return self.finish(dbgs, [(idf2, idf2.t[:], 64), (gate_all, gate_all.t[:].rearrange("p a b -> p (a b)"), 64)])
            self.phase_J(hx2, wg, wu, wd, idx_all, gate_all, ffn, ffr)
            self.phase_K(x1r, ffn, bcd, fgb, out)
            return self.finish(dbgs, [])

    def finish(self, dbgs, dumps):
        C = self.C
        o = 0
        for (r, ap, w) in dumps:
            self.store("sp", dbgs, dbgs.t.ap()[:, o:o + w], r, ap); o += w
        self.outputs.append("dbgs") if "dbgs" in self.dbg else None
        C.barrier()
        return self.nc

    def phase_A(self, cT, ada_w, abT, g1T, g2b, abb, modsT, AB, bcd):
        C = self.C; M = self.M
        C.push()
        bc = C.sb("bc", [P, 4, D], F32)
        ct = C.sb("ct", [P, 64], F32); sct = C.sb("sct", [P, 32, 2], F32)
        abt = C.sb("abt", [P, 192], F32); g1t = C.sb("g1t", [P, 32], F32)
        self.load("sp", ct, ct.t[:], cT, cT.t.ap())
        self.load("sp", abt, abt.t[:], abT, abT.t.ap())
        self.load("sp", g1t, g1t.t[:], g1T, g1T.t.ap())
        C.op("act", lambda e: e.activation(out=sct.t[:].rearrange("p a b -> p (a b)"), in_=ct.t[:], func=AF.Silu), reads=[ct], writes=[sct])
        wbuf = [C.sb("aw%d" % i, [P, 8, 512], F32) for i in range(5)]
        row = [C.sb("row%d" % i, [2, 512], F32) for i in range(2)]
        awv = ada_w.t.ap().rearrange("(kc p) n -> p kc n", p=P)
        wi = 0
        bmap = {2: 0, 5: 1, 4: 2, 3: 3}
        for nb in range(48):
            pr = self.nextps()
            halves = []
            for h in range(4):
                wb = wbuf[wi % 5]; wi += 1
                self.load("sp" if (wi % 2) else "act", wb, wb.t[:], ada_w, awv[:, h * 8:(h + 1) * 8, nb * 512:(nb + 1) * 512])
                halves.append(wb)
            def fn(e, halves=halves, pr=pr):
                inst = None
                for h in range(4):
                    for k in range(8):
                        kc = h * 8 + k
                        inst = e.matmul(pr.t[0:2, :], sct.t[:, kc, :], halves[h].t[:, k, :], start=(kc == 0), stop=(kc == 31))
                return inst
            C.op("pe", fn, reads=[sct, *halves], writes=[pr])
            rw = row[nb % 2]
            C.op("dve", lambda e, rw=rw, pr=pr: e.tensor_copy(rw.t[:], pr.t[0:2, :]), reads=[pr], writes=[rw])
            pt = self.nextps()
            def fn2(e, rw=rw, pt=pt):
                inst = None
                for j in range(4):
                    inst = e.transpose(pt.t[:, j * 2:j * 2 + 2], rw.t[0:2, j * 128:(j + 1) * 128], M("ident", 2)[:, 0:2])
                return inst
            C.op("pe", fn2, reads=[rw, self.mt], writes=[pt])
            C.op("dve", lambda e, pt=pt, nb=nb: e.tensor_copy(modsT.t[:, nb * 4:(nb + 1) * 4, :].rearrange("p a b -> p (a b)"), pt.t[:, 0:8]), reads=[pt], writes=[modsT])
            mod = nb // 8
            if mod in bmap:
                pb = self.nextps()
                C.op("pe", lambda e, pb=pb, rw=rw: e.matmul(pb.t[:, :], M("sel0", 2), rw.t[0:2, :], start=True, stop=True), reads=[rw, self.mt], writes=[pb])
                j = bmap[mod]; cb = (nb % 8) * 512
                C.op("act", lambda e, pb=pb, j=j, cb=cb: e.activation(out=bc.t[:, j, cb:cb + 512], in_=pb.t[:, :], func=AF.Copy), reads=[pb], writes=[bc])
        for r in range(2):
            C.op("dve", lambda e, r=r: e.tensor_tensor(out=modsT.t[:, :, r], in0=modsT.t[:, :, r], in1=abt.t[:], op=ALU.add), reads=[modsT, abt], writes=[modsT])
        for r in range(2):
            C.op("dve", lambda e, r=r: e.scalar_tensor_tensor(out=AB.t[:, 2 * r, :], in0=modsT.t[:, 32:64, r], scalar=1.0, in1=g1t.t[:], op0=ALU.add, op1=ALU.mult), reads=[modsT, g1t], writes=[AB])
            C.op("dve", lambda e, r=r: e.tensor_copy(AB.t[:, 2 * r + 1, :], modsT.t[:, 0:32, r]), reads=[modsT], writes=[AB])
        tmpb = C.sb("tmpb", [P, D], F32)
        for j in range(4):
            self.load("sp", tmpb, tmpb.t[:], abb, abb.t.ap()[:, j * D:(j + 1) * D])
            C.op("dve", lambda e, j=j: e.tensor_tensor(out=bc.t[:, j, :], in0=bc.t[:, j, :], in1=tmpb.t[:], op=ALU.add), reads=[bc, tmpb], writes=[bc])
        self.load("sp", tmpb, tmpb.t[:], g2b, g2b.t.ap())
        C.op("dve", lambda e: e.scalar_tensor_tensor(out=bc.t[:, 2, :], in0=bc.t[:, 2, :], scalar=1.0, in1=tmpb.t[:], op0=ALU.add, op1=ALU.mult), reads=[bc, tmpb], writes=[bc])
        self.store("sp", bcd, bcd.t.ap(), bc, bc.t[:].rearrange("p a b -> p (a b)"))
        C.pop()

    def psb(self, pr):
        return pr.t[:, :].bitcast(BF16)

    def rms_stats(self, xt, junk, st, width):
        C = self.C; M = self.M
        C.op("act", lambda e: e.activation(out=junk.t[:, 0:width], in_=xt.t[:, 0:width], func=AF.Square, accum_out=st.t[:, 0:1]), reads=[xt], writes=[junk, st])
        C.op("act", lambda e: e.activation(out=st.t[:, 2:3], in_=st.t[:, 0:1], func=AF.Sqrt, scale=1.0 / width, bias=M("cols")[:, 6:7]), reads=[st, self.mt], writes=[st])
        C.op("dve", lambda e: e.reciprocal(st.t[:, 1:2], st.t[:, 2:3]), reads=[st], writes=[st])

    def phase_B(self, x, ctx, AB, hxT, hcT):
        C = self.C; M = self.M
        C.push()
        xt = [C.sb("xt%d" % i, [P, D], F32) for i in range(2)]
        xn = [C.sb("xn%d" % i, [P, D], BF16) for i in range(2)]
        junk = C.sb("junkB", [P, D], BF16)
        sts = [C.sb("stB%d" % i, [P, 4], F32) for i in range(2)]
        hb = [C.sb("hbB%d" % i, [P, 32, 512], BF16) for i in range(2)]
        hview = hxT.t.ap().rearrange("(kc p) t -> p kc t", p=P)
        ev = 0
        for tt in range(NT + 2):
            isc = tt >= NT
            X = xt[tt % 2]; XN = xn[tt % 2]; st = sts[tt % 2]
            if isc:
                self.load("sp", X, X.t[:], ctx, ctx.t.ap()[(tt - NT) * P:(tt - NT + 1) * P, :])
            else:
                self.load("sp" if tt % 2 else "act", X, X.t[:], x, x.t.ap()[tt * P:(tt + 1) * P, :])
            self.rms_stats(X, junk, st, D)
            C.op("act", lambda e, X=X, XN=XN, st=st: e.activation(out=XN.t[:], in_=X.t[:], func=AF.Copy, scale=st.t[:, 1:2]), reads=[X, st], writes=[XN])
            a_i = 2 if isc else 0
            H = hb[(tt // 4) % 2]
            for g in range(4):
                pr = self.nextps()
                pv = self.psb(pr)
                def fn(e, XN=XN, pv=pv, g=g):
                    inst = None
                    for j in range(8):
                        kc = g * 8 + j
                        inst = e.transpose(pv[:, j * P:(j + 1) * P], XN.t[:, kc * P:(kc + 1) * P], self.identb.t[:])
                    return inst
                C.op("pe", fn, reads=[XN, self.identb], writes=[pr])
                for j in range(8):
                    kc = g * 8 + j
                    if isc:
                        dst, dap = hcT, hcT.t[:, kc, (tt - NT) * P:(tt - NT + 1) * P]
                    else:
                        dst, dap = H, H.t[:, kc, (tt % 4) * P:(tt % 4 + 1) * P]
                    ev += 1
                    if ev % 2:
                        C.op("dve", lambda e, dap=dap, pv=pv, j=j, kc=kc, a_i=a_i: e.tensor_scalar(out=dap, in0=pv[:, j * P:(j + 1) * P], scalar1=AB.t[:, a_i, kc:kc + 1], scalar2=AB.t[:, a_i + 1, kc:kc + 1], op0=ALU.mult, op1=ALU.add), reads=[pr, AB], writes=[dst])
                    else:
                        C.op("act", lambda e, dap=dap, pv=pv, j=j, kc=kc, a_i=a_i: e.activation(out=dap, in_=pv[:, j * P:(j + 1) * P], func=AF.Identity, scale=AB.t[:, a_i, kc:kc + 1], bias=AB.t[:, a_i + 1, kc:kc + 1]), reads=[pr, AB], writes=[dst])
            if (not isc) and tt % 4 == 3:
                tb = tt // 4
                self.store("sp", hxT, hview[:, :, tb * 512:(tb + 1) * 512], H, H.t[:])
        C.pop()

    def load_w_bf(self, wsrc, view, stg, wb):
        C = self.C
        for q in range(4):
            s = stg[self.stg_i % len(stg)]; self.stg_i += 1
            self.load("sp" if q % 2 else "act", s, s.t[:], wsrc, view[:, q * 8:(q + 1) * 8, :])
            self.copy(self.cast_eng(), wb, wb.t[:, q * 8:(q + 1) * 8, :], s, s.t[:])

    def rotary(self, pr, rt, rtap, dst, dap, tmps):
        C = self.C
        pv = pr.t[:, :].rearrange("p (h two f) -> p h two f", h=4, two=2)
        x1 = pv[:, :, 0, :]; x2 = pv[:, :, 1, :]
        cs = rtap[:, 0, :].rearrange("p (h f) -> p h f", h=4); sn = rtap[:, 1, :].rearrange("p (h f) -> p h f", h=4)
        t1, t2 = tmps
        t1v = t1.t[:, :].rearrange("p (h f) -> p h f", h=4); t2v = t2.t[:, :].rearrange("p (h f) -> p h f", h=4)
        dv = dap.rearrange("p (h two f) -> p h two f", h=4, two=2)
        C.op("dve", lambda e: e.tensor_tensor(out=t1v, in0=x1, in1=cs, op=ALU.mult), reads=[pr, rt], writes=[t1])
        C.op("dve", lambda e: e.tensor_tensor(out=t2v, in0=x2, in1=sn, op=ALU.mult), reads=[pr, rt], writes=[t2])
        C.op("dve", lambda e: e.tensor_tensor(out=dv[:, :, 0, :], in0=t1v, in1=t2v, op=ALU.subtract), reads=[t1, t2], writes=[dst])
        C.op("dve", lambda e: e.tensor_tensor(out=t1v, in0=x1, in1=sn, op=ALU.mult), reads=[pr, rt], writes=[t1])
        C.op("dve", lambda e: e.tensor_tensor(out=t2v, in0=x2, in1=cs, op=ALU.mult), reads=[pr, rt], writes=[t2])
        C.op("dve", lambda e: e.tensor_tensor(out=dv[:, :, 1, :], in0=t1v, in1=t2v, op=ALU.add), reads=[t1, t2], writes=[dst])

    def phase_C(self, hcT, w_in, rot_ck, rdec, lg, Sf, Sb):
        C = self.C; M = self.M
        C.push()
        rd = C.sb("rd", [P, 16], F32)
        self.load("sp", rd, rd.t[:], rdec, rdec.t.ap())
        C.op("act", lambda e: e.activation(out=rd.t[:], in_=rd.t[:], func=AF.Exp, scale=-1.0), reads=[rd], writes=[rd])
        C.op("act", lambda e: e.activation(out=rd.t[:], in_=rd.t[:], func=AF.Ln, bias=M("cols")[:, 7:8]), reads=[rd, self.mt], writes=[rd])
        C.op("dve", lambda e: e.tensor_scalar(out=lg.t[:], in0=rd.t[:], scalar1=-1.0, scalar2=None, op0=ALU.mult), reads=[rd], writes=[lg])
        wctx = C.sb("wctx", [P, 2, 16], F32)
        for tt in range(2):
            for d in range(2):
                col = 2 + tt if d == 0 else 4 + tt
                C.op("act", lambda e, tt=tt, d=d, col=col: e.activation(out=wctx.t[:, tt, d * 8:(d + 1) * 8], in_=lg.t[:, d * 8:(d + 1) * 8], func=AF.Exp, scale=M("cols")[:, col:col + 1]), reads=[lg, self.mt], writes=[wctx])
        import os
        cstop = int(os.environ.get("CSTOP", "9"))
        if cstop <= 1:
            C.pop(); return
        stg = [C.sb("stgC%d" % i, [P, 8, 512], F32) for i in range(3)]
        wb = [C.sb("wbC%d" % i, [P, 32, 512], BF16) for i in range(2)]
        kct = C.sb("kct", [P, 2, KC], BF16); vct = C.sb("vct", [P, 2, VC], BF16)
        rt = C.sb("rtC", [P, 2, 2, 256], F32)
        tm = [C.sb("tmC%d" % i, [P, 256], F32) for i in range(2)]
        self.load("sp", rt, rt.t[:].rearrange("p a b c -> p a (b c)"), rot_ck, rot_ck.t.ap().rearrange("(a p) c -> p a c", p=P))
        wv = w_in.t.ap().rearrange("(kc p) n -> p kc n", p=P)
        for nb in range(6):
            W = wb[nb % 2]
            self.load_w_bf(w_in, wv[:, :, KV0 + nb * 512:KV0 + (nb + 1) * 512], stg, W)
            for tt in range(2):
                pr = self.nextps()
                self.mm(pr, pr.t[:, :], [(hcT.t[:, kc, tt * P:(tt + 1) * P], W.t[:, kc, :]) for kc in range(32)], [hcT, W])
                if nb < 2 and cstop <= 2:
                    self.copy("act", kct, kct.t[:, tt, nb * 512:(nb + 1) * 512], pr, pr.t[:, :])
                elif nb < 2:
                    self.rotary(pr, rt, rt.t[:, tt], kct, kct.t[:, tt, nb * 512:(nb + 1) * 512], tm)
                else:
                    self.copy("act", vct, vct.t[:, tt, (nb - 2) * 512:(nb - 1) * 512], pr, pr.t[:, :])
        if cstop <= 3:
            C.pop(); return
        kd = [C.sb("kdC%d" % i, [P, P], BF16) for i in range(4)]
        ki = 0
        for d in range(2):
            for h in range(8):
                pr = self.nextps()
                pairs = []
                kds = []
                for tt in range(2):
                    k_ = kd[ki % 4]; ki += 1
                    C.op("dve", lambda e, k_=k_, tt=tt, h=h, d=d: e.tensor_scalar(out=k_.t[:], in0=kct.t[:, tt, h * P:(h + 1) * P], scalar1=wctx.t[:, tt, d * 8 + h:d * 8 + h + 1], scalar2=None, op0=ALU.mult), reads=[kct, wctx], writes=[k_])
                    pairs.append((k_.t[:], vct.t[:, tt, h * 256:(h + 1) * 256])); kds.append(k_)
                if cstop <= 4:
                    continue
                self.mm(pr, pr.t[:, 0:256], pairs, [vct, *kds])
                if cstop <= 5:
                    continue
                C.op("dve", lambda e, pr=pr, d=d, h=h: e.tensor_copy(Sf.t[:, d * 8 + h, :], pr.t[:, 0:256]), reads=[pr], writes=[Sf])
                if cstop <= 6:
                    continue
                C.op("pool", lambda e, d=d, h=h: e.tensor_copy(Sb.t[:, d * 8 + h, :], Sf.t[:, d * 8 + h, :]), reads=[Sf], writes=[Sb])
        C.pop()


    def phase_D(self, hxT, w_in, swT, rot_x, rot_xk, v_tm, x1_tm, x2_cm, sg, qT_d, kT_d, k_tm, vr_tm):
        C = self.C; M = self.M
        C.push()
        stg = [C.sb("stgD%d" % i, [P, 2, 512], F32) for i in range(3)]
        wb = [C.sb("wbD%d" % i, [P, 32, 512], BF16) for i in range(2)]
        hb = [C.sb("hbD%d" % i, [P, 32, 512], BF16) for i in range(2)]
        prow = [C.sb("prow%d" % i, [P, 4098], BF16) for i in range(4)]
        swt = C.sb("swt", [P, 48, 4], F32)
        self.load("sp", swt, swt.t[:].rearrange("p a b -> p (a b)"), swT, swT.t.ap())
        ctmp = C.sb("ctmp", [P, 1024], F32)
        uo = [C.sb("uo%d" % i, [P, 1024], BF16) for i in range(2)]
        utm = [C.sb("utm%d" % i, [P, 8, P], BF16) for i in range(2)]
        rt = [C.sb("rtD%d" % i, [P, 2, 256], F32) for i in range(2)]
        tm = [C.sb("tmD%d" % i, [P, 256], F32) for i in range(2)]
        qk = [C.sb("qk%d" % i, [P, 512], BF16) for i in range(2)]
        qkT = [C.sb("qkT%d" % i, [P, 4, P], BF16) for i in range(2)]
        ot = [C.sb("otD%d" % i, [P, 512], BF16) for i in range(2)]
        for pw in prow:
            C.op("dve", lambda e, pw=pw: e.memset(pw.t[:, 0:1], 0.0), writes=[pw])
            C.op("dve", lambda e, pw=pw: e.memset(pw.t[:, 4097:4098], 0.0), writes=[pw])
        wv = w_in.t.ap().rearrange("(kc p) n -> p kc n", p=P)
        hv = hxT.t.ap().rearrange("(kc p) t -> p kc t", p=P)
        groups = []
        for g in range(28):
            col0 = g * 512
            if g < 12: kind = "hy"
            elif g < 14: kind = "q"
            elif g < 22: kind = "gate"
            elif g < 24: kind = "k"
            else: kind = "v"
            groups.append((col0, kind))
        cnt = dict(stg=0, hb=0, uo=0, utm=0, rt=0, qk=0, qkT=0, ot=0, ev=0)

        def nxt(lst, key):
            r = lst[cnt[key] % len(lst)]; cnt[key] += 1; return r
        def wpieces(g, lo, hi):
            if g >= len(groups):
                return
            c0 = groups[g][0]; Wn = wb[g % 2]
            for q in range(lo, hi):
                s_ = nxt(stg, "stg")
                self.load("sp", s_, s_.t[:], w_in, wv[:, q * 2:(q + 1) * 2, c0:c0 + 512])
                self.copy(self.cast_eng(), Wn, Wn.t[:, q * 2:(q + 1) * 2, :], s_, s_.t[:])

        def hjob(k):
            def job():
                Hn = hb[k % 2]
                self.load("sp", Hn, Hn.t[:], hxT, hv[:, :, (k % 8) * 512:(k % 8 + 1) * 512])
                return Hn
            return job
        hst = Stream([hjob(k) for k in range(28 * 8)])
        wpieces(0, 0, 16)
        for gi, (col0, kind) in enumerate(groups):
            W = wb[gi % 2]
            for tb in range(8):
                H = hst.next()
                wpieces(gi + 1, tb * 2, tb * 2 + 2)
                if kind in ("hy", "gate"):
                    for ct in range(4):
                        pr = self.nextps()
                        self.mm(pr, pr.t[:, :], [(W.t[:, kc, ct * P:(ct + 1) * P], H.t[:, kc, :]) for kc in range(32)], [W, H])
                        if kind == "hy":
                            pw = prow[ct]
                            cnt["ev"] += 1
                            self.copy("act" if cnt["ev"] % 2 else "dve", pw, pw.t[:, 1 + tb * 512:1 + (tb + 1) * 512], pr, pr.t[:, :])
                        else:
                            O = nxt(ot, "ot")
                            C.op("act", lambda e, O=O, pr=pr: e.activation(out=O.t[:], in_=pr.t[:, :], func=AF.Silu), reads=[pr], writes=[O])
                            r0 = (col0 - 7168) + ct * P
                            self.store("sp", sg, sg.t.ap()[r0:r0 + P, tb * 512:(tb + 1) * 512], O, O.t[:])
                else:
                    for tt in range(4):
                        pr = self.nextps()
                        self.mm(pr, pr.t[:, :], [(H.t[:, kc, tt * P:(tt + 1) * P], W.t[:, kc, :]) for kc in range(32)], [W, H])
                        tok0 = tb * 512 + tt * P
                        if kind in ("q", "k"):
                            R = nxt(rt, "rt")
                            tab = rot_x if kind == "q" else rot_xk
                            self.load("sp", R, R.t[:].rearrange("p a b -> p (a b)"), tab, tab.t.ap()[tok0:tok0 + P, :])
                            Q = nxt(qk, "qk")
                            self.rotary(pr, R, R.t[:], Q, Q.t[:, :], tm)
                            if kind == "k":
                                c0 = col0 - 11264
                                self.store("sp", k_tm, k_tm.t.ap()[tok0:tok0 + P, c0:c0 + 512], Q, Q.t[:])
                                dstT = kT_d; hh0 = c0 // P
                            else:
                                dstT = qT_d; hh0 = (col0 - 6144) // P
                            pt = self.nextps(); pv = self.psb(pt)

                            def fnT(e, pv=pv, Q=Q):
                                inst = None
                                for h in range(4):
                                    inst = e.transpose(pv[:, h * P:(h + 1) * P], Q.t[:, h * P:(h + 1) * P], self.identb.t[:])
                                return inst
                            C.op("pe", fnT, reads=[Q, self.identb], writes=[pt])
                            QT = nxt(qkT, "qkT")
                            self.copy("act", QT, QT.t[:].rearrange("p a b -> p (a b)"), pt, pv[:, 0:512])
                            self.store("sp", dstT, dstT.t.ap().rearrange("(h p) t -> p h t", p=P)[:, hh0:hh0 + 4, tok0:tok0 + P], QT, QT.t[:])
                        else:
                            O = nxt(ot, "ot")
                            cnt["ev"] += 1
                            self.copy("act" if cnt["ev"] % 2 else "dve", O, O.t[:], pr, pr.t[:, :])
                            c0 = col0 - 12288
                            self.store("sp", vr_tm, vr_tm.t.ap()[tok0:tok0 + P, c0:c0 + 512], O, O.t[:])
            if kind == "hy":
                for ct in range(4):
                    colt = col0 // P + ct
                    pw = prow[ct]
                    for ch in range(4):
                        s0 = ch * 1024
                        C.op("dve", lambda e, pw=pw, s0=s0, colt=colt: e.tensor_scalar(out=ctmp.t[:], in0=pw.t[:, s0:s0 + 1024], scalar1=swt.t[:, colt, 0:1], scalar2=swt.t[:, colt, 3:4], op0=ALU.mult, op1=ALU.add), reads=[pw, swt], writes=[ctmp])
                        C.op("dve", lambda e, pw=pw, s0=s0, colt=colt: e.scalar_tensor_tensor(out=ctmp.t[:], in0=pw.t[:, s0 + 1:s0 + 1025], scalar=swt.t[:, colt, 1:2], in1=ctmp.t[:], op0=ALU.mult, op1=ALU.add), reads=[pw, swt, ctmp], writes=[ctmp])
                        U = nxt(uo, "uo")
                        C.op("dve", lambda e, pw=pw, s0=s0, colt=colt, U=U: e.scalar_tensor_tensor(out=U.t[:], in0=pw.t[:, s0 + 2:s0 + 1026], scalar=swt.t[:, colt, 2:3], in1=ctmp.t[:], op0=ALU.mult, op1=ALU.add), reads=[pw, swt, ctmp], writes=[U])
                        if col0 >= 4096:
                            r0 = (col0 - 4096) + ct * P
                            self.store("sp", x2_cm, x2_cm.t.ap()[r0:r0 + P, s0:s0 + 1024], U, U.t[:])
                        else:
                            dst = v_tm if col0 < 2048 else x1_tm
                            cc0 = (col0 % 2048) + ct * P
                            pt = self.nextps(); pv = self.psb(pt)

                            def fnU(e, pv=pv, U=U):
                                inst = None
                                for j in range(8):
                                    inst = e.transpose(pv[:, j * P:(j + 1) * P], U.t[:, j * P:(j + 1) * P], self.identb.t[:])
                                return inst
                            C.op("pe", fnU, reads=[U, self.identb], writes=[pt])
                            UT = nxt(utm, "utm")
                            cnt["ev"] += 1
                            self.copy("act" if cnt["ev"] % 2 else "pool" if False else "act", UT, UT.t[:].rearrange("p a b -> p (a b)"), pt, pv[:, 0:1024])
                            self.store("sp", dst, dst.t.ap().rearrange("(tt p) c -> p tt c", p=P)[:, ch * 8:(ch + 1) * 8, cc0:cc0 + P], UT, UT.t[:])
        C.pop()


    def slab_stream(self, cst, seq, slabs):
        def mk(k, mt):
            def job():
                SL = slabs[k % len(slabs)]
                self.load("sp", SL, SL.t[:].rearrange("p a b c -> p (a b c)"), cst, cst.t.ap()[mt])
                return SL
            return job
        return Stream([mk(k, mt) for k, mt in enumerate(seq)])

    def phase_E(self, fw1, fw2, fw3, fw4, fvec, zT, negt, deltab, hyb, cst, fscale, spec):
        C = self.C; M = self.M
        PI = math.pi
        C.push()
        h3 = C.sb("h3", [64, T], F32)
        fv = C.sb("fv", [64, 4], F32)
        self.load("sp", fv, fv.t[:], fvec, fvec.t.ap())
        arg = C.sb("argE", [64, 512], F32); mk = C.sb("mkE", [64, 512], F32)
        C.push()
        zt = C.sb("zt", [33, T], F32); hA = C.sb("hA", [64, T], F32); hB = C.sb("hB", [64, T], F32)
        w1 = C.sb("w1", [33, 64], F32); w2 = C.sb("w2", [64, 64], F32); w3 = C.sb("w3", [64, 64], F32)
        self.load("sp", zt, zt.t[:], zT, zT.t.ap())
        self.load("sp", w1, w1.t[:], fw1, fw1.t.ap()); self.load("sp", w2, w2.t[:], fw2, fw2.t.ap()); self.load("sp", w3, w3.t[:], fw3, fw3.t.ap())
        layers = [(zt, 33, w1, hA), (hA, 64, w2, hB), (hB, 64, w3, h3)]
        for li, (src, kk, w, dst) in enumerate(layers):
            for nb in range(8):
                pr = self.nextps()
                C.op("pe", lambda e, pr=pr, w=w, src=src, kk=kk, nb=nb: e.matmul(pr.t[0:64, :], w.t[0:kk, :], src.t[0:kk, nb * 512:(nb + 1) * 512], start=True, stop=True), reads=[w, src], writes=[pr])
                C.op("dve", lambda e, pr=pr, li=li: e.tensor_scalar(out=arg.t[:], in0=pr.t[0:64, :], scalar1=fv.t[:, li:li + 1], scalar2=fv.t[:, 3:4], op0=ALU.add, op1=ALU.mult), reads=[pr, fv], writes=[arg])
                C.op("dve", lambda e: e.tensor_scalar(out=mk.t[:], in0=arg.t[:], scalar1=PI, scalar2=-2.0 * PI, op0=ALU.is_gt, op1=ALU.mult), reads=[arg], writes=[mk])
                C.op("dve", lambda e: e.tensor_tensor(out=arg.t[:], in0=arg.t[:], in1=mk.t[:], op=ALU.add), reads=[arg, mk], writes=[arg])
                C.op("dve", lambda e: e.tensor_scalar(out=mk.t[:], in0=arg.t[:], scalar1=-PI, scalar2=2.0 * PI, op0=ALU.is_lt, op1=ALU.mult), reads=[arg], writes=[mk])
                C.op("dve", lambda e: e.tensor_tensor(out=arg.t[:], in0=arg.t[:], in1=mk.t[:], op=ALU.add), reads=[arg, mk], writes=[arg])
                C.op("act", lambda e, dst=dst, nb=nb: e.activation(out=dst.t[:, nb * 512:(nb + 1) * 512], in_=arg.t[:], func=AF.Sin), reads=[arg], writes=[dst])
        C.pop()
        w4 = C.sb("w4", [64, 2, 512], F32); w4b = C.sb("w4b", [64, 2, 512], BF16)
        h3b = C.sb("h3b", [64, T], BF16)
        C.op("dve", lambda e: e.tensor_copy(h3b.t[:], h3.t[:]), reads=[h3], writes=[h3b])
        ngt = C.sb("ngt", [P, NT], F32); delt = C.sb("delt", [P, HYW], F32); hybt = C.sb("hybt", [P, 512], F32); fsc = C.sb("fsc", [P, NFT], F32)
        self.load("sp", ngt, ngt.t[:], negt, negt.t.ap()); self.load("sp", delt, delt.t[:], deltab, deltab.t.ap())
        self.load("sp", fsc, fsc.t[:], fscale, fscale.t.ap())
        edt = C.sb("edt", [P, 32, 2, 512], BF16)
        slabs = [C.sb("slabE%d" % i, [P, 2, NFT, P], BF16) for i in range(2)]
        sst = self.slab_stream(cst, [mt for _ in range(8) for mt in range(NFT)], slabs)
        dect = C.sb("dect", [P, 512], F32)
        tf = [C.sb("tfE%d" % i, [P, 512], F32) for i in range(2)]; tb = [C.sb("tbE%d" % i, [P, 512], F32) for i in range(2)]
        af = C.sb("afE", [P, 512], F32); ab = [C.sb("abE%d" % i, [P, 512], F32) for i in range(2)]
        rn = C.sb("rnE", [P, 512], F32)
        stt = [C.sb("stE%d" % i, [P, 2, 512], F32) for i in range(2)]
        pacc = self.pacc
        specv = spec.t.ap().rearrange("k (two c) -> k two c", two=2)
        si = 0
        for o in range(2):
            for cb in range(4):
                colF = o * 2048 + cb * 512; colB = 4096 + colF
                self.load("sp", hybt, hybt.t[:], hyb, hyb.t.ap()[:, colF:colF + 512])
                self.load("sp", w4, w4.t[:, 0, :], fw4, fw4.t.ap()[:, colF:colF + 512])
                self.load("sp", w4, w4.t[:, 1, :], fw4, fw4.t.ap()[:, colB:colB + 512])
                C.op("dve", lambda e: e.tensor_copy(w4b.t[:].rearrange("p a b -> p (a b)"), w4.t[:].rearrange("p a b -> p (a b)")), reads=[w4], writes=[w4b])
                for lt in range(NT):
                    TF = tf[lt % 2]; TB = tb[lt % 2]; AB_ = ab[lt % 2]
                    prF = self.nextps(); prB = self.nextps()
                    C.op("pe", lambda e, prF=prF, lt=lt, colF=colF: e.matmul(prF.t[:, :], h3b.t[:, lt * P:(lt + 1) * P], w4b.t[:, 0, :], start=True, stop=True), reads=[h3b, w4b], writes=[prF])
                    C.op("pe", lambda e, prB=prB, lt=lt, colB=colB: e.matmul(prB.t[:, :], h3b.t[:, lt * P:(lt + 1) * P], w4b.t[:, 1, :], start=True, stop=True), reads=[h3b, w4b], writes=[prB])
                    C.op("act", lambda e, lt=lt, cb=cb: e.activation(out=dect.t[:], in_=delt.t[:, cb * 512:(cb + 1) * 512], func=AF.Exp, scale=ngt.t[:, lt:lt + 1]), reads=[delt, ngt], writes=[dect])
                    C.op("dve", lambda e, TF=TF, prF=prF: e.tensor_tensor(out=TF.t[:], in0=prF.t[:, :], in1=dect.t[:], op=ALU.mult), reads=[prF, dect], writes=[TF])
                    C.op("dve", lambda e, TB=TB, prB=prB: e.tensor_tensor(out=TB.t[:], in0=prB.t[:, :], in1=dect.t[:], op=ALU.mult), reads=[prB, dect], writes=[TB])
                    if lt == 0:
                        C.op("dve", lambda e, TB=TB: e.memset(TB.t[0:1, :], 0.0), reads=[TB], writes=[TB])
                    C.op("pool", lambda e, TF=TF, TB=TB, lt=lt: e.tensor_tensor(out=edt.t[:, lt, 0, :], in0=TF.t[:], in1=TB.t[:], op=ALU.add), reads=[TF, TB], writes=[edt])
                    C.op("pool", lambda e, TF=TF, TB=TB, lt=lt: e.tensor_tensor(out=edt.t[:, lt, 1, :], in0=TF.t[:], in1=TB.t[:], op=ALU.subtract), reads=[TF, TB], writes=[edt])
                    C.op("act", lambda e, TF=TF: e.activation(out=af.t[:], in_=TF.t[:], func=AF.Abs), reads=[TF], writes=[af])
                    C.op("act", lambda e, TB=TB, AB_=AB_: e.activation(out=AB_.t[:], in_=TB.t[:], func=AF.Abs), reads=[TB], writes=[AB_])
                    C.op("pool", lambda e, AB_=AB_: e.tensor_tensor(out=AB_.t[:], in0=AB_.t[:], in1=af.t[:], op=ALU.add), reads=[AB_, af], writes=[AB_])
                    C.op("pe", lambda e, AB_=AB_, lt=lt: e.matmul(pacc.t[:, :], M("ones"), AB_.t[:], start=(lt == 0), stop=(lt == NT - 1)), reads=[AB_, self.mt], writes=[pacc])
                C.op("dve", lambda e: e.reciprocal(rn.t[:], pacc.t[:, :]), reads=[pacc], writes=[rn])
                for mt in range(NFT):
                    SL = sst.next()
                    prR = self.nextps(); prW = self.nextps()
                    self.mm(prR, prR.t[:, :], [(SL.t[:, 0, kt, :], edt.t[:, kt, 0, :]) for kt in range(NT)], [SL, edt])
                    self.mm(prW, prW.t[:, :], [(SL.t[:, 1, kt, :], edt.t[:, kt, 1, :]) for kt in range(NT)], [SL, edt])
                    ST = stt[si % 2]; si += 1
                    C.op("dve", lambda e, ST=ST, prR=prR: e.tensor_tensor(out=ST.t[:, 0, :], in0=prR.t[:, :], in1=rn.t[:], op=ALU.mult), reads=[prR, rn], writes=[ST])
                    C.op("dve", lambda e, ST=ST, colF=colF: e.tensor_tensor(out=ST.t[:, 0, :], in0=ST.t[:, 0, :], in1=hybt.t[:], op=ALU.add), reads=[ST, hybt], writes=[ST])
                    C.op("dve", lambda e, ST=ST, prW=prW: e.tensor_tensor(out=ST.t[:, 1, :], in0=prW.t[:, :], in1=rn.t[:], op=ALU.mult), reads=[prW, rn], writes=[ST])
                    C.op("pool", lambda e, ST=ST, mt=mt: e.tensor_scalar(out=ST.t[:].rearrange("p a b -> p (a b)"), in0=ST.t[:].rearrange("p a b -> p (a b)"), scalar1=fsc.t[:, mt:mt + 1], scalar2=None, op0=ALU.mult), reads=[ST, fsc], writes=[ST])
                    self.store("sp", spec, specv[mt * P:(mt + 1) * P, :, colF:colF + 512], ST, ST.t[:])
        C.pop()

    def phase_F(self, v_tm, x1_tm, x2_cm, spec, cst, yT):
        C = self.C; M = self.M
        C.push()
        utm = C.sb("utmF", [P, NT, 512], BF16); x1t = C.sb("x1tF", [P, NT, 512], BF16)
        Y = C.sb("YF", [P, NFT, 2, 512], BF16)
        slabs = [C.sb("slabF%d" % i, [P, 2, NFT, P], BF16) for i in range(2)]
        spt = [C.sb("sptF%d" % i, [P, 2, 512], F32) for i in range(2)]
        sst = self.slab_stream(cst, [m_ for _ in range(8) for m_ in (list(range(NFT)) + list(range(NT)))], slabs)

        def spjob(k):
            cb_, rem = divmod(k, 2 * NFT); o_, mt_ = divmod(rem, NFT)
            c0_ = o_ * 2048 + cb_ * 512

            def job():
                SPn = spt[k % 2]
                self.load("sp", SPn, SPn.t[:], spec, specv[mt_ * P:(mt_ + 1) * P, :, c0_:c0_ + 512])
                return SPn
            return job
        t4 = [C.sb("t4F%d" % i, [P, 512], F32) for i in range(4)]
        yb = [C.sb("ybF%d" % i, [P, 512], BF16) for i in range(2)]
        x2t = [C.sb("x2tF%d" % i, [P, 4, P], BF16) for i in range(2)]
        yo = [C.sb("yoF%d" % i, [P, 4, P], BF16) for i in range(2)]
        specv = spec.t.ap().rearrange("k (two c) -> k two c", two=2)
        cnt = dict(sp=0, yb=0, x2=0, yo=0)
        spst = Stream([spjob(k) for k in range(4 * 2 * NFT)])

        def conv(o, cb, cbfn):
            c0 = o * 2048 + cb * 512
            for mt in range(NFT):
                SL = sst.next()
                prR = self.nextps(); prI = self.nextps()
                self.mm(prR, prR.t[:, :], [(SL.t[:, 0, kt, :], utm.t[:, kt, :]) for kt in range(NT)], [SL, utm])
                self.mm(prI, prI.t[:, :], [(SL.t[:, 1, kt, :], utm.t[:, kt, :]) for kt in range(NT)], [SL, utm])
                SP = spst.next()
                a, b_, c_, d_ = t4
                C.op("dve", lambda e, prR=prR, SP=SP: e.tensor_tensor(out=a.t[:], in0=prR.t[:, :], in1=SP.t[:, 0, :], op=ALU.mult), reads=[prR, SP], writes=[a])
                C.op("dve", lambda e, prI=prI, SP=SP: e.tensor_tensor(out=b_.t[:], in0=prI.t[:, :], in1=SP.t[:, 1, :], op=ALU.mult), reads=[prI, SP], writes=[b_])
                C.op("pool", lambda e, mt=mt: e.tensor_tensor(out=Y.t[:, mt, 0, :], in0=a.t[:], in1=b_.t[:], op=ALU.subtract), reads=[a, b_], writes=[Y])
                C.op("dve", lambda e, prR=prR, SP=SP: e.tensor_tensor(out=c_.t[:], in0=prR.t[:, :], in1=SP.t[:, 1, :], op=ALU.mult), reads=[prR, SP], writes=[c_])
                C.op("dve", lambda e, prI=prI, SP=SP: e.tensor_tensor(out=d_.t[:], in0=prI.t[:, :], in1=SP.t[:, 0, :], op=ALU.mult), reads=[prI, SP], writes=[d_])
                C.op("pool", lambda e, mt=mt: e.tensor_tensor(out=Y.t[:, mt, 1, :], in0=c_.t[:], in1=d_.t[:], op=ALU.add), reads=[c_, d_], writes=[Y])
            for it in range(NT):
                SL = sst.next()
                pr = self.nextps()
                self.mm(pr, pr.t[:, :], [(SL.t[:, 0, kt, :], Y.t[:, kt, 0, :]) for kt in range(NFT)] + [(SL.t[:, 1, kt, :], Y.t[:, kt, 1, :]) for kt in range(NFT)], [SL, Y])
                cbfn(it, pr)

        for cb in range(4):
            self.load("sp", utm, utm.t[:], v_tm, v_tm.t.ap().rearrange("(tt p) c -> p tt c", p=P)[:, :, cb * 512:(cb + 1) * 512])
            self.load("act", x1t, x1t.t[:], x1_tm, x1_tm.t.ap().rearrange("(tt p) c -> p tt c", p=P)[:, :, cb * 512:(cb + 1) * 512])

            def f1(it, pr):
                C.op("dve", lambda e: e.tensor_tensor(out=utm.t[:, it, :], in0=pr.t[:, :], in1=x1t.t[:, it, :], op=ALU.mult), reads=[pr, x1t], writes=[utm])

            def f2(it, pr, cb=cb):
                YB = yb[cnt["yb"] % 2]; cnt["yb"] += 1
                self.copy("act", YB, YB.t[:], pr, pr.t[:, :])
                pt = self.nextps(); pv = self.psb(pt)

                def fnT(e):
                    inst = None
                    for j in range(4):
                        inst = e.transpose(pv[:, j * P:(j + 1) * P], YB.t[:, j * P:(j + 1) * P], self.identb.t[:])
                    return inst
                C.op("pe", fnT, reads=[YB, self.identb], writes=[pt])
                X2 = x2t[cnt["x2"] % 2]; cnt["x2"] += 1
                self.load("sp", X2, X2.t[:], x2_cm, x2_cm.t.ap().rearrange("(j p) t -> p j t", p=P)[:, cb * 4:(cb + 1) * 4, it * P:(it + 1) * P])
                YO = yo[cnt["yo"] % 2]; cnt["yo"] += 1
                C.op("dve", lambda e: e.tensor_tensor(out=YO.t[:].rearrange("p a b -> p (a b)"), in0=pv[:, 0:512], in1=X2.t[:].rearrange("p a b -> p (a b)"), op=ALU.mult), reads=[pt, X2], writes=[YO])
                self.store("sp", yT, yT.t.ap().rearrange("(j p) t -> p j t", p=P)[:, cb * 4:(cb + 1) * 4, it * P:(it + 1) * P], YO, YO.t[:])
            conv(0, cb, f1)
            conv(1, cb, f2)
        C.pop()


    def phase_G(self, qT_d, kT_d, k_tm, vr_tm, sg, Sfd, lg, yT):
        C = self.C; M = self.M
        C.push()
        Sf = [C.sb("SfG%d" % i, [P, 256], F32) for i in range(16)]
        Sb = [C.sb("SbG%d" % i, [P, 256], BF16) for i in range(16)]
        for i in range(16):
            self.load("sp" if i % 2 else "act", Sf[i], Sf[i].t[:], Sfd, Sfd.t.ap()[:, i * 256:(i + 1) * 256])
            C.op("pool", lambda e, i=i: e.tensor_copy(Sb[i].t[:], Sf[i].t[:]), reads=[Sf[i]], writes=[Sb[i]])
        decT = C.sb("decT", [P, 16, P], F32); qdec = C.sb("qdec", [P, 16, P], F32)
        kdec = C.sb("kdec", [P, 16], F32); cdec = C.sb("cdec", [P, 16], F32)
        for d in range(2):
            for h in range(8):
                dh = d * 8 + h
                C.op("act", lambda e, d=d, dh=dh: e.activation(out=decT.t[:, dh, :], in_=M("diffF" if d == 0 else "diffB"), func=AF.Exp, scale=lg.t[:, dh:dh + 1]), reads=[lg, self.mt], writes=[decT])
                C.op("dve", lambda e, d=d, dh=dh: e.tensor_tensor(out=decT.t[:, dh, :], in0=decT.t[:, dh, :], in1=M("maskF" if d == 0 else "maskB"), op=ALU.mult), reads=[decT, self.mt], writes=[decT])
                C.op("act", lambda e, d=d, dh=dh: e.activation(out=qdec.t[:, dh, :], in_=M("ip1" if d == 0 else "i128m"), func=AF.Exp, scale=lg.t[:, dh:dh + 1]), reads=[lg, self.mt], writes=[qdec])
            C.op("act", lambda e, d=d: e.activation(out=kdec.t[:, d * 8:(d + 1) * 8], in_=lg.t[:, d * 8:(d + 1) * 8], func=AF.Exp, scale=M("cols")[:, d:d + 1]), reads=[lg, self.mt], writes=[kdec])
        C.op("act", lambda e: e.activation(out=cdec.t[:], in_=lg.t[:], func=AF.Exp, scale=128.0), reads=[lg], writes=[cdec])
        nb_ = 3
        qTc = [[C.sb("qTc%d%d" % (d, i), [P, 8, P], BF16) for i in range(nb_)] for d in range(2)]
        kTc = [[C.sb("kTc%d%d" % (d, i), [P, 8, P], BF16) for i in range(nb_)] for d in range(2)]
        ktm = [[C.sb("ktm%d%d" % (d, i), [P, KC], BF16) for i in range(nb_)] for d in range(2)]
        vtm = [[C.sb("vtm%d%d" % (d, i), [P, VC], BF16) for i in range(nb_)] for d in range(2)]
        sgc = [[C.sb("sgc%d%d" % (d, i), [P, 16, P], BF16) for i in range(nb_)] for d in range(2)]
        ytl = [[C.sb("ytl%d%d" % (d, i), [P, 16, P], BF16) for i in range(nb_)] for d in range(2)]
        qv = qT_d.t.ap().rearrange("(h p) t -> p h t", p=P); kv = kT_d.t.ap().rearrange("(h p) t -> p h t", p=P)
        sgv = sg.t.ap().rearrange("(ha p) t -> p ha t", p=P); yv = yT.t.ap().rearrange("(ha p) t -> p ha t", p=P)
        attm = [C.sb("attmP%d" % i, [P, P], BF16) for i in range(4)]
        qd = [C.sb("qdP%d" % i, [P, P], BF16) for i in range(4)]
        kd = [C.sb("kdP%d" % i, [P, P], BF16) for i in range(4)]
        sq = [C.sb("sqP%d" % i, [P, 256], BF16) for i in range(3)]
        sd = [C.sb("sdP%d" % i, [P, P], F32) for i in range(3)]
        rs = [C.sb("rsP%d" % i, [P, P], F32) for i in range(3)]
        on = [C.sb("onP%d" % i, [P, 256], F32) for i in range(3)]
        items = []
        for s_ in range(NT):
            for d in range(2):
                c = s_ if d == 0 else NT - 1 - s_
                for h in range(8):
                    items.append(dict(s=s_, d=d, c=c, h=h, dh=d * 8 + h, n=len(items)))

        def stage_load(it):
            d = it["d"]; bi = it["s"] % nb_; c = it["c"]
            bufs = dict(Q=qTc[d][bi], Kt=kTc[d][bi], KM=ktm[d][bi], V=vtm[d][bi], G=sgc[d][bi], YT=ytl[d][bi])
            if it["h"] == 0:
                tk = slice(c * P, (c + 1) * P)
                self.load("sp", bufs["Q"], bufs["Q"].t[:], qT_d, qv[:, :, tk]); self.load("sp", bufs["Kt"], bufs["Kt"].t[:], kT_d, kv[:, :, tk])
                self.load("sp", bufs["KM"], bufs["KM"].t[:], k_tm, k_tm.t.ap()[tk, :]); self.load("sp", bufs["V"], bufs["V"].t[:], vr_tm, vr_tm.t.ap()[tk, :])
                self.load("sp", bufs["G"], bufs["G"].t[:], sg, sgv[:, d * 16:(d + 1) * 16, tk])
            it.update(bufs)

        def stage_a(it):
            h = it["h"]; dh = it["dh"]; n = it["n"]
            Q = it["Q"]; Kt = it["Kt"]; KM_ = it["KM"]
            AT = attm[n % 4]; QD = qd[n % 4]; KD = kd[n % 4]
            it.update(AT=AT, QD=QD, KD=KD)
            pa = self.nextps()
            self.mm(pa, pa.t[:, 0:P], [(Kt.t[:, h, :], Q.t[:, h, :])], [Kt, Q])
            C.op("dve", lambda e: e.tensor_tensor(out=AT.t[:], in0=pa.t[:, 0:P], in1=decT.t[:, dh, :], op=ALU.mult), reads=[pa, decT], writes=[AT])
            C.op("pool", lambda e: e.tensor_tensor(out=QD.t[:], in0=Q.t[:, h, :], in1=qdec.t[:, dh, :], op=ALU.mult), reads=[Q, qdec], writes=[QD])
            C.op("pool", lambda e: e.tensor_scalar(out=KD.t[:], in0=KM_.t[:, h * P:(h + 1) * P], scalar1=kdec.t[:, dh:dh + 1], scalar2=None, op0=ALU.mult), reads=[KM_, kdec], writes=[KD])

        def stage_b(it):
            h = it["h"]; dh = it["dh"]; n = it["n"]; V = it["V"]; AT = it["AT"]; QD = it["QD"]; KD = it["KD"]
            SQ = sq[n % 3]
            po = self.nextps()
            it.update(po=po, SQ=SQ)

            def fo(e):
                inst = None
                for a_ in range(2):
                    e.matmul(po.t[:, a_ * P:(a_ + 1) * P], V.t[:, h * 256 + a_ * P:h * 256 + (a_ + 1) * P], AT.t[:], start=True, stop=False)
                    inst = e.matmul(po.t[:, a_ * P:(a_ + 1) * P], Sb[dh].t[:, a_ * P:(a_ + 1) * P], QD.t[:], start=False, stop=True)
                return inst
            C.op("pe", fo, reads=[V, AT, QD, Sb[dh]], writes=[po])
            C.op("act", lambda e: e.activation(out=SQ.t[:], in_=po.t[:, 0:256], func=AF.Square), reads=[po], writes=[SQ])
            psn = self.nextps()
            self.mm(psn, psn.t[:, 0:256], [(KD.t[:], V.t[:, h * 256:(h + 1) * 256])], [KD, V])
            C.op("dve", lambda e: e.scalar_tensor_tensor(out=Sf[dh].t[:], in0=Sf[dh].t[:], scalar=cdec.t[:, dh:dh + 1], in1=psn.t[:, 0:256], op0=ALU.mult, op1=ALU.add), reads=[Sf[dh], cdec, psn], writes=[Sf[dh]])
            C.op("pool", lambda e: e.tensor_copy(Sb[dh].t[:], Sf[dh].t[:]), reads=[Sf[dh]], writes=[Sb[dh]])

        def stage_c(it):
            h = it["h"]; n = it["n"]; po = it["po"]; SQ = it["SQ"]; G = it["G"]; YT = it["YT"]
            SD = sd[n % 3]; RS = rs[n % 3]; ON = on[n % 3]
            pss = self.nextps()
            self.mm(pss, pss.t[:, 0:P], [(self.onesb.t[:], SQ.t[:, 0:P]), (self.onesb.t[:], SQ.t[:, P:256])], [self.onesb, SQ])
            C.op("act", lambda e: e.activation(out=SD.t[:], in_=pss.t[:, 0:P], func=AF.Sqrt, scale=1.0 / 256.0, bias=M("cols")[:, 6:7]), reads=[pss, self.mt], writes=[SD])
            C.op("dve", lambda e: e.reciprocal(RS.t[:], SD.t[:]), reads=[SD], writes=[RS])
            for a_ in range(2):
                C.op("dve", lambda e, a_=a_: e.tensor_tensor(out=ON.t[:, a_ * P:(a_ + 1) * P], in0=po.t[:, a_ * P:(a_ + 1) * P], in1=RS.t[:], op=ALU.mult), reads=[po, RS], writes=[ON])
            C.op("pool", lambda e: e.tensor_tensor(out=YT.t[:, h * 2:h * 2 + 2, :], in0=ON.t[:].rearrange("p (a b) -> p a b", a=2), in1=G.t[:, h * 2:h * 2 + 2, :], op=ALU.mult), reads=[ON, G], writes=[YT])
            if h == 7:
                tk = slice(it["c"] * P, (it["c"] + 1) * P)
                self.store("sp", yT, yv[:, 16 + it["d"] * 16:16 + (it["d"] + 1) * 16, tk], YT, YT.t[:])
        NI = len(items)
        for i in range(NI + 2):
            if i < NI:
                stage_load(items[i]); stage_a(items[i])
            if 0 <= i - 1 < NI:
                stage_b(items[i - 1])
            if 0 <= i - 2 < NI:
                stage_c(items[i - 2])
        C.pop()


    def phase_H(self, yT, w_out, x, bcd, x1r, hx2, rwT, aff_tm):
        C = self.C; M = self.M
        yv = yT.t.ap().rearrange("(kc p) t -> p kc t", p=P)
        C.push()
        ya = [C.sb("yaH%d" % i, [P, 16, 512], BF16) for i in range(2)]
        yb_ = [C.sb("ybH%d" % i, [P, 16, 512], BF16) for i in range(2)]
        for tb in range(8):
            A_ = ya[tb % 2]; B_ = yb_[tb % 2]; ts_ = slice(tb * 512, (tb + 1) * 512)
            self.load("sp", A_, A_.t[:], yT, yv[:, 16:32, ts_]); self.load("sp", B_, B_.t[:], yT, yv[:, 32:48, ts_])
            C.op("dve", lambda e, A_=A_, B_=B_: e.tensor_tensor(out=A_.t[:].rearrange("p a b -> p (a b)"), in0=A_.t[:].rearrange("p a b -> p (a b)"), in1=B_.t[:].rearrange("p a b -> p (a b)"), op=ALU.add), reads=[A_, B_], writes=[A_])
            self.store("sp", yT, yv[:, 16:32, ts_], A_, A_.t[:])
        C.pop()
        C.push()
        stg = [C.sb("stgH%d" % i, [P, 8, 512], F32) for i in range(2)]
        wb = [C.sb("wbH%d" % i, [P, 32, 512], BF16) for i in range(2)]
        g1s = [C.sb("g1s%d" % i, [P, 512], F32) for i in range(2)]
        yh = [C.sb("yh%d" % i, [P, 32, 512], BF16) for i in range(2)]
        xs_ = [C.sb("xsH%d" % i, [P, 4, 512], F32) for i in range(2)]
        ob = [C.sb("obH%d" % i, [P, 4, 512], F32) for i in range(2)]
        wv = w_out.t.ap().rearrange("(kc p) n -> p kc n", p=P)
        xv = x.t.ap().rearrange("(tt p) c -> p tt c", p=P); ov = x1r.t.ap().rearrange("(tt p) c -> p tt c", p=P)
        sgi = [0]

        def wpieces(db, lo, hi):
            if db >= 8:
                return
            Wn = wb[db % 2]
            for q in range(lo, hi):
                s_ = stg[sgi[0] % 2]; sgi[0] += 1
                self.load("sp", s_, s_.t[:], w_out, wv[:, q * 8:(q + 1) * 8, db * 512:(db + 1) * 512])
                self.copy(self.cast_eng(), Wn, Wn.t[:, q * 8:(q + 1) * 8, :], s_, s_.t[:])
            if hi == 4:
                Gn = g1s[db % 2]
                self.load("sp", Gn, Gn.t[:], bcd, bcd.t.ap()[:, db * 512:(db + 1) * 512])

        def yjob(k):
            def job():
                db_, tb_ = divmod(k, 8)
                Yn = yh[k % 2]; Xn = xs_[k % 2]
                self.load("sp", Yn, Yn.t[:], yT, yv[:, 0:32, tb_ * 512:(tb_ + 1) * 512])
                self.load("sp", Xn, Xn.t[:], x, xv[:, tb_ * 4:(tb_ + 1) * 4, db_ * 512:(db_ + 1) * 512])
                return (Yn, Xn)
            return job
        yst = Stream([yjob(k) for k in range(64)])
        wpieces(0, 0, 4)
        n = 0
        for db in range(8):
            W = wb[db % 2]; G1 = g1s[db % 2]
            for tb in range(8):
                Y, X = yst.next()
                if tb % 2 == 0:
                    wpieces(db + 1, tb // 2, tb // 2 + 1)
                O = ob[n % 2]; n += 1
                for tt in range(4):
                    pr = self.nextps()
                    self.mm(pr, pr.t[:, :], [(Y.t[:, kc, tt * P:(tt + 1) * P], W.t[:, kc, :]) for kc in range(32)], [Y, W])
                    C.op("dve", lambda e, O=O, pr=pr, G1=G1, tt=tt: e.tensor_tensor(out=O.t[:, tt, :], in0=pr.t[:, :], in1=G1.t[:], op=ALU.mult), reads=[pr, G1], writes=[O])
                C.op("dve", lambda e, O=O, X=X: e.tensor_tensor(out=O.t[:].rearrange("p a b -> p (a b)"), in0=O.t[:].rearrange("p a b -> p (a b)"), in1=X.t[:].rearrange("p a b -> p (a b)"), op=ALU.add), reads=[O, X], writes=[O])
                self.store("sp", x1r, ov[:, tb * 4:(tb + 1) * 4, db * 512:(db + 1) * 512], O, O.t[:])
        C.pop()
        C.push()
        A2 = C.sb("A2b", [P, D], F32); B2 = C.sb("B2b", [P, D], F32)
        self.load("sp", A2, A2.t[:], bcd, bcd.t.ap()[:, 2 * D:3 * D]); self.load("act", B2, B2.t[:], bcd, bcd.t.ap()[:, 3 * D:4 * D])
        rw = C.sb("rw", [P, 32, NE], F32)
        self.load("sp", rw, rw.t[:].rearrange("p a b -> p (a b)"), rwT, rwT.t.ap())
        xt = [C.sb("xtH%d" % i, [P, D], F32) for i in range(2)]
        h2 = [C.sb("h2H%d" % i, [P, D], F32) for i in range(2)]
        hb16 = [C.sb("hb16%d" % i, [P, D], BF16) for i in range(2)]
        junk = C.sb("junkH", [P, D], BF16)
        sts = [C.sb("stH%d" % i, [P, 4], F32) for i in range(2)]
        h2T = [C.sb("h2T%d" % i, [P, 32, P], F32) for i in range(2)]
        sm = [C.sb("smH%d" % i, [P, 4], F32) for i in range(2)]
        ex = [C.sb("exH%d" % i, [P, NE], F32) for i in range(2)]
        for tt in range(NT):
            X = xt[tt % 2]; H2 = h2[tt % 2]; HB = hb16[tt % 2]; st = sts[tt % 2]; HT = h2T[tt % 2]; SM = sm[tt % 2]; EX = ex[tt % 2]
            tk = slice(tt * P, (tt + 1) * P)
            self.load("sp" if tt % 2 else "act", X, X.t[:], x1r, x1r.t.ap()[tk, :])
            self.rms_stats(X, junk, st, D)
            C.op("dve", lambda e, H2=H2, X=X, st=st: e.scalar_tensor_tensor(out=H2.t[:], in0=X.t[:], scalar=st.t[:, 1:2], in1=A2.t[:], op0=ALU.mult, op1=ALU.mult), reads=[X, st, A2], writes=[H2])
            C.op("pool", lambda e, H2=H2: e.tensor_tensor(out=H2.t[:], in0=H2.t[:], in1=B2.t[:], op=ALU.add), reads=[H2, B2], writes=[H2])
            C.op("act", lambda e, HB=HB, H2=H2: e.activation(out=HB.t[:], in_=H2.t[:], func=AF.Copy), reads=[H2], writes=[HB])
            self.store("sp", hx2, hx2.t.ap()[tk, :], HB, HB.t[:])
            for g in range(8):
                pt = self.nextps()

                def fnT(e, pt=pt, H2=H2, g=g):
                    inst = None
                    for j in range(4):
                        kc = g * 4 + j
                        inst = e.transpose(pt.t[:, j * P:(j + 1) * P], H2.t[:, kc * P:(kc + 1) * P], M("ident"))
                    return inst
                C.op("pe", fnT, reads=[H2, self.mt], writes=[pt])
                self.copy("act" if g % 2 else "dve", HT, HT.t[:, g * 4:(g + 1) * 4, :].rearrange("p a b -> p (a b)"), pt, pt.t[:, :])
            pl = self.nextps()
            self.mm(pl, pl.t[:, 0:NE], [(HT.t[:, kc, :], rw.t[:, kc, :]) for kc in range(32)], [HT, rw])
            C.op("dve", lambda e, SM=SM, pl=pl: e.tensor_reduce(out=SM.t[:, 0:1], in_=pl.t[:, 0:NE], axis=mybir.AxisListType.X, op=ALU.max), reads=[pl], writes=[SM])
            C.op("dve", lambda e, SM=SM: e.tensor_scalar(out=SM.t[:, 1:2], in0=SM.t[:, 0:1], scalar1=-1.0, scalar2=None, op0=ALU.mult), reads=[SM], writes=[SM])
            C.op("act", lambda e, EX=EX, pl=pl, SM=SM: e.activation(out=EX.t[:], in_=pl.t[:, 0:NE], func=AF.Exp, bias=SM.t[:, 1:2], accum_out=SM.t[:, 2:3]), reads=[pl, SM], writes=[EX, SM])
            C.op("dve", lambda e, SM=SM: e.reciprocal(SM.t[:, 3:4], SM.t[:, 2:3]), reads=[SM], writes=[SM])
            C.op("dve", lambda e, EX=EX, SM=SM, tt=tt: e.tensor_scalar(out=aff_tm.t[:, tt, :], in0=EX.t[:], scalar1=SM.t[:, 3:4], scalar2=None, op0=ALU.mult), reads=[EX, SM], writes=[aff_tm])
        C.pop()


    def phase_I(self, aff_tm, idx_all, gate_all):
        C = self.C; M = self.M
        C.push()
        affT = C.sb("affT", [NE, T], F32); junk = C.sb("junkI", [NE, T], F32)
        for g in range(8):
            pt = self.nextps()

            def fnT(e, pt=pt, g=g):
                inst = None
                for j in range(4):
                    tt = g * 4 + j
                    inst = e.transpose(pt.t[0:NE, j * P:(j + 1) * P], aff_tm.t[:, tt, :], M("ident"))
                return inst
            C.op("pe", fnT, reads=[aff_tm, self.mt], writes=[pt])
            self.copy("dve", affT, affT.t[:, g * 512:(g + 1) * 512], pt, pt.t[0:NE, :])
        bs = C.sb("bsI", [NE, 8], F32)
        C.op("dve", lambda e: e.memset(bs.t[:, 0:1], 0.0), writes=[bs])
        C.op("dve", lambda e: e.memset(bs.t[:, 1:2], 1.0), reads=[bs], writes=[bs])
        for it in range(34):
            C.op("dve", lambda e: e.tensor_scalar(out=bs.t[:, 2:3], in0=bs.t[:, 0:1], scalar1=bs.t[:, 1:2], scalar2=0.5, op0=ALU.add, op1=ALU.mult), reads=[bs], writes=[bs])
            C.op("dve", lambda e: e.tensor_scalar(out=junk.t[:], in0=affT.t[:], scalar1=bs.t[:, 2:3], scalar2=0.0, op0=ALU.is_ge, op1=ALU.add, accum_out=bs.t[:, 3:4]), reads=[affT, bs], writes=[junk, bs])
            C.op("dve", lambda e: e.tensor_scalar(out=bs.t[:, 4:5], in0=bs.t[:, 3:4], scalar1=float(CAP) - 0.5, scalar2=None, op0=ALU.is_gt), reads=[bs], writes=[bs])
            C.op("dve", lambda e: e.tensor_tensor(out=bs.t[:, 5:6], in0=bs.t[:, 2:3], in1=bs.t[:, 0:1], op=ALU.subtract), reads=[bs], writes=[bs])
            C.op("dve", lambda e: e.tensor_tensor(out=bs.t[:, 6:7], in0=bs.t[:, 1:2], in1=bs.t[:, 2:3], op=ALU.subtract), reads=[bs], writes=[bs])
            C.op("dve", lambda e: e.scalar_tensor_tensor(out=bs.t[:, 0:1], in0=bs.t[:, 5:6], scalar=bs.t[:, 4:5], in1=bs.t[:, 0:1], op0=ALU.mult, op1=ALU.add), reads=[bs], writes=[bs])
            C.op("dve", lambda e: e.scalar_tensor_tensor(out=bs.t[:, 1:2], in0=bs.t[:, 6:7], scalar=bs.t[:, 4:5], in1=bs.t[:, 2:3], op0=ALU.mult, op1=ALU.add), reads=[bs], writes=[bs])
        thrB = C.sb("thrB", [NE, P], F32)
        C.op("dve", lambda e: e.tensor_scalar(out=thrB.t[:], in0=M("ones", NE), scalar1=bs.t[:, 0:1], scalar2=None, op0=ALU.mult), reads=[bs, self.mt], writes=[thrB])
        pb = self.nextps()
        C.op("pe", lambda e: e.matmul(pb.t[:, 0:NE], thrB.t[:], M("ident", NE)[:, 0:NE], start=True, stop=True), reads=[thrB, self.mt], writes=[pb])
        thr = C.sb("thrI", [P, NE], F32)
        self.copy("dve", thr, thr.t[:], pb, pb.t[:, 0:NE])
        mask = C.sb("maskI", [P, NT, NE], F32); slot = C.sb("slotI", [P, NT, NE], F32); msum = C.sb("msumI", [P, NE], F32)
        for tt in range(NT):
            C.op("dve", lambda e, tt=tt: e.tensor_tensor(out=mask.t[:, tt, :], in0=aff_tm.t[:, tt, :], in1=thr.t[:], op=ALU.is_ge), reads=[aff_tm, thr], writes=[mask])
        C.op("dve", lambda e: e.memset(msum.t[:], 0.0), writes=[msum])
        for tt in range(NT):
            pr = self.nextps()

            def fn(e, pr=pr, tt=tt):
                e.matmul(pr.t[:, 0:NE], M("tri"), mask.t[:, tt, :], start=True, stop=False)
                return e.matmul(pr.t[:, 0:NE], M("ones"), msum.t[:], start=False, stop=True)
            C.op("pe", fn, reads=[mask, msum, self.mt], writes=[pr])
            self.copy("act", slot, slot.t[:, tt, :], pr, pr.t[:, 0:NE])
            C.op("dve", lambda e, tt=tt: e.tensor_tensor(out=msum.t[:], in0=msum.t[:], in1=mask.t[:, tt, :], op=ALU.add), reads=[msum, mask], writes=[msum])
        sv = slot.t[:].rearrange("p a b -> p (a b)"); mv = mask.t[:].rearrange("p a b -> p (a b)")
        C.op("dve", lambda e: e.scalar_tensor_tensor(out=sv, in0=sv, scalar=1.0, in1=mv, op0=ALU.add, op1=ALU.mult), reads=[slot, mask], writes=[slot])
        C.op("dve", lambda e: e.tensor_scalar(out=sv, in0=sv, scalar1=-1.0, scalar2=None, op0=ALU.add), reads=[slot], writes=[slot])
        if "dbgs" in self.dbg:
            self.store("sp", self.dbgs_res, self.dbgs_res.t.ap()[:, 128:640], slot, sv)
            self.store("sp", self.dbgs_res, self.dbgs_res.t.ap()[:, 640:1152], mask, mv)
            self.store("sp", self.dbgs_res, self.dbgs_res.t.ap()[:, 1152:1168], thr, thr.t[:])
        rhsE = C.sb("rhsE", [P, NT, 4], BF16)
        C.op("dve", lambda e: e.tensor_copy(rhsE.t[:, :, 0:2], M("tvals").rearrange("p (a b) -> p a b", b=2)), reads=[self.mt], writes=[rhsE])
        ahi = C.sb("ahiI", [P, NT], BF16); ahf = C.sb("ahfI", [P, NT], F32); alo = C.sb("aloI", [P, NT], F32)
        oh = [[C.sb("ohI%d_%d" % (b, i), [P, CAP], BF16) for i in range(NT)] for b in range(2)]
        idf = C.sb("idfI", [P, 4], F32); pis = C.sb("pisI", [P, 16], F32)
        for ex in range(NL):
            C.op("dve", lambda e, ex=ex: e.tensor_copy(ahi.t[:], aff_tm.t[:, :, ex]), reads=[aff_tm], writes=[ahi])
            C.op("dve", lambda e: e.tensor_copy(ahf.t[:], ahi.t[:]), reads=[ahi], writes=[ahf])
            C.op("dve", lambda e, ex=ex: e.tensor_tensor(out=alo.t[:], in0=aff_tm.t[:, :, ex], in1=ahf.t[:], op=ALU.subtract), reads=[aff_tm, ahf], writes=[alo])
            C.op("dve", lambda e: e.tensor_copy(rhsE.t[:, :, 2], ahi.t[:]), reads=[ahi, rhsE], writes=[rhsE])
            C.op("dve", lambda e: e.tensor_copy(rhsE.t[:, :, 3], alo.t[:]), reads=[alo, rhsE], writes=[rhsE])
            pi = self.nextps()
            OHs = oh[ex % 2]
            for tt in range(NT):
                OH = OHs[tt]
                C.op("dve", lambda e, OH=OH, tt=tt, ex=ex: e.tensor_scalar(out=OH.t[:], in0=M("iota512"), scalar1=slot.t[:, tt, ex:ex + 1], scalar2=None, op0=ALU.is_equal), reads=[slot, self.mt], writes=[OH])

            def fm(e, OHs=OHs, pi=pi):
                inst = None
                for st in range(4):
                    for tt in range(NT):
                        inst = e.matmul(pi.t[:, st * 4:(st + 1) * 4], OHs[tt].t[:, st * P:(st + 1) * P], rhsE.t[:, tt, :], start=(tt == 0), stop=(tt == NT - 1))
                return inst
            C.op("pe", fm, reads=[*OHs, rhsE], writes=[pi])
            C.op("dve", lambda e, pi=pi: e.tensor_copy(pis.t[:], pi.t[:, 0:16]), reads=[pi], writes=[pis])
            pv = pis.t[:].rearrange("p (a b) -> p a b", b=4)
            C.op("dve", lambda e, pv=pv: e.scalar_tensor_tensor(out=idf.t[:], in0=pv[:, :, 0], scalar=64.0, in1=pv[:, :, 1], op0=ALU.mult, op1=ALU.add), reads=[pis], writes=[idf])
            C.op("dve", lambda e, ex=ex: e.tensor_copy(idx_all.t[:, ex, :], idf.t[:]), reads=[idf], writes=[idx_all])
            C.op("dve", lambda e, ex=ex, pv=pv: e.tensor_tensor(out=gate_all.t[:, ex, :], in0=pv[:, :, 2], in1=pv[:, :, 3], op=ALU.add), reads=[pis], writes=[gate_all])
        C.pop()

    def phase_J(self, hx2, wg, wu, wd, idx_all, gate_all, ffn, ffr):
        C = self.C; M = self.M
        C.push()
        zt = C.sb("ztJ", [P, 512], F32)
        C.op("dve", lambda e: e.memset(zt.t[:], 0.0), writes=[zt])
        fres = [Res("ffnres%d" % i) for i in range(8)]
        for db in range(8):
            for tt in range(NT):
                C.dma("sp" if tt % 2 else "act", lambda e, db=db, tt=tt: e.dma_start(out=ffn[db].t.ap()[tt * P:(tt + 1) * P, :], in_=zt.t[:]), reads=[zt], writes=[fres[db]], semres=zt)
        stg = [C.sb("stgJ%d" % i, [P, 4096], F32) for i in range(3)]
        wgb = [C.sb("wgb%d" % i, [P, 32, P], BF16) for i in range(2)]
        wub = [C.sb("wub%d" % i, [P, 32, P], BF16) for i in range(2)]
        wdb = [C.sb("wdb%d" % i, [P, 16, 512], BF16) for i in range(2)]
        xs = [C.sb("xsJ%d" % i, [P, D], BF16) for i in range(2)]
        xsT = C.sb("xsT", [P, 32, CAP], BF16); hidT = C.sb("hidT", [P, 16, CAP], BF16)
        sgt = [C.sb("sgt%d" % i, [P, CAP], F32) for i in range(2)]
        ot = [C.sb("otJ%d" % i, [P, 512], F32) for i in range(4)]
        cnt = dict(stg=0, ot=0, ev=0)

        def nstg():
            r = stg[cnt["stg"] % 3]; cnt["stg"] += 1; return r
        def gujob(ex, fi):
            def job():
                WG = wgb[fi % 2]; WU = wub[fi % 2]
                gv = wg.t.ap()[ex].rearrange("(kc p) f -> p kc f", p=P); uv = wu.t.ap()[ex].rearrange("(kc p) f -> p kc f", p=P)
                for (src, view, Wd_) in ((wg, gv, WG), (wu, uv, WU)):
                    S_ = nstg()
                    self.load("sp", S_, S_.t[:].rearrange("p (a b) -> p a b", a=32), src, view[:, :, fi * P:(fi + 1) * P])
                    self.copy(self.cast_eng(), Wd_, Wd_.t[:].rearrange("p a b -> p (a b)"), S_, S_.t[:])
                return (WG, WU)
            return job

        def djob(ex, db):
            def job():
                WD = wdb[db % 2]
                dv = wd.t.ap()[ex].rearrange("(fc p) d -> p fc d", p=P)
                for hf in range(2):
                    S_ = nstg()
                    self.load("sp", S_, S_.t[:].rearrange("p (a b) -> p a b", a=8), wd, dv[:, hf * 8:(hf + 1) * 8, db * 512:(db + 1) * 512])
                    self.copy(self.cast_eng(), WD, WD.t[:, hf * 8:(hf + 1) * 8, :].rearrange("p a b -> p (a b)"), S_, S_.t[:])
                return WD
            return job
        wst = Stream([j for ex in range(NL) for j in ([gujob(ex, fi) for fi in range(16)] + [djob(ex, db) for db in range(8)])])
        for ex in range(NL):
            for st in range(4):
                X = xs[st % 2]
                C.dma("pool", lambda e, X=X, ex=ex, st=st: e.indirect_dma_start(out=X.t[:], out_offset=None, in_=hx2.t.ap(), in_offset=bass.IndirectOffsetOnAxis(ap=idx_all.t[:, ex, st:st + 1], axis=0)), reads=[hx2, idx_all], writes=[X])
                for g in range(8):
                    pt = self.nextps(); pv = self.psb(pt)

                    def fnT(e, pv=pv, X=X, g=g):
                        inst = None
                        for j in range(4):
                            kc = g * 4 + j
                            inst = e.transpose(pv[:, j * P:(j + 1) * P], X.t[:, kc * P:(kc + 1) * P], self.identb.t[:])
                        return inst
                    C.op("pe", fnT, reads=[X, self.identb], writes=[pt])
                    cnt["ev"] += 1
                    self.copy("act" if cnt["ev"] % 2 else "dve", xsT, xsT.t[:, g * 4:(g + 1) * 4, st * P:(st + 1) * P], pt, pv[:, 0:512].rearrange("p (a b) -> p a b", a=4))
            for fi in range(16):
                WG, WU = wst.next()
                pg = self.nextps(); pu = self.nextps()
                self.mm(pg, pg.t[:, :], [(WG.t[:, kc, :], xsT.t[:, kc, :]) for kc in range(32)], [WG, xsT])
                self.mm(pu, pu.t[:, :], [(WU.t[:, kc, :], xsT.t[:, kc, :]) for kc in range(32)], [WU, xsT])
                SG = sgt[fi % 2]
                C.op("act", lambda e, SG=SG, pg=pg: e.activation(out=SG.t[:], in_=pg.t[:, :], func=AF.Silu), reads=[pg], writes=[SG])
                C.op("dve", lambda e, SG=SG, pu=pu, fi=fi: e.tensor_tensor(out=hidT.t[:, fi, :], in0=pu.t[:, :], in1=SG.t[:], op=ALU.mult), reads=[pu, SG], writes=[hidT])
            for db in range(8):
                WD = wst.next()
                for st in range(4):
                    po = self.nextps()
                    self.mm(po, po.t[:, :], [(hidT.t[:, fc, st * P:(st + 1) * P], WD.t[:, fc, :]) for fc in range(16)], [hidT, WD])
                    O = ot[cnt["ot"] % 4]; cnt["ot"] += 1
                    C.op("act", lambda e, O=O, po=po, ex=ex, st=st: e.activation(out=O.t[:], in_=po.t[:, :], func=AF.Copy, scale=gate_all.t[:, ex, st:st + 1]), reads=[po, gate_all], writes=[O])
                    rd_ = [O, idx_all] + ([] if st == 0 else [fres[db]])
                    wr_ = [fres[db]] if st == 0 else []
                    C.dma("pool", lambda e, O=O, ex=ex, st=st, db=db: e.indirect_dma_start(out=ffn[db].t.ap(), out_offset=bass.IndirectOffsetOnAxis(ap=idx_all.t[:, ex, st:st + 1], axis=0), in_=O.t[:], in_offset=None, compute_op=ALU.add), reads=rd_, writes=wr_, semres=O)
        self.fres = fres
        C.pop()

    def phase_K(self, x1r, ffn, bcd, fgb, out):
        C = self.C; M = self.M
        C.push()
        g2 = C.sb("gt2b", [P, D], F32); fg = C.sb("fgbt", [P, D], F32)
        self.load("sp", g2, g2.t[:], bcd, bcd.t.ap()[:, D:2 * D]); self.load("act", fg, fg.t[:], fgb, fgb.t.ap())
        xt = [C.sb("xtK%d" % i, [P, D], F32) for i in range(2)]
        ft = [C.sb("ftK%d" % i, [P, D], F32) for i in range(2)]
        junk = C.sb("junkK", [P, D], BF16)
        sts = [C.sb("stK%d" % i, [P, 4], F32) for i in range(2)]
        for tt in range(NT):
            X = xt[tt % 2]; Fd = ft[tt % 2]; st = sts[tt % 2]
            tk = slice(tt * P, (tt + 1) * P)
            self.load("sp", X, X.t[:], x1r, x1r.t.ap()[tk, :])
            for db in range(8):
                C.dma("act" if db % 2 else "sp", lambda e, Fd=Fd, db=db, tk=tk: e.dma_start(out=Fd.t[:, db * 512:(db + 1) * 512], in_=ffn[db].t.ap()[tk, :]), reads=[self.fres[db]], writes=[Fd])
            C.op("dve", lambda e, Fd=Fd: e.tensor_tensor(out=Fd.t[:], in0=Fd.t[:], in1=g2.t[:], op=ALU.mult), reads=[Fd, g2], writes=[Fd])
            C.op("pool", lambda e, Fd=Fd, X=X: e.tensor_tensor(out=Fd.t[:], in0=Fd.t[:], in1=X.t[:], op=ALU.add), reads=[Fd, X], writes=[Fd])
            self.rms_stats(Fd, junk, st, D)
            C.op("dve", lambda e, Fd=Fd, st=st, X=X: e.scalar_tensor_tensor(out=X.t[:], in0=Fd.t[:], scalar=st.t[:, 1:2], in1=fg.t[:], op0=ALU.mult, op1=ALU.mult), reads=[Fd, st, fg, X], writes=[X])
            self.store("sp", out, out.t.ap()[tk, :], X, X.t[:])
        C.pop()


def prep_inputs(inp, b, names, r=0):
    hc = host_constants()
    f = lambda a: np.ascontiguousarray(np.asarray(a, np.float32))
    bro = lambda v: np.ascontiguousarray(np.broadcast_to(np.asarray(v, np.float32).reshape(1, -1), (P, np.asarray(v).size)))
    m = {}
    m["x"] = f(inp["x"][b]); m["ctx"] = f(inp["ctx"][b])
    cc = np.stack([col_layout(inp["c"][b]), col_layout(inp["c_ctx"])], axis=2)
    m["cT"] = f(cc.reshape(P, 64))
    m["ada_w"] = f(inp["ada_w"][0]); m["abT"] = col_layout(inp["ada_b"][0])
    ab = np.asarray(inp["ada_b"][0], np.float32).reshape(6, D)
    m["abb"] = np.ascontiguousarray(np.concatenate([bro(ab[2]), bro(ab[5]), bro(ab[4]), bro(ab[3])], axis=1))
    m["g1T"] = col_layout(inp["norm1_g"][0]); m["g2b"] = bro(inp["norm2_g"][0]); m["fgb"] = bro(inp["final_g"])
    m["w_in"] = f(inp["w_in"][0]); m["w_out"] = f(inp["w_out"][0])
    sw = np.asarray(inp["hy_short_w"][0], np.float32); sb_ = np.asarray(inp["hy_short_b"][0], np.float32)
    swt = np.stack([col_layout(sw[0]), col_layout(sw[1]), col_layout(sw[2]), col_layout(sb_)], axis=2)
    m["swT"] = f(swt.reshape(P, 48 * 4))
    m["fw1"] = f(inp["hy_f_w1"][0]); m["fw2"] = f(inp["hy_f_w2"][0]); m["fw3"] = f(inp["hy_f_w3"][0]); m["fw4"] = f(inp["hy_f_w4"][0])
    m["fvec"] = f(np.stack([inp["hy_f_b1"][0], inp["hy_f_b2"][0], inp["hy_f_b3"][0], inp["hy_sin_freq"][0]], axis=1))
    m["hyb"] = bro(np.asarray(inp["hy_bias"][0]).reshape(-1)); m["rdec"] = bro(np.asarray(inp["ret_decay"][0]).reshape(-1))
    perm = [(e + NL * r) % NE for e in range(NE)]
    rw = np.asarray(inp["router_w"][0], np.float32)[:, perm].reshape(32, P, NE).transpose(1, 0, 2)
    m["rwT"] = f(rw.reshape(P, 32 * NE))
    m["cst"] = hc["cst"].reshape(NFT, P, 2 * NFT * P); m["fscale"] = hc["fscale"]
    m["zT"] = hc["zT"]; m["negt"] = hc["negt"]; m["deltab"] = hc["deltab"]
    m["rot_x"] = hc["rot_x"].reshape(T, 512); m["rot_c"] = hc["rot_c"].reshape(LC, 512)
    m["rot_xk"] = hc["rot_xk"].reshape(T, 512); m["rot_ck"] = hc["rot_ck"].reshape(LC, 512)
    m["misc"] = hc["misc"]
    if "wg" in names:
        sl = slice(NL * r, NL * (r + 1))
        m["wg"] = f(inp["exp_w_gate"][0][sl]); m["wu"] = f(inp["exp_w_up"][0][sl]); m["wd"] = f(inp["exp_w_down"][0][sl])
    return {k: v for k, v in m.items() if k in names}


def run(inputs, upto="all", dbg=(), cores=8):
    kb = K(upto=upto, dbg=dbg)
    nc = kb.build()
    names = set(kb.inputs.keys())
    maps = {}
    in_maps = []
    for cid in range(cores):
        b = cid // 2
        if b not in maps:
            maps[b] = prep_inputs(inputs, b, names, 0)
        in_maps.append(maps[b])
    res = run_bass_kernel_spmd(nc, in_maps, core_ids=list(range(cores)))
    return res, kb


def kernel(**inputs):
    res, kb = run(inputs)
    out = np.stack([np.asarray(res.results[2 * b]["out"], np.float32) for b in range(4)], axis=0)
    return out
```
